# Optimizing a Trainium2 kernel written in Bass

```python
import math
import numpy as np
import jax, jax.numpy as jnp
from jax import lax

D_MODEL = 2048
BATCH = 2
SEQ = 8192
DEPTH = 1

F32 = jnp.float32
D_MIX = D_MODEL
SSM_WIDTH = D_MIX // 2
SSM_CH_PER_GROUP = 16
SSM_GROUPS = SSM_WIDTH // SSM_CH_PER_GROUP
SSM_STATE = 64
DT_MIN = 1e-3
DT_MAX = 1e-1
NSA_HEADS = 16
NSA_KV_HEADS = 2
HEAD_DIM = 64
Q_PER_KV = NSA_HEADS // NSA_KV_HEADS
NSA_WIDTH = NSA_HEADS * HEAD_DIM
KV_WIDTH = NSA_KV_HEADS * HEAD_DIM
N_BRANCH = 3
IN_COLS = SSM_WIDTH + NSA_WIDTH + 6 * KV_WIDTH + NSA_HEADS * N_BRANCH
CMP_BLOCK = 32
CMP_STRIDE = 16
CMP_HIDDEN = 128
SEL_BLOCK = 64
SEL_TOPK = 16
WINDOW = 512
Q_BLOCK = 128
ROPE_THETA = 10000.0
N_EXPERTS = 64
TOP_K = 8
N_EXPERT_GROUPS = 8
TOPK_GROUPS = 4
EXPERT_FF = 512
SHARED_FF = 512
ROUTED_SCALE = 2.5
MOE_BLOCK = 128
DEEPNORM_ALPHA = (2.0 * DEPTH) ** 0.25
DEEPNORM_BETA = (8.0 * DEPTH) ** -0.25
LN_EPS = 1e-5
NEG = -1e30
FORCE = 1e4

kernel_name = 'hymba_s5_nsa_moe_deepnorm'


def layer_norm(x, g, b):
    xf = x.astype(F32)
    mu = jnp.mean(xf, -1, keepdims=True)
    var = jnp.mean(jnp.square(xf - mu), -1, keepdims=True)
    return ((xf - mu) * lax.rsqrt(var + LN_EPS) * g.astype(F32) + b.astype(F32)).astype(x.dtype)


def rope(x, pos):
    half = HEAD_DIM // 2
    inv = ROPE_THETA ** (-jnp.arange(half, dtype=F32) / half)
    ang = pos[..., None, None] * inv
    cos, sin = jnp.cos(ang), jnp.sin(ang)
    xf = x.astype(F32)
    x1, x2 = xf[..., :half], xf[..., half:]
    return jnp.concatenate([x1 * cos - x2 * sin, x2 * cos + x1 * sin], -1).astype(x.dtype)


def s5_mixer(u, lam_re, lam_im, log_dt, b_re, b_im, c_re, c_im, d_skip, w_glu):
    bsz, L, _ = u.shape
    uf = u.astype(F32).reshape(bsz, L, SSM_GROUPS, SSM_CH_PER_GROUP)
    lr, li = lam_re.astype(F32), lam_im.astype(F32)
    dt = jnp.exp(log_dt.astype(F32))[:, None]
    mag = jnp.exp(lr * dt)
    ar, ai = mag * jnp.cos(li * dt), mag * jnp.sin(li * dt)
    zr, zi = ar - 1.0, ai
    den = lr * lr + li * li
    fr, fi = (zr * lr + zi * li) / den, (zi * lr - zr * li) / den
    br_, bi_ = b_re.astype(F32), b_im.astype(F32)
    bbr = fr[..., None] * br_ - fi[..., None] * bi_
    bbi = fr[..., None] * bi_ + fi[..., None] * br_
    xr = jnp.einsum('blgh,gph->lbgp', uf, bbr)
    xi = jnp.einsum('blgh,gph->lbgp', uf, bbi)
    ar_t = jnp.broadcast_to(ar[None, None], (L, 1) + ar.shape)
    ai_t = jnp.broadcast_to(ai[None, None], (L, 1) + ai.shape)

    def combine(e1, e2):
        a1r, a1i, b1r, b1i = e1
        a2r, a2i, b2r, b2i = e2
        return (a1r * a2r - a1i * a2i, a1r * a2i + a1i * a2r,
                a2r * b1r - a2i * b1i + b2r, a2r * b1i + a2i * b1r + b2i)

    _, _, sr, si = lax.associative_scan(combine, (ar_t, ai_t, xr, xi), axis=0)
    y = jnp.einsum('lbgp,ghp->blgh', sr, c_re.astype(F32)) - jnp.einsum('lbgp,ghp->blgh', si, c_im.astype(F32))
    y = y + d_skip.astype(F32) * uf
    y = jax.nn.gelu(y.reshape(bsz, L, SSM_WIDTH).astype(u.dtype))
    return y * jax.nn.sigmoid(y @ w_glu)


def attend(scores, mask):
    s = jnp.where(mask, scores.astype(F32), NEG)
    return jax.nn.softmax(s, axis=-1) * mask


def nsa_mixer(q, k_cmp, v_cmp, k_sel, v_sel, k_win, v_win, gate_logits, positions,
              cmp_pos_k, cmp_pos_v, w_cmp_k1, w_cmp_k2, w_cmp_v1, w_cmp_v2):
    bsz, L = q.shape[:2]
    pos = positions.astype(F32)
    n_cmp = (L - CMP_BLOCK) // CMP_STRIDE + 1
    n_sel = L // SEL_BLOCK
    topk = min(SEL_TOPK, n_sel)
    n_qblk = L // Q_BLOCK
    scale = HEAD_DIM ** -0.5
    cidx = np.arange(n_cmp)[:, None] * CMP_STRIDE + np.arange(CMP_BLOCK)[None, :]

    def compress(kv, pe, w1, w2):
        blk = kv[:, cidx] + pe[:, None, :]
        blk = jnp.moveaxis(blk, 3, 2).reshape(bsz, n_cmp, NSA_KV_HEADS, CMP_BLOCK * HEAD_DIM)
        return jax.nn.gelu(blk @ w1) @ w2

    kc = rope(compress(k_cmp, cmp_pos_k, w_cmp_k1, w_cmp_k2), jnp.mean(pos[:, cidx], -1))
    vc = compress(v_cmp, cmp_pos_v, w_cmp_v1, w_cmp_v2)
    q = rope(q, pos)
    ks_t = jnp.transpose(rope(k_sel, pos), (0, 2, 1, 3))
    vs_t = jnp.transpose(v_sel, (0, 2, 1, 3))
    pad = ((0, 0), (WINDOW, 0), (0, 0), (0, 0))
    kw_p = jnp.pad(rope(k_win, pos), pad)
    vw_p = jnp.pad(v_win, pad)

    cs = np.arange(n_cmp) * CMP_STRIDE
    ce = cs + CMP_BLOCK - 1
    ss = np.arange(n_sel) * SEL_BLOCK
    se = ss + SEL_BLOCK - 1
    overlap = jnp.asarray(((cs[:, None] <= se[None, :]) & (ce[:, None] >= ss[None, :])).astype(np.float32))
    cmp_end = jnp.asarray(ce)
    sel_start = jnp.asarray(ss)
    blk_ids = jnp.arange(n_sel)
    b_ix = jnp.arange(bsz)[:, None, None]
    h_ix = jnp.arange(NSA_KV_HEADS)[None, :, None]

    q_blk = jnp.moveaxis(q.reshape(bsz, n_qblk, Q_BLOCK, NSA_KV_HEADS, Q_PER_KV, HEAD_DIM), 1, 0)
    gates = jax.nn.sigmoid(gate_logits)
    g_blk = jnp.moveaxis(gates.reshape(bsz, n_qblk, Q_BLOCK, NSA_KV_HEADS, Q_PER_KV, N_BRANCH), 1, 0)

    def block_fn(args):
        qi, qb, gb = args
        t = qi * Q_BLOCK + jnp.arange(Q_BLOCK)
        s_c = jnp.einsum('bqkgd,bnkd->bkgqn', qb, kc) * scale
        p_c = attend(s_c, cmp_end[None, :] <= t[:, None])
        o_c = jnp.einsum('bkgqn,bnkd->bqkgd', p_c.astype(vc.dtype), vc)
        imp = jnp.einsum('bkgqn,ns->bkqs', p_c, overlap)
        cur = t // SEL_BLOCK
        forced = (blk_ids[None, :] == 0) | (blk_ids[None, :] == cur[:, None]) | (blk_ids[None, :] == cur[:, None] - 1)
        valid = sel_start[None, :] <= t[:, None]
        score = jnp.where(forced, FORCE, jnp.where(valid, imp, -1.0))
        _, sel = lax.top_k(score, topk)
        tok = (sel[..., None] * SEL_BLOCK + jnp.arange(SEL_BLOCK)).reshape(bsz, NSA_KV_HEADS, Q_BLOCK * topk * SEL_BLOCK)
        kg = ks_t[b_ix, h_ix, tok].reshape(bsz, NSA_KV_HEADS, Q_BLOCK, topk * SEL_BLOCK, HEAD_DIM)
        vg = vs_t[b_ix, h_ix, tok].reshape(bsz, NSA_KV_HEADS, Q_BLOCK, topk * SEL_BLOCK, HEAD_DIM)
        tok = tok.reshape(bsz, NSA_KV_HEADS, Q_BLOCK, topk * SEL_BLOCK)
        s_s = jnp.einsum('bqkgd,bkqsd->bkgqs', qb, kg) * scale
        p_s = attend(s_s, (tok <= t[:, None])[:, :, None])
        o_s = jnp.einsum('bkgqs,bkqsd->bqkgd', p_s.astype(vg.dtype), vg)
        kwb = lax.dynamic_slice_in_dim(kw_p, qi * Q_BLOCK, Q_BLOCK + WINDOW, axis=1)
        vwb = lax.dynamic_slice_in_dim(vw_p, qi * Q_BLOCK, Q_BLOCK + WINDOW, axis=1)
        kpos = qi * Q_BLOCK - WINDOW + jnp.arange(Q_BLOCK + WINDOW)
        diff = t[:, None] - kpos[None, :]
        wmask = (diff >= 0) & (diff < WINDOW) & (kpos[None, :] >= 0)
        s_w = jnp.einsum('bqkgd,bskd->bkgqs', qb, kwb) * scale
        p_w = attend(s_w, wmask)
        o_w = jnp.einsum('bkgqs,bskd->bqkgd', p_w.astype(vwb.dtype), vwb)
        return gb[..., 0:1] * o_c + gb[..., 1:2] * o_s + gb[..., 2:3] * o_w

    out = lax.map(block_fn, (jnp.arange(n_qblk), q_blk, g_blk))
    return jnp.moveaxis(out, 0, 1).reshape(bsz, L, NSA_WIDTH)


def swiglu(x, wg, wu, wd):
    return (jax.nn.silu(x @ wg) * (x @ wu)) @ wd


def moe_ffn(x, w_router, router_bias, w_gate, w_up, w_down, ws_gate, ws_up, ws_down):
    bsz, L, d = x.shape
    xt = x.reshape(-1, d)
    n_tok = xt.shape[0]
    affin = jax.nn.sigmoid((xt @ w_router).astype(F32))
    biased = affin + router_bias.astype(F32)
    grp = biased.reshape(n_tok, N_EXPERT_GROUPS, N_EXPERTS // N_EXPERT_GROUPS)
    grp_score = jnp.sum(lax.top_k(grp, 2)[0], -1)
    _, gsel = lax.top_k(grp_score, TOPK_GROUPS)
    gmask = jnp.sum(jax.nn.one_hot(gsel, N_EXPERT_GROUPS, dtype=F32), 1)
    gmask = jnp.repeat(gmask, N_EXPERTS // N_EXPERT_GROUPS, axis=1) > 0
    _, eidx = lax.top_k(jnp.where(gmask, biased, NEG), TOP_K)
    w = jnp.take_along_axis(affin, eidx, axis=1)
    w = w / jnp.sum(w, -1, keepdims=True) * ROUTED_SCALE
    n_asg = n_tok * TOP_K
    flat_e = eidx.reshape(-1)
    order = jnp.argsort(flat_e)
    e_sorted = flat_e[order]
    tok_sorted = (order // TOP_K).astype(jnp.int32)
    w_sorted = w.reshape(-1)[order]
    counts = jnp.zeros(N_EXPERTS, jnp.int32).at[flat_e].add(1)
    padded = (counts + MOE_BLOCK - 1) // MOE_BLOCK * MOE_BLOCK
    pad_end = jnp.cumsum(padded)
    pad_start = pad_end - padded
    start = jnp.cumsum(counts) - counts
    dest = pad_start[e_sorted] + jnp.arange(n_asg) - start[e_sorted]
    n_blk = -(-(n_asg + N_EXPERTS * (MOE_BLOCK - 1)) // MOE_BLOCK)
    buf_tok = jnp.zeros(n_blk * MOE_BLOCK, jnp.int32).at[dest].set(tok_sorted).reshape(n_blk, MOE_BLOCK)
    buf_w = jnp.zeros(n_blk * MOE_BLOCK, F32).at[dest].set(w_sorted).reshape(n_blk, MOE_BLOCK)
    blk_exp = jnp.minimum(jnp.searchsorted(pad_end, jnp.arange(n_blk) * MOE_BLOCK, side='right'), N_EXPERTS - 1)

    def step(acc, args):
        e, tk, wk = args
        yb = swiglu(xt[tk], w_gate[e], w_up[e], w_down[e])
        return acc.at[tk].add(yb * wk[:, None].astype(yb.dtype)), None

    routed, _ = lax.scan(step, jnp.zeros_like(xt), (blk_exp, buf_tok, buf_w))
    shared = swiglu(xt, ws_gate, ws_up, ws_down)
    return (routed + shared).reshape(bsz, L, d)


def hybrid_layer(x, positions, w_in, lam_re, lam_im, log_dt, ssm_b_re, ssm_b_im, ssm_c_re, ssm_c_im, ssm_d,
                 w_glu, cmp_pos_k, cmp_pos_v, w_cmp_k1, w_cmp_k2, w_cmp_v1, w_cmp_v2, w_out, ln1_g, ln1_b,
                 w_router, router_bias, w_gate, w_up, w_down, ws_gate, ws_up, ws_down, ln2_g, ln2_b):
    bsz, L, _ = x.shape
    sizes = [SSM_WIDTH, NSA_WIDTH] + [KV_WIDTH] * 6 + [NSA_HEADS * N_BRANCH]
    offsets = tuple(int(o) for o in np.cumsum(sizes)[:-1])
    proj = x @ w_in
    u, q, k_c, v_c, k_s, v_s, k_w, v_w, g = jnp.split(proj, offsets, axis=-1)
    kv = lambda t: t.reshape(bsz, L, NSA_KV_HEADS, HEAD_DIM)
    y_ssm = s5_mixer(u, lam_re, lam_im, log_dt, ssm_b_re, ssm_b_im, ssm_c_re, ssm_c_im, ssm_d, w_glu)
    y_nsa = nsa_mixer(q.reshape(bsz, L, NSA_HEADS, HEAD_DIM), kv(k_c), kv(v_c), kv(k_s), kv(v_s), kv(k_w), kv(v_w),
                      g.reshape(bsz, L, NSA_HEADS, N_BRANCH), positions,
                      cmp_pos_k, cmp_pos_v, w_cmp_k1, w_cmp_k2, w_cmp_v1, w_cmp_v2)
    mix = jnp.concatenate([y_ssm, y_nsa], -1) @ w_out
    x = layer_norm(DEEPNORM_ALPHA * x + mix, ln1_g, ln1_b)
    ffn = moe_ffn(x, w_router, router_bias, w_gate, w_up, w_down, ws_gate, ws_up, ws_down)
    return layer_norm(DEEPNORM_ALPHA * x + ffn, ln2_g, ln2_b)


def setup_inputs(seed: int = 0) -> dict:
    key = jax.random.key(seed)
    keys = iter(jax.random.split(key, 48))

    def nrm(shape, scale):
        return scale * jax.random.normal(next(keys), (DEPTH,) + shape, F32)

    x = jax.random.normal(next(keys), (BATCH, SEQ, D_MODEL), F32)
    offs = jax.random.randint(next(keys), (BATCH, 1), 0, 1024, jnp.int32)
    positions = offs + jnp.arange(SEQ, dtype=jnp.int32)[None, :]
    n_idx = jnp.arange(SSM_STATE, dtype=F32)
    lam_re = -0.5 + nrm((SSM_GROUPS, SSM_STATE), 0.01)
    lam_im = jnp.broadcast_to(math.pi * n_idx, (DEPTH, SSM_GROUPS, SSM_STATE)) + nrm((SSM_GROUPS, SSM_STATE), 0.01)
    log_dt = jax.random.uniform(next(keys), (DEPTH, SSM_GROUPS), F32, math.log(DT_MIN), math.log(DT_MAX))
    return {
        'x': x,
        'positions': positions,
        'w_in': nrm((D_MODEL, IN_COLS), D_MODEL ** -0.5),
        'lam_re': lam_re,
        'lam_im': lam_im,
        'log_dt': log_dt,
        'ssm_b_re': nrm((SSM_GROUPS, SSM_STATE, SSM_CH_PER_GROUP), (2.0 * SSM_CH_PER_GROUP) ** -0.5),
        'ssm_b_im': nrm((SSM_GROUPS, SSM_STATE, SSM_CH_PER_GROUP), (2.0 * SSM_CH_PER_GROUP) ** -0.5),
        'ssm_c_re': nrm((SSM_GROUPS, SSM_CH_PER_GROUP, SSM_STATE), (2.0 * SSM_STATE) ** -0.5),
        'ssm_c_im': nrm((SSM_GROUPS, SSM_CH_PER_GROUP, SSM_STATE), (2.0 * SSM_STATE) ** -0.5),
        'ssm_d': nrm((SSM_GROUPS, SSM_CH_PER_GROUP), 1.0),
        'w_glu': nrm((SSM_WIDTH, SSM_WIDTH), SSM_WIDTH ** -0.5),
        'cmp_pos_k': nrm((CMP_BLOCK, HEAD_DIM), 0.02),
        'cmp_pos_v': nrm((CMP_BLOCK, HEAD_DIM), 0.02),
        'w_cmp_k1': nrm((CMP_BLOCK * HEAD_DIM, CMP_HIDDEN), (CMP_BLOCK * HEAD_DIM) ** -0.5),
        'w_cmp_k2': nrm((CMP_HIDDEN, HEAD_DIM), CMP_HIDDEN ** -0.5),
        'w_cmp_v1': nrm((CMP_BLOCK * HEAD_DIM, CMP_HIDDEN), (CMP_BLOCK * HEAD_DIM) ** -0.5),
        'w_cmp_v2': nrm((CMP_HIDDEN, HEAD_DIM), CMP_HIDDEN ** -0.5),
        'w_out': nrm((D_MIX, D_MODEL), DEEPNORM_BETA * D_MIX ** -0.5),
        'ln1_g': 1.0 + nrm((D_MODEL,), 0.01),
        'ln1_b': nrm((D_MODEL,), 0.01),
        'w_router': nrm((D_MODEL, N_EXPERTS), D_MODEL ** -0.5),
        'router_bias': nrm((N_EXPERTS,), 0.01),
        'w_gate': nrm((N_EXPERTS, D_MODEL, EXPERT_FF), D_MODEL ** -0.5),
        'w_up': nrm((N_EXPERTS, D_MODEL, EXPERT_FF), D_MODEL ** -0.5),
        'w_down': nrm((N_EXPERTS, EXPERT_FF, D_MODEL), DEEPNORM_BETA * EXPERT_FF ** -0.5),
        'ws_gate': nrm((D_MODEL, SHARED_FF), D_MODEL ** -0.5),
        'ws_up': nrm((D_MODEL, SHARED_FF), D_MODEL ** -0.5),
        'ws_down': nrm((SHARED_FF, D_MODEL), DEEPNORM_BETA * SHARED_FF ** -0.5),
        'ln2_g': 1.0 + nrm((D_MODEL,), 0.01),
        'ln2_b': nrm((D_MODEL,), 0.01),
    }


def reference(x, positions, w_in, lam_re, lam_im, log_dt, ssm_b_re, ssm_b_im, ssm_c_re, ssm_c_im, ssm_d,
              w_glu, cmp_pos_k, cmp_pos_v, w_cmp_k1, w_cmp_k2, w_cmp_v1, w_cmp_v2, w_out, ln1_g, ln1_b,
              w_router, router_bias, w_gate, w_up, w_down, ws_gate, ws_up, ws_down, ln2_g, ln2_b):
    params = (w_in, lam_re, lam_im, log_dt, ssm_b_re, ssm_b_im, ssm_c_re, ssm_c_im, ssm_d,
              w_glu, cmp_pos_k, cmp_pos_v, w_cmp_k1, w_cmp_k2, w_cmp_v1, w_cmp_v2, w_out, ln1_g, ln1_b,
              w_router, router_bias, w_gate, w_up, w_down, ws_gate, ws_up, ws_down, ln2_g, ln2_b)
    for layer in range(DEPTH):
        x = hybrid_layer(x, positions, *(p[layer] for p in params))
    return x
```

```python
import math
from contextlib import ExitStack

import numpy as np
import concourse.bass as bass
import concourse.mybir as mybir
from concourse.bass_utils import run_bass_kernel_spmd

F32 = mybir.dt.float32
BF16 = mybir.dt.bfloat16
I32 = mybir.dt.int32
AF = mybir.ActivationFunctionType
ALU = mybir.AluOpType
AX = mybir.AxisListType

D = 2048
SEQ = 8192
W = 8192
NOWN = 2048
NG = W // 512
OWN_G0 = (W - NOWN) // 512
ALPHA = 2.0 ** 0.25
LN_EPS = 1e-5
TWO_PI = 2.0 * math.pi


class Sched:
    SEM_LIMIT = 20000

    def __init__(self, nc, es, n_dma_sems=40):
        self.nc = nc
        self.es = es
        self.E = {'pe': nc.tensor, 'act': nc.scalar, 'dve': nc.vector, 'pool': nc.gpsimd, 'sp': nc.sync}
        self.cur = {}
        self.known = {e: {} for e in self.E}
        self.lastw = {}
        self.reads = {}
        self.nsem = 0
        self.dma_slots = {'hw': [[self._newsem('dmah'), 0] for _ in range(n_dma_sems // 2)],
                          'sw': [[self._newsem('dmas'), 0] for _ in range(n_dma_sems // 2)]}
        self.dma_rr = {'hw': 0, 'sw': 0}
        self.ninst = {e: 0 for e in self.E}

    def _newsem(self, name):
        self.nsem += 1
        return self.es.enter_context(self.nc.semaphore('%s_%d' % (name, self.nsem)))

    def _tick(self, eng):
        c = self.cur.get(eng)
        if c is None or c[1] >= self.SEM_LIMIT:
            c = [self._newsem(eng), 0]
            self.cur[eng] = c
        c[1] += 1
        return c[0], c[1]

    def _wait(self, eng, dep):
        sem, val, deng = dep
        k = id(sem)
        if self.known[eng].get(k, 0) >= val:
            return
        self.E[eng].wait_ge(sem, val)
        self.known[eng][k] = val

    def _deps(self, eng, r, w):
        deps = []
        for res in r:
            lw = self.lastw.get(res)
            if lw is not None:
                deps.append(lw)
        for res in w:
            lw = self.lastw.get(res)
            if lw is not None:
                deps.append(lw)
            rd = self.reads.get(res)
            if rd:
                deps.extend(rd[0].values())
                deps.extend(rd[1])
        for d in deps:
            if d[2] == eng and eng == 'pe':
                continue
            self._wait(eng, d)

    def _commit(self, me, r, w):
        for res in w:
            self.lastw[res] = me
            self.reads[res] = ({}, [])
        for res in r:
            if res in w:
                continue
            rd = self.reads.get(res)
            if rd is None:
                rd = ({}, [])
                self.reads[res] = rd
            if me[2] == 'dma':
                rd[1].append(me)
            else:
                rd[0][me[2]] = me

    def op(self, eng, fn, r=(), w=()):
        self._deps(eng, r, w)
        sem, val = self._tick(eng)
        inst = fn(self.E[eng])
        inst.then_inc(sem, 1)
        self.ninst[eng] += 1
        me = (sem, val, eng)
        self._commit(me, r, w)
        return me

    def dma(self, q, out, in_, r=(), w=(), **kw):
        self._deps(q, r, w)
        kind = 'sw' if q == 'pool' else 'hw'
        slots = self.dma_slots[kind]
        slot = slots[self.dma_rr[kind]]
        self.dma_rr[kind] = (self.dma_rr[kind] + 1) % len(slots)
        if slot[1] > 0:
            self._wait(q, (slot[0], slot[1], 'dma'))
        if slot[1] >= self.SEM_LIMIT:
            slot[0] = self._newsem('dmax')
            slot[1] = 0
        slot[1] += 16
        self.E[q].dma_start(out=out, in_=in_, **kw).then_inc(slot[0], 16)
        self.ninst[q] += 1
        me = (slot[0], slot[1], 'dma')
        self._commit(me, r, w)
        return me

    def barrier(self):
        marks = []
        for e, c in self.cur.items():
            if c[1] > 0:
                marks.append((c[0], c[1], e))
        for slots in self.dma_slots.values():
            for slot in slots:
                if slot[1] > 0:
                    marks.append((slot[0], slot[1], 'dma'))
        for eng in self.E:
            for m in marks:
                if m[2] == eng and eng == 'pe':
                    continue
                self._wait(eng, m)
        self.lastw = {}
        self.reads = {}


C_U = 0
C_KC = 8 * 128
C_VC = 9 * 128
NC1 = 10 * 128
C_KS = 0
C_Q = 128
C_KW = 9 * 128
C_VS = 10 * 128
C_VW = 11 * 128
C_G = 12 * 128
NC2 = 12 * 128 + 48
NCOL = NC1 + NC2


def q_head_order():
    return [h for i in range(8) for h in (i, 8 + i)]


def w_in_columns():
    u = np.arange(0, 1024)
    q = np.arange(1024, 2048).reshape(16, 64)[q_head_order()].reshape(-1)
    kc = np.arange(2048, 2176)
    vc = np.arange(2176, 2304)
    ks = np.arange(2304, 2432)
    vs = np.arange(2432, 2560)
    kw = np.arange(2560, 2688)
    vw = np.arange(2688, 2816)
    g = np.arange(2816, 2864)
    cols = np.concatenate([u, kc, vc, ks, q, kw, vs, vw, g])
    assert cols.shape[0] == NCOL
    return cols


class Prog:
    def __init__(self, upto='all', dbg=()):
        self.upto = upto
        self.dbg = set(dbg)
        self.nc = bass.Bass("TRN2", target_bir_lowering=False)
        self.nq = 16
        self.nst = 32
        self.ntt = 16
        self.nexp = 65
        self.nhalf = 2
        self.skip = set()
        self.ext_scratch = set()
        self.dbg_qi, self.dbg_k = 0, 0
        self.ins = {}
        self.outs = {}

    def din(self, name, shape, dt=F32):
        t = self.nc.dram_tensor(name, list(shape), dt, kind="ExternalInput").ap()
        self.ins[name] = t
        return t

    def dout(self, name, shape, dt=F32):
        t = self.nc.dram_tensor(name, list(shape), dt, kind="ExternalOutput").ap()
        self.outs[name] = t
        return t

    def dscr(self, name, shape, dt=F32):
        if name in getattr(self, 'ext_scratch', ()):
            return self.din(name, shape, dt)
        return self.nc.dram_tensor(name, list(shape), dt, kind="Internal").ap()

    def sb(self, es, name, shape, dt=F32):
        self._nsb = getattr(self, '_nsb', 0) + 1
        return es.enter_context(self.nc.sbuf_tensor("%s_%d" % (name, self._nsb), list(shape), dt))

    def range_reduce(self, S, eng, fr, r, ti, tf, rtag, wtags):
        MAGIC = 12582912.0
        S.op(eng, lambda e: e.tensor_scalar(out=tf, in0=r, scalar1=MAGIC, scalar2=-MAGIC, op0=ALU.add, op1=ALU.add), r=[rtag], w=[wtags[1]])
        S.op(eng, lambda e: e.tensor_tensor(out=fr, in0=r, in1=tf, op=ALU.subtract), r=[rtag, wtags[1]], w=[wtags[2]])

    def build(self):
        nc = self.nc
        P = self
        xT = P.din("xT", [D, W])
        x_own = P.din("x_own", [NOWN, D])
        posw = P.din("posw", [1, W], I32)
        posc = P.din("posc", [1, 512], I32)
        kvalid = P.din("kvalid", [128, 64])
        nvalid = P.din("nvalid", [128, 4])
        padcol = P.din("padcol", [128, 2])
        w_in = P.din("w_in", [D, NCOL])
        ropecol = P.din("ropecol", [128, 4])
        identf = P.din("identf", [128, 128])
        pswap = P.din("pswap", [128, 128])
        P.din("w_cmp_k1", [2048, 128]); P.din("w_cmp_v1", [2048, 128])
        P.din("w_cmp_k2", [128, 64]); P.din("w_cmp_v2", [128, 64])
        P.din("cmp_pos_kT", [128, 32]); P.din("cmp_pos_vT", [128, 32])
        P.din("omat", [128, 4, 128]); P.din("causalT", [128, 128]); P.din("antiT", [128, 128])
        P.din("cmask", [128, 16, 2, 128]); P.din("sidx", [128, 128]); P.din("qhalf", [128, 1])
        P.din("s5_lam", [128, 32, 3]); P.din("s5_b", [128, 32, 2, 16]); P.din("s5_c", [128, 32, 2, 16]); P.din("s5_d", [32, 32])
        P.din("iota1", [128, 512]); P.din("iotar", [128, 512]); P.din("w_glu", [1024, 1024])
        P.din("w_out", [D, D]); P.din("ln1_g", [1, D]); P.din("ln1_b", [1, D]); P.din("ln2_g", [1, D]); P.din("ln2_b", [1, D])
        P.din("w_router", [D, 64]); P.din("router_bias", [1, 64])
        P.din("w_gate", [64, D, 512]); P.din("w_up", [64, D, 512]); P.din("w_down", [64, 512, D])
        P.din("ws_gate", [D, 512]); P.din("ws_up", [D, 512]); P.din("ws_down", [512, D])
        uT_s = P.dscr("uT_s", [1024, W], BF16)
        kcT_s = P.dscr("kcT_s", [128, W], BF16)
        vcT_s = P.dscr("vcT_s", [128, W], BF16)

        with ExitStack() as es0:
            S = Sched(nc, es0)
            self.S = S
            psbig = es0.enter_context(nc.psum_tensor("psbig", [128, 4096], F32))
            self.psbig = psbig
            ps = [psbig[:, i * 512:(i + 1) * 512] for i in range(8)]
            c_ident = P.sb(es0, "c_ident", [128, 128])
            c_identb = P.sb(es0, "c_identb", [128, 128], BF16)
            c_pswap = P.sb(es0, "c_pswap", [128, 128], BF16)
            c_rope = P.sb(es0, "c_rope", [128, 4])
            c_kvalid = P.sb(es0, "c_kvalid", [128, 64])
            c_nvalid = P.sb(es0, "c_nvalid", [128, 4])
            c_pad = P.sb(es0, "c_pad", [128, 2])
            S.dma('sp', c_ident[:], identf[:, :], w=['c_ident'])
            S.dma('pool', c_identb[:], identf[:, :], w=['c_identb'])
            S.dma('pool', c_pswap[:], pswap[:, :], w=['c_pswap'])
            S.dma('sp', c_rope[:], ropecol[:, :], w=['c_rope'])
            S.dma('sp', c_kvalid[:], kvalid[:, :], w=['c_kvalid'])
            S.dma('sp', c_nvalid[:], nvalid[:, :], w=['c_nvalid'])
            S.dma('sp', c_pad[:], padcol[:, :], w=['c_pad'])
            self.c_rope, self.c_pswap, self.c_ident, self.c_identb = c_rope, c_pswap, c_ident, c_identb
            self.c_kvalid, self.c_nvalid, self.c_pad = c_kvalid, c_nvalid, c_pad
            S.barrier()

            if 'p1' not in self.skip:
                self.phase1(S, ps, 1, locals())
            if self.upto == 'p1a':
                self.finish_debug(S, locals())
                return nc
            ysT_s = P.dscr("ysT_s", [1024, NOWN], BF16)
            if 'yT_all' in self.dbg:
                d_yT = P.dout("dbg_yT_all", [128, 8, NOWN], BF16)
            if 'p3' not in self.skip:
                self.phase3(S, ps, locals())
            self.dbg.discard('yT_all')
            if self.upto == 'p3':
                self.finish_debug(S, locals())
                return nc

            es1 = es0.enter_context(ExitStack())
            ksT = P.sb(es1, "ksT", [128, W], BF16)
            kwT = P.sb(es1, "kwT", [128, NOWN + 512], BF16)
            qT = P.sb(es1, "qT", [128, 16, 8, 128], BF16)
            Vs = P.sb(es1, "Vs", [128, 64, 2, 65], BF16)
            Vw = P.sb(es1, "Vw", [128, 20, 2, 65], BF16)
            gates = P.sb(es1, "gates", [128, 16, 48])
            if 'p1' not in self.skip:
                self.phase1(S, ps, 2, locals())
            if self.upto == 'p1':
                self.finish_debug(S, locals())
                return nc
            kcT = P.sb(es1, "kcT", [128, 512], BF16)
            Vc1O = P.sb(es1, "Vc1O", [128, 4, 2, 193], BF16)
            if 'p2' not in self.skip:
                self.phase2(S, ps, locals())
            if self.upto == 'p2':
                self.finish_debug(S, locals())
                return nc
            ynT_s = P.dscr("ynT_s", [1024, NOWN], BF16)
            if 'selb' in self.dbg:
                d_selb = P.dout("dbg_selb", [128, 128], BF16)
                d_imp = P.dout("dbg_imp", [128, 128])
                self.dbg.discard('selb')
                self.dbg_sel = True
            if 'p4' not in self.skip:
                self.phase4(S, ps, locals())
            if self.upto == 'p4':
                self.finish_debug(S, locals())
                return nc
            es1.close()
            x1_s = P.dscr("x1_s", [NOWN, D])
            x1T_s = P.dscr("x1T_s", [D, NOWN], BF16)
            gT_s = P.dscr("gT_s", [64, NOWN])
            if 'p5' not in self.skip:
                self.phase5(S, ps, locals())
            if self.upto == 'p5':
                self.finish_debug(S, locals())
                return nc
            out = P.dout("out", [NOWN, D])
            self.phase6(S, ps, locals())
            self.finish_debug(S, locals())
        return nc

    def rope_tables(self, S, eng, pos_i, cs, scol, tagp, tmp):
        c_rope = self.c_rope
        posf, r, fr, ti, tf = tmp
        C, Ss = cs
        S.op(eng, lambda e: e.tensor_copy(out=posf, in_=pos_i), r=[tagp + 'pi'], w=[tagp + 'pf'])
        S.op(eng, lambda e: e.tensor_scalar(out=r, in0=posf, scalar1=scol, scalar2=c_rope[:, 0:1], op0=ALU.add, op1=ALU.mult),
             r=[tagp + 'pf', 'c_rope'], w=[tagp + 'r'])
        self.range_reduce(S, eng, fr, r, ti, tf, tagp + 'r', [tagp + 'ti', tagp + 'tf', tagp + 'fr'])
        S.op('act', lambda e: e.activation(out=Ss, in_=fr, func=AF.Sin, scale=c_rope[:, 1:2]), r=[tagp + 'fr', 'c_rope'], w=[tagp + 'S'])
        S.op(eng, lambda e: e.tensor_scalar(out=r, in0=r, scalar1=0.25, scalar2=None, op0=ALU.add), r=[tagp + 'r'], w=[tagp + 'r'])
        self.range_reduce(S, eng, fr, r, ti, tf, tagp + 'r', [tagp + 'ti', tagp + 'tf', tagp + 'fr'])
        S.op('act', lambda e: e.activation(out=C, in_=fr, func=AF.Sin, scale=TWO_PI), r=[tagp + 'fr'], w=[tagp + 'C'])

    def rope_apply(self, S, src_ps, src_tag, dst, dst_tag, C, Ss, ctag, stag, raw, t1, t2, ps_sw, k):
        c_pswap = self.c_pswap
        S.op('act', lambda e: e.copy(out=raw, in_=src_ps), r=[src_tag], w=[('raw', k)])
        S.op('pe', lambda e: e.matmul(ps_sw, lhsT=c_pswap[:], rhs=raw, start=True, stop=True), r=[('raw', k), 'c_pswap'], w=[('pssw', k)])
        S.op('dve', lambda e: e.tensor_tensor(out=t1, in0=raw, in1=C, op=ALU.mult), r=[('raw', k), ctag], w=[('t1', k)])
        S.op('dve', lambda e: e.tensor_tensor(out=t2, in0=ps_sw, in1=Ss, op=ALU.mult), r=[('pssw', k), stag], w=[('t2', k)])
        if len(dst.shape) == 3:
            S.op('dve', lambda e: e.tensor_tensor(out=dst, in0=t1.rearrange("p (a b) -> p a b", b=128), in1=t2.rearrange("p (a b) -> p a b", b=128), op=ALU.add),
                 r=[('t1', k), ('t2', k)], w=[dst_tag])
        else:
            S.op('dve', lambda e: e.tensor_tensor(out=dst, in0=t1, in1=t2, op=ALU.add), r=[('t1', k), ('t2', k)], w=[dst_tag])

    def phase1(self, S, ps, npass, L):
        nc = self.nc
        P = self
        xT = self.ins["xT"]
        w_in = self.ins["w_in"]
        posw = self.ins["posw"]
        c_kvalid = self.c_kvalid
        if npass == 1:
            uT_s, kcT_s, vcT_s = L['uT_s'], L['kcT_s'], L['vcT_s']
            wc0, wn = 0, NC1
        else:
            ksT, kwT, qT, Vs, Vw, gates = (L[k] for k in ['ksT', 'kwT', 'qT', 'Vs', 'Vw', 'gates'])
            wc0, wn = NC1, NC2
        with ExitStack() as es:
            w_sb = P.sb(es, "w_sb", [128, 16, wn], BF16)
            xg = [P.sb(es, "xg%d" % i, [128, 16, 512], BF16) for i in range(2)]
            if npass == 1:
                ust = [P.sb(es, "ust%d" % i, [128, 512], BF16) for i in range(4)]
            else:
                raw = [P.sb(es, "raw%d" % i, [128, 512], BF16) for i in range(2)]
                t1 = [P.sb(es, "t1_%d" % i, [128, 512]) for i in range(2)]
                t2 = [P.sb(es, "t2_%d" % i, [128, 512]) for i in range(2)]
                Ct = [P.sb(es, "Ct%d" % i, [128, 512]) for i in range(2)]
                St = [P.sb(es, "St%d" % i, [128, 512]) for i in range(2)]
                posi = [P.sb(es, "posi%d" % i, [128, 512], I32) for i in range(2)]
                rtmp = (P.sb(es, "r_pf", [128, 512]), P.sb(es, "r_r", [128, 512]), P.sb(es, "r_fr", [128, 512]),
                        P.sb(es, "r_ti", [128, 512], I32), P.sb(es, "r_tf", [128, 512]))
            wv = w_in.rearrange("(c p) n -> p c n", p=128)
            wblocks = [(c0, min(wn, c0 + 800)) for c0 in range(0, wn, 800)]
            for (c0, c1) in wblocks:
                for dcg in range(0, 16, 4):
                    S.dma('pool', w_sb[:, dcg:dcg + 4, c0:c1], wv[:, dcg:dcg + 4, wc0 + c0:wc0 + c1], w=[('w_sb', c0, dcg)])
            wtags = [('w_sb', c0, dcg) for (c0, c1) in wblocks for dcg in range(0, 16, 4)]
            xv = xT.rearrange("(c p) t -> p c t", p=128)
            nfm = 0
            ropek = 0
            nst = 0
            for g in range(NG):
                own = g >= OWN_G0
                xb = xg[g % 2]
                xtag = ('xg', g % 2)
                for dcg in range(0, 16, 4):
                    S.dma('pool', xb[:, dcg:dcg + 4, :], xv[:, dcg:dcg + 4, g * 512:(g + 1) * 512], w=[(xtag, dcg)])
                xtags = [(xtag, dcg) for dcg in range(0, 16, 4)]
                if npass == 1:
                    fm = [('u', i, C_U + 128 * i) for i in range(8)] + [('kc', 0, C_KC), ('vc', 0, C_VC)]
                else:
                    pi = posi[g % 2]
                    tp = 'rt%d' % (g % 2)
                    S.dma('sp', pi[:], posw[0:1, g * 512:(g + 1) * 512].to_broadcast([128, 512]), w=[tp + 'pi'])
                    C, Ss = Ct[g % 2], St[g % 2]
                    self.rope_tables(S, 'dve', pi[:], (C[:], Ss[:]), 0.0, tp, tuple(t[:] for t in rtmp))
                    fm = [('ks', 0, C_KS)]
                    if own:
                        fm += [('q', i, C_Q + 128 * i) for i in range(8)]
                    if g >= OWN_G0 - 1:
                        fm += [('kw', 0, C_KW)]
                for (kind, i, c0) in fm:
                    bank = nfm % 4
                    nfm += 1
                    pt = ps[bank]
                    ptag = ('ps', bank)
                    for dc in range(16):
                        S.op('pe', lambda e: e.matmul(pt[:, :], lhsT=w_sb[:, dc, c0:c0 + 128], rhs=xb[:, dc, :], start=(dc == 0), stop=(dc == 15)),
                             r=(wtags + xtags) if dc in (0, 15) else [], w=[ptag])
                    if kind in ('u', 'kc', 'vc'):
                        ub = ust[nst % 4]
                        utag = ('ust', nst % 4)
                        nst += 1
                        S.op('act', lambda e: e.copy(out=ub[:], in_=pt[:, :]), r=[ptag], w=[utag])
                        if kind == 'u':
                            dst = uT_s[i * 128:(i + 1) * 128, g * 512:(g + 1) * 512]
                        elif kind == 'kc':
                            dst = kcT_s[:, g * 512:(g + 1) * 512]
                        else:
                            dst = vcT_s[:, g * 512:(g + 1) * 512]
                        S.dma('sp', dst, ub[:], r=[utag], w=[('scr', kind, i, g)])
                    else:
                        if kind == 'ks':
                            dst, dtag = ksT[:, g * 512:(g + 1) * 512], ('ksT', g)
                        elif kind == 'q':
                            go = g - OWN_G0
                            dst, dtag = qT[:, 4 * go:4 * go + 4, i, :], ('qT', i, go)
                        else:
                            go = g - (OWN_G0 - 1)
                            dst, dtag = kwT[:, go * 512:(go + 1) * 512], ('kwT', go)
                        k = ropek % 2
                        ropek += 1
                        self.rope_apply(S, pt[:, :], ptag, dst, dtag, C[:], Ss[:], tp + 'C', tp + 'S',
                                        raw[k][:], t1[k][:], t2[k][:], ps[4 + k][:, :], k)
                if npass == 2:
                    for tt in range(4):
                        wt = g * 4 + tt
                        bank = 6 + (wt % 2)
                        pt = ps[bank]
                        ptag = ('ps', bank)
                        ncols = 128 + (176 if g >= OWN_G0 - 1 else 0)
                        for dc in range(16):
                            S.op('pe', lambda e: e.matmul(pt[:, 0:ncols], lhsT=xb[:, dc, tt * 128:(tt + 1) * 128], rhs=w_sb[:, dc, C_VS:C_VS + ncols],
                                                          start=(dc == 0), stop=(dc == 15)),
                                 r=(wtags + xtags) if dc in (0, 15) else [], w=[ptag])
                        S.op('act', lambda e: e.copy(out=Vs[:, wt, :, 0:64], in_=pt[:, 0:128].rearrange("p (h d) -> p h d", h=2)), r=[ptag], w=[('Vs', wt)])
                        if g >= OWN_G0 - 1:
                            lw = wt - (OWN_G0 - 1) * 4
                            S.op('act', lambda e: e.copy(out=Vw[:, lw, :, 0:64], in_=pt[:, 128:256].rearrange("p (h d) -> p h d", h=2)), r=[ptag], w=[('Vw', lw)])
                        if own:
                            lo = wt - OWN_G0 * 4
                            S.op('act', lambda e: e.activation(out=gates[:, lo, :], in_=pt[:, 256:304], func=AF.Sigmoid), r=[ptag], w=[('gates', lo)])
            if npass == 2:
                for h in range(2):
                    S.op('dve', lambda e: e.tensor_copy(out=Vs[:, :, h, 64], in_=c_kvalid[:, 0:64]), r=['c_kvalid'], w=[('Vs1', h)])
                    S.op('dve', lambda e: e.tensor_copy(out=Vw[:, :, h, 64], in_=c_kvalid[:, 44:64]), r=['c_kvalid'], w=[('Vw1', h)])
            S.barrier()

    def phase2(self, S, ps, L):
        P = self
        kcT_s, vcT_s = L['kcT_s'], L['vcT_s']
        kcT, Vc1O = L['kcT'], L['Vc1O']
        posc = self.ins['posc']
        c_nvalid = self.c_nvalid
        with ExitStack() as es:
            raws = [P.sb(es, "cr_k", [128, W], BF16), P.sb(es, "cr_v", [128, W], BF16)]
            w1 = [P.sb(es, "w1k", [128, 32, 128], BF16), P.sb(es, "w1v", [128, 32, 128], BF16)]
            w2k = [P.sb(es, "w2k%d" % h, [128, 128], BF16) for h in range(2)]
            w2v = P.sb(es, "w2v", [128, 64], BF16)
            peT = [P.sb(es, "peTk", [128, 32], BF16), P.sb(es, "peTv", [128, 32], BF16)]
            cb = P.sb(es, "cb", [128, 2])
            gT = [P.sb(es, "gT%d" % h, [128, 512], BF16) for h in range(2)]
            Omat = P.sb(es, "Omat", [128, 4, 128], BF16)
            posi = P.sb(es, "cposi", [128, 512], I32)
            Ct, St = P.sb(es, "cCt", [128, 512]), P.sb(es, "cSt", [128, 512])
            rtmp = (P.sb(es, "c_pf", [128, 512]), P.sb(es, "c_r", [128, 512]), P.sb(es, "c_fr", [128, 512]),
                    P.sb(es, "c_ti", [128, 512], I32), P.sb(es, "c_tf", [128, 512]))
            raw, t1, t2 = P.sb(es, "craw", [128, 512], BF16), P.sb(es, "ct1", [128, 512]), P.sb(es, "ct2", [128, 512])
            S.dma('sp', raws[0][:], kcT_s, w=['cr0'])
            S.dma('sp', raws[1][:], vcT_s, w=['cr1'])
            for kv, nm in enumerate(['w_cmp_k1', 'w_cmp_v1']):
                src = self.ins[nm].rearrange("(i d) c -> d i c", d=64)
                for h in range(2):
                    S.dma('pool', w1[kv][64 * h:64 * h + 64, :, :], src, w=[('w1', kv, h)])
            for h in range(2):
                S.op('dve', lambda e: e.memset(w2k[h][:], 0.0), w=[('w2k', h)])
                S.dma('pool', w2k[h][:, 64 * h:64 * h + 64], self.ins['w_cmp_k2'], w=[('w2k', h)])
                S.op('dve', lambda e: e.memset(gT[h][:], 0.0), w=[('gT', h)])
            S.dma('pool', w2v[:], self.ins['w_cmp_v2'], w=['w2v'])
            S.dma('pool', peT[0][:], self.ins['cmp_pos_kT'], w=['peT0'])
            S.dma('pool', peT[1][:], self.ins['cmp_pos_vT'], w=['peT1'])
            S.dma('pool', Omat[:], self.ins['omat'], w=['Omat'])
            S.dma('sp', posi[:], posc[0:1, :].to_broadcast([128, 512]), w=['cpi'])
            self.rope_tables(S, 'pool', posi[:], (Ct[:], St[:]), 15.5, 'c', tuple(t[:] for t in rtmp))
            for kv in range(2):
                for i in range(32):
                    S.op('pe', lambda e: e.matmul(ps[7][:, 0:1], lhsT=w1[kv][0:64, i, :], rhs=peT[kv][0:64, i:i + 1], start=(i == 0), stop=(i == 31)),
                         r=[('w1', kv, 0), 'peT%d' % kv], w=[('ps', 7)])
                S.op('act', lambda e: e.copy(out=cb[:, kv:kv + 1], in_=ps[7][:, 0:1]), r=[('ps', 7)], w=[('cb', kv)])
                for h in range(2):
                    pt = ps[h]
                    for i in range(32):
                        S.op('pe', lambda e: e.matmul(pt[:, 0:511], lhsT=w1[kv][64 * h:64 * h + 64, i, :],
                                                      rhs=raws[kv][64 * h:64 * h + 64, i:i + 16 * 510 + 1:16], start=(i == 0), stop=(i == 31)),
                             r=[('w1', kv, h), 'cr%d' % kv], w=[('ps', h)])
                    S.op('act', lambda e: e.activation(out=gT[h][:, 0:511], in_=pt[:, 0:511], func=AF.Gelu_apprx_tanh, bias=cb[:, kv:kv + 1]),
                         r=[('ps', h), ('cb', kv)], w=[('gT', h)])
                if kv == 0:
                    for h in range(2):
                        S.op('pe', lambda e: e.matmul(ps[2][:, :], lhsT=w2k[h][:], rhs=gT[h][:], start=(h == 0), stop=(h == 1)),
                             r=[('gT', h), ('w2k', h)], w=[('ps', 2)])
                    self.rope_apply(S, ps[2][:, :], ('ps', 2), kcT[:], 'kcT', Ct[:], St[:], 'cC', 'cS', raw[:], t1[:], t2[:], ps[3][:, :], 0)
                else:
                    for a_ in range(4):
                        for h in range(2):
                            S.op('pe', lambda e: e.matmul(ps[2][:, (a_ * 2 + h) * 64:(a_ * 2 + h + 1) * 64], lhsT=gT[h][:, a_ * 128:(a_ + 1) * 128], rhs=w2v[:],
                                                          start=True, stop=True), r=[('gT', h), 'w2v'], w=[('ps', 2)])
                    for a_ in range(4):
                        S.op('dve', lambda e: e.tensor_scalar(out=Vc1O[:, a_, :, 0:64], in0=ps[2][:, a_ * 128:(a_ + 1) * 128].rearrange("p (h d) -> p h d", h=2),
                                                              scalar1=c_nvalid[:, a_:a_ + 1], scalar2=None, op0=ALU.mult),
                             r=[('ps', 2), 'c_nvalid'], w=[('Vc1O', a_)])
                        for h in range(2):
                            S.op('dve', lambda e: e.tensor_copy(out=Vc1O[:, a_, h, 64:65], in_=c_nvalid[:, a_:a_ + 1]), r=['c_nvalid'], w=[('Vc1O', a_)])
                            S.op('dve', lambda e: e.tensor_scalar(out=Vc1O[:, a_, h, 65:193], in0=Omat[:, a_, :], scalar1=c_nvalid[:, a_:a_ + 1], scalar2=None, op0=ALU.mult),
                                 r=['Omat', 'c_nvalid'], w=[('Vc1O', a_)])
            S.barrier()

    def phase3(self, S, ps, L):
        P = self
        uT_s, ysT_s = L['uT_s'], L['ysT_s']
        c_ident = self.c_ident
        nst = self.nst
        with ExitStack() as es:
            lam = P.sb(es, "lam", [128, 32, 3])
            sbD = P.sb(es, "sbD", [32, 32])
            iota1 = P.sb(es, "iota1", [128, 512])
            iotar = P.sb(es, "iotar", [128, 512])
            wglu = P.sb(es, "wglu", [128, 8, 1024], BF16)
            S.dma('sp', lam[:], self.ins['s5_lam'], w=['lam'])
            S.dma('sp', sbD[:], self.ins['s5_d'], w=['sbD'])
            S.dma('sp', iota1[:], self.ins['iota1'], w=['iota1'])
            S.dma('sp', iotar[:], self.ins['iotar'], w=['iotar'])
            S.dma('pool', wglu[:], self.ins['w_glu'].rearrange("(c p) n -> p c n", p=128), w=['wglu'])
            v = {}
            for nm in ['dt', 'mag', 'thc', 'sn', 'cs', 'ar', 'ai', 'zr', 'den', 'fr', 'fi', 'ta', 'tb', 'tfr', 'ttf', 'lrdt', 'm512']:
                v[nm] = P.sb(es, "v_" + nm, [128, 32])
            v_ti = P.sb(es, "v_ti", [128, 32], I32)
            lhsB = P.sb(es, "lhsB", [32, 32, 2, 128], BF16)
            bdC = P.sb(es, "bdC", [128, 32, 2, 32], BF16)
            dd = P.sb(es, "Ddiag", [32, 32, 32], BF16)
            esp = ExitStack()
            sbB = P.sb(esp, "sbB", [128, 32, 2, 16])
            sbC = P.sb(esp, "sbC", [128, 32, 2, 16])
            bd = P.sb(esp, "bdB", [128, 32, 2, 32])
            bt1 = P.sb(esp, "bt1", [128, 32, 16])
            bt2 = P.sb(esp, "bt2", [128, 32, 16])
            S.dma('sp', sbB[:], self.ins['s5_b'], w=['sbB'])
            S.dma('sp', sbC[:], self.ins['s5_c'], w=['sbC'])
            lr, li, ldt = lam[:, :, 0], lam[:, :, 1], lam[:, :, 2]
            TT = lambda o, a_, b_, op, r, w: S.op('dve', lambda e: e.tensor_tensor(out=o, in0=a_, in1=b_, op=op), r=r, w=w)
            TS = lambda o, a_, s1, op, r, w: S.op('dve', lambda e: e.tensor_scalar(out=o, in0=a_, scalar1=s1, scalar2=None, op0=op), r=r, w=w)
            S.op('act', lambda e: e.activation(out=v['dt'][:], in_=ldt, func=AF.Exp), r=['lam'], w=['v_dt'])
            TT(v['ta'][:], lr, v['dt'][:], ALU.mult, ['lam', 'v_dt'], ['v_ta'])
            S.op('act', lambda e: e.activation(out=v['mag'][:], in_=v['ta'][:], func=AF.Exp), r=['v_ta'], w=['v_mag'])
            S.op('act', lambda e: e.activation(out=v['m512'][:], in_=v['ta'][:], func=AF.Exp, scale=512.0), r=['v_ta'], w=['v_m512'])
            S.op('dve', lambda e: e.tensor_copy(out=v['lrdt'][:], in_=v['ta'][:]), r=['v_ta'], w=['v_lrdt'])
            TT(v['thc'][:], li, v['dt'][:], ALU.mult, ['lam', 'v_dt'], ['v_thc'])
            TS(v['thc'][:], v['thc'][:], 1.0 / TWO_PI, ALU.mult, ['v_thc'], ['v_thc'])
            self.range_reduce(S, 'dve', v['tfr'][:], v['thc'][:], v_ti[:], v['ttf'][:], 'v_thc', ['v_ti', 'v_ttf', 'v_tfr'])
            S.op('act', lambda e: e.activation(out=v['sn'][:], in_=v['tfr'][:], func=AF.Sin, scale=TWO_PI), r=['v_tfr'], w=['v_sn'])
            TS(v['tb'][:], v['thc'][:], 0.25, ALU.add, ['v_thc'], ['v_tb'])
            self.range_reduce(S, 'dve', v['tfr'][:], v['tb'][:], v_ti[:], v['ttf'][:], 'v_tb', ['v_ti', 'v_ttf', 'v_tfr'])
            S.op('act', lambda e: e.activation(out=v['cs'][:], in_=v['tfr'][:], func=AF.Sin, scale=TWO_PI), r=['v_tfr'], w=['v_cs'])
            TT(v['ar'][:], v['mag'][:], v['cs'][:], ALU.mult, ['v_mag', 'v_cs'], ['v_ar'])
            TT(v['ai'][:], v['mag'][:], v['sn'][:], ALU.mult, ['v_mag', 'v_sn'], ['v_ai'])
            TS(v['zr'][:], v['ar'][:], -1.0, ALU.add, ['v_ar'], ['v_zr'])
            TT(v['den'][:], lr, lr, ALU.mult, ['lam'], ['v_den'])
            TT(v['ta'][:], li, li, ALU.mult, ['lam'], ['v_ta'])
            TT(v['den'][:], v['den'][:], v['ta'][:], ALU.add, ['v_den', 'v_ta'], ['v_den'])
            S.op('dve', lambda e: e.reciprocal(out=v['den'][:], in_=v['den'][:]), r=['v_den'], w=['v_den'])
            TT(v['ta'][:], v['zr'][:], lr, ALU.mult, ['v_zr', 'lam'], ['v_ta'])
            TT(v['tb'][:], v['ai'][:], li, ALU.mult, ['v_ai', 'lam'], ['v_tb'])
            TT(v['fr'][:], v['ta'][:], v['tb'][:], ALU.add, ['v_ta', 'v_tb'], ['v_fr'])
            TT(v['fr'][:], v['fr'][:], v['den'][:], ALU.mult, ['v_fr', 'v_den'], ['v_fr'])
            TT(v['ta'][:], v['ai'][:], lr, ALU.mult, ['v_ai', 'lam'], ['v_ta'])
            TT(v['tb'][:], v['zr'][:], li, ALU.mult, ['v_zr', 'lam'], ['v_tb'])
            TT(v['fi'][:], v['ta'][:], v['tb'][:], ALU.subtract, ['v_ta', 'v_tb'], ['v_fi'])
            TT(v['fi'][:], v['fi'][:], v['den'][:], ALU.mult, ['v_fi', 'v_den'], ['v_fi'])
            S.op('pool', lambda e: e.memset(bd[:], 0.0), w=['bd'])
            frb = v['fr'][:].unsqueeze(2).to_broadcast([128, 32, 16])
            fib = v['fi'][:].unsqueeze(2).to_broadcast([128, 32, 16])
            br_, bi_ = sbB[:, :, 0, :], sbB[:, :, 1, :]
            TT(bt1[:], br_, frb, ALU.mult, ['sbB', 'v_fr'], ['bt1'])
            TT(bt2[:], bi_, fib, ALU.mult, ['sbB', 'v_fi'], ['bt2'])
            for gl in range(2):
                sl = slice(64 * gl, 64 * gl + 64)
                TT(bd[sl, :, 0, 16 * gl:16 * gl + 16], bt1[sl], bt2[sl], ALU.subtract, ['bt1', 'bt2', 'bd'], ['bd'])
            TT(bt1[:], bi_, frb, ALU.mult, ['sbB', 'v_fr', 'bd'], ['bt1'])
            TT(bt2[:], br_, fib, ALU.mult, ['sbB', 'v_fi', 'bd'], ['bt2'])
            for gl in range(2):
                sl = slice(64 * gl, 64 * gl + 64)
                TT(bd[sl, :, 1, 16 * gl:16 * gl + 16], bt1[sl], bt2[sl], ALU.add, ['bt1', 'bt2', 'bd'], ['bd'])
            for st in range(nst):
                for ri in range(2):
                    S.op('pe', lambda e: e.transpose(out=ps[0][0:32, ri * 128:(ri + 1) * 128], in_=bd[:, st, ri, :], identity=c_ident[:]), r=['bd', 'c_ident'], w=[('ps', 0)])
                S.op('act', lambda e: e.copy(out=lhsB[:, st, :, :], in_=ps[0][0:32, 0:256].rearrange("p (a b) -> p a b", b=128)), r=[('ps', 0)], w=['lhsB'])
            S.op('pool', lambda e: e.memset(bdC[:], 0.0), w=['bdC'])
            for gl in range(2):
                sl = slice(64 * gl, 64 * gl + 64)
                S.op('dve', lambda e: e.tensor_copy(out=bdC[sl, :, 0, 16 * gl:16 * gl + 16], in_=sbC[sl, :, 0, :]), r=['sbC', 'bdC'], w=['bdC'])
                TS(bdC[sl, :, 1, 16 * gl:16 * gl + 16], sbC[sl, :, 1, :], -1.0, ALU.mult, ['sbC', 'bdC'], ['bdC'])
            TT(dd[:], c_ident[0:32, 0:32].unsqueeze(1).to_broadcast([32, 32, 32]), sbD[:].unsqueeze(2).to_broadcast([32, 32, 32]), ALU.mult, ['c_ident', 'sbD'], ['dd'])

            S.barrier()
            esp.close()
            ub = [P.sb(es, "ub%d" % i, [32, W], BF16) for i in range(2)]
            Ct = [P.sb(es, "sCt%d" % i, [128, 512]) for i in range(2)]
            St = [P.sb(es, "sSt%d" % i, [128, 512]) for i in range(2)]
            magb = [P.sb(es, "magb%d" % i, [128, 512]) for i in range(2)]
            rtmp = (P.sb(es, "s_r", [128, 512]), P.sb(es, "s_fr", [128, 512]), None, P.sb(es, "s_tf", [128, 512]))
            Gt = P.sb(es, "s_G", [128, 512])
            rtmp2 = [P.sb(es, "s_fr%d" % i, [128, 512]) for i in range(3)]
            DA = [P.sb(es, "DA%d" % i, [128, 1024]) for i in range(2)]
            DB = [P.sb(es, "DB%d" % i, [128, 1024]) for i in range(2)]
            acol = [P.sb(es, "acol%d" % i, [128, 4]) for i in range(2)]
            junk = P.sb(es, "junk", [128, 1024], BF16)
            Ecol = P.sb(es, "Ecol", [128, 12, 2])
            Sst = [P.sb(es, "Sst%d" % i, [128, 2]) for i in range(2)]
            tst = P.sb(es, "tst", [128, 2])
            m_ = [[P.sb(es, "m%d_%d" % (k_, i), [128, 512]) for i in range(4)] for k_ in range(2)]
            mo = [P.sb(es, "mo%d" % i, [128, 512]) for i in range(4)]
            zre = [P.sb(es, "zre%d" % i, [128, 512]) for i in range(2)]
            zim = [P.sb(es, "zim%d" % i, [128, 512]) for i in range(2)]
            sre, sim_ = P.sb(es, "sre", [128, 512], BF16), P.sb(es, "sim", [128, 512], BF16)
            S0 = P.sb(es, "S0", [128, 2])
            tcol = P.sb(es, "tcol", [128, 2])
            nident = P.sb(es, "nident", [128, 128])
            S.op('dve', lambda e: e.tensor_scalar(out=nident[:], in0=c_ident[:], scalar1=-1.0, scalar2=None, op0=ALU.mult), r=['c_ident'], w=['nident'])
            yT_all = P.sb(es, "yT_all", [128, 8, NOWN], BF16)
            if nst < 32:
                S.op('pool', lambda e: e.memset(yT_all[:], 0.0), w=['yT_all'])
            def prepA(st):
                S.dma('sp', ub[st % 2][:], uT_s[st * 32:(st + 1) * 32, :], w=[('ub', st % 2)])
                C, Sn, mb = Ct[st % 2], St[st % 2], magb[st % 2]
                tp = 's5t%d' % (st % 2)
                da, db = DA[st % 2], DB[st % 2]
                r_, fr_, tf_ = rtmp[0][:], rtmp[1][:], rtmp[3][:]
                E_ = 'dve'
                S.op(E_, lambda e: e.tensor_scalar(out=r_, in0=iota1[:], scalar1=v['thc'][:, st:st + 1], scalar2=None, op0=ALU.mult), r=['iota1', 'v_thc'], w=['s_r'])
                self.range_reduce(S, E_, fr_, r_, None, tf_, 's_r', ['s_ti', 's_tf', 's_fr'])
                S.op('act', lambda e: e.activation(out=Sn[:], in_=fr_, func=AF.Sin, scale=TWO_PI), r=['s_fr'], w=[tp + 'S'])
                S.op(E_, lambda e: e.tensor_scalar(out=r_, in0=r_, scalar1=0.25, scalar2=None, op0=ALU.add), r=['s_r'], w=['s_r'])
                self.range_reduce(S, E_, rtmp2[0][:], r_, None, tf_, 's_r', ['s_ti', 's_tf', 's_fr2'])
                S.op('act', lambda e: e.activation(out=C[:], in_=rtmp2[0][:], func=AF.Sin, scale=TWO_PI), r=['s_fr2'], w=[tp + 'C'])
                S.op(E_, lambda e: e.tensor_scalar(out=r_, in0=iotar[:], scalar1=v['thc'][:, st:st + 1], scalar2=None, op0=ALU.mult), r=['iotar', 'v_thc'], w=['s_r'])
                self.range_reduce(S, E_, rtmp2[1][:], r_, None, tf_, 's_r', ['s_ti', 's_tf', 's_fr3'])
                S.op('act', lambda e: e.activation(out=db[:, 0:512], in_=rtmp2[1][:], func=AF.Sin, scale=TWO_PI), r=['s_fr3'], w=[tp + 'DB'])
                S.op(E_, lambda e: e.tensor_scalar(out=r_, in0=r_, scalar1=0.25, scalar2=None, op0=ALU.add), r=['s_r'], w=['s_r'])
                self.range_reduce(S, E_, rtmp2[2][:], r_, None, tf_, 's_r', ['s_ti', 's_tf', 's_fr4'])
                S.op('act', lambda e: e.activation(out=da[:, 0:512], in_=rtmp2[2][:], func=AF.Sin, scale=TWO_PI), r=['s_fr4'], w=[tp + 'DA'])
                S.op('act', lambda e: e.activation(out=Gt[:], in_=iotar[:], func=AF.Exp, scale=v['lrdt'][:, st:st + 1]), r=['iotar', 'v_lrdt'], w=['s_G'])

            def prepB(st):
                C, Sn, mb = Ct[st % 2], St[st % 2], magb[st % 2]
                tp = 's5t%d' % (st % 2)
                da, db, ac = DA[st % 2], DB[st % 2], acol[st % 2]
                E_ = 'dve'
                S.op(E_, lambda e: e.tensor_copy(out=mb[:], in_=v['mag'][:, st:st + 1].to_broadcast([128, 512])), r=['v_mag'], w=[tp + 'M'])
                S.op(E_, lambda e: e.tensor_tensor(out=da[:, 0:512], in0=da[:, 0:512], in1=Gt[:], op=ALU.mult), r=[tp + 'DA', 's_G'], w=[tp + 'DA'])
                S.op(E_, lambda e: e.tensor_tensor(out=db[:, 0:512], in0=db[:, 0:512], in1=Gt[:], op=ALU.mult), r=[tp + 'DB', 's_G'], w=[tp + 'DB'])
                S.op(E_, lambda e: e.tensor_copy(out=db[:, 512:1024], in_=da[:, 0:512]), r=[tp + 'DA', tp + 'DB'], w=[tp + 'DB'])
                S.op(E_, lambda e: e.tensor_scalar(out=da[:, 512:1024], in0=db[:, 0:512], scalar1=-1.0, scalar2=None, op0=ALU.mult), r=[tp + 'DB', tp + 'DA'], w=[tp + 'DA'])
                S.op(E_, lambda e: e.tensor_tensor(out=ac[:, 0:1], in0=C[:, 511:512], in1=v['m512'][:, st:st + 1], op=ALU.mult), r=[tp + 'C', 'v_m512'], w=[tp + 'A'])
                S.op(E_, lambda e: e.tensor_tensor(out=ac[:, 1:2], in0=Sn[:, 511:512], in1=v['m512'][:, st:st + 1], op=ALU.mult), r=[tp + 'S', 'v_m512'], w=[tp + 'A'])
                S.op(E_, lambda e: e.tensor_scalar(out=ac[:, 2:3], in0=ac[:, 1:2], scalar1=-1.0, scalar2=None, op0=ALU.mult), r=[tp + 'A'], w=[tp + 'A'])

            prepA(0)
            prepB(0)
            for st in range(nst):
                u_ = ub[st % 2]
                utag = ('ub', st % 2)
                C, Sn, mb = Ct[st % 2], St[st % 2], magb[st % 2]
                tp = 's5t%d' % (st % 2)
                if st + 1 < nst:
                    prepA(st + 1)
                def front(g):
                    usl = u_[:, g * 512:(g + 1) * 512]
                    mm = m_[g % 2]
                    mt = lambda i: ('m', g % 2, i)
                    wb = 2 + 2 * (g % 2)
                    S.op('pe', lambda e: e.matmul(ps[0][:, :], lhsT=lhsB[:, st, 0, :], rhs=usl, start=True, stop=True), r=['lhsB', utag], w=[('ps', 0)])
                    S.op('pe', lambda e: e.matmul(ps[1][:, :], lhsT=lhsB[:, st, 1, :], rhs=usl, start=True, stop=True), r=['lhsB', utag], w=[('ps', 1)])
                    TT(mm[0][:], ps[0][:, :], C[:], ALU.mult, [('ps', 0), tp + 'C'], [mt(0)])
                    TT(mm[1][:], ps[1][:, :], Sn[:], ALU.mult, [('ps', 1), tp + 'S'], [mt(1)])
                    TT(mm[2][:], ps[1][:, :], C[:], ALU.mult, [('ps', 1), tp + 'C'], [mt(2)])
                    TT(mm[3][:], ps[0][:, :], Sn[:], ALU.mult, [('ps', 0), tp + 'S'], [mt(3)])
                    S.op('pe', lambda e: e.matmul(ps[wb][:, :], lhsT=c_ident[:], rhs=mm[0][:], start=True, stop=False), r=['c_ident', mt(0)], w=[('ps', wb)])
                    S.op('pe', lambda e: e.matmul(ps[wb][:, :], lhsT=c_ident[:], rhs=mm[1][:], start=False, stop=True), r=['c_ident', mt(1)], w=[('ps', wb)])
                    S.op('pe', lambda e: e.matmul(ps[wb + 1][:, :], lhsT=c_ident[:], rhs=mm[2][:], start=True, stop=False), r=['c_ident', mt(2)], w=[('ps', wb + 1)])
                    S.op('pe', lambda e: e.matmul(ps[wb + 1][:, :], lhsT=nident[:], rhs=mm[3][:], start=False, stop=True), r=['nident', mt(3)], w=[('ps', wb + 1)])

                def back(g):
                    own = g >= OWN_G0
                    usl = u_[:, g * 512:(g + 1) * 512]
                    wb = 2 + 2 * (g % 2)
                    zr_, zi_ = zre[g % 2], zim[g % 2]
                    S.op('dve', lambda e: e.tensor_tensor_scan(out=zr_[:], data0=mb[:], data1=ps[wb][:, :], initial=S0[:, 0:1], op0=ALU.mult, op1=ALU.add),
                         r=[tp + 'M', ('ps', wb), 'S0'], w=[('zre', g % 2)])
                    S.op('dve', lambda e: e.tensor_tensor_scan(out=zi_[:], data0=mb[:], data1=ps[wb + 1][:, :], initial=S0[:, 1:2], op0=ALU.mult, op1=ALU.add),
                         r=[tp + 'M', ('ps', wb + 1), 'S0'], w=[('zim', g % 2)])
                    c5, s5 = C[:, 511:512], Sn[:, 511:512]
                    TT(tcol[:, 0:1], zi_[:, 511:512], s5, ALU.mult, [('zim', g % 2), tp + 'S'], ['tcol'])
                    TT(tcol[:, 1:2], zi_[:, 511:512], c5, ALU.mult, [('zim', g % 2), tp + 'C'], ['tcol'])
                    S.op('dve', lambda e: e.scalar_tensor_tensor(out=S0[:, 0:1], in0=zr_[:, 511:512], scalar=c5, in1=tcol[:, 0:1], op0=ALU.mult, op1=ALU.subtract),
                         r=[('zre', g % 2), tp + 'C', 'tcol', 'S0'], w=['S0'])
                    S.op('dve', lambda e: e.scalar_tensor_tensor(out=S0[:, 1:2], in0=zr_[:, 511:512], scalar=s5, in1=tcol[:, 1:2], op0=ALU.mult, op1=ALU.add),
                         r=[('zre', g % 2), tp + 'S', 'tcol', 'S0'], w=['S0'])
                    if own:
                        TT(mo[0][:], zr_[:], C[:], ALU.mult, [('zre', g % 2), tp + 'C', 'mo0'], ['mo0'])
                        TT(mo[1][:], zi_[:], Sn[:], ALU.mult, [('zim', g % 2), tp + 'S', 'mo1'], ['mo1'])
                        TT(mo[2][:], zr_[:], Sn[:], ALU.mult, [('zre', g % 2), tp + 'S', 'mo2'], ['mo2'])
                        TT(mo[3][:], zi_[:], C[:], ALU.mult, [('zim', g % 2), tp + 'C', 'mo3'], ['mo3'])
                        S.op('pool', lambda e: e.tensor_tensor(out=sre[:], in0=mo[0][:], in1=mo[1][:], op=ALU.subtract), r=['mo0', 'mo1'], w=['sre'])
                        S.op('pool', lambda e: e.tensor_tensor(out=sim_[:], in0=mo[2][:], in1=mo[3][:], op=ALU.add), r=['mo2', 'mo3'], w=['sim'])
                        S.op('pe', lambda e: e.matmul(ps[6][0:32, :], lhsT=bdC[:, st, 0, :], rhs=sre[:], start=True, stop=False), r=['bdC', 'sre'], w=[('ps', 6)])
                        S.op('pe', lambda e: e.matmul(ps[6][0:32, :], lhsT=bdC[:, st, 1, :], rhs=sim_[:], start=False, stop=False), r=['bdC', 'sim'], w=[('ps', 6)])
                        S.op('pe', lambda e: e.matmul(ps[6][0:32, :], lhsT=dd[:, st, :], rhs=usl, start=False, stop=True), r=['dd', utag], w=[('ps', 6)])
                        go = g - OWN_G0
                        po = (st % 4) * 32
                        S.op('act', lambda e: e.activation(out=yT_all[po:po + 32, st // 4, go * 512:(go + 1) * 512], in_=ps[6][0:32, :], func=AF.Gelu_apprx_tanh),
                             r=[('ps', 6)], w=['yT_all'])

                da, db, ac = DA[st % 2], DB[st % 2], acol[st % 2]
                S.op('dve', lambda e: e.memset(Sst[0][:], 0.0), r=[('Sst', 0)], w=[('Sst', 0)])
                for g in range(OWN_G0):
                    usl = u_[:, g * 512:(g + 1) * 512]
                    xb = (g % 2) * 2
                    S.op('pe', lambda e: e.matmul(ps[xb][:, :], lhsT=lhsB[:, st, 0, :], rhs=usl, start=True, stop=True), r=['lhsB', utag], w=[('ps', xb)])
                    S.op('pe', lambda e: e.matmul(ps[xb + 1][:, :], lhsT=lhsB[:, st, 1, :], rhs=usl, start=True, stop=True), r=['lhsB', utag], w=[('ps', xb + 1)])
                    xcat = self.psbig[:, xb * 512:(xb + 2) * 512]
                    S.op('dve', lambda e: e.scalar_tensor_tensor(out=junk[:], in0=xcat, scalar=1.0, in1=da[:], op0=ALU.mult, op1=ALU.mult, accum_out=Ecol[:, g, 0:1]),
                         r=[('ps', xb), ('ps', xb + 1), tp + 'DA', 'junk'], w=['junk', ('E', g)])
                    S.op('dve', lambda e: e.scalar_tensor_tensor(out=junk[:], in0=xcat, scalar=1.0, in1=db[:], op0=ALU.mult, op1=ALU.mult, accum_out=Ecol[:, g, 1:2]),
                         r=[('ps', xb), ('ps', xb + 1), tp + 'DB', 'junk'], w=['junk', ('E', g)])
                    cur, nxt = Sst[g % 2], Sst[(g + 1) % 2]
                    ct, nt = ('Sst', g % 2), ('Sst', (g + 1) % 2)
                    S.op('act', lambda e: e.activation(out=tst[:, 0:1], in_=cur[:, 0:1], func=AF.Identity, scale=ac[:, 0:1], bias=Ecol[:, g, 0:1]), r=[ct, tp + 'A', ('E', g)], w=['tst'])
                    S.op('act', lambda e: e.activation(out=nxt[:, 0:1], in_=cur[:, 1:2], func=AF.Identity, scale=ac[:, 2:3], bias=tst[:, 0:1]), r=[ct, tp + 'A', 'tst'], w=[nt])
                    S.op('act', lambda e: e.activation(out=tst[:, 1:2], in_=cur[:, 1:2], func=AF.Identity, scale=ac[:, 0:1], bias=Ecol[:, g, 1:2]), r=[ct, tp + 'A', ('E', g)], w=['tst'])
                    S.op('act', lambda e: e.activation(out=nxt[:, 1:2], in_=cur[:, 0:1], func=AF.Identity, scale=ac[:, 1:2], bias=tst[:, 1:2]), r=[ct, tp + 'A', 'tst'], w=[nt])
                S.op('act', lambda e: e.copy(out=S0[:], in_=Sst[OWN_G0 % 2][:]), r=[('Sst', OWN_G0 % 2), 'S0'], w=['S0'])
                if st + 1 < nst:
                    prepB(st + 1)
                front(OWN_G0)
                for g in range(OWN_G0, NG):
                    if g + 1 < NG:
                        front(g + 1)
                    back(g)
            if 'yT_all' in self.dbg:
                S.dma('sp', L['d_yT'], yT_all[:], r=['yT_all'], w=['d_yT'])
            sg = [P.sb(es, "sg%d" % i, [128, 512]) for i in range(2)]
            yo = [P.sb(es, "yo%d" % i, [128, 512], BF16) for i in range(2)]
            n = 0
            for cc in range(8):
                for tg in range(4):
                    b = 4 + n % 2
                    for kc in range(8):
                        S.op('pe', lambda e: e.matmul(ps[b][:, :], lhsT=wglu[:, kc, cc * 128:(cc + 1) * 128], rhs=yT_all[:, kc, tg * 512:(tg + 1) * 512],
                                                      start=(kc == 0), stop=(kc == 7)), r=['wglu', 'yT_all'], w=[('ps', b)])
                    S.op('act', lambda e: e.activation(out=sg[n % 2][:], in_=ps[b][:, :], func=AF.Sigmoid), r=[('ps', b)], w=[('sg', n % 2)])
                    TT(yo[n % 2][:], yT_all[:, cc, tg * 512:(tg + 1) * 512], sg[n % 2][:], ALU.mult, ['yT_all', ('sg', n % 2)], [('yo', n % 2)])
                    S.dma('sp', ysT_s[cc * 128:(cc + 1) * 128, tg * 512:(tg + 1) * 512], yo[n % 2][:], r=[('yo', n % 2)], w=[('ysT_s', cc, tg)])
                    n += 1
            S.barrier()

    def phase4(self, S, ps, L):
        P = self
        ksT, kwT, qT, Vs, Vw, gates, kcT, Vc1O = (L[k] for k in ['ksT', 'kwT', 'qT', 'Vs', 'Vw', 'gates', 'kcT', 'Vc1O'])
        ynT_s = L['ynT_s']
        c_identb, c_ident, c_pad = self.c_identb, self.c_ident, self.c_pad
        nq = self.nq
        with ExitStack() as es:
            causalT = P.sb(es, "causalT", [128, 128], BF16)
            antiT = P.sb(es, "antiT", [128, 128], BF16)
            cmask = P.sb(es, "cmask", [128, 16, 2, 128], BF16)
            sidx = P.sb(es, "sidx", [128, 128])
            qhalf = P.sb(es, "qhalf", [128, 1])
            real = P.sb(es, "real", [128, 128])
            first = P.sb(es, "first", [128, 128])
            rm2 = P.sb(es, "rm2", [128, 128])
            S.dma('pool', causalT[:], self.ins['causalT'], w=['causalT'])
            S.dma('pool', antiT[:], self.ins['antiT'], w=['antiT'])
            S.dma('pool', cmask[:], self.ins['cmask'], w=['cmask'])
            S.dma('sp', sidx[:], self.ins['sidx'], w=['sidx'])
            S.dma('sp', qhalf[:], self.ins['qhalf'], w=['qhalf'])
            S.op('dve', lambda e: e.tensor_scalar(out=first[:], in0=sidx[:], scalar1=c_pad[:, 0:1], scalar2=None, op0=ALU.subtract), r=['sidx', 'c_pad'], w=['first'])
            S.op('dve', lambda e: e.tensor_scalar(out=real[:], in0=first[:], scalar1=0.0, scalar2=None, op0=ALU.is_ge), r=['first'], w=['real'])
            S.op('dve', lambda e: e.tensor_scalar(out=first[:], in0=first[:], scalar1=0.0, scalar2=None, op0=ALU.is_equal), r=['first'], w=['first'])
            S.op('dve', lambda e: e.tensor_scalar(out=rm2[:], in0=real[:], scalar1=-2.0, scalar2=None, op0=ALU.add), r=['real'], w=['rm2'])
            et = [P.sb(es, "e%d" % i, [128, 512], BF16) for i in range(4)]
            mk = [P.sb(es, "mk%d" % i, [128, 128], BF16) for i in range(3)]
            o_all = P.sb(es, "o_all", [128, 8, 2, 64])
            den8 = P.sb(es, "den8", [128, 8])
            rg8 = P.sb(es, "rg8", [128, 8])
            imp = P.sb(es, "imp", [128, 128])
            A_ = P.sb(es, "A_", [128, 128])
            vld = P.sb(es, "vld", [128, 128])
            fo = P.sb(es, "fo", [128, 128])
            score = P.sb(es, "score", [128, 128])
            swk = P.sb(es, "swk", [128, 128])
            top = P.sb(es, "top", [128, 16])
            selb = P.sb(es, "selb", [128, 128], BF16)
            maskq = P.sb(es, "maskq", [128, W], BF16)
            yst = [P.sb(es, "yst%d" % i, [128, 8, 128], BF16) for i in range(2)]
            cnt = {'u': 0, 'm': 0}

            pend = []
            LA = 3
            cnt['i'] = 0

            def flush(n=0):
                while len(pend) > n:
                    pend.pop(0)()

            def unit(k, qi, keys, ktag, Vrhs, vtag, ncols, mask, mtag, accbank, first_, last_):
                for hf in range(2):
                    i = cnt['i']
                    cnt['i'] += 1
                    sb_ = i % 4
                    pt = ps[sb_]
                    e_ = et[i % 4]
                    etag = ('e', i % 4)
                    S.op('pe', lambda e: e.matmul(pt[:, :], lhsT=keys, rhs=qT[64 * k:64 * k + 64, qi, 4 * hf:4 * hf + 4, :],
                                                  start=True, stop=True), r=[ktag, 'qT'], w=[('ps', sb_)])
                    S.op('act', lambda e: e.activation(out=e_[:], in_=pt[:, :], func=AF.Exp, scale=0.125), r=[('ps', sb_)], w=[etag])
                    if mask is not None:
                        S.op('dve', lambda e: e.tensor_tensor(out=e_[:].rearrange("p (h q) -> p h q", h=4), in0=e_[:].rearrange("p (h q) -> p h q", h=4),
                                                              in1=mask.unsqueeze(1).to_broadcast([128, 4, 128]), op=ALU.mult), r=[etag, mtag], w=[etag])

                    def B(hf=hf, e_=e_, etag=etag, Vrhs=Vrhs, vtag=vtag, ncols=ncols, accbank=accbank, first_=first_, last_=last_):
                        for hh in range(4):
                            j = 4 * hf + hh
                            bank, c0 = accbank(j)
                            S.op('pe', lambda e: e.matmul(ps[bank][:, c0:c0 + ncols], lhsT=e_[:, hh * 128:(hh + 1) * 128], rhs=Vrhs,
                                                          start=(first_ and c0 == 0), stop=last_, skip_group_check=True),
                                 r=[etag, vtag], w=[('ps', bank)])
                    pend.append(B)
                    flush(LA)

            def evac(k, qi, accbank, br, init):
                for j in range(8):
                    bank, c0 = accbank(j)
                    S.op('dve', lambda e: e.tensor_copy(out=den8[:, j:j + 1], in_=ps[bank][:, c0 + 64:c0 + 65]), r=[('ps', bank)], w=['den8'])
                S.op('dve', lambda e: e.tensor_scalar(out=den8[:], in0=den8[:], scalar1=1e-30, scalar2=None, op0=ALU.max), r=['den8'], w=['den8'])
                S.op('dve', lambda e: e.reciprocal(out=den8[:], in_=den8[:]), r=['den8'], w=['den8'])
                gsl = gates[:, qi, 24 * k + br:24 * k + br + 22:3]
                S.op('dve', lambda e: e.tensor_tensor(out=rg8[:], in0=den8[:], in1=gsl, op=ALU.mult), r=['den8', 'gates'], w=['rg8'])
                for j in range(8):
                    bank, c0 = accbank(j)
                    if init:
                        S.op('dve', lambda e: e.tensor_scalar(out=o_all[:, j, k, :], in0=ps[bank][:, c0:c0 + 64], scalar1=rg8[:, j:j + 1], scalar2=None, op0=ALU.mult),
                             r=[('ps', bank), 'rg8'], w=['o_all'])
                    else:
                        S.op('dve', lambda e: e.scalar_tensor_tensor(out=o_all[:, j, k, :], in0=ps[bank][:, c0:c0 + 64], scalar=rg8[:, j:j + 1], in1=o_all[:, j, k, :],
                                                                     op0=ALU.mult, op1=ALU.add), r=[('ps', bank), 'rg8', 'o_all'], w=['o_all'])

            for qi in range(nq):
                wt = 48 + qi
                for k in range(2):
                    accC = lambda j: (4 + j // 2, (j % 2) * 193)
                    for a_ in range(4):
                        m_ = cmask[:, qi, a_ - 2, :] if a_ >= 2 else None
                        unit(k, qi, kcT[64 * k:64 * k + 64, a_ * 128:(a_ + 1) * 128], 'kcT', Vc1O[:, a_, k, :], 'Vc1O', 193, m_, 'cmask', accC, a_ == 0, a_ == 3)
                    flush()
                    evac(k, qi, accC, 0, True)
                    for j in range(8):
                        bank, c0 = accC(j)
                        if j == 0:
                            S.op('dve', lambda e: e.tensor_scalar(out=imp[:], in0=ps[bank][:, c0 + 65:c0 + 193], scalar1=den8[:, j:j + 1], scalar2=None, op0=ALU.mult),
                                 r=[('ps', bank), 'den8'], w=['imp'])
                        else:
                            S.op('dve', lambda e: e.scalar_tensor_tensor(out=imp[:], in0=ps[bank][:, c0 + 65:c0 + 193], scalar=den8[:, j:j + 1], in1=imp[:],
                                                                         op0=ALU.mult, op1=ALU.add), r=[('ps', bank), 'den8', 'imp'], w=['imp'])
                    S.op('dve', lambda e: e.tensor_scalar(out=A_[:], in0=sidx[:], scalar1=float(-2 * wt), scalar2=qhalf[:, 0:1], op0=ALU.add, op1=ALU.subtract),
                         r=['sidx', 'qhalf'], w=['A_'])
                    S.op('dve', lambda e: e.tensor_scalar(out=vld[:], in0=A_[:], scalar1=0.0, scalar2=None, op0=ALU.is_le), r=['A_'], w=['vld'])
                    S.op('dve', lambda e: e.tensor_scalar(out=fo[:], in0=A_[:], scalar1=-1.0, scalar2=None, op0=ALU.is_ge), r=['A_'], w=['fo'])
                    S.op('dve', lambda e: e.tensor_tensor(out=vld[:], in0=vld[:], in1=real[:], op=ALU.mult), r=['vld', 'real'], w=['vld'])
                    S.op('dve', lambda e: e.tensor_tensor(out=fo[:], in0=fo[:], in1=first[:], op=ALU.max), r=['fo', 'first'], w=['fo'])
                    S.op('dve', lambda e: e.tensor_tensor(out=fo[:], in0=fo[:], in1=vld[:], op=ALU.mult), r=['fo', 'vld'], w=['fo'])
                    S.op('dve', lambda e: e.scalar_tensor_tensor(out=score[:], in0=imp[:], scalar=1.0, in1=vld[:], op0=ALU.add, op1=ALU.mult), r=['imp', 'vld'], w=['score'])
                    S.op('dve', lambda e: e.scalar_tensor_tensor(out=score[:], in0=fo[:], scalar=1e4, in1=score[:], op0=ALU.mult, op1=ALU.add), r=['fo', 'score'], w=['score'])
                    S.op('dve', lambda e: e.tensor_tensor(out=score[:], in0=score[:], in1=rm2[:], op=ALU.add), r=['score', 'rm2'], w=['score'])
                    S.op('dve', lambda e: e.max(out=top[:, 0:8], in_=score[:]), r=['score'], w=['top'])
                    S.op('dve', lambda e: e.match_replace(out=swk[:], in_to_replace=top[:, 0:8], in_values=score[:], imm_value=-1e30), r=['score', 'top'], w=['swk'])
                    S.op('dve', lambda e: e.max(out=top[:, 8:16], in_=swk[:]), r=['swk'], w=['top'])
                    S.op('dve', lambda e: e.tensor_scalar(out=selb[:], in0=score[:], scalar1=top[:, 15:16], scalar2=None, op0=ALU.is_ge), r=['score', 'top'], w=['selb'])
                    if getattr(self, 'dbg_sel', False) and qi == self.dbg_qi and k == self.dbg_k:
                        S.dma('sp', L['d_selb'], selb[:], r=['selb'], w=['d_selb'])
                        S.dma('sp', L['d_imp'], imp[:], r=['imp'], w=['d_imp'])
                    S.op('dve', lambda e: e.tensor_copy(out=maskq[:].rearrange("p (s j) -> p s j", j=64), in_=selb[:].unsqueeze(2).to_broadcast([128, 128, 64])),
                         r=['selb'], w=['maskq'])
                    accS = lambda j: (4 + j // 4, (j % 4) * 65)
                    for kt in range(wt + 1):
                        mi = cnt['m'] % 2
                        cnt['m'] += 1
                        mb = 6 + mi
                        S.op('pe', lambda e: e.matmul(ps[mb][:, 0:128], lhsT=maskq[:, kt * 128:(kt + 1) * 128], rhs=c_identb[:],
                                                      start=True, stop=True), r=['maskq', 'c_identb'], w=[('ps', mb)])
                        if kt == wt:
                            S.op('dve', lambda e: e.tensor_tensor(out=mk[mi][:], in0=ps[mb][:, 0:128], in1=causalT[:], op=ALU.mult), r=[('ps', mb), 'causalT'], w=[('mk', mi)])
                        else:
                            S.op('act', lambda e: e.copy(out=mk[mi][:], in_=ps[mb][:, 0:128]), r=[('ps', mb)], w=[('mk', mi)])
                        unit(k, qi, ksT[64 * k:64 * k + 64, kt * 128:(kt + 1) * 128], 'ksT', Vs[:, kt, k, :], 'Vs', 65, mk[mi][:], ('mk', mi), accS, kt == 0, kt == wt)
                    flush()
                    evac(k, qi, accS, 1, False)
                    for wi, wkt in enumerate(range(wt - 4, wt + 1)):
                        if wkt == wt:
                            m_, mt_ = causalT[:], 'causalT'
                        elif wkt == wt - 4:
                            m_, mt_ = antiT[:], 'antiT'
                        else:
                            m_, mt_ = None, None
                        lw = wkt - 44
                        unit(k, qi, kwT[64 * k:64 * k + 64, lw * 128:(lw + 1) * 128], 'kwT', Vw[:, lw, k, :], 'Vw', 65, m_, mt_, accS, wi == 0, wi == 4)
                    flush()
                    evac(k, qi, accS, 2, False)
                ys = yst[qi % 2]
                for half in range(2):
                    tb = 6 + half
                    for i4 in range(4):
                        i = half * 4 + i4
                        S.op('pe', lambda e: e.transpose(out=ps[tb][:, i4 * 128:(i4 + 1) * 128], in_=o_all[:, i, :, :], identity=c_ident[:]),
                             r=['o_all', 'c_ident'], w=[('ps', tb)])
                    S.op('act', lambda e: e.copy(out=ys[:, half * 4:half * 4 + 4, :], in_=ps[tb][:, :].rearrange("p (a b) -> p a b", b=128)),
                         r=[('ps', tb)], w=[('yst', qi % 2)])
                S.dma('sp', ynT_s.rearrange("(i p) t -> p i t", p=128)[:, :, qi * 128:(qi + 1) * 128], ys[:], r=[('yst', qi % 2)], w=[('ynT_s', qi)])
            S.barrier()

    def layer_norm(self, S, h, htag, out, otag, g_bc, b_bc, tmp, k):
        stats, mv = tmp
        for c in range(4):
            S.op('dve', lambda e: e.bn_stats(out=stats[:, c, :], in_=h[:, c * 512:(c + 1) * 512]), r=[htag], w=[('lnst', k)])
        S.op('dve', lambda e: e.bn_aggr(out=mv[:, 0:2], in_=stats[:].rearrange("p a b -> p (a b)")), r=[('lnst', k)], w=[('lnmv', k)])
        S.op('dve', lambda e: e.tensor_scalar(out=mv[:, 2:3], in0=mv[:, 1:2], scalar1=LN_EPS, scalar2=None, op0=ALU.add), r=[('lnmv', k)], w=[('lnmv', k)])
        S.op('act', lambda e: e.activation(out=mv[:, 2:3], in_=mv[:, 2:3], func=AF.Sqrt), r=[('lnmv', k)], w=[('lnmv', k)])
        S.op('dve', lambda e: e.reciprocal(out=mv[:, 2:3], in_=mv[:, 2:3]), r=[('lnmv', k)], w=[('lnmv', k)])
        S.op('dve', lambda e: e.tensor_scalar(out=h, in0=h, scalar1=mv[:, 0:1], scalar2=mv[:, 2:3], op0=ALU.subtract, op1=ALU.mult),
             r=[htag, ('lnmv', k)], w=[htag])
        S.op('dve', lambda e: e.tensor_tensor(out=h, in0=h, in1=g_bc, op=ALU.mult), r=[htag, 'ln_g'], w=[htag])
        S.op('dve', lambda e: e.tensor_tensor(out=out, in0=h, in1=b_bc, op=ALU.add), r=[htag, 'ln_b'], w=[otag])

    def phase5(self, S, ps, L):
        P = self
        ysT_s, ynT_s, x1_s, x1T_s, gT_s = L['ysT_s'], L['ynT_s'], L['x1_s'], L['x1T_s'], L['gT_s']
        x_own = self.ins['x_own']
        c_ident = self.c_ident
        ntt = self.ntt
        with ExitStack() as es:
            wo = P.sb(es, "wo", [128, 16, D], BF16)
            mixT = [P.sb(es, "mixT%d" % i, [128, 16, 128], BF16) for i in range(2)]
            g_bc = P.sb(es, "ln1g", [128, D])
            b_bc = P.sb(es, "ln1b", [128, D])
            wr = P.sb(es, "wr", [128, 16, 64])
            rb = P.sb(es, "rb", [128, 64])
            wov = self.ins['w_out'].rearrange("(c p) n -> p c n", p=128)
            for dcg in range(0, 16, 2):
                S.dma('pool', wo[:, dcg:dcg + 2, :], wov[:, dcg:dcg + 2, :], w=[('wo', dcg)])
            wotags = [('wo', dcg) for dcg in range(0, 16, 2)]
            S.dma('sp', g_bc[:], self.ins['ln1_g'][0:1, :].to_broadcast([128, D]), w=['ln_g'])
            S.dma('sp', b_bc[:], self.ins['ln1_b'][0:1, :].to_broadcast([128, D]), w=['ln_b'])
            S.dma('sp', wr[:], self.ins['w_router'].rearrange("(c p) n -> p c n", p=128), w=['wr'])
            S.dma('sp', rb[:], self.ins['router_bias'][0:1, :].to_broadcast([128, 64]), w=['rb'])
            xo = [P.sb(es, "xo%d" % i, [128, D]) for i in range(2)]
            h_ = [P.sb(es, "h%d" % i, [128, D]) for i in range(2)]
            x1 = [P.sb(es, "x1_%d" % i, [128, D]) for i in range(2)]
            lnt = (P.sb(es, "lnstats", [128, 4, 6]), P.sb(es, "lnmv", [128, 4]))
            xTf = P.sb(es, "xTf", [128, 16, 128])
            xTb = [P.sb(es, "xTb%d" % i, [128, 16, 128], BF16) for i in range(2)]
            r_ = {}
            for nm, shp in [('aff', [128, 64]), ('bia', [128, 64]), ('m1', [128, 8]), ('eq', [128, 64]), ('b2', [128, 64]), ('m2', [128, 8]), ('gs', [128, 8]),
                            ('t8', [128, 8]), ('gm', [128, 8]), ('pen', [128, 8]), ('msk', [128, 64]), ('em', [128, 64]), ('wv', [128, 64]), ('ss', [128, 2]),
                            ('gate', [128, 64])]:
                r_[nm] = P.sb(es, "r_" + nm, shp)
            gst = [P.sb(es, "gst%d" % i, [64, 128]) for i in range(2)]
            TT = lambda o, a_, b_, op, r, w: S.op('dve', lambda e: e.tensor_tensor(out=o, in0=a_, in1=b_, op=op), r=r, w=w)
            TS = lambda o, a_, s1, op, r, w: S.op('dve', lambda e: e.tensor_scalar(out=o, in0=a_, scalar1=s1, scalar2=None, op0=op), r=r, w=w)
            for tt in range(ntt):
                k = tt % 2
                S.dma('sp', xo[k][:], x_own[tt * 128:(tt + 1) * 128, :], w=[('xo', k)])
                S.dma('sp', mixT[k][:, 0:8, :], ysT_s.rearrange("(c p) t -> p c t", p=128)[:, :, tt * 128:(tt + 1) * 128], w=[('mixT0', k)])
                S.dma('sp', mixT[k][:, 8:16, :], ynT_s.rearrange("(c p) t -> p c t", p=128)[:, :, tt * 128:(tt + 1) * 128], w=[('mixT1', k)])
                for dg in range(4):
                    for kc in range(16):
                        S.op('pe', lambda e: e.matmul(ps[dg][:, :], lhsT=mixT[k][:, kc, :], rhs=wo[:, kc, dg * 512:(dg + 1) * 512],
                                                      start=(kc == 0), stop=(kc == 15)), r=wotags + [('mixT0', k), ('mixT1', k)] if kc in (0, 15) else [], w=[('ps', dg)])
                    S.op('dve', lambda e: e.scalar_tensor_tensor(out=h_[k][:, dg * 512:(dg + 1) * 512], in0=xo[k][:, dg * 512:(dg + 1) * 512], scalar=ALPHA,
                                                                 in1=ps[dg][:, :], op0=ALU.mult, op1=ALU.add), r=[('xo', k), ('ps', dg)], w=[('h', k)])
                import os as _os
                cut = int(_os.environ.get('P5CUT', '9'))
                if cut <= 1:
                    S.dma('sp', x1_s[tt * 128:(tt + 1) * 128, :], h_[k][:], r=[('h', k)], w=[('x1_s', tt)])
                    continue
                self.layer_norm(S, h_[k][:], ('h', k), x1[k][:], ('x1', k), g_bc[:], b_bc[:], lnt, 0)
                S.dma('sp', x1_s[tt * 128:(tt + 1) * 128, :], x1[k][:], r=[('x1', k)], w=[('x1_s', tt)])
                if cut <= 2:
                    continue
                for q4 in range(4):
                    tb = 4 + q4 % 2
                    for i4 in range(4):
                        dc = q4 * 4 + i4
                        S.op('pe', lambda e: e.transpose(out=ps[tb][:, i4 * 128:(i4 + 1) * 128], in_=x1[k][:, dc * 128:(dc + 1) * 128], identity=c_ident[:]),
                             r=[('x1', k), 'c_ident'], w=[('ps', tb)])
                    S.op('act', lambda e: e.copy(out=xTf[:, q4 * 4:q4 * 4 + 4, :], in_=ps[tb][:, :].rearrange("p (a b) -> p a b", b=128)), r=[('ps', tb)], w=['xTf'])
                    S.op('dve', lambda e: e.tensor_copy(out=xTb[k][:, q4 * 4:q4 * 4 + 4, :], in_=xTf[:, q4 * 4:q4 * 4 + 4, :]), r=['xTf'], w=[('xTb', k)])
                S.dma('sp', x1T_s.rearrange("(c p) t -> p c t", p=128)[:, :, tt * 128:(tt + 1) * 128], xTb[k][:], r=[('xTb', k)], w=[('x1T_s', tt)])
                if cut <= 3:
                    continue
                for dc in range(16):
                    S.op('pe', lambda e: e.matmul(ps[6][:, 0:64], lhsT=xTf[:, dc, :], rhs=wr[:, dc, :], start=(dc == 0), stop=(dc == 15)), r=['xTf', 'wr'], w=[('ps', 6)])
                S.op('act', lambda e: e.activation(out=r_['aff'][:], in_=ps[6][:, 0:64], func=AF.Sigmoid), r=[('ps', 6)], w=['aff'])
                if cut <= 4:
                    continue
                TT(r_['bia'][:], r_['aff'][:], rb[:], ALU.add, ['aff', 'rb'], ['bia'])
                b3 = r_['bia'][:].rearrange("p (g e) -> p g e", e=8)
                S.op('dve', lambda e: e.tensor_reduce(out=r_['m1'][:], in_=b3, axis=AX.X, op=ALU.max), r=['bia'], w=['m1'])
                TT(r_['eq'][:].rearrange("p (g e) -> p g e", e=8), b3, r_['m1'][:].unsqueeze(2).to_broadcast([128, 8, 8]), ALU.is_equal, ['bia', 'm1'], ['eq'])
                S.op('dve', lambda e: e.scalar_tensor_tensor(out=r_['b2'][:], in0=r_['eq'][:], scalar=-1e9, in1=r_['bia'][:], op0=ALU.mult, op1=ALU.add), r=['eq', 'bia'], w=['b2'])
                S.op('dve', lambda e: e.tensor_reduce(out=r_['m2'][:], in_=r_['b2'][:].rearrange("p (g e) -> p g e", e=8), axis=AX.X, op=ALU.max), r=['b2'], w=['m2'])
                TT(r_['gs'][:], r_['m1'][:], r_['m2'][:], ALU.add, ['m1', 'm2'], ['gs'])
                S.op('dve', lambda e: e.max(out=r_['t8'][:], in_=r_['gs'][:]), r=['gs'], w=['t8'])
                TS(r_['gm'][:], r_['gs'][:], r_['t8'][:, 3:4], ALU.is_ge, ['gs', 't8'], ['gm'])
                S.op('dve', lambda e: e.tensor_scalar(out=r_['pen'][:], in0=r_['gm'][:], scalar1=-1.0, scalar2=1e9, op0=ALU.add, op1=ALU.mult), r=['gm'], w=['pen'])
                m3 = r_['msk'][:].rearrange("p (g e) -> p g e", e=8)
                TT(m3, b3, r_['gm'][:].unsqueeze(2).to_broadcast([128, 8, 8]), ALU.mult, ['bia', 'gm'], ['msk'])
                TT(m3, m3, r_['pen'][:].unsqueeze(2).to_broadcast([128, 8, 8]), ALU.add, ['msk', 'pen'], ['msk'])
                S.op('dve', lambda e: e.max(out=r_['t8'][:], in_=r_['msk'][:]), r=['msk', 'gm'], w=['t8'])
                TS(r_['em'][:], r_['msk'][:], r_['t8'][:, 7:8], ALU.is_ge, ['msk', 't8'], ['em'])
                TT(r_['wv'][:], r_['aff'][:], r_['em'][:], ALU.mult, ['aff', 'em'], ['wv'])
                S.op('dve', lambda e: e.tensor_reduce(out=r_['ss'][:, 0:1], in_=r_['wv'][:], axis=AX.X, op=ALU.add), r=['wv'], w=['ss'])
                S.op('dve', lambda e: e.reciprocal(out=r_['ss'][:, 1:2], in_=r_['ss'][:, 0:1]), r=['ss'], w=['ss'])
                S.op('dve', lambda e: e.tensor_scalar(out=r_['gate'][:], in0=r_['wv'][:], scalar1=r_['ss'][:, 1:2], scalar2=2.5, op0=ALU.mult, op1=ALU.mult), r=['wv', 'ss'], w=['gate'])
                S.op('pe', lambda e: e.transpose(out=ps[7][0:64, 0:128], in_=r_['gate'][:], identity=c_ident[:]), r=['gate', 'c_ident'], w=[('ps', 7)])
                S.op('act', lambda e: e.copy(out=gst[k][:], in_=ps[7][0:64, 0:128]), r=[('ps', 7)], w=[('gst', k)])
                S.dma('sp', gT_s[:, tt * 128:(tt + 1) * 128], gst[k][:], r=[('gst', k)], w=[('gT_s', tt)])
            S.barrier()

    def phase6(self, S, ps, L):
        P = self
        x1_s, x1T_s, gT_s = L['x1_s'], L['x1T_s'], L['gT_s']
        out = L['out']
        c_ident = self.c_ident
        nexp = self.nexp
        with ExitStack() as es:
            acc = P.sb(es, "acc", [128, 8, D])
            xT = P.sb(es, "x1T", [128, 16, 1024], BF16)
            gT = P.sb(es, "gTsb", [64, 1024])
            g_bc = P.sb(es, "ln2g", [128, D])
            b_bc = P.sb(es, "ln2b", [128, D])
            S.dma('sp', g_bc[:], self.ins['ln2_g'][0:1, :].to_broadcast([128, D]), w=['ln_g'])
            S.dma('sp', b_bc[:], self.ins['ln2_b'][0:1, :].to_broadcast([128, D]), w=['ln_b'])
            wgb = [P.sb(es, "wgb%d" % i, [128, 16, 256], BF16) for i in range(3)]
            wub = [P.sb(es, "wub%d" % i, [128, 16, 256], BF16) for i in range(3)]
            wdb = [P.sb(es, "wdb%d" % i, [128, 2, D], BF16) for i in range(2)]
            hT = [P.sb(es, "hT%d" % i, [128, 2, 1024], BF16) for i in range(2)]
            gbc = [[P.sb(es, "gbc%d_%d" % (i, tg), [128, 512]) for tg in range(2)] for i in range(1)]
            sg = [P.sb(es, "sgm%d" % i, [128, 512]) for i in range(2)]
            tm = [P.sb(es, "tm%d" % i, [128, 512]) for i in range(2)]
            ev = [P.sb(es, "ev%d" % i, [128, 512]) for i in range(2)]
            lnt = (P.sb(es, "ln2stats", [128, 4, 6]), P.sb(es, "ln2mv", [128, 4]))
            cnt = {'gu': 0, 'y': 0, 'ev': 0}
            units = [(e_, fh) for e_ in range(nexp) for fh in range(2)]
            NU = len(units)

            def wsrc(e_):
                if e_ == 64:
                    return self.ins['ws_gate'], self.ins['ws_up'], self.ins['ws_down']
                return self.ins['w_gate'][e_], self.ins['w_up'][e_], self.ins['w_down'][e_]

            def loads_gu(gu):
                e_, fh = units[gu % NU]
                wgs, wus, wds = wsrc(e_)
                b3 = gu % 3
                S.dma('pool', wgb[b3][:], wgs.rearrange("(c p) f -> p c f", p=128)[:, :, fh * 256:(fh + 1) * 256], w=[('wg', b3)])
                S.dma('pool', wub[b3][:], wus.rearrange("(c p) f -> p c f", p=128)[:, :, fh * 256:(fh + 1) * 256], w=[('wu', b3)])

            def loads_d(gu):
                e_, fh = units[gu % NU]
                wgs, wus, wds = wsrc(e_)
                b2 = gu % 2
                S.dma('pool', wdb[b2][:], wds[fh * 256:(fh + 1) * 256, :].rearrange("(c p) d -> p c d", p=128), w=[('wd', b2)])

            def gate_bc(gu):
                e_, fh = units[gu % NU]
                if e_ == 64 or fh != 0:
                    return
                gi = 0
                for tg in range(2):
                    S.op('pe', lambda e: e.matmul(ps[6][:, :], lhsT=c_ident[0:64, e_:e_ + 1].to_broadcast([64, 128]), rhs=gT[:, tg * 512:(tg + 1) * 512],
                                                  start=True, stop=True), r=['c_ident', 'gTsb'], w=[('ps', 6)])
                    S.op('act', lambda e: e.copy(out=gbc[gi][tg][:], in_=ps[6][:, :]), r=[('ps', 6)], w=[('gbc', gi, tg)])

            def GU(gu):
                e_, fh = units[gu % NU]
                shared = (e_ == 64)
                b3 = gu % 3
                wg_, wu_ = wgb[b3], wub[b3]
                hb = hT[gu % 2]
                htag = ('hT', gu % 2)
                gi = 0
                for tg in range(2):
                    for fc in range(2):
                        bg = (cnt['gu'] % 2) * 2
                        cnt['gu'] += 1
                        k = cnt['gu'] % 2
                        for dc in range(16):
                            S.op('pe', lambda e: e.matmul(ps[bg][:, :], lhsT=wg_[:, dc, fc * 128:(fc + 1) * 128], rhs=xT[:, dc, tg * 512:(tg + 1) * 512],
                                                          start=(dc == 0), stop=(dc == 15)), r=[('wg', b3), 'x1T'] if dc in (0, 15) else [], w=[('ps', bg)])
                        for dc in range(16):
                            S.op('pe', lambda e: e.matmul(ps[bg + 1][:, :], lhsT=wu_[:, dc, fc * 128:(fc + 1) * 128], rhs=xT[:, dc, tg * 512:(tg + 1) * 512],
                                                          start=(dc == 0), stop=(dc == 15)), r=[('wu', b3), 'x1T'] if dc in (0, 15) else [], w=[('ps', bg + 1)])
                        S.op('act', lambda e: e.activation(out=sg[k][:], in_=ps[bg][:, :], func=AF.Silu), r=[('ps', bg)], w=[('sg', k)])
                        hdst = hb[:, fc, tg * 512:(tg + 1) * 512]
                        if shared:
                            S.op('dve', lambda e: e.tensor_tensor(out=hdst, in0=ps[bg + 1][:, :], in1=sg[k][:], op=ALU.mult), r=[('ps', bg + 1), ('sg', k)], w=[htag])
                        else:
                            S.op('dve', lambda e: e.tensor_tensor(out=tm[k][:], in0=ps[bg + 1][:, :], in1=sg[k][:], op=ALU.mult), r=[('ps', bg + 1), ('sg', k)], w=[('tm', k)])
                            S.op('dve', lambda e: e.tensor_tensor(out=hdst, in0=tm[k][:], in1=gbc[gi][tg][:], op=ALU.mult), r=[('tm', k), ('gbc', gi, tg)], w=[htag])

            def DN(gu):
                b2 = gu % 2
                wd_ = wdb[b2]
                hb = hT[gu % 2]
                htag = ('hT', gu % 2)
                for t8 in range(8):
                    for dg in range(4):
                        by = (4, 5, 7)[cnt['y'] % 3]
                        cnt['y'] += 1
                        for fc in range(2):
                            S.op('pe', lambda e: e.matmul(ps[by][:, :], lhsT=hb[:, fc, t8 * 128:(t8 + 1) * 128], rhs=wd_[:, fc, dg * 512:(dg + 1) * 512],
                                                          start=(fc == 0), stop=(fc == 1)), r=[htag, ('wd', b2)], w=[('ps', by)])
                        asl = acc[:, t8, dg * 512:(dg + 1) * 512]
                        atag = ('acc', t8, dg)
                        S.op('dve', lambda e: e.tensor_tensor(out=asl, in0=asl, in1=ps[by][:, :], op=ALU.add), r=[('ps', by), atag], w=[atag])

            acctags = [('acc', t8, dg) for t8 in range(8) for dg in range(4)] + [('accrow', t8) for t8 in range(8)]
            for half in range(self.nhalf):
                tk0 = half * 1024
                S.dma('sp', xT[:], x1T_s.rearrange("(c p) t -> p c t", p=128)[:, :, tk0:tk0 + 1024], w=['x1T'])
                S.dma('sp', gT[:], gT_s[:, tk0:tk0 + 1024], w=['gTsb'])
                S.dma('sp', acc[:], x1_s[tk0:tk0 + 1024, :].rearrange("(t p) d -> p t d", p=128), w=acctags)
                for t8 in range(8):
                    S.op('act', lambda e: e.activation(out=acc[:, t8, :], in_=acc[:, t8, :], func=AF.Copy, scale=ALPHA),
                         r=[('acc', t8, dg) for dg in range(4)], w=[('acc', t8, dg) for dg in range(4)] + [('accrow', t8)])
                g0 = half * NU
                for i in range(min(3, NU)):
                    loads_gu(g0 + i)
                for i in range(min(2, NU)):
                    loads_d(g0 + i)
                gate_bc(g0)
                GU(g0)
                for ui in range(NU):
                    gu = g0 + ui
                    if ui + 1 < NU:
                        gate_bc(gu + 1)
                        GU(gu + 1)
                        if ui + 3 < NU:
                            loads_gu(gu + 3)
                    DN(gu)
                    if ui + 2 < NU:
                        loads_d(gu + 2)
                for t8 in range(8):
                    k = t8 % 2
                    k = 0
                    S.op('dve', lambda e: e.tensor_copy(out=lnt[1][:, 3:4], in_=acc[:, t8, 0:1]), r=[('acc', t8, dg) for dg in range(4)], w=[('accrow', t8)] + [('acc', t8, dg) for dg in range(4)])
                    self.layer_norm(S, acc[:, t8, :], ('accrow', t8), acc[:, t8, :], ('accrow', t8), g_bc[:], b_bc[:], lnt, 1)
                    r0 = tk0 + t8 * 128
                    S.dma('sp', out[r0:r0 + 128, :], acc[:, t8, :], r=[('accrow', t8)], w=[('out', r0)])
            S.barrier()

    def finish_debug(self, S, L):
        for name in sorted(self.dbg):
            t = L[name]
            if name == 'ynT_s':
                t = t[:, 0:self.nq * 128]
            shape = list(t.shape)
            o = self.dout("dbg_" + name, shape, t.dtype)
            S.dma('sp', o, t if isinstance(t, bass.AP) else t[:], w=[('dbg', name)])
        S.barrier()


def host_constants():
    c = {}
    c["identf"] = np.eye(128, dtype=np.float32)
    pm = np.zeros((128, 128), np.float32)
    for m in range(128):
        k = (m % 64 + 32) % 64 + 64 * (m // 64)
        pm[k, m] = 1.0
    c["pswap"] = pm
    rc = np.zeros((128, 4), np.float32)
    inv = 10000.0 ** (-(np.arange(32, dtype=np.float64)) / 32.0)
    for p in range(128):
        rc[p, 0] = inv[p % 32] / TWO_PI
        rc[p, 1] = (-TWO_PI if (p % 64) < 32 else TWO_PI)
    c["ropecol"] = rc
    n_ = (np.arange(4)[None, :, None] * 128 + np.arange(128)[:, None, None])
    s_ = np.arange(128)[None, None, :]
    c["omat"] = ((16 * n_ <= 64 * s_ + 63) & (16 * n_ + 31 >= 64 * s_)).astype(np.float32)
    kk = np.arange(128)[:, None]
    qq = np.arange(128)[None, :]
    c["causalT"] = (kk <= qq).astype(np.float32)
    c["antiT"] = (kk > qq).astype(np.float32)
    nl = np.arange(128)[:, None, None, None]
    qi = np.arange(16)[None, :, None, None]
    aa = np.arange(2)[None, None, :, None] + 2
    ql = np.arange(128)[None, None, None, :]
    c["cmask"] = (16 * (128 * aa + nl) + 31 <= (48 + qi) * 128 + ql).astype(np.float32)
    c["sidx"] = np.broadcast_to(np.arange(128, dtype=np.float32)[None, :], (128, 128)).copy()
    c["qhalf"] = (np.arange(128) >= 64).astype(np.float32)[:, None]
    c["iota1"] = np.broadcast_to(np.arange(1, 513, dtype=np.float32)[None, :], (128, 512)).copy()
    c["iotar"] = np.broadcast_to((511.0 - np.arange(512, dtype=np.float32))[None, :], (128, 512)).copy()
    return c


def core_inputs(c, x, positions, shared):
    b, j = c // 4, c % 4
    t0 = NOWN * j
    pad = W - (t0 + NOWN)
    m = {}
    xT = np.zeros((D, W), np.float32)
    xT[:, pad:] = x[b, :t0 + NOWN, :].T
    m["xT"] = xT
    m["x_own"] = np.ascontiguousarray(x[b, t0:t0 + NOWN, :])
    pw = np.zeros((1, W), np.int32)
    pw[0, pad:] = positions[b, :t0 + NOWN]
    m["posw"] = pw
    pc = np.zeros((1, 512), np.int32)
    pc[0, :] = pw[0, np.minimum(np.arange(512) * 16, W - 1)]
    m["posc"] = pc
    widx = np.arange(64)[None, :] * 128 + np.arange(128)[:, None]
    m["kvalid"] = (widx >= pad).astype(np.float32)
    nidx = np.arange(4)[None, :] * 128 + np.arange(128)[:, None]
    m["nvalid"] = (nidx >= pad // 16).astype(np.float32)
    pc2 = np.zeros((128, 2), np.float32)
    pc2[:, 0] = pad // 64
    pc2[:, 1] = pad
    m["padcol"] = pc2
    m.update(shared)
    return m


def make_shared(inp):
    sh = host_constants()
    sh["w_in"] = np.ascontiguousarray(inp["w_in"][0][:, w_in_columns()])
    for nm in ["w_cmp_k1", "w_cmp_v1", "w_cmp_k2", "w_cmp_v2"]:
        sh[nm] = np.ascontiguousarray(inp[nm][0])
    G2 = lambda a: np.asarray(a[0]).reshape(32, 2, *a.shape[2:])
    lam = np.stack([G2(inp["lam_re"]), G2(inp["lam_im"]),
                    np.broadcast_to(G2(inp["log_dt"])[:, :, None], (32, 2, 64))], -1)
    sh["s5_lam"] = np.ascontiguousarray(lam.transpose(1, 2, 0, 3).reshape(128, 32, 3)).astype(np.float32)
    bb = np.stack([G2(inp["ssm_b_re"]), G2(inp["ssm_b_im"])], 3)
    sh["s5_b"] = np.ascontiguousarray(bb.transpose(1, 2, 0, 3, 4).reshape(128, 32, 2, 16)).astype(np.float32)
    cc = np.stack([G2(inp["ssm_c_re"]), G2(inp["ssm_c_im"])], 2)
    sh["s5_c"] = np.ascontiguousarray(cc.transpose(1, 4, 0, 2, 3).reshape(128, 32, 2, 16)).astype(np.float32)
    sh["s5_d"] = np.ascontiguousarray(G2(inp["ssm_d"]).transpose(1, 2, 0).reshape(32, 32)).astype(np.float32)
    sh["w_glu"] = np.ascontiguousarray(inp["w_glu"][0])
    wo = np.asarray(inp["w_out"][0])
    sh["w_out"] = np.ascontiguousarray(np.concatenate([wo[:1024], wo[1024:].reshape(16, 64, D)[q_head_order()].reshape(1024, D)], 0))
    for nm in ["ln1_g", "ln1_b", "ln2_g", "ln2_b", "w_router", "router_bias", "w_gate", "w_up", "w_down", "ws_gate", "ws_up", "ws_down"]:
        sh[nm] = np.ascontiguousarray(inp[nm][0])
    for nm in ["cmp_pos_k", "cmp_pos_v"]:
        t = np.asarray(inp[nm][0]).T
        sh[nm + "T"] = np.ascontiguousarray(np.concatenate([t, t], 0))
    return sh


_CACHE = {}


def kernel(**inputs):
    x = np.asarray(inputs["x"], np.float32)
    positions = np.asarray(inputs["positions"], np.int32)
    shared = make_shared(inputs)
    if 'nc' not in _CACHE:
        _CACHE['nc'] = Prog('all').build()
    nc = _CACHE['nc']
    in_maps = [core_inputs(c, x, positions, shared) for c in range(8)]
    res = run_bass_kernel_spmd(nc, in_maps, core_ids=list(range(8)))
    out = np.zeros((2, SEQ, D), np.float32)
    for c in range(8):
        b, j = c // 4, c % 4
        out[b, j * NOWN:(j + 1) * NOWN] = res.results[c]["out"]
    return out
```

```python
import math
from contextlib import ExitStack

import numpy as np
import concourse.bass as bass
import concourse.mybir as mybir
from concourse.bass_utils import run_bass_kernel_spmd

F32 = mybir.dt.float32
BF16 = mybir.dt.bfloat16
I32 = mybir.dt.int32
AF = mybir.ActivationFunctionType
ALU = mybir.AluOpType
AX = mybir.AxisListType

D = 2048
SEQ = 8192
W = 8192
NOWN = 2048
NG = W // 512
OWN_G0 = (W - NOWN) // 512
ALPHA = 2.0 ** 0.25
LN_EPS = 1e-5
TWO_PI = 2.0 * math.pi


class Sched:
    SEM_LIMIT = 20000

    def __init__(self, nc, es, n_dma_sems=40):
        self.nc = nc
        self.es = es
        self.E = {'pe': nc.tensor, 'act': nc.scalar, 'dve': nc.vector, 'pool': nc.gpsimd, 'sp': nc.sync}
        self.cur = {}
        self.known = {e: {} for e in self.E}
        self.lastw = {}
        self.reads = {}
        self.nsem = 0
        self.dma_slots = {'hw': [[self._newsem('dmah'), 0] for _ in range(n_dma_sems // 2)],
                          'sw': [[self._newsem('dmas'), 0] for _ in range(n_dma_sems // 2)]}
        self.dma_rr = {'hw': 0, 'sw': 0}
        self.ninst = {e: 0 for e in self.E}

    def _newsem(self, name):
        self.nsem += 1
        return self.es.enter_context(self.nc.semaphore('%s_%d' % (name, self.nsem)))

    def _tick(self, eng):
        c = self.cur.get(eng)
        if c is None or c[1] >= self.SEM_LIMIT:
            c = [self._newsem(eng), 0]
            self.cur[eng] = c
        c[1] += 1
        return c[0], c[1]

    def _wait(self, eng, dep):
        sem, val, deng = dep
        k = id(sem)
        if self.known[eng].get(k, 0) >= val:
            return
        self.E[eng].wait_ge(sem, val)
        self.known[eng][k] = val

    def _deps(self, eng, r, w):
        deps = []
        for res in r:
            lw = self.lastw.get(res)
            if lw is not None:
                deps.append(lw)
        for res in w:
            lw = self.lastw.get(res)
            if lw is not None:
                deps.append(lw)
            rd = self.reads.get(res)
            if rd:
                deps.extend(rd[0].values())
                deps.extend(rd[1])
        for d in deps:
            if d[2] == eng and eng == 'pe':
                continue
            self._wait(eng, d)

    def _commit(self, me, r, w):
        for res in w:
            self.lastw[res] = me
            self.reads[res] = ({}, [])
        for res in r:
            if res in w:
                continue
            rd = self.reads.get(res)
            if rd is None:
                rd = ({}, [])
                self.reads[res] = rd
            if me[2] == 'dma':
                rd[1].append(me)
            else:
                rd[0][me[2]] = me

    def op(self, eng, fn, r=(), w=()):
        self._deps(eng, r, w)
        sem, val = self._tick(eng)
        inst = fn(self.E[eng])
        inst.then_inc(sem, 1)
        self.ninst[eng] += 1
        me = (sem, val, eng)
        self._commit(me, r, w)
        return me

    def dma(self, q, out, in_, r=(), w=(), **kw):
        self._deps(q, r, w)
        kind = 'sw' if q == 'pool' else 'hw'
        slots = self.dma_slots[kind]
        slot = slots[self.dma_rr[kind]]
        self.dma_rr[kind] = (self.dma_rr[kind] + 1) % len(slots)
        if slot[1] > 0:
            self._wait(q, (slot[0], slot[1], 'dma'))
        if slot[1] >= self.SEM_LIMIT:
            slot[0] = self._newsem('dmax')
            slot[1] = 0
        slot[1] += 16
        self.E[q].dma_start(out=out, in_=in_, **kw).then_inc(slot[0], 16)
        self.ninst[q] += 1
        me = (slot[0], slot[1], 'dma')
        self._commit(me, r, w)
        return me

    def barrier(self):
        marks = []
        for e, c in self.cur.items():
            if c[1] > 0:
                marks.append((c[0], c[1], e))
        for slots in self.dma_slots.values():
            for slot in slots:
                if slot[1] > 0:
                    marks.append((slot[0], slot[1], 'dma'))
        for eng in self.E:
            for m in marks:
                if m[2] == eng and eng == 'pe':
                    continue
                self._wait(eng, m)
        self.lastw = {}
        self.reads = {}


C_U = 0
C_KC = 8 * 128
C_VC = 9 * 128
NC1 = 10 * 128
C_KS = 0
C_Q = 128
C_KW = 9 * 128
C_VS = 10 * 128
C_VW = 11 * 128
C_G = 12 * 128
NC2 = 12 * 128 + 48
NCOL = NC1 + NC2


def q_head_order():
    return [h for i in range(8) for h in (i, 8 + i)]


def w_in_columns():
    u = np.arange(0, 1024)
    q = np.arange(1024, 2048).reshape(16, 64)[q_head_order()].reshape(-1)
    kc = np.arange(2048, 2176)
    vc = np.arange(2176, 2304)
    ks = np.arange(2304, 2432)
    vs = np.arange(2432, 2560)
    kw = np.arange(2560, 2688)
    vw = np.arange(2688, 2816)
    g = np.arange(2816, 2864)
    cols = np.concatenate([u, kc, vc, ks, q, kw, vs, vw, g])
    assert cols.shape[0] == NCOL
    return cols


class Prog:
    def __init__(self, upto='all', dbg=()):
        self.upto = upto
        self.dbg = set(dbg)
        self.nc = bass.Bass("TRN2", target_bir_lowering=False)
        self.nq = 16
        self.nst = 32
        self.ntt = 16
        self.nexp = 65
        self.nhalf = 2
        self.skip = set()
        self.ext_scratch = set()
        self.dbg_qi, self.dbg_k = 0, 0
        self.ins = {}
        self.outs = {}

    def din(self, name, shape, dt=F32):
        t = self.nc.dram_tensor(name, list(shape), dt, kind="ExternalInput").ap()
        self.ins[name] = t
        return t

    def dout(self, name, shape, dt=F32):
        t = self.nc.dram_tensor(name, list(shape), dt, kind="ExternalOutput").ap()
        self.outs[name] = t
        return t

    def dscr(self, name, shape, dt=F32):
        if name in getattr(self, 'ext_scratch', ()):
            return self.din(name, shape, dt)
        return self.nc.dram_tensor(name, list(shape), dt, kind="Internal").ap()

    def sb(self, es, name, shape, dt=F32):
        self._nsb = getattr(self, '_nsb', 0) + 1
        return es.enter_context(self.nc.sbuf_tensor("%s_%d" % (name, self._nsb), list(shape), dt))

    def range_reduce(self, S, eng, fr, r, ti, tf, rtag, wtags):
        MAGIC = 12582912.0
        S.op(eng, lambda e: e.tensor_scalar(out=tf, in0=r, scalar1=MAGIC, scalar2=-MAGIC, op0=ALU.add, op1=ALU.add), r=[rtag], w=[wtags[1]])
        S.op(eng, lambda e: e.tensor_tensor(out=fr, in0=r, in1=tf, op=ALU.subtract), r=[rtag, wtags[1]], w=[wtags[2]])

    def build(self):
        nc = self.nc
        P = self
        xT = P.din("xT", [D, W])
        x_own = P.din("x_own", [NOWN, D])
        posw = P.din("posw", [1, W], I32)
        posc = P.din("posc", [1, 512], I32)
        kvalid = P.din("kvalid", [128, 64])
        nvalid = P.din("nvalid", [128, 4])
        padcol = P.din("padcol", [128, 2])
        w_in = P.din("w_in", [D, NCOL])
        ropecol = P.din("ropecol", [128, 4])
        identf = P.din("identf", [128, 128])
        pswap = P.din("pswap", [128, 128])
        P.din("w_cmp_k1", [2048, 128]); P.din("w_cmp_v1", [2048, 128])
        P.din("w_cmp_k2", [128, 64]); P.din("w_cmp_v2", [128, 64])
        P.din("cmp_pos_kT", [128, 32]); P.din("cmp_pos_vT", [128, 32])
        P.din("omat", [128, 4, 128]); P.din("causalT", [128, 128]); P.din("antiT", [128, 128])
        P.din("cmask", [128, 16, 2, 128]); P.din("sidx", [128, 128]); P.din("qhalf", [128, 1])
        P.din("s5_lam", [128, 32, 3]); P.din("s5_b", [128, 32, 2, 16]); P.din("s5_c", [128, 32, 2, 16]); P.din("s5_d", [32, 32])
        P.din("iota1", [128, 512]); P.din("iotar", [128, 512]); P.din("w_glu", [1024, 1024])
        P.din("w_out", [D, D]); P.din("ln1_g", [1, D]); P.din("ln1_b", [1, D]); P.din("ln2_g", [1, D]); P.din("ln2_b", [1, D])
        P.din("w_router", [D, 64]); P.din("router_bias", [1, 64])
        P.din("w_gate", [64, D, 512]); P.din("w_up", [64, D, 512]); P.din("w_down", [64, 512, D])
        P.din("ws_gate", [D, 512]); P.din("ws_up", [D, 512]); P.din("ws_down", [512, D])
        uT_s = P.dscr("uT_s", [1024, W], BF16)
        kcT_s = P.dscr("kcT_s", [128, W], BF16)
        vcT_s = P.dscr("vcT_s", [128, W], BF16)

        with ExitStack() as es0:
            S = Sched(nc, es0)
            self.S = S
            psbig = es0.enter_context(nc.psum_tensor("psbig", [128, 4096], F32))
            self.psbig = psbig
            ps = [psbig[:, i * 512:(i + 1) * 512] for i in range(8)]
            c_ident = P.sb(es0, "c_ident", [128, 128])
            c_identb = P.sb(es0, "c_identb", [128, 128], BF16)
            c_pswap = P.sb(es0, "c_pswap", [128, 128], BF16)
            c_rope = P.sb(es0, "c_rope", [128, 4])
            c_kvalid = P.sb(es0, "c_kvalid", [128, 64])
            c_nvalid = P.sb(es0, "c_nvalid", [128, 4])
            c_pad = P.sb(es0, "c_pad", [128, 2])
            S.dma('sp', c_ident[:], identf[:, :], w=['c_ident'])
            S.dma('pool', c_identb[:], identf[:, :], w=['c_identb'])
            S.dma('pool', c_pswap[:], pswap[:, :], w=['c_pswap'])
            S.dma('sp', c_rope[:], ropecol[:, :], w=['c_rope'])
            S.dma('sp', c_kvalid[:], kvalid[:, :], w=['c_kvalid'])
            S.dma('sp', c_nvalid[:], nvalid[:, :], w=['c_nvalid'])
            S.dma('sp', c_pad[:], padcol[:, :], w=['c_pad'])
            self.c_rope, self.c_pswap, self.c_ident, self.c_identb = c_rope, c_pswap, c_ident, c_identb
            self.c_kvalid, self.c_nvalid, self.c_pad = c_kvalid, c_nvalid, c_pad
            S.barrier()

            if 'p1' not in self.skip:
                self.phase1(S, ps, 1, locals())
            if self.upto == 'p1a':
                self.finish_debug(S, locals())
                return nc
            ysT_s = P.dscr("ysT_s", [1024, NOWN], BF16)
            if 'yT_all' in self.dbg:
                d_yT = P.dout("dbg_yT_all", [128, 8, NOWN], BF16)
            if 'p3' not in self.skip:
                self.phase3(S, ps, locals())
            self.dbg.discard('yT_all')
            if self.upto == 'p3':
                self.finish_debug(S, locals())
                return nc

            es1 = es0.enter_context(ExitStack())
            ksT = P.sb(es1, "ksT", [128, W], BF16)
            kwT = P.sb(es1, "kwT", [128, NOWN + 512], BF16)
            qT = P.sb(es1, "qT", [128, 16, 8, 128], BF16)
            Vs = P.sb(es1, "Vs", [128, 64, 2, 65], BF16)
            Vw = P.sb(es1, "Vw", [128, 20, 2, 65], BF16)
            gates = P.sb(es1, "gates", [128, 16, 48])
            if 'p1' not in self.skip:
                self.phase1(S, ps, 2, locals())
            if self.upto == 'p1':
                self.finish_debug(S, locals())
                return nc
            kcT = P.sb(es1, "kcT", [128, 512], BF16)
            Vc1O = P.sb(es1, "Vc1O", [128, 4, 2, 193], BF16)
            if 'p2' not in self.skip:
                self.phase2(S, ps, locals())
            if self.upto == 'p2':
                self.finish_debug(S, locals())
                return nc
            ynT_s = P.dscr("ynT_s", [1024, NOWN], BF16)
            if 'selb' in self.dbg:
                d_selb = P.dout("dbg_selb", [128, 128], BF16)
                d_imp = P.dout("dbg_imp", [128, 128])
                self.dbg.discard('selb')
                self.dbg_sel = True
            if 'p4' not in self.skip:
                self.phase4(S, ps, locals())
            if self.upto == 'p4':
                self.finish_debug(S, locals())
                return nc
            es1.close()
            x1_s = P.dscr("x1_s", [NOWN, D])
            x1T_s = P.dscr("x1T_s", [D, NOWN], BF16)
            gT_s = P.dscr("gT_s", [64, NOWN])
            if 'p5' not in self.skip:
                self.phase5(S, ps, locals())
            if self.upto == 'p5':
                self.finish_debug(S, locals())
                return nc
            out = P.dout("out", [NOWN, D])
            self.phase6(S, ps, locals())
            self.finish_debug(S, locals())
        return nc

    def rope_tables(self, S, eng, pos_i, cs, scol, tagp, tmp):
        c_rope = self.c_rope
        posf, r, fr, ti, tf = tmp
        C, Ss = cs
        S.op(eng, lambda e: e.tensor_copy(out=posf, in_=pos_i), r=[tagp + 'pi'], w=[tagp + 'pf'])
        S.op(eng, lambda e: e.tensor_scalar(out=r, in0=posf, scalar1=scol, scalar2=c_rope[:, 0:1], op0=ALU.add, op1=ALU.mult),
             r=[tagp + 'pf', 'c_rope'], w=[tagp + 'r'])
        self.range_reduce(S, eng, fr, r, ti, tf, tagp + 'r', [tagp + 'ti', tagp + 'tf', tagp + 'fr'])
        S.op('act', lambda e: e.activation(out=Ss, in_=fr, func=AF.Sin, scale=c_rope[:, 1:2]), r=[tagp + 'fr', 'c_rope'], w=[tagp + 'S'])
        S.op(eng, lambda e: e.tensor_scalar(out=r, in0=r, scalar1=0.25, scalar2=None, op0=ALU.add), r=[tagp + 'r'], w=[tagp + 'r'])
        self.range_reduce(S, eng, fr, r, ti, tf, tagp + 'r', [tagp + 'ti', tagp + 'tf', tagp + 'fr'])
        S.op('act', lambda e: e.activation(out=C, in_=fr, func=AF.Sin, scale=TWO_PI), r=[tagp + 'fr'], w=[tagp + 'C'])

    def rope_apply(self, S, src_ps, src_tag, dst, dst_tag, C, Ss, ctag, stag, raw, t1, t2, ps_sw, k):
        c_pswap = self.c_pswap
        S.op('act', lambda e: e.copy(out=raw, in_=src_ps), r=[src_tag], w=[('raw', k)])
        S.op('pe', lambda e: e.matmul(ps_sw, lhsT=c_pswap[:], rhs=raw, start=True, stop=True), r=[('raw', k), 'c_pswap'], w=[('pssw', k)])
        S.op('dve', lambda e: e.tensor_tensor(out=t1, in0=raw, in1=C, op=ALU.mult), r=[('raw', k), ctag], w=[('t1', k)])
        S.op('dve', lambda e: e.tensor_tensor(out=t2, in0=ps_sw, in1=Ss, op=ALU.mult), r=[('pssw', k), stag], w=[('t2', k)])
        if len(dst.shape) == 3:
            S.op('dve', lambda e: e.tensor_tensor(out=dst, in0=t1.rearrange("p (a b) -> p a b", b=128), in1=t2.rearrange("p (a b) -> p a b", b=128), op=ALU.add),
                 r=[('t1', k), ('t2', k)], w=[dst_tag])
        else:
            S.op('dve', lambda e: e.tensor_tensor(out=dst, in0=t1, in1=t2, op=ALU.add), r=[('t1', k), ('t2', k)], w=[dst_tag])

    def phase1(self, S, ps, npass, L):
        nc = self.nc
        P = self
        xT = self.ins["xT"]
        w_in = self.ins["w_in"]
        posw = self.ins["posw"]
        c_kvalid = self.c_kvalid
        if npass == 1:
            uT_s, kcT_s, vcT_s = L['uT_s'], L['kcT_s'], L['vcT_s']
            wc0, wn = 0, NC1
        else:
            ksT, kwT, qT, Vs, Vw, gates = (L[k] for k in ['ksT', 'kwT', 'qT', 'Vs', 'Vw', 'gates'])
            wc0, wn = NC1, NC2
        with ExitStack() as es:
            w_sb = P.sb(es, "w_sb", [128, 16, wn], BF16)
            xg = [P.sb(es, "xg%d" % i, [128, 16, 512], BF16) for i in range(2)]
            if npass == 1:
                ust = [P.sb(es, "ust%d" % i, [128, 512], BF16) for i in range(4)]
            else:
                raw = [P.sb(es, "raw%d" % i, [128, 512], BF16) for i in range(2)]
                t1 = [P.sb(es, "t1_%d" % i, [128, 512]) for i in range(2)]
                t2 = [P.sb(es, "t2_%d" % i, [128, 512]) for i in range(2)]
                Ct = [P.sb(es, "Ct%d" % i, [128, 512]) for i in range(2)]
                St = [P.sb(es, "St%d" % i, [128, 512]) for i in range(2)]
                posi = [P.sb(es, "posi%d" % i, [128, 512], I32) for i in range(2)]
                rtmp = (P.sb(es, "r_pf", [128, 512]), P.sb(es, "r_r", [128, 512]), P.sb(es, "r_fr", [128, 512]),
                        P.sb(es, "r_ti", [128, 512], I32), P.sb(es, "r_tf", [128, 512]))
            wv = w_in.rearrange("(c p) n -> p c n", p=128)
            wblocks = [(c0, min(wn, c0 + 800)) for c0 in range(0, wn, 800)]
            for (c0, c1) in wblocks:
                for dcg in range(0, 16, 4):
                    S.dma('pool', w_sb[:, dcg:dcg + 4, c0:c1], wv[:, dcg:dcg + 4, wc0 + c0:wc0 + c1], w=[('w_sb', c0, dcg)])
            wtags = [('w_sb', c0, dcg) for (c0, c1) in wblocks for dcg in range(0, 16, 4)]
            xv = xT.rearrange("(c p) t -> p c t", p=128)
            nfm = 0
            ropek = 0
            nst = 0
            for g in range(NG):
                own = g >= OWN_G0
                xb = xg[g % 2]
                xtag = ('xg', g % 2)
                for dcg in range(0, 16, 4):
                    S.dma('pool', xb[:, dcg:dcg + 4, :], xv[:, dcg:dcg + 4, g * 512:(g + 1) * 512], w=[(xtag, dcg)])
                xtags = [(xtag, dcg) for dcg in range(0, 16, 4)]
                if npass == 1:
                    fm = [('u', i, C_U + 128 * i) for i in range(8)] + [('kc', 0, C_KC), ('vc', 0, C_VC)]
                else:
                    pi = posi[g % 2]
                    tp = 'rt%d' % (g % 2)
                    S.dma('sp', pi[:], posw[0:1, g * 512:(g + 1) * 512].to_broadcast([128, 512]), w=[tp + 'pi'])
                    C, Ss = Ct[g % 2], St[g % 2]
                    self.rope_tables(S, 'dve', pi[:], (C[:], Ss[:]), 0.0, tp, tuple(t[:] for t in rtmp))
                    fm = [('ks', 0, C_KS)]
                    if own:
                        fm += [('q', i, C_Q + 128 * i) for i in range(8)]
                    if g >= OWN_G0 - 1:
                        fm += [('kw', 0, C_KW)]
                for (kind, i, c0) in fm:
                    bank = nfm % 4
                    nfm += 1
                    pt = ps[bank]
                    ptag = ('ps', bank)
                    for dc in range(16):
                        S.op('pe', lambda e: e.matmul(pt[:, :], lhsT=w_sb[:, dc, c0:c0 + 128], rhs=xb[:, dc, :], start=(dc == 0), stop=(dc == 15)),
                             r=(wtags + xtags) if dc in (0, 15) else [], w=[ptag])
                    if kind in ('u', 'kc', 'vc'):
                        ub = ust[nst % 4]
                        utag = ('ust', nst % 4)
                        nst += 1
                        S.op('act', lambda e: e.copy(out=ub[:], in_=pt[:, :]), r=[ptag], w=[utag])
                        if kind == 'u':
                            dst = uT_s[i * 128:(i + 1) * 128, g * 512:(g + 1) * 512]
                        elif kind == 'kc':
                            dst = kcT_s[:, g * 512:(g + 1) * 512]
                        else:
                            dst = vcT_s[:, g * 512:(g + 1) * 512]
                        S.dma('sp', dst, ub[:], r=[utag], w=[('scr', kind, i, g)])
                    else:
                        if kind == 'ks':
                            dst, dtag = ksT[:, g * 512:(g + 1) * 512], ('ksT', g)
                        elif kind == 'q':
                            go = g - OWN_G0
                            dst, dtag = qT[:, 4 * go:4 * go + 4, i, :], ('qT', i, go)
                        else:
                            go = g - (OWN_G0 - 1)
                            dst, dtag = kwT[:, go * 512:(go + 1) * 512], ('kwT', go)
                        k = ropek % 2
                        ropek += 1
                        self.rope_apply(S, pt[:, :], ptag, dst, dtag, C[:], Ss[:], tp + 'C', tp + 'S',
                                        raw[k][:], t1[k][:], t2[k][:], ps[4 + k][:, :], k)
                if npass == 2:
                    for tt in range(4):
                        wt = g * 4 + tt
                        bank = 6 + (wt % 2)
                        pt = ps[bank]
                        ptag = ('ps', bank)
                        ncols = 128 + (176 if g >= OWN_G0 - 1 else 0)
                        for dc in range(16):
                            S.op('pe', lambda e: e.matmul(pt[:, 0:ncols], lhsT=xb[:, dc, tt * 128:(tt + 1) * 128], rhs=w_sb[:, dc, C_VS:C_VS + ncols],
                                                          start=(dc == 0), stop=(dc == 15)),
                                 r=(wtags + xtags) if dc in (0, 15) else [], w=[ptag])
                        S.op('act', lambda e: e.copy(out=Vs[:, wt, :, 0:64], in_=pt[:, 0:128].rearrange("p (h d) -> p h d", h=2)), r=[ptag], w=[('Vs', wt)])
                        if g >= OWN_G0 - 1:
                            lw = wt - (OWN_G0 - 1) * 4
                            S.op('act', lambda e: e.copy(out=Vw[:, lw, :, 0:64], in_=pt[:, 128:256].rearrange("p (h d) -> p h d", h=2)), r=[ptag], w=[('Vw', lw)])
                        if own:
                            lo = wt - OWN_G0 * 4
                            S.op('act', lambda e: e.activation(out=gates[:, lo, :], in_=pt[:, 256:304], func=AF.Sigmoid), r=[ptag], w=[('gates', lo)])
            if npass == 2:
                for h in range(2):
                    S.op('dve', lambda e: e.tensor_copy(out=Vs[:, :, h, 64], in_=c_kvalid[:, 0:64]), r=['c_kvalid'], w=[('Vs1', h)])
                    S.op('dve', lambda e: e.tensor_copy(out=Vw[:, :, h, 64], in_=c_kvalid[:, 44:64]), r=['c_kvalid'], w=[('Vw1', h)])
            S.barrier()

    def phase2(self, S, ps, L):
        P = self
        kcT_s, vcT_s = L['kcT_s'], L['vcT_s']
        kcT, Vc1O = L['kcT'], L['Vc1O']
        posc = self.ins['posc']
        c_nvalid = self.c_nvalid
        with ExitStack() as es:
            raws = [P.sb(es, "cr_k", [128, W], BF16), P.sb(es, "cr_v", [128, W], BF16)]
            w1 = [P.sb(es, "w1k", [128, 32, 128], BF16), P.sb(es, "w1v", [128, 32, 128], BF16)]
            w2k = [P.sb(es, "w2k%d" % h, [128, 128], BF16) for h in range(2)]
            w2v = P.sb(es, "w2v", [128, 64], BF16)
            peT = [P.sb(es, "peTk", [128, 32], BF16), P.sb(es, "peTv", [128, 32], BF16)]
            cb = P.sb(es, "cb", [128, 2])
            gT = [P.sb(es, "gT%d" % h, [128, 512], BF16) for h in range(2)]
            Omat = P.sb(es, "Omat", [128, 4, 128], BF16)
            posi = P.sb(es, "cposi", [128, 512], I32)
            Ct, St = P.sb(es, "cCt", [128, 512]), P.sb(es, "cSt", [128, 512])
            rtmp = (P.sb(es, "c_pf", [128, 512]), P.sb(es, "c_r", [128, 512]), P.sb(es, "c_fr", [128, 512]),
                    P.sb(es, "c_ti", [128, 512], I32), P.sb(es, "c_tf", [128, 512]))
            raw, t1, t2 = P.sb(es, "craw", [128, 512], BF16), P.sb(es, "ct1", [128, 512]), P.sb(es, "ct2", [128, 512])
            S.dma('sp', raws[0][:], kcT_s, w=['cr0'])
            S.dma('sp', raws[1][:], vcT_s, w=['cr1'])
            for kv, nm in enumerate(['w_cmp_k1', 'w_cmp_v1']):
                src = self.ins[nm].rearrange("(i d) c -> d i c", d=64)
                for h in range(2):
                    S.dma('pool', w1[kv][64 * h:64 * h + 64, :, :], src, w=[('w1', kv, h)])
            for h in range(2):
                S.op('dve', lambda e: e.memset(w2k[h][:], 0.0), w=[('w2k', h)])
                S.dma('pool', w2k[h][:, 64 * h:64 * h + 64], self.ins['w_cmp_k2'], w=[('w2k', h)])
                S.op('dve', lambda e: e.memset(gT[h][:], 0.0), w=[('gT', h)])
            S.dma('pool', w2v[:], self.ins['w_cmp_v2'], w=['w2v'])
            S.dma('pool', peT[0][:], self.ins['cmp_pos_kT'], w=['peT0'])
            S.dma('pool', peT[1][:], self.ins['cmp_pos_vT'], w=['peT1'])
            S.dma('pool', Omat[:], self.ins['omat'], w=['Omat'])
            S.dma('sp', posi[:], posc[0:1, :].to_broadcast([128, 512]), w=['cpi'])
            self.rope_tables(S, 'pool', posi[:], (Ct[:], St[:]), 15.5, 'c', tuple(t[:] for t in rtmp))
            for kv in range(2):
                for i in range(32):
                    S.op('pe', lambda e: e.matmul(ps[7][:, 0:1], lhsT=w1[kv][0:64, i, :], rhs=peT[kv][0:64, i:i + 1], start=(i == 0), stop=(i == 31)),
                         r=[('w1', kv, 0), 'peT%d' % kv], w=[('ps', 7)])
                S.op('act', lambda e: e.copy(out=cb[:, kv:kv + 1], in_=ps[7][:, 0:1]), r=[('ps', 7)], w=[('cb', kv)])
                for h in range(2):
                    pt = ps[h]
                    for i in range(32):
                        S.op('pe', lambda e: e.matmul(pt[:, 0:511], lhsT=w1[kv][64 * h:64 * h + 64, i, :],
                                                      rhs=raws[kv][64 * h:64 * h + 64, i:i + 16 * 510 + 1:16], start=(i == 0), stop=(i == 31)),
                             r=[('w1', kv, h), 'cr%d' % kv], w=[('ps', h)])
                    S.op('act', lambda e: e.activation(out=gT[h][:, 0:511], in_=pt[:, 0:511], func=AF.Gelu_apprx_tanh, bias=cb[:, kv:kv + 1]),
                         r=[('ps', h), ('cb', kv)], w=[('gT', h)])
                if kv == 0:
                    for h in range(2):
                        S.op('pe', lambda e: e.matmul(ps[2][:, :], lhsT=w2k[h][:], rhs=gT[h][:], start=(h == 0), stop=(h == 1)),
                             r=[('gT', h), ('w2k', h)], w=[('ps', 2)])
                    self.rope_apply(S, ps[2][:, :], ('ps', 2), kcT[:], 'kcT', Ct[:], St[:], 'cC', 'cS', raw[:], t1[:], t2[:], ps[3][:, :], 0)
                else:
                    for a_ in range(4):
                        for h in range(2):
                            S.op('pe', lambda e: e.matmul(ps[2][:, (a_ * 2 + h) * 64:(a_ * 2 + h + 1) * 64], lhsT=gT[h][:, a_ * 128:(a_ + 1) * 128], rhs=w2v[:],
                                                          start=True, stop=True), r=[('gT', h), 'w2v'], w=[('ps', 2)])
                    for a_ in range(4):
                        S.op('dve', lambda e: e.tensor_scalar(out=Vc1O[:, a_, :, 0:64], in0=ps[2][:, a_ * 128:(a_ + 1) * 128].rearrange("p (h d) -> p h d", h=2),
                                                              scalar1=c_nvalid[:, a_:a_ + 1], scalar2=None, op0=ALU.mult),
                             r=[('ps', 2), 'c_nvalid'], w=[('Vc1O', a_)])
                        for h in range(2):
                            S.op('dve', lambda e: e.tensor_copy(out=Vc1O[:, a_, h, 64:65], in_=c_nvalid[:, a_:a_ + 1]), r=['c_nvalid'], w=[('Vc1O', a_)])
                            S.op('dve', lambda e: e.tensor_scalar(out=Vc1O[:, a_, h, 65:193], in0=Omat[:, a_, :], scalar1=c_nvalid[:, a_:a_ + 1], scalar2=None, op0=ALU.mult),
                                 r=['Omat', 'c_nvalid'], w=[('Vc1O', a_)])
            S.barrier()

    def phase3(self, S, ps, L):
        P = self
        uT_s, ysT_s = L['uT_s'], L['ysT_s']
        c_ident = self.c_ident
        nst = self.nst
        with ExitStack() as es:
            lam = P.sb(es, "lam", [128, 32, 3])
            sbD = P.sb(es, "sbD", [32, 32])
            iota1 = P.sb(es, "iota1", [128, 512])
            iotar = P.sb(es, "iotar", [128, 512])
            wglu = P.sb(es, "wglu", [128, 8, 1024], BF16)
            S.dma('sp', lam[:], self.ins['s5_lam'], w=['lam'])
            S.dma('sp', sbD[:], self.ins['s5_d'], w=['sbD'])
            S.dma('sp', iota1[:], self.ins['iota1'], w=['iota1'])
            S.dma('sp', iotar[:], self.ins['iotar'], w=['iotar'])
            S.dma('pool', wglu[:], self.ins['w_glu'].rearrange("(c p) n -> p c n", p=128), w=['wglu'])
            v = {}
            for nm in ['dt', 'mag', 'thc', 'sn', 'cs', 'ar', 'ai', 'zr', 'den', 'fr', 'fi', 'ta', 'tb', 'tfr', 'ttf', 'lrdt', 'm512']:
                v[nm] = P.sb(es, "v_" + nm, [128, 32])
            v_ti = P.sb(es, "v_ti", [128, 32], I32)
            lhsB = P.sb(es, "lhsB", [32, 32, 2, 128], BF16)
            bdC = P.sb(es, "bdC", [128, 32, 2, 32], BF16)
            dd = P.sb(es, "Ddiag", [32, 32, 32], BF16)
            esp = ExitStack()
            sbB = P.sb(esp, "sbB", [128, 32, 2, 16])
            sbC = P.sb(esp, "sbC", [128, 32, 2, 16])
            bd = P.sb(esp, "bdB", [128, 32, 2, 32])
            bt1 = P.sb(esp, "bt1", [128, 32, 16])
            bt2 = P.sb(esp, "bt2", [128, 32, 16])
            S.dma('sp', sbB[:], self.ins['s5_b'], w=['sbB'])
            S.dma('sp', sbC[:], self.ins['s5_c'], w=['sbC'])
            lr, li, ldt = lam[:, :, 0], lam[:, :, 1], lam[:, :, 2]
            TT = lambda o, a_, b_, op, r, w: S.op('dve', lambda e: e.tensor_tensor(out=o, in0=a_, in1=b_, op=op), r=r, w=w)
            TS = lambda o, a_, s1, op, r, w: S.op('dve', lambda e: e.tensor_scalar(out=o, in0=a_, scalar1=s1, scalar2=None, op0=op), r=r, w=w)
            S.op('act', lambda e: e.activation(out=v['dt'][:], in_=ldt, func=AF.Exp), r=['lam'], w=['v_dt'])
            TT(v['ta'][:], lr, v['dt'][:], ALU.mult, ['lam', 'v_dt'], ['v_ta'])
            S.op('act', lambda e: e.activation(out=v['mag'][:], in_=v['ta'][:], func=AF.Exp), r=['v_ta'], w=['v_mag'])
            S.op('act', lambda e: e.activation(out=v['m512'][:], in_=v['ta'][:], func=AF.Exp, scale=512.0), r=['v_ta'], w=['v_m512'])
            S.op('dve', lambda e: e.tensor_copy(out=v['lrdt'][:], in_=v['ta'][:]), r=['v_ta'], w=['v_lrdt'])
            TT(v['thc'][:], li, v['dt'][:], ALU.mult, ['lam', 'v_dt'], ['v_thc'])
            TS(v['thc'][:], v['thc'][:], 1.0 / TWO_PI, ALU.mult, ['v_thc'], ['v_thc'])
            self.range_reduce(S, 'dve', v['tfr'][:], v['thc'][:], v_ti[:], v['ttf'][:], 'v_thc', ['v_ti', 'v_ttf', 'v_tfr'])
            S.op('act', lambda e: e.activation(out=v['sn'][:], in_=v['tfr'][:], func=AF.Sin, scale=TWO_PI), r=['v_tfr'], w=['v_sn'])
            TS(v['tb'][:], v['thc'][:], 0.25, ALU.add, ['v_thc'], ['v_tb'])
            self.range_reduce(S, 'dve', v['tfr'][:], v['tb'][:], v_ti[:], v['ttf'][:], 'v_tb', ['v_ti', 'v_ttf', 'v_tfr'])
            S.op('act', lambda e: e.activation(out=v['cs'][:], in_=v['tfr'][:], func=AF.Sin, scale=TWO_PI), r=['v_tfr'], w=['v_cs'])
            TT(v['ar'][:], v['mag'][:], v['cs'][:], ALU.mult, ['v_mag', 'v_cs'], ['v_ar'])
            TT(v['ai'][:], v['mag'][:], v['sn'][:], ALU.mult, ['v_mag', 'v_sn'], ['v_ai'])
            TS(v['zr'][:], v['ar'][:], -1.0, ALU.add, ['v_ar'], ['v_zr'])
            TT(v['den'][:], lr, lr, ALU.mult, ['lam'], ['v_den'])
            TT(v['ta'][:], li, li, ALU.mult, ['lam'], ['v_ta'])
            TT(v['den'][:], v['den'][:], v['ta'][:], ALU.add, ['v_den', 'v_ta'], ['v_den'])
            S.op('dve', lambda e: e.reciprocal(out=v['den'][:], in_=v['den'][:]), r=['v_den'], w=['v_den'])
            TT(v['ta'][:], v['zr'][:], lr, ALU.mult, ['v_zr', 'lam'], ['v_ta'])
            TT(v['tb'][:], v['ai'][:], li, ALU.mult, ['v_ai', 'lam'], ['v_tb'])
            TT(v['fr'][:], v['ta'][:], v['tb'][:], ALU.add, ['v_ta', 'v_tb'], ['v_fr'])
            TT(v['fr'][:], v['fr'][:], v['den'][:], ALU.mult, ['v_fr', 'v_den'], ['v_fr'])
            TT(v['ta'][:], v['ai'][:], lr, ALU.mult, ['v_ai', 'lam'], ['v_ta'])
            TT(v['tb'][:], v['zr'][:], li, ALU.mult, ['v_zr', 'lam'], ['v_tb'])
            TT(v['fi'][:], v['ta'][:], v['tb'][:], ALU.subtract, ['v_ta', 'v_tb'], ['v_fi'])
            TT(v['fi'][:], v['fi'][:], v['den'][:], ALU.mult, ['v_fi', 'v_den'], ['v_fi'])
            S.op('pool', lambda e: e.memset(bd[:], 0.0), w=['bd'])
            frb = v['fr'][:].unsqueeze(2).to_broadcast([128, 32, 16])
            fib = v['fi'][:].unsqueeze(2).to_broadcast([128, 32, 16])
            br_, bi_ = sbB[:, :, 0, :], sbB[:, :, 1, :]
            TT(bt1[:], br_, frb, ALU.mult, ['sbB', 'v_fr'], ['bt1'])
            TT(bt2[:], bi_, fib, ALU.mult, ['sbB', 'v_fi'], ['bt2'])
            for gl in range(2):
                sl = slice(64 * gl, 64 * gl + 64)
                TT(bd[sl, :, 0, 16 * gl:16 * gl + 16], bt1[sl], bt2[sl], ALU.subtract, ['bt1', 'bt2', 'bd'], ['bd'])
            TT(bt1[:], bi_, frb, ALU.mult, ['sbB', 'v_fr', 'bd'], ['bt1'])
            TT(bt2[:], br_, fib, ALU.mult, ['sbB', 'v_fi', 'bd'], ['bt2'])
            for gl in range(2):
                sl = slice(64 * gl, 64 * gl + 64)
                TT(bd[sl, :, 1, 16 * gl:16 * gl + 16], bt1[sl], bt2[sl], ALU.add, ['bt1', 'bt2', 'bd'], ['bd'])
            for st in range(nst):
                for ri in range(2):
                    S.op('pe', lambda e: e.transpose(out=ps[0][0:32, ri * 128:(ri + 1) * 128], in_=bd[:, st, ri, :], identity=c_ident[:]), r=['bd', 'c_ident'], w=[('ps', 0)])
                S.op('act', lambda e: e.copy(out=lhsB[:, st, :, :], in_=ps[0][0:32, 0:256].rearrange("p (a b) -> p a b", b=128)), r=[('ps', 0)], w=['lhsB'])
            S.op('pool', lambda e: e.memset(bdC[:], 0.0), w=['bdC'])
            for gl in range(2):
                sl = slice(64 * gl, 64 * gl + 64)
                S.op('dve', lambda e: e.tensor_copy(out=bdC[sl, :, 0, 16 * gl:16 * gl + 16], in_=sbC[sl, :, 0, :]), r=['sbC', 'bdC'], w=['bdC'])
                TS(bdC[sl, :, 1, 16 * gl:16 * gl + 16], sbC[sl, :, 1, :], -1.0, ALU.mult, ['sbC', 'bdC'], ['bdC'])
            TT(dd[:], c_ident[0:32, 0:32].unsqueeze(1).to_broadcast([32, 32, 32]), sbD[:].unsqueeze(2).to_broadcast([32, 32, 32]), ALU.mult, ['c_ident', 'sbD'], ['dd'])

            S.barrier()
            esp.close()
            ub = [P.sb(es, "ub%d" % i, [32, W], BF16) for i in range(2)]
            Ct = [P.sb(es, "sCt%d" % i, [128, 512]) for i in range(2)]
            St = [P.sb(es, "sSt%d" % i, [128, 512]) for i in range(2)]
            magb = [P.sb(es, "magb%d" % i, [128, 512]) for i in range(2)]
            rtmp = (P.sb(es, "s_r", [128, 512]), P.sb(es, "s_fr", [128, 512]), None, P.sb(es, "s_tf", [128, 512]))
            Gt = P.sb(es, "s_G", [128, 512])
            rtmp2 = [P.sb(es, "s_fr%d" % i, [128, 512]) for i in range(3)]
            DA = [P.sb(es, "DA%d" % i, [128, 1024]) for i in range(2)]
            DB = [P.sb(es, "DB%d" % i, [128, 1024]) for i in range(2)]
            acol = [P.sb(es, "acol%d" % i, [128, 4]) for i in range(2)]
            junk = P.sb(es, "junk", [128, 1024], BF16)
            Ecol = P.sb(es, "Ecol", [128, 12, 2])
            Sst = [P.sb(es, "Sst%d" % i, [128, 2]) for i in range(2)]
            tst = P.sb(es, "tst", [128, 2])
            m_ = [[P.sb(es, "m%d_%d" % (k_, i), [128, 512]) for i in range(4)] for k_ in range(2)]
            mo = [P.sb(es, "mo%d" % i, [128, 512]) for i in range(4)]
            zre = [P.sb(es, "zre%d" % i, [128, 512]) for i in range(2)]
            zim = [P.sb(es, "zim%d" % i, [128, 512]) for i in range(2)]
            sre, sim_ = P.sb(es, "sre", [128, 512], BF16), P.sb(es, "sim", [128, 512], BF16)
            S0 = P.sb(es, "S0", [128, 2])
            tcol = P.sb(es, "tcol", [128, 2])
            nident = P.sb(es, "nident", [128, 128])
            S.op('dve', lambda e: e.tensor_scalar(out=nident[:], in0=c_ident[:], scalar1=-1.0, scalar2=None, op0=ALU.mult), r=['c_ident'], w=['nident'])
            yT_all = P.sb(es, "yT_all", [128, 8, NOWN], BF16)
            if nst < 32:
                S.op('pool', lambda e: e.memset(yT_all[:], 0.0), w=['yT_all'])
            def prepA(st):
                S.dma('sp', ub[st % 2][:], uT_s[st * 32:(st + 1) * 32, :], w=[('ub', st % 2)])
                C, Sn, mb = Ct[st % 2], St[st % 2], magb[st % 2]
                tp = 's5t%d' % (st % 2)
                da, db = DA[st % 2], DB[st % 2]
                r_, fr_, tf_ = rtmp[0][:], rtmp[1][:], rtmp[3][:]
                E_ = 'dve'
                S.op(E_, lambda e: e.tensor_scalar(out=r_, in0=iota1[:], scalar1=v['thc'][:, st:st + 1], scalar2=None, op0=ALU.mult), r=['iota1', 'v_thc'], w=['s_r'])
                self.range_reduce(S, E_, fr_, r_, None, tf_, 's_r', ['s_ti', 's_tf', 's_fr'])
                S.op('act', lambda e: e.activation(out=Sn[:], in_=fr_, func=AF.Sin, scale=TWO_PI), r=['s_fr'], w=[tp + 'S'])
                S.op(E_, lambda e: e.tensor_scalar(out=r_, in0=r_, scalar1=0.25, scalar2=None, op0=ALU.add), r=['s_r'], w=['s_r'])
                self.range_reduce(S, E_, rtmp2[0][:], r_, None, tf_, 's_r', ['s_ti', 's_tf', 's_fr2'])
                S.op('act', lambda e: e.activation(out=C[:], in_=rtmp2[0][:], func=AF.Sin, scale=TWO_PI), r=['s_fr2'], w=[tp + 'C'])
                S.op(E_, lambda e: e.tensor_scalar(out=r_, in0=iotar[:], scalar1=v['thc'][:, st:st + 1], scalar2=None, op0=ALU.mult), r=['iotar', 'v_thc'], w=['s_r'])
                self.range_reduce(S, E_, rtmp2[1][:], r_, None, tf_, 's_r', ['s_ti', 's_tf', 's_fr3'])
                S.op('act', lambda e: e.activation(out=db[:, 0:512], in_=rtmp2[1][:], func=AF.Sin, scale=TWO_PI), r=['s_fr3'], w=[tp + 'DB'])
                S.op(E_, lambda e: e.tensor_scalar(out=r_, in0=r_, scalar1=0.25, scalar2=None, op0=ALU.add), r=['s_r'], w=['s_r'])
                self.range_reduce(S, E_, rtmp2[2][:], r_, None, tf_, 's_r', ['s_ti', 's_tf', 's_fr4'])
                S.op('act', lambda e: e.activation(out=da[:, 0:512], in_=rtmp2[2][:], func=AF.Sin, scale=TWO_PI), r=['s_fr4'], w=[tp + 'DA'])
                S.op('act', lambda e: e.activation(out=Gt[:], in_=iotar[:], func=AF.Exp, scale=v['lrdt'][:, st:st + 1]), r=['iotar', 'v_lrdt'], w=['s_G'])

            def prepB(st):
                C, Sn, mb = Ct[st % 2], St[st % 2], magb[st % 2]
                tp = 's5t%d' % (st % 2)
                da, db, ac = DA[st % 2], DB[st % 2], acol[st % 2]
                E_ = 'dve'
                S.op(E_, lambda e: e.tensor_copy(out=mb[:], in_=v['mag'][:, st:st + 1].to_broadcast([128, 512])), r=['v_mag'], w=[tp + 'M'])
                S.op(E_, lambda e: e.tensor_tensor(out=da[:, 0:512], in0=da[:, 0:512], in1=Gt[:], op=ALU.mult), r=[tp + 'DA', 's_G'], w=[tp + 'DA'])
                S.op(E_, lambda e: e.tensor_tensor(out=db[:, 0:512], in0=db[:, 0:512], in1=Gt[:], op=ALU.mult), r=[tp + 'DB', 's_G'], w=[tp + 'DB'])
                S.op(E_, lambda e: e.tensor_copy(out=db[:, 512:1024], in_=da[:, 0:512]), r=[tp + 'DA', tp + 'DB'], w=[tp + 'DB'])
                S.op(E_, lambda e: e.tensor_scalar(out=da[:, 512:1024], in0=db[:, 0:512], scalar1=-1.0, scalar2=None, op0=ALU.mult), r=[tp + 'DB', tp + 'DA'], w=[tp + 'DA'])
                S.op(E_, lambda e: e.tensor_tensor(out=ac[:, 0:1], in0=C[:, 511:512], in1=v['m512'][:, st:st + 1], op=ALU.mult), r=[tp + 'C', 'v_m512'], w=[tp + 'A'])
                S.op(E_, lambda e: e.tensor_tensor(out=ac[:, 1:2], in0=Sn[:, 511:512], in1=v['m512'][:, st:st + 1], op=ALU.mult), r=[tp + 'S', 'v_m512'], w=[tp + 'A'])
                S.op(E_, lambda e: e.tensor_scalar(out=ac[:, 2:3], in0=ac[:, 1:2], scalar1=-1.0, scalar2=None, op0=ALU.mult), r=[tp + 'A'], w=[tp + 'A'])

            prepA(0)
            prepB(0)
            for st in range(nst):
                u_ = ub[st % 2]
                utag = ('ub', st % 2)
                C, Sn, mb = Ct[st % 2], St[st % 2], magb[st % 2]
                tp = 's5t%d' % (st % 2)
                if st + 1 < nst:
                    prepA(st + 1)
                def front(g):
                    usl = u_[:, g * 512:(g + 1) * 512]
                    mm = m_[g % 2]
                    mt = lambda i: ('m', g % 2, i)
                    wb = 2 + 2 * (g % 2)
                    S.op('pe', lambda e: e.matmul(ps[0][:, :], lhsT=lhsB[:, st, 0, :], rhs=usl, start=True, stop=True), r=['lhsB', utag], w=[('ps', 0)])
                    S.op('pe', lambda e: e.matmul(ps[1][:, :], lhsT=lhsB[:, st, 1, :], rhs=usl, start=True, stop=True), r=['lhsB', utag], w=[('ps', 1)])
                    TT(mm[0][:], ps[0][:, :], C[:], ALU.mult, [('ps', 0), tp + 'C'], [mt(0)])
                    TT(mm[1][:], ps[1][:, :], Sn[:], ALU.mult, [('ps', 1), tp + 'S'], [mt(1)])
                    TT(mm[2][:], ps[1][:, :], C[:], ALU.mult, [('ps', 1), tp + 'C'], [mt(2)])
                    TT(mm[3][:], ps[0][:, :], Sn[:], ALU.mult, [('ps', 0), tp + 'S'], [mt(3)])
                    S.op('pe', lambda e: e.matmul(ps[wb][:, :], lhsT=c_ident[:], rhs=mm[0][:], start=True, stop=False), r=['c_ident', mt(0)], w=[('ps', wb)])
                    S.op('pe', lambda e: e.matmul(ps[wb][:, :], lhsT=c_ident[:], rhs=mm[1][:], start=False, stop=True), r=['c_ident', mt(1)], w=[('ps', wb)])
                    S.op('pe', lambda e: e.matmul(ps[wb + 1][:, :], lhsT=c_ident[:], rhs=mm[2][:], start=True, stop=False), r=['c_ident', mt(2)], w=[('ps', wb + 1)])
                    S.op('pe', lambda e: e.matmul(ps[wb + 1][:, :], lhsT=nident[:], rhs=mm[3][:], start=False, stop=True), r=['nident', mt(3)], w=[('ps', wb + 1)])

                def back(g):
                    own = g >= OWN_G0
                    usl = u_[:, g * 512:(g + 1) * 512]
                    wb = 2 + 2 * (g % 2)
                    zr_, zi_ = zre[g % 2], zim[g % 2]
                    S.op('dve', lambda e: e.tensor_tensor_scan(out=zr_[:], data0=mb[:], data1=ps[wb][:, :], initial=S0[:, 0:1], op0=ALU.mult, op1=ALU.add),
                         r=[tp + 'M', ('ps', wb), 'S0'], w=[('zre', g % 2)])
                    S.op('dve', lambda e: e.tensor_tensor_scan(out=zi_[:], data0=mb[:], data1=ps[wb + 1][:, :], initial=S0[:, 1:2], op0=ALU.mult, op1=ALU.add),
                         r=[tp + 'M', ('ps', wb + 1), 'S0'], w=[('zim', g % 2)])
                    c5, s5 = C[:, 511:512], Sn[:, 511:512]
                    TT(tcol[:, 0:1], zi_[:, 511:512], s5, ALU.mult, [('zim', g % 2), tp + 'S'], ['tcol'])
                    TT(tcol[:, 1:2], zi_[:, 511:512], c5, ALU.mult, [('zim', g % 2), tp + 'C'], ['tcol'])
                    S.op('dve', lambda e: e.scalar_tensor_tensor(out=S0[:, 0:1], in0=zr_[:, 511:512], scalar=c5, in1=tcol[:, 0:1], op0=ALU.mult, op1=ALU.subtract),
                         r=[('zre', g % 2), tp + 'C', 'tcol', 'S0'], w=['S0'])
                    S.op('dve', lambda e: e.scalar_tensor_tensor(out=S0[:, 1:2], in0=zr_[:, 511:512], scalar=s5, in1=tcol[:, 1:2], op0=ALU.mult, op1=ALU.add),
                         r=[('zre', g % 2), tp + 'S', 'tcol', 'S0'], w=['S0'])
                    if own:
                        TT(mo[0][:], zr_[:], C[:], ALU.mult, [('zre', g % 2), tp + 'C', 'mo0'], ['mo0'])
                        TT(mo[1][:], zi_[:], Sn[:], ALU.mult, [('zim', g % 2), tp + 'S', 'mo1'], ['mo1'])
                        TT(mo[2][:], zr_[:], Sn[:], ALU.mult, [('zre', g % 2), tp + 'S', 'mo2'], ['mo2'])
                        TT(mo[3][:], zi_[:], C[:], ALU.mult, [('zim', g % 2), tp + 'C', 'mo3'], ['mo3'])
                        S.op('pool', lambda e: e.tensor_tensor(out=sre[:], in0=mo[0][:], in1=mo[1][:], op=ALU.subtract), r=['mo0', 'mo1'], w=['sre'])
                        S.op('pool', lambda e: e.tensor_tensor(out=sim_[:], in0=mo[2][:], in1=mo[3][:], op=ALU.add), r=['mo2', 'mo3'], w=['sim'])
                        S.op('pe', lambda e: e.matmul(ps[6][0:32, :], lhsT=bdC[:, st, 0, :], rhs=sre[:], start=True, stop=False), r=['bdC', 'sre'], w=[('ps', 6)])
                        S.op('pe', lambda e: e.matmul(ps[6][0:32, :], lhsT=bdC[:, st, 1, :], rhs=sim_[:], start=False, stop=False), r=['bdC', 'sim'], w=[('ps', 6)])
                        S.op('pe', lambda e: e.matmul(ps[6][0:32, :], lhsT=dd[:, st, :], rhs=usl, start=False, stop=True), r=['dd', utag], w=[('ps', 6)])
                        go = g - OWN_G0
                        po = (st % 4) * 32
                        S.op('act', lambda e: e.activation(out=yT_all[po:po + 32, st // 4, go * 512:(go + 1) * 512], in_=ps[6][0:32, :], func=AF.Gelu_apprx_tanh),
                             r=[('ps', 6)], w=['yT_all'])

                da, db, ac = DA[st % 2], DB[st % 2], acol[st % 2]
                S.op('dve', lambda e: e.memset(Sst[0][:], 0.0), r=[('Sst', 0)], w=[('Sst', 0)])
                for g in range(OWN_G0):
                    usl = u_[:, g * 512:(g + 1) * 512]
                    xb = (g % 2) * 2
                    S.op('pe', lambda e: e.matmul(ps[xb][:, :], lhsT=lhsB[:, st, 0, :], rhs=usl, start=True, stop=True), r=['lhsB', utag], w=[('ps', xb)])
                    S.op('pe', lambda e: e.matmul(ps[xb + 1][:, :], lhsT=lhsB[:, st, 1, :], rhs=usl, start=True, stop=True), r=['lhsB', utag], w=[('ps', xb + 1)])
                    xcat = self.psbig[:, xb * 512:(xb + 2) * 512]
                    S.op('dve', lambda e: e.scalar_tensor_tensor(out=junk[:], in0=xcat, scalar=1.0, in1=da[:], op0=ALU.mult, op1=ALU.mult, accum_out=Ecol[:, g, 0:1]),
                         r=[('ps', xb), ('ps', xb + 1), tp + 'DA', 'junk'], w=['junk', ('E', g)])
                    S.op('dve', lambda e: e.scalar_tensor_tensor(out=junk[:], in0=xcat, scalar=1.0, in1=db[:], op0=ALU.mult, op1=ALU.mult, accum_out=Ecol[:, g, 1:2]),
                         r=[('ps', xb), ('ps', xb + 1), tp + 'DB', 'junk'], w=['junk', ('E', g)])
                    cur, nxt = Sst[g % 2], Sst[(g + 1) % 2]
                    ct, nt = ('Sst', g % 2), ('Sst', (g + 1) % 2)
                    S.op('act', lambda e: e.activation(out=tst[:, 0:1], in_=cur[:, 0:1], func=AF.Identity, scale=ac[:, 0:1], bias=Ecol[:, g, 0:1]), r=[ct, tp + 'A', ('E', g)], w=['tst'])
                    S.op('act', lambda e: e.activation(out=nxt[:, 0:1], in_=cur[:, 1:2], func=AF.Identity, scale=ac[:, 2:3], bias=tst[:, 0:1]), r=[ct, tp + 'A', 'tst'], w=[nt])
                    S.op('act', lambda e: e.activation(out=tst[:, 1:2], in_=cur[:, 1:2], func=AF.Identity, scale=ac[:, 0:1], bias=Ecol[:, g, 1:2]), r=[ct, tp + 'A', ('E', g)], w=['tst'])
                    S.op('act', lambda e: e.activation(out=nxt[:, 1:2], in_=cur[:, 0:1], func=AF.Identity, scale=ac[:, 1:2], bias=tst[:, 1:2]), r=[ct, tp + 'A', 'tst'], w=[nt])
                S.op('act', lambda e: e.copy(out=S0[:], in_=Sst[OWN_G0 % 2][:]), r=[('Sst', OWN_G0 % 2), 'S0'], w=['S0'])
                if st + 1 < nst:
                    prepB(st + 1)
                front(OWN_G0)
                for g in range(OWN_G0, NG):
                    if g + 1 < NG:
                        front(g + 1)
                    back(g)
            if 'yT_all' in self.dbg:
                S.dma('sp', L['d_yT'], yT_all[:], r=['yT_all'], w=['d_yT'])
            sg = [P.sb(es, "sg%d" % i, [128, 512]) for i in range(2)]
            yo = [P.sb(es, "yo%d" % i, [128, 512], BF16) for i in range(2)]
            n = 0
            for cc in range(8):
                for tg in range(4):
                    b = 4 + n % 2
                    for kc in range(8):
                        S.op('pe', lambda e: e.matmul(ps[b][:, :], lhsT=wglu[:, kc, cc * 128:(cc + 1) * 128], rhs=yT_all[:, kc, tg * 512:(tg + 1) * 512],
                                                      start=(kc == 0), stop=(kc == 7)), r=['wglu', 'yT_all'], w=[('ps', b)])
                    S.op('act', lambda e: e.activation(out=sg[n % 2][:], in_=ps[b][:, :], func=AF.Sigmoid), r=[('ps', b)], w=[('sg', n % 2)])
                    TT(yo[n % 2][:], yT_all[:, cc, tg * 512:(tg + 1) * 512], sg[n % 2][:], ALU.mult, ['yT_all', ('sg', n % 2)], [('yo', n % 2)])
                    S.dma('sp', ysT_s[cc * 128:(cc + 1) * 128, tg * 512:(tg + 1) * 512], yo[n % 2][:], r=[('yo', n % 2)], w=[('ysT_s', cc, tg)])
                    n += 1
            S.barrier()

    def phase4(self, S, ps, L):
        P = self
        ksT, kwT, qT, Vs, Vw, gates, kcT, Vc1O = (L[k] for k in ['ksT', 'kwT', 'qT', 'Vs', 'Vw', 'gates', 'kcT', 'Vc1O'])
        ynT_s = L['ynT_s']
        c_identb, c_ident, c_pad = self.c_identb, self.c_ident, self.c_pad
        nq = self.nq
        with ExitStack() as es:
            causalT = P.sb(es, "causalT", [128, 128], BF16)
            antiT = P.sb(es, "antiT", [128, 128], BF16)
            cmask = P.sb(es, "cmask", [128, 16, 2, 128], BF16)
            sidx = P.sb(es, "sidx", [128, 128])
            qhalf = P.sb(es, "qhalf", [128, 1])
            real = P.sb(es, "real", [128, 128])
            first = P.sb(es, "first", [128, 128])
            rm2 = P.sb(es, "rm2", [128, 128])
            S.dma('pool', causalT[:], self.ins['causalT'], w=['causalT'])
            S.dma('pool', antiT[:], self.ins['antiT'], w=['antiT'])
            S.dma('pool', cmask[:], self.ins['cmask'], w=['cmask'])
            S.dma('sp', sidx[:], self.ins['sidx'], w=['sidx'])
            S.dma('sp', qhalf[:], self.ins['qhalf'], w=['qhalf'])
            S.op('dve', lambda e: e.tensor_scalar(out=first[:], in0=sidx[:], scalar1=c_pad[:, 0:1], scalar2=None, op0=ALU.subtract), r=['sidx', 'c_pad'], w=['first'])
            S.op('dve', lambda e: e.tensor_scalar(out=real[:], in0=first[:], scalar1=0.0, scalar2=None, op0=ALU.is_ge), r=['first'], w=['real'])
            S.op('dve', lambda e: e.tensor_scalar(out=first[:], in0=first[:], scalar1=0.0, scalar2=None, op0=ALU.is_equal), r=['first'], w=['first'])
            S.op('dve', lambda e: e.tensor_scalar(out=rm2[:], in0=real[:], scalar1=-2.0, scalar2=None, op0=ALU.add), r=['real'], w=['rm2'])
            et = [P.sb(es, "e%d" % i, [128, 512], BF16) for i in range(4)]
            mk = [P.sb(es, "mk%d" % i, [128, 128], BF16) for i in range(3)]
            o_all = P.sb(es, "o_all", [128, 8, 2, 64])
            den8 = P.sb(es, "den8", [128, 8])
            rg8 = P.sb(es, "rg8", [128, 8])
            imp = P.sb(es, "imp", [128, 128])
            A_ = P.sb(es, "A_", [128, 128])
            vld = P.sb(es, "vld", [128, 128])
            fo = P.sb(es, "fo", [128, 128])
            score = P.sb(es, "score", [128, 128])
            swk = P.sb(es, "swk", [128, 128])
            top = P.sb(es, "top", [128, 16])
            selb = P.sb(es, "selb", [128, 128], BF16)
            maskq = P.sb(es, "maskq", [128, W], BF16)
            yst = [P.sb(es, "yst%d" % i, [128, 8, 128], BF16) for i in range(2)]
            cnt = {'u': 0, 'm': 0}

            pend = []
            LA = 3
            cnt['i'] = 0

            def flush(n=0):
                while len(pend) > n:
                    pend.pop(0)()

            def unit(k, qi, keys, ktag, Vrhs, vtag, ncols, mask, mtag, accbank, first_, last_):
                for hf in range(2):
                    i = cnt['i']
                    cnt['i'] += 1
                    sb_ = i % 4
                    pt = ps[sb_]
                    e_ = et[i % 4]
                    etag = ('e', i % 4)
                    S.op('pe', lambda e: e.matmul(pt[:, :], lhsT=keys, rhs=qT[64 * k:64 * k + 64, qi, 4 * hf:4 * hf + 4, :],
                                                  start=True, stop=True), r=[ktag, 'qT'], w=[('ps', sb_)])
                    S.op('act', lambda e: e.activation(out=e_[:], in_=pt[:, :], func=AF.Exp, scale=0.125), r=[('ps', sb_)], w=[etag])
                    if mask is not None:
                        S.op('dve', lambda e: e.tensor_tensor(out=e_[:].rearrange("p (h q) -> p h q", h=4), in0=e_[:].rearrange("p (h q) -> p h q", h=4),
                                                              in1=mask.unsqueeze(1).to_broadcast([128, 4, 128]), op=ALU.mult), r=[etag, mtag], w=[etag])

                    def B(hf=hf, e_=e_, etag=etag, Vrhs=Vrhs, vtag=vtag, ncols=ncols, accbank=accbank, first_=first_, last_=last_):
                        for hh in range(4):
                            j = 4 * hf + hh
                            bank, c0 = accbank(j)
                            S.op('pe', lambda e: e.matmul(ps[bank][:, c0:c0 + ncols], lhsT=e_[:, hh * 128:(hh + 1) * 128], rhs=Vrhs,
                                                          start=(first_ and c0 == 0), stop=last_, skip_group_check=True),
                                 r=[etag, vtag], w=[('ps', bank)])
                    pend.append(B)
                    flush(LA)

            def evac(k, qi, accbank, br, init):
                for j in range(8):
                    bank, c0 = accbank(j)
                    S.op('dve', lambda e: e.tensor_copy(out=den8[:, j:j + 1], in_=ps[bank][:, c0 + 64:c0 + 65]), r=[('ps', bank)], w=['den8'])
                S.op('dve', lambda e: e.tensor_scalar(out=den8[:], in0=den8[:], scalar1=1e-30, scalar2=None, op0=ALU.max), r=['den8'], w=['den8'])
                S.op('dve', lambda e: e.reciprocal(out=den8[:], in_=den8[:]), r=['den8'], w=['den8'])
                gsl = gates[:, qi, 24 * k + br:24 * k + br + 22:3]
                S.op('dve', lambda e: e.tensor_tensor(out=rg8[:], in0=den8[:], in1=gsl, op=ALU.mult), r=['den8', 'gates'], w=['rg8'])
                for j in range(8):
                    bank, c0 = accbank(j)
                    if init:
                        S.op('dve', lambda e: e.tensor_scalar(out=o_all[:, j, k, :], in0=ps[bank][:, c0:c0 + 64], scalar1=rg8[:, j:j + 1], scalar2=None, op0=ALU.mult),
                             r=[('ps', bank), 'rg8'], w=['o_all'])
                    else:
                        S.op('dve', lambda e: e.scalar_tensor_tensor(out=o_all[:, j, k, :], in0=ps[bank][:, c0:c0 + 64], scalar=rg8[:, j:j + 1], in1=o_all[:, j, k, :],
                                                                     op0=ALU.mult, op1=ALU.add), r=[('ps', bank), 'rg8', 'o_all'], w=['o_all'])

            for qi in range(nq):
                wt = 48 + qi
                for k in range(2):
                    accC = lambda j: (4 + j // 2, (j % 2) * 193)
                    for a_ in range(4):
                        m_ = cmask[:, qi, a_ - 2, :] if a_ >= 2 else None
                        unit(k, qi, kcT[64 * k:64 * k + 64, a_ * 128:(a_ + 1) * 128], 'kcT', Vc1O[:, a_, k, :], 'Vc1O', 193, m_, 'cmask', accC, a_ == 0, a_ == 3)
                    flush()
                    evac(k, qi, accC, 0, True)
                    for j in range(8):
                        bank, c0 = accC(j)
                        if j == 0:
                            S.op('dve', lambda e: e.tensor_scalar(out=imp[:], in0=ps[bank][:, c0 + 65:c0 + 193], scalar1=den8[:, j:j + 1], scalar2=None, op0=ALU.mult),
                                 r=[('ps', bank), 'den8'], w=['imp'])
                        else:
                            S.op('dve', lambda e: e.scalar_tensor_tensor(out=imp[:], in0=ps[bank][:, c0 + 65:c0 + 193], scalar=den8[:, j:j + 1], in1=imp[:],
                                                                         op0=ALU.mult, op1=ALU.add), r=[('ps', bank), 'den8', 'imp'], w=['imp'])
                    S.op('dve', lambda e: e.tensor_scalar(out=A_[:], in0=sidx[:], scalar1=float(-2 * wt), scalar2=qhalf[:, 0:1], op0=ALU.add, op1=ALU.subtract),
                         r=['sidx', 'qhalf'], w=['A_'])
                    S.op('dve', lambda e: e.tensor_scalar(out=vld[:], in0=A_[:], scalar1=0.0, scalar2=None, op0=ALU.is_le), r=['A_'], w=['vld'])
                    S.op('dve', lambda e: e.tensor_scalar(out=fo[:], in0=A_[:], scalar1=-1.0, scalar2=None, op0=ALU.is_ge), r=['A_'], w=['fo'])
                    S.op('dve', lambda e: e.tensor_tensor(out=vld[:], in0=vld[:], in1=real[:], op=ALU.mult), r=['vld', 'real'], w=['vld'])
                    S.op('dve', lambda e: e.tensor_tensor(out=fo[:], in0=fo[:], in1=first[:], op=ALU.max), r=['fo', 'first'], w=['fo'])
                    S.op('dve', lambda e: e.tensor_tensor(out=fo[:], in0=fo[:], in1=vld[:], op=ALU.mult), r=['fo', 'vld'], w=['fo'])
                    S.op('dve', lambda e: e.scalar_tensor_tensor(out=score[:], in0=imp[:], scalar=1.0, in1=vld[:], op0=ALU.add, op1=ALU.mult), r=['imp', 'vld'], w=['score'])
                    S.op('dve', lambda e: e.scalar_tensor_tensor(out=score[:], in0=fo[:], scalar=1e4, in1=score[:], op0=ALU.mult, op1=ALU.add), r=['fo', 'score'], w=['score'])
                    S.op('dve', lambda e: e.tensor_tensor(out=score[:], in0=score[:], in1=rm2[:], op=ALU.add), r=['score', 'rm2'], w=['score'])
                    S.op('dve', lambda e: e.max(out=top[:, 0:8], in_=score[:]), r=['score'], w=['top'])
                    S.op('dve', lambda e: e.match_replace(out=swk[:], in_to_replace=top[:, 0:8], in_values=score[:], imm_value=-1e30), r=['score', 'top'], w=['swk'])
                    S.op('dve', lambda e: e.max(out=top[:, 8:16], in_=swk[:]), r=['swk'], w=['top'])
                    S.op('dve', lambda e: e.tensor_scalar(out=selb[:], in0=score[:], scalar1=top[:, 15:16], scalar2=None, op0=ALU.is_ge), r=['score', 'top'], w=['selb'])
                    if getattr(self, 'dbg_sel', False) and qi == self.dbg_qi and k == self.dbg_k:
                        S.dma('sp', L['d_selb'], selb[:], r=['selb'], w=['d_selb'])
                        S.dma('sp', L['d_imp'], imp[:], r=['imp'], w=['d_imp'])
                    S.op('dve', lambda e: e.tensor_copy(out=maskq[:].rearrange("p (s j) -> p s j", j=64), in_=selb[:].unsqueeze(2).to_broadcast([128, 128, 64])),
                         r=['selb'], w=['maskq'])
                    accS = lambda j: (4 + j // 4, (j % 4) * 65)
                    for kt in range(wt + 1):
                        mi = cnt['m'] % 2
                        cnt['m'] += 1
                        mb = 6 + mi
                        S.op('pe', lambda e: e.matmul(ps[mb][:, 0:128], lhsT=maskq[:, kt * 128:(kt + 1) * 128], rhs=c_identb[:],
                                                      start=True, stop=True), r=['maskq', 'c_identb'], w=[('ps', mb)])
                        if kt == wt:
                            S.op('dve', lambda e: e.tensor_tensor(out=mk[mi][:], in0=ps[mb][:, 0:128], in1=causalT[:], op=ALU.mult), r=[('ps', mb), 'causalT'], w=[('mk', mi)])
                        else:
                            S.op('act', lambda e: e.copy(out=mk[mi][:], in_=ps[mb][:, 0:128]), r=[('ps', mb)], w=[('mk', mi)])
                        unit(k, qi, ksT[64 * k:64 * k + 64, kt * 128:(kt + 1) * 128], 'ksT', Vs[:, kt, k, :], 'Vs', 65, mk[mi][:], ('mk', mi), accS, kt == 0, kt == wt)
                    flush()
                    evac(k, qi, accS, 1, False)
                    for wi, wkt in enumerate(range(wt - 4, wt + 1)):
                        if wkt == wt:
                            m_, mt_ = causalT[:], 'causalT'
                        elif wkt == wt - 4:
                            m_, mt_ = antiT[:], 'antiT'
                        else:
                            m_, mt_ = None, None
                        lw = wkt - 44
                        unit(k, qi, kwT[64 * k:64 * k + 64, lw * 128:(lw + 1) * 128], 'kwT', Vw[:, lw, k, :], 'Vw', 65, m_, mt_, accS, wi == 0, wi == 4)
                    flush()
                    evac(k, qi, accS, 2, False)
                ys = yst[qi % 2]
                for half in range(2):
                    tb = 6 + half
                    for i4 in range(4):
                        i = half * 4 + i4
                        S.op('pe', lambda e: e.transpose(out=ps[tb][:, i4 * 128:(i4 + 1) * 128], in_=o_all[:, i, :, :], identity=c_ident[:]),
                             r=['o_all', 'c_ident'], w=[('ps', tb)])
                    S.op('act', lambda e: e.copy(out=ys[:, half * 4:half * 4 + 4, :], in_=ps[tb][:, :].rearrange("p (a b) -> p a b", b=128)),
                         r=[('ps', tb)], w=[('yst', qi % 2)])
                S.dma('sp', ynT_s.rearrange("(i p) t -> p i t", p=128)[:, :, qi * 128:(qi + 1) * 128], ys[:], r=[('yst', qi % 2)], w=[('ynT_s', qi)])
            S.barrier()

    def layer_norm(self, S, h, htag, out, otag, g_bc, b_bc, tmp, k):
        stats, mv = tmp
        for c in range(4):
            S.op('dve', lambda e: e.bn_stats(out=stats[:, c, :], in_=h[:, c * 512:(c + 1) * 512]), r=[htag], w=[('lnst', k)])
        S.op('dve', lambda e: e.bn_aggr(out=mv[:, 0:2], in_=stats[:].rearrange("p a b -> p (a b)")), r=[('lnst', k)], w=[('lnmv', k)])
        S.op('dve', lambda e: e.tensor_scalar(out=mv[:, 2:3], in0=mv[:, 1:2], scalar1=LN_EPS, scalar2=None, op0=ALU.add), r=[('lnmv', k)], w=[('lnmv', k)])
        S.op('act', lambda e: e.activation(out=mv[:, 2:3], in_=mv[:, 2:3], func=AF.Sqrt), r=[('lnmv', k)], w=[('lnmv', k)])
        S.op('dve', lambda e: e.reciprocal(out=mv[:, 2:3], in_=mv[:, 2:3]), r=[('lnmv', k)], w=[('lnmv', k)])
        S.op('dve', lambda e: e.tensor_scalar(out=h, in0=h, scalar1=mv[:, 0:1], scalar2=mv[:, 2:3], op0=ALU.subtract, op1=ALU.mult),
             r=[htag, ('lnmv', k)], w=[htag])
        S.op('dve', lambda e: e.tensor_tensor(out=h, in0=h, in1=g_bc, op=ALU.mult), r=[htag, 'ln_g'], w=[htag])
        S.op('dve', lambda e: e.tensor_tensor(out=out, in0=h, in1=b_bc, op=ALU.add), r=[htag, 'ln_b'], w=[otag])

    def phase5(self, S, ps, L):
        P = self
        ysT_s, ynT_s, x1_s, x1T_s, gT_s = L['ysT_s'], L['ynT_s'], L['x1_s'], L['x1T_s'], L['gT_s']
        x_own = self.ins['x_own']
        c_ident = self.c_ident
        ntt = self.ntt
        with ExitStack() as es:
            wo = P.sb(es, "wo", [128, 16, D], BF16)
            mixT = [P.sb(es, "mixT%d" % i, [128, 16, 128], BF16) for i in range(2)]
            g_bc = P.sb(es, "ln1g", [128, D])
            b_bc = P.sb(es, "ln1b", [128, D])
            wr = P.sb(es, "wr", [128, 16, 64])
            rb = P.sb(es, "rb", [128, 64])
            wov = self.ins['w_out'].rearrange("(c p) n -> p c n", p=128)
            for dcg in range(0, 16, 2):
                S.dma('pool', wo[:, dcg:dcg + 2, :], wov[:, dcg:dcg + 2, :], w=[('wo', dcg)])
            wotags = [('wo', dcg) for dcg in range(0, 16, 2)]
            S.dma('sp', g_bc[:], self.ins['ln1_g'][0:1, :].to_broadcast([128, D]), w=['ln_g'])
            S.dma('sp', b_bc[:], self.ins['ln1_b'][0:1, :].to_broadcast([128, D]), w=['ln_b'])
            S.dma('sp', wr[:], self.ins['w_router'].rearrange("(c p) n -> p c n", p=128), w=['wr'])
            S.dma('sp', rb[:], self.ins['router_bias'][0:1, :].to_broadcast([128, 64]), w=['rb'])
            xo = [P.sb(es, "xo%d" % i, [128, D]) for i in range(2)]
            h_ = [P.sb(es, "h%d" % i, [128, D]) for i in range(2)]
            x1 = [P.sb(es, "x1_%d" % i, [128, D]) for i in range(2)]
            lnt = (P.sb(es, "lnstats", [128, 4, 6]), P.sb(es, "lnmv", [128, 4]))
            xTf = P.sb(es, "xTf", [128, 16, 128])
            xTb = [P.sb(es, "xTb%d" % i, [128, 16, 128], BF16) for i in range(2)]
            r_ = {}
            for nm, shp in [('aff', [128, 64]), ('bia', [128, 64]), ('m1', [128, 8]), ('eq', [128, 64]), ('b2', [128, 64]), ('m2', [128, 8]), ('gs', [128, 8]),
                            ('t8', [128, 8]), ('gm', [128, 8]), ('pen', [128, 8]), ('msk', [128, 64]), ('em', [128, 64]), ('wv', [128, 64]), ('ss', [128, 2]),
                            ('gate', [128, 64])]:
                r_[nm] = P.sb(es, "r_" + nm, shp)
            gst = [P.sb(es, "gst%d" % i, [64, 128]) for i in range(2)]
            TT = lambda o, a_, b_, op, r, w: S.op('dve', lambda e: e.tensor_tensor(out=o, in0=a_, in1=b_, op=op), r=r, w=w)
            TS = lambda o, a_, s1, op, r, w: S.op('dve', lambda e: e.tensor_scalar(out=o, in0=a_, scalar1=s1, scalar2=None, op0=op), r=r, w=w)
            for tt in range(ntt):
                k = tt % 2
                S.dma('sp', xo[k][:], x_own[tt * 128:(tt + 1) * 128, :], w=[('xo', k)])
                S.dma('sp', mixT[k][:, 0:8, :], ysT_s.rearrange("(c p) t -> p c t", p=128)[:, :, tt * 128:(tt + 1) * 128], w=[('mixT0', k)])
                S.dma('sp', mixT[k][:, 8:16, :], ynT_s.rearrange("(c p) t -> p c t", p=128)[:, :, tt * 128:(tt + 1) * 128], w=[('mixT1', k)])
                for dg in range(4):
                    for kc in range(16):
                        S.op('pe', lambda e: e.matmul(ps[dg][:, :], lhsT=mixT[k][:, kc, :], rhs=wo[:, kc, dg * 512:(dg + 1) * 512],
                                                      start=(kc == 0), stop=(kc == 15)), r=wotags + [('mixT0', k), ('mixT1', k)] if kc in (0, 15) else [], w=[('ps', dg)])
                    S.op('dve', lambda e: e.scalar_tensor_tensor(out=h_[k][:, dg * 512:(dg + 1) * 512], in0=xo[k][:, dg * 512:(dg + 1) * 512], scalar=ALPHA,
                                                                 in1=ps[dg][:, :], op0=ALU.mult, op1=ALU.add), r=[('xo', k), ('ps', dg)], w=[('h', k)])
                import os as _os
                cut = int(_os.environ.get('P5CUT', '9'))
                if cut <= 1:
                    S.dma('sp', x1_s[tt * 128:(tt + 1) * 128, :], h_[k][:], r=[('h', k)], w=[('x1_s', tt)])
                    continue
                self.layer_norm(S, h_[k][:], ('h', k), x1[k][:], ('x1', k), g_bc[:], b_bc[:], lnt, 0)
                S.dma('sp', x1_s[tt * 128:(tt + 1) * 128, :], x1[k][:], r=[('x1', k)], w=[('x1_s', tt)])
                if cut <= 2:
                    continue
                for q4 in range(4):
                    tb = 4 + q4 % 2
                    for i4 in range(4):
                        dc = q4 * 4 + i4
                        S.op('pe', lambda e: e.transpose(out=ps[tb][:, i4 * 128:(i4 + 1) * 128], in_=x1[k][:, dc * 128:(dc + 1) * 128], identity=c_ident[:]),
                             r=[('x1', k), 'c_ident'], w=[('ps', tb)])
                    S.op('act', lambda e: e.copy(out=xTf[:, q4 * 4:q4 * 4 + 4, :], in_=ps[tb][:, :].rearrange("p (a b) -> p a b", b=128)), r=[('ps', tb)], w=['xTf'])
                    S.op('dve', lambda e: e.tensor_copy(out=xTb[k][:, q4 * 4:q4 * 4 + 4, :], in_=xTf[:, q4 * 4:q4 * 4 + 4, :]), r=['xTf'], w=[('xTb', k)])
                S.dma('sp', x1T_s.rearrange("(c p) t -> p c t", p=128)[:, :, tt * 128:(tt + 1) * 128], xTb[k][:], r=[('xTb', k)], w=[('x1T_s', tt)])
                if cut <= 3:
                    continue
                for dc in range(16):
                    S.op('pe', lambda e: e.matmul(ps[6][:, 0:64], lhsT=xTf[:, dc, :], rhs=wr[:, dc, :], start=(dc == 0), stop=(dc == 15)), r=['xTf', 'wr'], w=[('ps', 6)])
                S.op('act', lambda e: e.activation(out=r_['aff'][:], in_=ps[6][:, 0:64], func=AF.Sigmoid), r=[('ps', 6)], w=['aff'])
                if cut <= 4:
                    continue
                TT(r_['bia'][:], r_['aff'][:], rb[:], ALU.add, ['aff', 'rb'], ['bia'])
                b3 = r_['bia'][:].rearrange("p (g e) -> p g e", e=8)
                S.op('dve', lambda e: e.tensor_reduce(out=r_['m1'][:], in_=b3, axis=AX.X, op=ALU.max), r=['bia'], w=['m1'])
                TT(r_['eq'][:].rearrange("p (g e) -> p g e", e=8), b3, r_['m1'][:].unsqueeze(2).to_broadcast([128, 8, 8]), ALU.is_equal, ['bia', 'm1'], ['eq'])
                S.op('dve', lambda e: e.scalar_tensor_tensor(out=r_['b2'][:], in0=r_['eq'][:], scalar=-1e9, in1=r_['bia'][:], op0=ALU.mult, op1=ALU.add), r=['eq', 'bia'], w=['b2'])
                S.op('dve', lambda e: e.tensor_reduce(out=r_['m2'][:], in_=r_['b2'][:].rearrange("p (g e) -> p g e", e=8), axis=AX.X, op=ALU.max), r=['b2'], w=['m2'])
                TT(r_['gs'][:], r_['m1'][:], r_['m2'][:], ALU.add, ['m1', 'm2'], ['gs'])
                S.op('dve', lambda e: e.max(out=r_['t8'][:], in_=r_['gs'][:]), r=['gs'], w=['t8'])
                TS(r_['gm'][:], r_['gs'][:], r_['t8'][:, 3:4], ALU.is_ge, ['gs', 't8'], ['gm'])
                S.op('dve', lambda e: e.tensor_scalar(out=r_['pen'][:], in0=r_['gm'][:], scalar1=-1.0, scalar2=1e9, op0=ALU.add, op1=ALU.mult), r=['gm'], w=['pen'])
                m3 = r_['msk'][:].rearrange("p (g e) -> p g e", e=8)
                TT(m3, b3, r_['gm'][:].unsqueeze(2).to_broadcast([128, 8, 8]), ALU.mult, ['bia', 'gm'], ['msk'])
                TT(m3, m3, r_['pen'][:].unsqueeze(2).to_broadcast([128, 8, 8]), ALU.add, ['msk', 'pen'], ['msk'])
                S.op('dve', lambda e: e.max(out=r_['t8'][:], in_=r_['msk'][:]), r=['msk', 'gm'], w=['t8'])
                TS(r_['em'][:], r_['msk'][:], r_['t8'][:, 7:8], ALU.is_ge, ['msk', 't8'], ['em'])
                TT(r_['wv'][:], r_['aff'][:], r_['em'][:], ALU.mult, ['aff', 'em'], ['wv'])
                S.op('dve', lambda e: e.tensor_reduce(out=r_['ss'][:, 0:1], in_=r_['wv'][:], axis=AX.X, op=ALU.add), r=['wv'], w=['ss'])
                S.op('dve', lambda e: e.reciprocal(out=r_['ss'][:, 1:2], in_=r_['ss'][:, 0:1]), r=['ss'], w=['ss'])
                S.op('dve', lambda e: e.tensor_scalar(out=r_['gate'][:], in0=r_['wv'][:], scalar1=r_['ss'][:, 1:2], scalar2=2.5, op0=ALU.mult, op1=ALU.mult), r=['wv', 'ss'], w=['gate'])
                S.op('pe', lambda e: e.transpose(out=ps[7][0:64, 0:128], in_=r_['gate'][:], identity=c_ident[:]), r=['gate', 'c_ident'], w=[('ps', 7)])
                S.op('act', lambda e: e.copy(out=gst[k][:], in_=ps[7][0:64, 0:128]), r=[('ps', 7)], w=[('gst', k)])
                S.dma('sp', gT_s[:, tt * 128:(tt + 1) * 128], gst[k][:], r=[('gst', k)], w=[('gT_s', tt)])
            S.barrier()

    def phase6(self, S, ps, L):
        P = self
        x1_s, x1T_s, gT_s = L['x1_s'], L['x1T_s'], L['gT_s']
        out = L['out']
        c_ident = self.c_ident
        nexp = self.nexp
        with ExitStack() as es:
            acc = P.sb(es, "acc", [128, 8, D])
            xT = P.sb(es, "x1T", [128, 16, 1024], BF16)
            gT = P.sb(es, "gTsb", [64, 1024])
            g_bc = P.sb(es, "ln2g", [128, D])
            b_bc = P.sb(es, "ln2b", [128, D])
            S.dma('sp', g_bc[:], self.ins['ln2_g'][0:1, :].to_broadcast([128, D]), w=['ln_g'])
            S.dma('sp', b_bc[:], self.ins['ln2_b'][0:1, :].to_broadcast([128, D]), w=['ln_b'])
            wgb = [P.sb(es, "wgb%d" % i, [128, 16, 256], BF16) for i in range(2)]
            wub = [P.sb(es, "wub%d" % i, [128, 16, 256], BF16) for i in range(2)]
            wdb = [P.sb(es, "wdb%d" % i, [128, 4, D], BF16) for i in range(2)]
            hT = [P.sb(es, "hT%d" % i, [128, 4, 1024], BF16) for i in range(2)]
            gbc = [P.sb(es, "gbc%d" % tg, [128, 512]) for tg in range(2)]
            sg = [P.sb(es, "sgm%d" % i, [128, 512]) for i in range(2)]
            tm = [P.sb(es, "tm%d" % i, [128, 512]) for i in range(1)] * 2
            lnt = (P.sb(es, "ln2stats", [128, 4, 6]), P.sb(es, "ln2mv", [128, 4]))
            cnt = {'gu': 0, 'y': 0}
            NE = nexp

            def wsrc(e_):
                if e_ == 64:
                    return self.ins['ws_gate'], self.ins['ws_up'], self.ins['ws_down']
                return self.ins['w_gate'][e_], self.ins['w_up'][e_], self.ins['w_down'][e_]

            def loads_gu(ge, fh):
                wgs, wus, wds = wsrc(ge % NE)
                S.dma('pool', wgb[fh][:], wgs.rearrange("(c p) f -> p c f", p=128)[:, :, fh * 256:(fh + 1) * 256], w=[('wg', fh)])
                S.dma('pool', wub[fh][:], wus.rearrange("(c p) f -> p c f", p=128)[:, :, fh * 256:(fh + 1) * 256], w=[('wu', fh)])

            def loads_d(ge):
                wgs, wus, wds = wsrc(ge % NE)
                S.dma('pool', wdb[ge % 2][:], wds.rearrange("(c p) d -> p c d", p=128), w=[('wd', ge % 2)])

            def gate_bc(ge):
                e_ = ge % NE
                if e_ == 64:
                    return
                for tg in range(2):
                    S.op('pe', lambda e: e.matmul(ps[6][:, :], lhsT=c_ident[0:64, e_:e_ + 1].to_broadcast([64, 128]), rhs=gT[:, tg * 512:(tg + 1) * 512],
                                                  start=True, stop=True), r=['c_ident', 'gTsb'], w=[('ps', 6)])
                    S.op('act', lambda e: e.copy(out=gbc[tg][:], in_=ps[6][:, :]), r=[('ps', 6)], w=[('gbc', tg)])

            def GU(ge, fh):
                e_ = ge % NE
                shared = (e_ == 64)
                wg_, wu_ = wgb[fh], wub[fh]
                hb = hT[ge % 2]
                htag = ('hT', ge % 2)
                for tg in range(2):
                    for fc in range(2):
                        bg = (cnt['gu'] % 2) * 2
                        cnt['gu'] += 1
                        k = cnt['gu'] % 2
                        for dc in range(16):
                            S.op('pe', lambda e: e.matmul(ps[bg][:, :], lhsT=wg_[:, dc, fc * 128:(fc + 1) * 128], rhs=xT[:, dc, tg * 512:(tg + 1) * 512],
                                                          start=(dc == 0), stop=(dc == 15)), r=[('wg', fh), 'x1T'] if dc in (0, 15) else [], w=[('ps', bg)])
                        for dc in range(16):
                            S.op('pe', lambda e: e.matmul(ps[bg + 1][:, :], lhsT=wu_[:, dc, fc * 128:(fc + 1) * 128], rhs=xT[:, dc, tg * 512:(tg + 1) * 512],
                                                          start=(dc == 0), stop=(dc == 15)), r=[('wu', fh), 'x1T'] if dc in (0, 15) else [], w=[('ps', bg + 1)])
                        S.op('act', lambda e: e.activation(out=sg[k][:], in_=ps[bg][:, :], func=AF.Silu), r=[('ps', bg)], w=[('sg', k)])
                        hdst = hb[:, fh * 2 + fc, tg * 512:(tg + 1) * 512]
                        if shared:
                            S.op('dve', lambda e: e.tensor_tensor(out=hdst, in0=ps[bg + 1][:, :], in1=sg[k][:], op=ALU.mult), r=[('ps', bg + 1), ('sg', k)], w=[htag])
                        else:
                            S.op('dve', lambda e: e.tensor_tensor(out=tm[k][:], in0=ps[bg + 1][:, :], in1=sg[k][:], op=ALU.mult), r=[('ps', bg + 1), ('sg', k)], w=[('tm', 0)])
                            S.op('dve', lambda e: e.tensor_tensor(out=hdst, in0=tm[k][:], in1=gbc[tg][:], op=ALU.mult), r=[('tm', 0), ('gbc', tg)], w=[htag])

            def DN(ge):
                wd_ = wdb[ge % 2]
                hb = hT[ge % 2]
                htag = ('hT', ge % 2)
                for t8 in range(8):
                    for dg in range(4):
                        by = (4, 5, 7)[cnt['y'] % 3]
                        cnt['y'] += 1
                        for fc in range(4):
                            S.op('pe', lambda e: e.matmul(ps[by][:, :], lhsT=hb[:, fc, t8 * 128:(t8 + 1) * 128], rhs=wd_[:, fc, dg * 512:(dg + 1) * 512],
                                                          start=(fc == 0), stop=(fc == 3)), r=[htag, ('wd', ge % 2)], w=[('ps', by)])
                        asl = acc[:, t8, dg * 512:(dg + 1) * 512]
                        atag = ('acc', t8, dg)
                        S.op('dve', lambda e: e.tensor_tensor(out=asl, in0=asl, in1=ps[by][:, :], op=ALU.add), r=[('ps', by), atag], w=[atag])

            acctags = [('acc', t8, dg) for t8 in range(8) for dg in range(4)] + [('accrow', t8) for t8 in range(8)]
            for half in range(self.nhalf):
                tk0 = half * 1024
                S.dma('sp', xT[:], x1T_s.rearrange("(c p) t -> p c t", p=128)[:, :, tk0:tk0 + 1024], w=['x1T'])
                S.dma('sp', gT[:], gT_s[:, tk0:tk0 + 1024], w=['gTsb'])
                S.dma('sp', acc[:], x1_s[tk0:tk0 + 1024, :].rearrange("(t p) d -> p t d", p=128), w=acctags)
                for t8 in range(8):
                    S.op('act', lambda e: e.activation(out=acc[:, t8, :], in_=acc[:, t8, :], func=AF.Copy, scale=ALPHA),
                         r=[('acc', t8, dg) for dg in range(4)], w=[('acc', t8, dg) for dg in range(4)] + [('accrow', t8)])
                g0 = half * NE
                loads_gu(g0, 0)
                loads_gu(g0, 1)
                loads_d(g0)
                gate_bc(g0)
                GU(g0, 0)
                if NE > 1:
                    loads_gu(g0 + 1, 0)
                GU(g0, 1)
                if NE > 1:
                    loads_gu(g0 + 1, 1)
                    loads_d(g0 + 1)
                for ei in range(NE):
                    ge = g0 + ei
                    if ei + 1 < NE:
                        gate_bc(ge + 1)
                        GU(ge + 1, 0)
                        if ei + 2 < NE:
                            loads_gu(ge + 2, 0)
                        GU(ge + 1, 1)
                        if ei + 2 < NE:
                            loads_gu(ge + 2, 1)
                    DN(ge)
                    if ei + 2 < NE:
                        loads_d(ge + 2)
                for t8 in range(8):
                    k = t8 % 2
                    k = 0
                    S.op('dve', lambda e: e.tensor_copy(out=lnt[1][:, 3:4], in_=acc[:, t8, 0:1]), r=[('acc', t8, dg) for dg in range(4)], w=[('accrow', t8)] + [('acc', t8, dg) for dg in range(4)])
                    self.layer_norm(S, acc[:, t8, :], ('accrow', t8), acc[:, t8, :], ('accrow', t8), g_bc[:], b_bc[:], lnt, 1)
                    r0 = tk0 + t8 * 128
                    S.dma('sp', out[r0:r0 + 128, :], acc[:, t8, :], r=[('accrow', t8)], w=[('out', r0)])
            S.barrier()

    def finish_debug(self, S, L):
        for name in sorted(self.dbg):
            t = L[name]
            if name == 'ynT_s':
                t = t[:, 0:self.nq * 128]
            shape = list(t.shape)
            o = self.dout("dbg_" + name, shape, t.dtype)
            S.dma('sp', o, t if isinstance(t, bass.AP) else t[:], w=[('dbg', name)])
        S.barrier()


def host_constants():
    c = {}
    c["identf"] = np.eye(128, dtype=np.float32)
    pm = np.zeros((128, 128), np.float32)
    for m in range(128):
        k = (m % 64 + 32) % 64 + 64 * (m // 64)
        pm[k, m] = 1.0
    c["pswap"] = pm
    rc = np.zeros((128, 4), np.float32)
    inv = 10000.0 ** (-(np.arange(32, dtype=np.float64)) / 32.0)
    for p in range(128):
        rc[p, 0] = inv[p % 32] / TWO_PI
        rc[p, 1] = (-TWO_PI if (p % 64) < 32 else TWO_PI)
    c["ropecol"] = rc
    n_ = (np.arange(4)[None, :, None] * 128 + np.arange(128)[:, None, None])
    s_ = np.arange(128)[None, None, :]
    c["omat"] = ((16 * n_ <= 64 * s_ + 63) & (16 * n_ + 31 >= 64 * s_)).astype(np.float32)
    kk = np.arange(128)[:, None]
    qq = np.arange(128)[None, :]
    c["causalT"] = (kk <= qq).astype(np.float32)
    c["antiT"] = (kk > qq).astype(np.float32)
    nl = np.arange(128)[:, None, None, None]
    qi = np.arange(16)[None, :, None, None]
    aa = np.arange(2)[None, None, :, None] + 2
    ql = np.arange(128)[None, None, None, :]
    c["cmask"] = (16 * (128 * aa + nl) + 31 <= (48 + qi) * 128 + ql).astype(np.float32)
    c["sidx"] = np.broadcast_to(np.arange(128, dtype=np.float32)[None, :], (128, 128)).copy()
    c["qhalf"] = (np.arange(128) >= 64).astype(np.float32)[:, None]
    c["iota1"] = np.broadcast_to(np.arange(1, 513, dtype=np.float32)[None, :], (128, 512)).copy()
    c["iotar"] = np.broadcast_to((511.0 - np.arange(512, dtype=np.float32))[None, :], (128, 512)).copy()
    return c


def core_inputs(c, x, positions, shared):
    b, j = c // 4, c % 4
    t0 = NOWN * j
    pad = W - (t0 + NOWN)
    m = {}
    xT = np.zeros((D, W), np.float32)
    xT[:, pad:] = x[b, :t0 + NOWN, :].T
    m["xT"] = xT
    m["x_own"] = np.ascontiguousarray(x[b, t0:t0 + NOWN, :])
    pw = np.zeros((1, W), np.int32)
    pw[0, pad:] = positions[b, :t0 + NOWN]
    m["posw"] = pw
    pc = np.zeros((1, 512), np.int32)
    pc[0, :] = pw[0, np.minimum(np.arange(512) * 16, W - 1)]
    m["posc"] = pc
    widx = np.arange(64)[None, :] * 128 + np.arange(128)[:, None]
    m["kvalid"] = (widx >= pad).astype(np.float32)
    nidx = np.arange(4)[None, :] * 128 + np.arange(128)[:, None]
    m["nvalid"] = (nidx >= pad // 16).astype(np.float32)
    pc2 = np.zeros((128, 2), np.float32)
    pc2[:, 0] = pad // 64
    pc2[:, 1] = pad
    m["padcol"] = pc2
    m.update(shared)
    return m


def make_shared(inp):
    sh = host_constants()
    sh["w_in"] = np.ascontiguousarray(inp["w_in"][0][:, w_in_columns()])
    for nm in ["w_cmp_k1", "w_cmp_v1", "w_cmp_k2", "w_cmp_v2"]:
        sh[nm] = np.ascontiguousarray(inp[nm][0])
    G2 = lambda a: np.asarray(a[0]).reshape(32, 2, *a.shape[2:])
    lam = np.stack([G2(inp["lam_re"]), G2(inp["lam_im"]),
                    np.broadcast_to(G2(inp["log_dt"])[:, :, None], (32, 2, 64))], -1)
    sh["s5_lam"] = np.ascontiguousarray(lam.transpose(1, 2, 0, 3).reshape(128, 32, 3)).astype(np.float32)
    bb = np.stack([G2(inp["ssm_b_re"]), G2(inp["ssm_b_im"])], 3)
    sh["s5_b"] = np.ascontiguousarray(bb.transpose(1, 2, 0, 3, 4).reshape(128, 32, 2, 16)).astype(np.float32)
    cc = np.stack([G2(inp["ssm_c_re"]), G2(inp["ssm_c_im"])], 2)
    sh["s5_c"] = np.ascontiguousarray(cc.transpose(1, 4, 0, 2, 3).reshape(128, 32, 2, 16)).astype(np.float32)
    sh["s5_d"] = np.ascontiguousarray(G2(inp["ssm_d"]).transpose(1, 2, 0).reshape(32, 32)).astype(np.float32)
    sh["w_glu"] = np.ascontiguousarray(inp["w_glu"][0])
    wo = np.asarray(inp["w_out"][0])
    sh["w_out"] = np.ascontiguousarray(np.concatenate([wo[:1024], wo[1024:].reshape(16, 64, D)[q_head_order()].reshape(1024, D)], 0))
    for nm in ["ln1_g", "ln1_b", "ln2_g", "ln2_b", "w_router", "router_bias", "w_gate", "w_up", "w_down", "ws_gate", "ws_up", "ws_down"]:
        sh[nm] = np.ascontiguousarray(inp[nm][0])
    for nm in ["cmp_pos_k", "cmp_pos_v"]:
        t = np.asarray(inp[nm][0]).T
        sh[nm + "T"] = np.ascontiguousarray(np.concatenate([t, t], 0))
    return sh


_CACHE = {}


def kernel(**inputs):
    x = np.asarray(inputs["x"], np.float32)
    positions = np.asarray(inputs["positions"], np.int32)
    shared = make_shared(inputs)
    if 'nc' not in _CACHE:
        _CACHE['nc'] = Prog('all').build()
    nc = _CACHE['nc']
    in_maps = [core_inputs(c, x, positions, shared) for c in range(8)]
    res = run_bass_kernel_spmd(nc, in_maps, core_ids=list(range(8)))
    out = np.zeros((2, SEQ, D), np.float32)
    for c in range(8):
        b, j = c // 4, c % 4
        out[b, j * NOWN:(j + 1) * NOWN] = res.results[c]["out"]
    return out
```

```python
import math
from contextlib import ExitStack

import numpy as np
import concourse.bass as bass
import concourse.mybir as mybir
from concourse.bass_utils import run_bass_kernel_spmd

F32 = mybir.dt.float32
BF16 = mybir.dt.bfloat16
I32 = mybir.dt.int32
AF = mybir.ActivationFunctionType
ALU = mybir.AluOpType
AX = mybir.AxisListType

D = 2048
SEQ = 8192
W = 8192
NOWN = 2048
NG = W // 512
OWN_G0 = (W - NOWN) // 512
ALPHA = 2.0 ** 0.25
LN_EPS = 1e-5
TWO_PI = 2.0 * math.pi


class Sched:
    SEM_LIMIT = 20000

    def __init__(self, nc, es, n_dma_sems=40):
        self.nc = nc
        self.es = es
        self.E = {'pe': nc.tensor, 'act': nc.scalar, 'dve': nc.vector, 'pool': nc.gpsimd, 'sp': nc.sync}
        self.cur = {}
        self.known = {e: {} for e in self.E}
        self.lastw = {}
        self.reads = {}
        self.nsem = 0
        self.dma_slots = {'hw': [[self._newsem('dmah'), 0] for _ in range(n_dma_sems // 2)],
                          'sw': [[self._newsem('dmas'), 0] for _ in range(n_dma_sems // 2)]}
        self.dma_rr = {'hw': 0, 'sw': 0}
        self.ninst = {e: 0 for e in self.E}

    def _newsem(self, name):
        self.nsem += 1
        return self.es.enter_context(self.nc.semaphore('%s_%d' % (name, self.nsem)))

    def _tick(self, eng):
        c = self.cur.get(eng)
        if c is None or c[1] >= self.SEM_LIMIT:
            c = [self._newsem(eng), 0]
            self.cur[eng] = c
        c[1] += 1
        return c[0], c[1]

    def _wait(self, eng, dep):
        sem, val, deng = dep
        k = id(sem)
        if self.known[eng].get(k, 0) >= val:
            return
        self.E[eng].wait_ge(sem, val)
        self.known[eng][k] = val

    def _deps(self, eng, r, w):
        deps = []
        for res in r:
            lw = self.lastw.get(res)
            if lw is not None:
                deps.append(lw)
        for res in w:
            lw = self.lastw.get(res)
            if lw is not None:
                deps.append(lw)
            rd = self.reads.get(res)
            if rd:
                deps.extend(rd[0].values())
                deps.extend(rd[1])
        for d in deps:
            if d[2] == eng and eng == 'pe':
                continue
            self._wait(eng, d)

    def _commit(self, me, r, w):
        for res in w:
            self.lastw[res] = me
            self.reads[res] = ({}, [])
        for res in r:
            if res in w:
                continue
            rd = self.reads.get(res)
            if rd is None:
                rd = ({}, [])
                self.reads[res] = rd
            if me[2] == 'dma':
                rd[1].append(me)
            else:
                rd[0][me[2]] = me

    def op(self, eng, fn, r=(), w=()):
        self._deps(eng, r, w)
        sem, val = self._tick(eng)
        inst = fn(self.E[eng])
        inst.then_inc(sem, 1)
        self.ninst[eng] += 1
        me = (sem, val, eng)
        self._commit(me, r, w)
        return me

    def dma(self, q, out, in_, r=(), w=(), **kw):
        self._deps(q, r, w)
        kind = 'sw' if q == 'pool' else 'hw'
        slots = self.dma_slots[kind]
        slot = slots[self.dma_rr[kind]]
        self.dma_rr[kind] = (self.dma_rr[kind] + 1) % len(slots)
        if slot[1] > 0:
            self._wait(q, (slot[0], slot[1], 'dma'))
        if slot[1] >= self.SEM_LIMIT:
            slot[0] = self._newsem('dmax')
            slot[1] = 0
        slot[1] += 16
        self.E[q].dma_start(out=out, in_=in_, **kw).then_inc(slot[0], 16)
        self.ninst[q] += 1
        me = (slot[0], slot[1], 'dma')
        self._commit(me, r, w)
        return me

    def barrier(self):
        marks = []
        for e, c in self.cur.items():
            if c[1] > 0:
                marks.append((c[0], c[1], e))
        for slots in self.dma_slots.values():
            for slot in slots:
                if slot[1] > 0:
                    marks.append((slot[0], slot[1], 'dma'))
        for eng in self.E:
            for m in marks:
                if m[2] == eng and eng == 'pe':
                    continue
                self._wait(eng, m)
        self.lastw = {}
        self.reads = {}


C_U = 0
C_KC = 8 * 128
C_VC = 9 * 128
NC1 = 10 * 128
C_KS = 0
C_Q = 128
C_KW = 9 * 128
C_VS = 10 * 128
C_VW = 11 * 128
C_G = 12 * 128
NC2 = 12 * 128 + 48
NCOL = NC1 + NC2


def q_head_order():
    return [h for i in range(8) for h in (i, 8 + i)]


def w_in_columns():
    u = np.arange(0, 1024)
    q = np.arange(1024, 2048).reshape(16, 64)[q_head_order()].reshape(-1)
    kc = np.arange(2048, 2176)
    vc = np.arange(2176, 2304)
    ks = np.arange(2304, 2432)
    vs = np.arange(2432, 2560)
    kw = np.arange(2560, 2688)
    vw = np.arange(2688, 2816)
    g = np.arange(2816, 2864)
    cols = np.concatenate([u, kc, vc, ks, q, kw, vs, vw, g])
    assert cols.shape[0] == NCOL
    return cols


class Prog:
    def __init__(self, upto='all', dbg=()):
        self.upto = upto
        self.dbg = set(dbg)
        self.nc = bass.Bass("TRN2", target_bir_lowering=False)
        self.nq = 16
        self.nst = 32
        self.ntt = 16
        self.nexp = 65
        self.nhalf = 2
        self.skip = set()
        self.ext_scratch = set()
        self.dbg_qi, self.dbg_k = 0, 0
        self.ins = {}
        self.outs = {}

    def din(self, name, shape, dt=F32):
        t = self.nc.dram_tensor(name, list(shape), dt, kind="ExternalInput").ap()
        self.ins[name] = t
        return t

    def dout(self, name, shape, dt=F32):
        t = self.nc.dram_tensor(name, list(shape), dt, kind="ExternalOutput").ap()
        self.outs[name] = t
        return t

    def dscr(self, name, shape, dt=F32):
        if name in getattr(self, 'ext_scratch', ()):
            return self.din(name, shape, dt)
        return self.nc.dram_tensor(name, list(shape), dt, kind="Internal").ap()

    def sb(self, es, name, shape, dt=F32):
        self._nsb = getattr(self, '_nsb', 0) + 1
        return es.enter_context(self.nc.sbuf_tensor("%s_%d" % (name, self._nsb), list(shape), dt))

    def range_reduce(self, S, eng, fr, r, ti, tf, rtag, wtags):
        MAGIC = 12582912.0
        S.op(eng, lambda e: e.tensor_scalar(out=tf, in0=r, scalar1=MAGIC, scalar2=-MAGIC, op0=ALU.add, op1=ALU.add), r=[rtag], w=[wtags[1]])
        S.op(eng, lambda e: e.tensor_tensor(out=fr, in0=r, in1=tf, op=ALU.subtract), r=[rtag, wtags[1]], w=[wtags[2]])

    def build(self):
        nc = self.nc
        P = self
        xT = P.din("xT", [D, W])
        x_own = P.din("x_own", [NOWN, D])
        posw = P.din("posw", [1, W], I32)
        posc = P.din("posc", [1, 512], I32)
        kvalid = P.din("kvalid", [128, 64])
        nvalid = P.din("nvalid", [128, 4])
        padcol = P.din("padcol", [128, 2])
        w_in = P.din("w_in", [D, NCOL])
        ropecol = P.din("ropecol", [128, 4])
        identf = P.din("identf", [128, 128])
        pswap = P.din("pswap", [128, 128])
        P.din("w_cmp_k1", [2048, 128]); P.din("w_cmp_v1", [2048, 128])
        P.din("w_cmp_k2", [128, 64]); P.din("w_cmp_v2", [128, 64])
        P.din("cmp_pos_kT", [128, 32]); P.din("cmp_pos_vT", [128, 32])
        P.din("omat", [128, 4, 128]); P.din("causalT", [128, 128]); P.din("antiT", [128, 128])
        P.din("cmask", [128, 16, 2, 128]); P.din("sidx", [128, 128]); P.din("qhalf", [128, 1])
        P.din("s5_lam", [128, 32, 3]); P.din("s5_b", [128, 32, 2, 16]); P.din("s5_c", [128, 32, 2, 16]); P.din("s5_d", [32, 32])
        P.din("iota1", [128, 512]); P.din("iotar", [128, 512]); P.din("w_glu", [1024, 1024])
        P.din("w_out", [D, D]); P.din("ln1_g", [1, D]); P.din("ln1_b", [1, D]); P.din("ln2_g", [1, D]); P.din("ln2_b", [1, D])
        P.din("w_router", [D, 64]); P.din("router_bias", [1, 64])
        P.din("w_gate", [64, D, 512]); P.din("w_up", [64, D, 512]); P.din("w_down", [64, 512, D])
        P.din("ws_gate", [D, 512]); P.din("ws_up", [D, 512]); P.din("ws_down", [512, D])
        uT_s = P.dscr("uT_s", [1024, W], BF16)
        kcT_s = P.dscr("kcT_s", [128, W], BF16)
        vcT_s = P.dscr("vcT_s", [128, W], BF16)

        with ExitStack() as es0:
            S = Sched(nc, es0)
            self.S = S
            psbig = es0.enter_context(nc.psum_tensor("psbig", [128, 4096], F32))
            self.psbig = psbig
            ps = [psbig[:, i * 512:(i + 1) * 512] for i in range(8)]
            c_ident = P.sb(es0, "c_ident", [128, 128])
            c_identb = P.sb(es0, "c_identb", [128, 128], BF16)
            c_pswap = P.sb(es0, "c_pswap", [128, 128], BF16)
            c_rope = P.sb(es0, "c_rope", [128, 4])
            c_kvalid = P.sb(es0, "c_kvalid", [128, 64])
            c_nvalid = P.sb(es0, "c_nvalid", [128, 4])
            c_pad = P.sb(es0, "c_pad", [128, 2])
            S.dma('sp', c_ident[:], identf[:, :], w=['c_ident'])
            S.dma('pool', c_identb[:], identf[:, :], w=['c_identb'])
            S.dma('pool', c_pswap[:], pswap[:, :], w=['c_pswap'])
            S.dma('sp', c_rope[:], ropecol[:, :], w=['c_rope'])
            S.dma('sp', c_kvalid[:], kvalid[:, :], w=['c_kvalid'])
            S.dma('sp', c_nvalid[:], nvalid[:, :], w=['c_nvalid'])
            S.dma('sp', c_pad[:], padcol[:, :], w=['c_pad'])
            self.c_rope, self.c_pswap, self.c_ident, self.c_identb = c_rope, c_pswap, c_ident, c_identb
            self.c_kvalid, self.c_nvalid, self.c_pad = c_kvalid, c_nvalid, c_pad
            S.barrier()

            if 'p1' not in self.skip:
                self.phase1(S, ps, 1, locals())
            if self.upto == 'p1a':
                self.finish_debug(S, locals())
                return nc
            ysT_s = P.dscr("ysT_s", [1024, NOWN], BF16)
            if 'yT_all' in self.dbg:
                d_yT = P.dout("dbg_yT_all", [128, 8, NOWN], BF16)
            if 'p3' not in self.skip:
                self.phase3(S, ps, locals())
            self.dbg.discard('yT_all')
            if self.upto == 'p3':
                self.finish_debug(S, locals())
                return nc

            es1 = es0.enter_context(ExitStack())
            ksT = P.sb(es1, "ksT", [128, W], BF16)
            kwT = P.sb(es1, "kwT", [128, NOWN + 512], BF16)
            qT = P.sb(es1, "qT", [128, 16, 8, 128], BF16)
            Vs = P.sb(es1, "Vs", [128, 64, 2, 65], BF16)
            Vw = P.sb(es1, "Vw", [128, 20, 2, 65], BF16)
            gates = P.sb(es1, "gates", [128, 16, 48])
            if 'p1' not in self.skip:
                self.phase1(S, ps, 2, locals())
            if self.upto == 'p1':
                self.finish_debug(S, locals())
                return nc
            kcT = P.sb(es1, "kcT", [128, 512], BF16)
            Vc1O = P.sb(es1, "Vc1O", [128, 4, 2, 193], BF16)
            if 'p2' not in self.skip:
                self.phase2(S, ps, locals())
            if self.upto == 'p2':
                self.finish_debug(S, locals())
                return nc
            ynT_s = P.dscr("ynT_s", [1024, NOWN], BF16)
            if 'selb' in self.dbg:
                d_selb = P.dout("dbg_selb", [128, 128], BF16)
                d_imp = P.dout("dbg_imp", [128, 128])
                self.dbg.discard('selb')
                self.dbg_sel = True
            if 'p4' not in self.skip:
                self.phase4(S, ps, locals())
            if self.upto == 'p4':
                self.finish_debug(S, locals())
                return nc
            es1.close()
            x1_s = P.dscr("x1_s", [NOWN, D])
            x1T_s = P.dscr("x1T_s", [D, NOWN], BF16)
            gT_s = P.dscr("gT_s", [64, NOWN])
            if 'p5' not in self.skip:
                self.phase5(S, ps, locals())
            if self.upto == 'p5':
                self.finish_debug(S, locals())
                return nc
            out = P.dout("out", [NOWN, D])
            self.phase6(S, ps, locals())
            self.finish_debug(S, locals())
        return nc

    def rope_tables(self, S, eng, pos_i, cs, scol, tagp, tmp):
        c_rope = self.c_rope
        posf, r, fr, ti, tf = tmp
        C, Ss = cs
        S.op(eng, lambda e: e.tensor_copy(out=posf, in_=pos_i), r=[tagp + 'pi'], w=[tagp + 'pf'])
        S.op(eng, lambda e: e.tensor_scalar(out=r, in0=posf, scalar1=scol, scalar2=c_rope[:, 0:1], op0=ALU.add, op1=ALU.mult),
             r=[tagp + 'pf', 'c_rope'], w=[tagp + 'r'])
        self.range_reduce(S, eng, fr, r, ti, tf, tagp + 'r', [tagp + 'ti', tagp + 'tf', tagp + 'fr'])
        S.op('act', lambda e: e.activation(out=Ss, in_=fr, func=AF.Sin, scale=c_rope[:, 1:2]), r=[tagp + 'fr', 'c_rope'], w=[tagp + 'S'])
        S.op(eng, lambda e: e.tensor_scalar(out=r, in0=r, scalar1=0.25, scalar2=None, op0=ALU.add), r=[tagp + 'r'], w=[tagp + 'r'])
        self.range_reduce(S, eng, fr, r, ti, tf, tagp + 'r', [tagp + 'ti', tagp + 'tf', tagp + 'fr'])
        S.op('act', lambda e: e.activation(out=C, in_=fr, func=AF.Sin, scale=TWO_PI), r=[tagp + 'fr'], w=[tagp + 'C'])

    def rope_apply(self, S, src_ps, src_tag, dst, dst_tag, C, Ss, ctag, stag, raw, t1, t2, ps_sw, k):
        c_pswap = self.c_pswap
        S.op('act', lambda e: e.copy(out=raw, in_=src_ps), r=[src_tag], w=[('raw', k)])
        S.op('pe', lambda e: e.matmul(ps_sw, lhsT=c_pswap[:], rhs=raw, start=True, stop=True), r=[('raw', k), 'c_pswap'], w=[('pssw', k)])
        S.op('dve', lambda e: e.tensor_tensor(out=t1, in0=raw, in1=C, op=ALU.mult), r=[('raw', k), ctag], w=[('t1', k)])
        S.op('dve', lambda e: e.tensor_tensor(out=t2, in0=ps_sw, in1=Ss, op=ALU.mult), r=[('pssw', k), stag], w=[('t2', k)])
        if isinstance(dst, tuple):
            for hp, d_ in enumerate(dst):
                sl = slice(64 * hp, 64 * hp + 64)
                S.op('dve', lambda e: e.tensor_tensor(out=d_, in0=t1[sl], in1=t2[sl], op=ALU.add), r=[('t1', k), ('t2', k)], w=[dst_tag])
        elif len(dst.shape) == 3:
            S.op('dve', lambda e: e.tensor_tensor(out=dst, in0=t1.rearrange("p (a b) -> p a b", b=128), in1=t2.rearrange("p (a b) -> p a b", b=128), op=ALU.add),
                 r=[('t1', k), ('t2', k)], w=[dst_tag])
        else:
            S.op('dve', lambda e: e.tensor_tensor(out=dst, in0=t1, in1=t2, op=ALU.add), r=[('t1', k), ('t2', k)], w=[dst_tag])

    def phase1(self, S, ps, npass, L):
        nc = self.nc
        P = self
        xT = self.ins["xT"]
        w_in = self.ins["w_in"]
        posw = self.ins["posw"]
        c_kvalid = self.c_kvalid
        if npass == 1:
            uT_s, kcT_s, vcT_s = L['uT_s'], L['kcT_s'], L['vcT_s']
            wc0, wn = 0, NC1
        else:
            ksT, kwT, qT, Vs, Vw, gates = (L[k] for k in ['ksT', 'kwT', 'qT', 'Vs', 'Vw', 'gates'])
            wc0, wn = NC1, NC2
        with ExitStack() as es:
            w_sb = P.sb(es, "w_sb", [128, 16, wn], BF16)
            xg = [P.sb(es, "xg%d" % i, [128, 16, 512], BF16) for i in range(2)]
            if npass == 1:
                ust = [P.sb(es, "ust%d" % i, [128, 512], BF16) for i in range(4)]
            else:
                raw = [P.sb(es, "raw%d" % i, [128, 512], BF16) for i in range(2)]
                t1 = [P.sb(es, "t1_%d" % i, [128, 512]) for i in range(2)]
                t2 = [P.sb(es, "t2_%d" % i, [128, 512]) for i in range(2)]
                Ct = [P.sb(es, "Ct%d" % i, [128, 512]) for i in range(2)]
                St = [P.sb(es, "St%d" % i, [128, 512]) for i in range(2)]
                posi = [P.sb(es, "posi%d" % i, [128, 512], I32) for i in range(2)]
                rtmp = (P.sb(es, "r_pf", [128, 512]), P.sb(es, "r_r", [128, 512]), P.sb(es, "r_fr", [128, 512]),
                        P.sb(es, "r_ti", [128, 512], I32), P.sb(es, "r_tf", [128, 512]))
            wv = w_in.rearrange("(c p) n -> p c n", p=128)
            wblocks = [(c0, min(wn, c0 + 800)) for c0 in range(0, wn, 800)]
            for (c0, c1) in wblocks:
                for dcg in range(0, 16, 4):
                    S.dma('pool', w_sb[:, dcg:dcg + 4, c0:c1], wv[:, dcg:dcg + 4, wc0 + c0:wc0 + c1], w=[('w_sb', c0, dcg)])
            wtags = [('w_sb', c0, dcg) for (c0, c1) in wblocks for dcg in range(0, 16, 4)]
            xv = xT.rearrange("(c p) t -> p c t", p=128)
            nfm = 0
            ropek = 0
            nst = 0
            for g in range(NG):
                own = g >= OWN_G0
                xb = xg[g % 2]
                xtag = ('xg', g % 2)
                for dcg in range(0, 16, 4):
                    S.dma('pool', xb[:, dcg:dcg + 4, :], xv[:, dcg:dcg + 4, g * 512:(g + 1) * 512], w=[(xtag, dcg)])
                xtags = [(xtag, dcg) for dcg in range(0, 16, 4)]
                if npass == 1:
                    fm = [('u', i, C_U + 128 * i) for i in range(8)] + [('kc', 0, C_KC), ('vc', 0, C_VC)]
                else:
                    pi = posi[g % 2]
                    tp = 'rt%d' % (g % 2)
                    S.dma('sp', pi[:], posw[0:1, g * 512:(g + 1) * 512].to_broadcast([128, 512]), w=[tp + 'pi'])
                    C, Ss = Ct[g % 2], St[g % 2]
                    self.rope_tables(S, 'dve', pi[:], (C[:], Ss[:]), 0.0, tp, tuple(t[:] for t in rtmp))
                    fm = [('ks', 0, C_KS)]
                    if own:
                        fm += [('q', i, C_Q + 128 * i) for i in range(8)]
                    if g >= OWN_G0 - 1:
                        fm += [('kw', 0, C_KW)]
                for (kind, i, c0) in fm:
                    bank = nfm % 4
                    nfm += 1
                    pt = ps[bank]
                    ptag = ('ps', bank)
                    for dc in range(16):
                        S.op('pe', lambda e: e.matmul(pt[:, :], lhsT=w_sb[:, dc, c0:c0 + 128], rhs=xb[:, dc, :], start=(dc == 0), stop=(dc == 15)),
                             r=(wtags + xtags) if dc in (0, 15) else [], w=[ptag])
                    if kind in ('u', 'kc', 'vc'):
                        ub = ust[nst % 4]
                        utag = ('ust', nst % 4)
                        nst += 1
                        S.op('act', lambda e: e.copy(out=ub[:], in_=pt[:, :]), r=[ptag], w=[utag])
                        if kind == 'u':
                            dst = uT_s[i * 128:(i + 1) * 128, g * 512:(g + 1) * 512]
                        elif kind == 'kc':
                            dst = kcT_s[:, g * 512:(g + 1) * 512]
                        else:
                            dst = vcT_s[:, g * 512:(g + 1) * 512]
                        S.dma('sp', dst, ub[:], r=[utag], w=[('scr', kind, i, g)])
                    else:
                        if kind == 'ks':
                            dst, dtag = ksT[:, g * 512:(g + 1) * 512], ('ksT', g)
                        elif kind == 'q':
                            go = g - OWN_G0
                            dst, dtag = qT[:, 4 * go:4 * go + 4, i, :], ('qT', i, go)
                        else:
                            go = g - (OWN_G0 - 1)
                            dst, dtag = kwT[:, go * 512:(go + 1) * 512], ('kwT', go)
                        k = ropek % 2
                        ropek += 1
                        self.rope_apply(S, pt[:, :], ptag, dst, dtag, C[:], Ss[:], tp + 'C', tp + 'S',
                                        raw[k][:], t1[k][:], t2[k][:], ps[4 + k][:, :], k)
                if npass == 2:
                    for tt in range(4):
                        wt = g * 4 + tt
                        bank = 6 + (wt % 2)
                        pt = ps[bank]
                        ptag = ('ps', bank)
                        ncols = 128 + (176 if g >= OWN_G0 - 1 else 0)
                        for dc in range(16):
                            S.op('pe', lambda e: e.matmul(pt[:, 0:ncols], lhsT=xb[:, dc, tt * 128:(tt + 1) * 128], rhs=w_sb[:, dc, C_VS:C_VS + ncols],
                                                          start=(dc == 0), stop=(dc == 15)),
                                 r=(wtags + xtags) if dc in (0, 15) else [], w=[ptag])
                        S.op('act', lambda e: e.copy(out=Vs[:, wt, :, 0:64], in_=pt[:, 0:128].rearrange("p (h d) -> p h d", h=2)), r=[ptag], w=[('Vs', wt)])
                        if g >= OWN_G0 - 1:
                            lw = wt - (OWN_G0 - 1) * 4
                            S.op('act', lambda e: e.copy(out=Vw[:, lw, :, 0:64], in_=pt[:, 128:256].rearrange("p (h d) -> p h d", h=2)), r=[ptag], w=[('Vw', lw)])
                        if own:
                            lo = wt - OWN_G0 * 4
                            S.op('act', lambda e: e.activation(out=gates[:, lo, :], in_=pt[:, 256:304], func=AF.Sigmoid), r=[ptag], w=[('gates', lo)])
            if npass == 2:
                for h in range(2):
                    S.op('dve', lambda e: e.tensor_copy(out=Vs[:, :, h, 64], in_=c_kvalid[:, 0:64]), r=['c_kvalid'], w=[('Vs1', h)])
                    S.op('dve', lambda e: e.tensor_copy(out=Vw[:, :, h, 64], in_=c_kvalid[:, 44:64]), r=['c_kvalid'], w=[('Vw1', h)])
            S.barrier()

    def phase2(self, S, ps, L):
        P = self
        kcT_s, vcT_s = L['kcT_s'], L['vcT_s']
        kcT, Vc1O = L['kcT'], L['Vc1O']
        posc = self.ins['posc']
        c_nvalid = self.c_nvalid
        with ExitStack() as es:
            raws = [P.sb(es, "cr_k", [128, W], BF16), P.sb(es, "cr_v", [128, W], BF16)]
            w1 = [P.sb(es, "w1k", [128, 32, 128], BF16), P.sb(es, "w1v", [128, 32, 128], BF16)]
            w2k = [P.sb(es, "w2k%d" % h, [128, 128], BF16) for h in range(2)]
            w2v = P.sb(es, "w2v", [128, 64], BF16)
            peT = [P.sb(es, "peTk", [128, 32], BF16), P.sb(es, "peTv", [128, 32], BF16)]
            cb = P.sb(es, "cb", [128, 2])
            gT = [P.sb(es, "gT%d" % h, [128, 512], BF16) for h in range(2)]
            Omat = P.sb(es, "Omat", [128, 4, 128], BF16)
            posi = P.sb(es, "cposi", [128, 512], I32)
            Ct, St = P.sb(es, "cCt", [128, 512]), P.sb(es, "cSt", [128, 512])
            rtmp = (P.sb(es, "c_pf", [128, 512]), P.sb(es, "c_r", [128, 512]), P.sb(es, "c_fr", [128, 512]),
                    P.sb(es, "c_ti", [128, 512], I32), P.sb(es, "c_tf", [128, 512]))
            raw, t1, t2 = P.sb(es, "craw", [128, 512], BF16), P.sb(es, "ct1", [128, 512]), P.sb(es, "ct2", [128, 512])
            S.dma('sp', raws[0][:], kcT_s, w=['cr0'])
            S.dma('sp', raws[1][:], vcT_s, w=['cr1'])
            for kv, nm in enumerate(['w_cmp_k1', 'w_cmp_v1']):
                src = self.ins[nm].rearrange("(i d) c -> d i c", d=64)
                for h in range(2):
                    S.dma('pool', w1[kv][64 * h:64 * h + 64, :, :], src, w=[('w1', kv, h)])
            for h in range(2):
                S.op('dve', lambda e: e.memset(w2k[h][:], 0.0), w=[('w2k', h)])
                S.dma('pool', w2k[h][:, 64 * h:64 * h + 64], self.ins['w_cmp_k2'], w=[('w2k', h)])
                S.op('dve', lambda e: e.memset(gT[h][:], 0.0), w=[('gT', h)])
            S.dma('pool', w2v[:], self.ins['w_cmp_v2'], w=['w2v'])
            S.dma('pool', peT[0][:], self.ins['cmp_pos_kT'], w=['peT0'])
            S.dma('pool', peT[1][:], self.ins['cmp_pos_vT'], w=['peT1'])
            S.dma('pool', Omat[:], self.ins['omat'], w=['Omat'])
            S.dma('sp', posi[:], posc[0:1, :].to_broadcast([128, 512]), w=['cpi'])
            self.rope_tables(S, 'pool', posi[:], (Ct[:], St[:]), 15.5, 'c', tuple(t[:] for t in rtmp))
            for kv in range(2):
                for i in range(32):
                    S.op('pe', lambda e: e.matmul(ps[7][:, 0:1], lhsT=w1[kv][0:64, i, :], rhs=peT[kv][0:64, i:i + 1], start=(i == 0), stop=(i == 31)),
                         r=[('w1', kv, 0), 'peT%d' % kv], w=[('ps', 7)])
                S.op('act', lambda e: e.copy(out=cb[:, kv:kv + 1], in_=ps[7][:, 0:1]), r=[('ps', 7)], w=[('cb', kv)])
                for h in range(2):
                    pt = ps[h]
                    for i in range(32):
                        S.op('pe', lambda e: e.matmul(pt[:, 0:511], lhsT=w1[kv][64 * h:64 * h + 64, i, :],
                                                      rhs=raws[kv][64 * h:64 * h + 64, i:i + 16 * 510 + 1:16], start=(i == 0), stop=(i == 31)),
                             r=[('w1', kv, h), 'cr%d' % kv], w=[('ps', h)])
                    S.op('act', lambda e: e.activation(out=gT[h][:, 0:511], in_=pt[:, 0:511], func=AF.Gelu_apprx_tanh, bias=cb[:, kv:kv + 1]),
                         r=[('ps', h), ('cb', kv)], w=[('gT', h)])
                if kv == 0:
                    for h in range(2):
                        S.op('pe', lambda e: e.matmul(ps[2][:, :], lhsT=w2k[h][:], rhs=gT[h][:], start=(h == 0), stop=(h == 1)),
                             r=[('gT', h), ('w2k', h)], w=[('ps', 2)])
                    self.rope_apply(S, ps[2][:, :], ('ps', 2), kcT[:], 'kcT', Ct[:], St[:], 'cC', 'cS', raw[:], t1[:], t2[:], ps[3][:, :], 0)
                else:
                    for a_ in range(4):
                        for h in range(2):
                            S.op('pe', lambda e: e.matmul(ps[2][:, (a_ * 2 + h) * 64:(a_ * 2 + h + 1) * 64], lhsT=gT[h][:, a_ * 128:(a_ + 1) * 128], rhs=w2v[:],
                                                          start=True, stop=True), r=[('gT', h), 'w2v'], w=[('ps', 2)])
                    for a_ in range(4):
                        S.op('dve', lambda e: e.tensor_scalar(out=Vc1O[:, a_, :, 0:64], in0=ps[2][:, a_ * 128:(a_ + 1) * 128].rearrange("p (h d) -> p h d", h=2),
                                                              scalar1=c_nvalid[:, a_:a_ + 1], scalar2=None, op0=ALU.mult),
                             r=[('ps', 2), 'c_nvalid'], w=[('Vc1O', a_)])
                        for h in range(2):
                            S.op('dve', lambda e: e.tensor_copy(out=Vc1O[:, a_, h, 64:65], in_=c_nvalid[:, a_:a_ + 1]), r=['c_nvalid'], w=[('Vc1O', a_)])
                            S.op('dve', lambda e: e.tensor_scalar(out=Vc1O[:, a_, h, 65:193], in0=Omat[:, a_, :], scalar1=c_nvalid[:, a_:a_ + 1], scalar2=None, op0=ALU.mult),
                                 r=['Omat', 'c_nvalid'], w=[('Vc1O', a_)])
            S.barrier()

    def phase3(self, S, ps, L):
        P = self
        uT_s, ysT_s = L['uT_s'], L['ysT_s']
        c_ident = self.c_ident
        nst = self.nst
        with ExitStack() as es:
            lam = P.sb(es, "lam", [128, 32, 3])
            sbD = P.sb(es, "sbD", [32, 32])
            iota1 = P.sb(es, "iota1", [128, 512])
            iotar = P.sb(es, "iotar", [128, 512])
            wglu = P.sb(es, "wglu", [128, 8, 1024], BF16)
            S.dma('sp', lam[:], self.ins['s5_lam'], w=['lam'])
            S.dma('sp', sbD[:], self.ins['s5_d'], w=['sbD'])
            S.dma('sp', iota1[:], self.ins['iota1'], w=['iota1'])
            S.dma('sp', iotar[:], self.ins['iotar'], w=['iotar'])
            S.dma('pool', wglu[:], self.ins['w_glu'].rearrange("(c p) n -> p c n", p=128), w=['wglu'])
            v = {}
            for nm in ['dt', 'mag', 'thc', 'sn', 'cs', 'ar', 'ai', 'zr', 'den', 'fr', 'fi', 'ta', 'tb', 'tfr', 'ttf', 'lrdt', 'm512']:
                v[nm] = P.sb(es, "v_" + nm, [128, 32])
            v_ti = P.sb(es, "v_ti", [128, 32], I32)
            lhsB = P.sb(es, "lhsB", [32, 32, 2, 128], BF16)
            bdC = P.sb(es, "bdC", [128, 32, 2, 32], BF16)
            dd = P.sb(es, "Ddiag", [32, 32, 32], BF16)
            esp = ExitStack()
            sbB = P.sb(esp, "sbB", [128, 32, 2, 16])
            sbC = P.sb(esp, "sbC", [128, 32, 2, 16])
            bd = P.sb(esp, "bdB", [128, 32, 2, 32])
            bt1 = P.sb(esp, "bt1", [128, 32, 16])
            bt2 = P.sb(esp, "bt2", [128, 32, 16])
            S.dma('sp', sbB[:], self.ins['s5_b'], w=['sbB'])
            S.dma('sp', sbC[:], self.ins['s5_c'], w=['sbC'])
            lr, li, ldt = lam[:, :, 0], lam[:, :, 1], lam[:, :, 2]
            TT = lambda o, a_, b_, op, r, w: S.op('dve', lambda e: e.tensor_tensor(out=o, in0=a_, in1=b_, op=op), r=r, w=w)
            TS = lambda o, a_, s1, op, r, w: S.op('dve', lambda e: e.tensor_scalar(out=o, in0=a_, scalar1=s1, scalar2=None, op0=op), r=r, w=w)
            S.op('act', lambda e: e.activation(out=v['dt'][:], in_=ldt, func=AF.Exp), r=['lam'], w=['v_dt'])
            TT(v['ta'][:], lr, v['dt'][:], ALU.mult, ['lam', 'v_dt'], ['v_ta'])
            S.op('act', lambda e: e.activation(out=v['mag'][:], in_=v['ta'][:], func=AF.Exp), r=['v_ta'], w=['v_mag'])
            S.op('act', lambda e: e.activation(out=v['m512'][:], in_=v['ta'][:], func=AF.Exp, scale=512.0), r=['v_ta'], w=['v_m512'])
            S.op('dve', lambda e: e.tensor_copy(out=v['lrdt'][:], in_=v['ta'][:]), r=['v_ta'], w=['v_lrdt'])
            TT(v['thc'][:], li, v['dt'][:], ALU.mult, ['lam', 'v_dt'], ['v_thc'])
            TS(v['thc'][:], v['thc'][:], 1.0 / TWO_PI, ALU.mult, ['v_thc'], ['v_thc'])
            self.range_reduce(S, 'dve', v['tfr'][:], v['thc'][:], v_ti[:], v['ttf'][:], 'v_thc', ['v_ti', 'v_ttf', 'v_tfr'])
            S.op('act', lambda e: e.activation(out=v['sn'][:], in_=v['tfr'][:], func=AF.Sin, scale=TWO_PI), r=['v_tfr'], w=['v_sn'])
            TS(v['tb'][:], v['thc'][:], 0.25, ALU.add, ['v_thc'], ['v_tb'])
            self.range_reduce(S, 'dve', v['tfr'][:], v['tb'][:], v_ti[:], v['ttf'][:], 'v_tb', ['v_ti', 'v_ttf', 'v_tfr'])
            S.op('act', lambda e: e.activation(out=v['cs'][:], in_=v['tfr'][:], func=AF.Sin, scale=TWO_PI), r=['v_tfr'], w=['v_cs'])
            TT(v['ar'][:], v['mag'][:], v['cs'][:], ALU.mult, ['v_mag', 'v_cs'], ['v_ar'])
            TT(v['ai'][:], v['mag'][:], v['sn'][:], ALU.mult, ['v_mag', 'v_sn'], ['v_ai'])
            TS(v['zr'][:], v['ar'][:], -1.0, ALU.add, ['v_ar'], ['v_zr'])
            TT(v['den'][:], lr, lr, ALU.mult, ['lam'], ['v_den'])
            TT(v['ta'][:], li, li, ALU.mult, ['lam'], ['v_ta'])
            TT(v['den'][:], v['den'][:], v['ta'][:], ALU.add, ['v_den', 'v_ta'], ['v_den'])
            S.op('dve', lambda e: e.reciprocal(out=v['den'][:], in_=v['den'][:]), r=['v_den'], w=['v_den'])
            TT(v['ta'][:], v['zr'][:], lr, ALU.mult, ['v_zr', 'lam'], ['v_ta'])
            TT(v['tb'][:], v['ai'][:], li, ALU.mult, ['v_ai', 'lam'], ['v_tb'])
            TT(v['fr'][:], v['ta'][:], v['tb'][:], ALU.add, ['v_ta', 'v_tb'], ['v_fr'])
            TT(v['fr'][:], v['fr'][:], v['den'][:], ALU.mult, ['v_fr', 'v_den'], ['v_fr'])
            TT(v['ta'][:], v['ai'][:], lr, ALU.mult, ['v_ai', 'lam'], ['v_ta'])
            TT(v['tb'][:], v['zr'][:], li, ALU.mult, ['v_zr', 'lam'], ['v_tb'])
            TT(v['fi'][:], v['ta'][:], v['tb'][:], ALU.subtract, ['v_ta', 'v_tb'], ['v_fi'])
            TT(v['fi'][:], v['fi'][:], v['den'][:], ALU.mult, ['v_fi', 'v_den'], ['v_fi'])
            S.op('pool', lambda e: e.memset(bd[:], 0.0), w=['bd'])
            frb = v['fr'][:].unsqueeze(2).to_broadcast([128, 32, 16])
            fib = v['fi'][:].unsqueeze(2).to_broadcast([128, 32, 16])
            br_, bi_ = sbB[:, :, 0, :], sbB[:, :, 1, :]
            TT(bt1[:], br_, frb, ALU.mult, ['sbB', 'v_fr'], ['bt1'])
            TT(bt2[:], bi_, fib, ALU.mult, ['sbB', 'v_fi'], ['bt2'])
            for gl in range(2):
                sl = slice(64 * gl, 64 * gl + 64)
                TT(bd[sl, :, 0, 16 * gl:16 * gl + 16], bt1[sl], bt2[sl], ALU.subtract, ['bt1', 'bt2', 'bd'], ['bd'])
            TT(bt1[:], bi_, frb, ALU.mult, ['sbB', 'v_fr', 'bd'], ['bt1'])
            TT(bt2[:], br_, fib, ALU.mult, ['sbB', 'v_fi', 'bd'], ['bt2'])
            for gl in range(2):
                sl = slice(64 * gl, 64 * gl + 64)
                TT(bd[sl, :, 1, 16 * gl:16 * gl + 16], bt1[sl], bt2[sl], ALU.add, ['bt1', 'bt2', 'bd'], ['bd'])
            for st in range(nst):
                for ri in range(2):
                    S.op('pe', lambda e: e.transpose(out=ps[0][0:32, ri * 128:(ri + 1) * 128], in_=bd[:, st, ri, :], identity=c_ident[:]), r=['bd', 'c_ident'], w=[('ps', 0)])
                S.op('act', lambda e: e.copy(out=lhsB[:, st, :, :], in_=ps[0][0:32, 0:256].rearrange("p (a b) -> p a b", b=128)), r=[('ps', 0)], w=['lhsB'])
            S.op('pool', lambda e: e.memset(bdC[:], 0.0), w=['bdC'])
            for gl in range(2):
                sl = slice(64 * gl, 64 * gl + 64)
                S.op('dve', lambda e: e.tensor_copy(out=bdC[sl, :, 0, 16 * gl:16 * gl + 16], in_=sbC[sl, :, 0, :]), r=['sbC', 'bdC'], w=['bdC'])
                TS(bdC[sl, :, 1, 16 * gl:16 * gl + 16], sbC[sl, :, 1, :], -1.0, ALU.mult, ['sbC', 'bdC'], ['bdC'])
            TT(dd[:], c_ident[0:32, 0:32].unsqueeze(1).to_broadcast([32, 32, 32]), sbD[:].unsqueeze(2).to_broadcast([32, 32, 32]), ALU.mult, ['c_ident', 'sbD'], ['dd'])

            S.barrier()
            esp.close()
            ub = [P.sb(es, "ub%d" % i, [32, W], BF16) for i in range(2)]
            Ct = [P.sb(es, "sCt%d" % i, [128, 512]) for i in range(2)]
            St = [P.sb(es, "sSt%d" % i, [128, 512]) for i in range(2)]
            magb = [P.sb(es, "magb%d" % i, [128, 512]) for i in range(2)]
            rtmp = (P.sb(es, "s_r", [128, 512]), P.sb(es, "s_fr", [128, 512]), None, P.sb(es, "s_tf", [128, 512]))
            Gt = P.sb(es, "s_G", [128, 512])
            rtmp2 = [P.sb(es, "s_fr%d" % i, [128, 512]) for i in range(3)]
            DA = [P.sb(es, "DA%d" % i, [128, 1024]) for i in range(2)]
            DB = [P.sb(es, "DB%d" % i, [128, 1024]) for i in range(2)]
            acol = [P.sb(es, "acol%d" % i, [128, 4]) for i in range(2)]
            junk = P.sb(es, "junk", [128, 1024], BF16)
            Ecol = P.sb(es, "Ecol", [128, 12, 2])
            Sst = [P.sb(es, "Sst%d" % i, [128, 2]) for i in range(2)]
            tst = P.sb(es, "tst", [128, 2])
            m_ = [[P.sb(es, "m%d_%d" % (k_, i), [128, 512]) for i in range(4)] for k_ in range(2)]
            mo = [P.sb(es, "mo%d" % i, [128, 512]) for i in range(4)]
            zre = [P.sb(es, "zre%d" % i, [128, 512]) for i in range(2)]
            zim = [P.sb(es, "zim%d" % i, [128, 512]) for i in range(2)]
            sre, sim_ = P.sb(es, "sre", [128, 512], BF16), P.sb(es, "sim", [128, 512], BF16)
            S0 = P.sb(es, "S0", [128, 2])
            tcol = P.sb(es, "tcol", [128, 2])
            nident = P.sb(es, "nident", [128, 128])
            S.op('dve', lambda e: e.tensor_scalar(out=nident[:], in0=c_ident[:], scalar1=-1.0, scalar2=None, op0=ALU.mult), r=['c_ident'], w=['nident'])
            yT_all = P.sb(es, "yT_all", [128, 8, NOWN], BF16)
            if nst < 32:
                S.op('pool', lambda e: e.memset(yT_all[:], 0.0), w=['yT_all'])
            def prepA(st):
                S.dma('sp', ub[st % 2][:], uT_s[st * 32:(st + 1) * 32, :], w=[('ub', st % 2)])
                C, Sn, mb = Ct[st % 2], St[st % 2], magb[st % 2]
                tp = 's5t%d' % (st % 2)
                da, db = DA[st % 2], DB[st % 2]
                r_, fr_, tf_ = rtmp[0][:], rtmp[1][:], rtmp[3][:]
                E_ = 'dve'
                S.op(E_, lambda e: e.tensor_scalar(out=r_, in0=iota1[:], scalar1=v['thc'][:, st:st + 1], scalar2=None, op0=ALU.mult), r=['iota1', 'v_thc'], w=['s_r'])
                self.range_reduce(S, E_, fr_, r_, None, tf_, 's_r', ['s_ti', 's_tf', 's_fr'])
                S.op('act', lambda e: e.activation(out=Sn[:], in_=fr_, func=AF.Sin, scale=TWO_PI), r=['s_fr'], w=[tp + 'S'])
                S.op(E_, lambda e: e.tensor_scalar(out=r_, in0=r_, scalar1=0.25, scalar2=None, op0=ALU.add), r=['s_r'], w=['s_r'])
                self.range_reduce(S, E_, rtmp2[0][:], r_, None, tf_, 's_r', ['s_ti', 's_tf', 's_fr2'])
                S.op('act', lambda e: e.activation(out=C[:], in_=rtmp2[0][:], func=AF.Sin, scale=TWO_PI), r=['s_fr2'], w=[tp + 'C'])
                S.op(E_, lambda e: e.tensor_scalar(out=r_, in0=iotar[:], scalar1=v['thc'][:, st:st + 1], scalar2=None, op0=ALU.mult), r=['iotar', 'v_thc'], w=['s_r'])
                self.range_reduce(S, E_, rtmp2[1][:], r_, None, tf_, 's_r', ['s_ti', 's_tf', 's_fr3'])
                S.op('act', lambda e: e.activation(out=db[:, 0:512], in_=rtmp2[1][:], func=AF.Sin, scale=TWO_PI), r=['s_fr3'], w=[tp + 'DB'])
                S.op(E_, lambda e: e.tensor_scalar(out=r_, in0=r_, scalar1=0.25, scalar2=None, op0=ALU.add), r=['s_r'], w=['s_r'])
                self.range_reduce(S, E_, rtmp2[2][:], r_, None, tf_, 's_r', ['s_ti', 's_tf', 's_fr4'])
                S.op('act', lambda e: e.activation(out=da[:, 0:512], in_=rtmp2[2][:], func=AF.Sin, scale=TWO_PI), r=['s_fr4'], w=[tp + 'DA'])
                S.op('act', lambda e: e.activation(out=Gt[:], in_=iotar[:], func=AF.Exp, scale=v['lrdt'][:, st:st + 1]), r=['iotar', 'v_lrdt'], w=['s_G'])

            def prepB(st):
                C, Sn, mb = Ct[st % 2], St[st % 2], magb[st % 2]
                tp = 's5t%d' % (st % 2)
                da, db, ac = DA[st % 2], DB[st % 2], acol[st % 2]
                E_ = 'dve'
                S.op(E_, lambda e: e.tensor_copy(out=mb[:], in_=v['mag'][:, st:st + 1].to_broadcast([128, 512])), r=['v_mag'], w=[tp + 'M'])
                S.op(E_, lambda e: e.tensor_tensor(out=da[:, 0:512], in0=da[:, 0:512], in1=Gt[:], op=ALU.mult), r=[tp + 'DA', 's_G'], w=[tp + 'DA'])
                S.op(E_, lambda e: e.tensor_tensor(out=db[:, 0:512], in0=db[:, 0:512], in1=Gt[:], op=ALU.mult), r=[tp + 'DB', 's_G'], w=[tp + 'DB'])
                S.op(E_, lambda e: e.tensor_copy(out=db[:, 512:1024], in_=da[:, 0:512]), r=[tp + 'DA', tp + 'DB'], w=[tp + 'DB'])
                S.op(E_, lambda e: e.tensor_scalar(out=da[:, 512:1024], in0=db[:, 0:512], scalar1=-1.0, scalar2=None, op0=ALU.mult), r=[tp + 'DB', tp + 'DA'], w=[tp + 'DA'])
                S.op(E_, lambda e: e.tensor_tensor(out=ac[:, 0:1], in0=C[:, 511:512], in1=v['m512'][:, st:st + 1], op=ALU.mult), r=[tp + 'C', 'v_m512'], w=[tp + 'A'])
                S.op(E_, lambda e: e.tensor_tensor(out=ac[:, 1:2], in0=Sn[:, 511:512], in1=v['m512'][:, st:st + 1], op=ALU.mult), r=[tp + 'S', 'v_m512'], w=[tp + 'A'])
                S.op(E_, lambda e: e.tensor_scalar(out=ac[:, 2:3], in0=ac[:, 1:2], scalar1=-1.0, scalar2=None, op0=ALU.mult), r=[tp + 'A'], w=[tp + 'A'])

            prepA(0)
            prepB(0)
            for st in range(nst):
                u_ = ub[st % 2]
                utag = ('ub', st % 2)
                C, Sn, mb = Ct[st % 2], St[st % 2], magb[st % 2]
                tp = 's5t%d' % (st % 2)
                if st + 1 < nst:
                    prepA(st + 1)
                def front(g):
                    usl = u_[:, g * 512:(g + 1) * 512]
                    mm = m_[g % 2]
                    mt = lambda i: ('m', g % 2, i)
                    wb = 2 + 2 * (g % 2)
                    S.op('pe', lambda e: e.matmul(ps[0][:, :], lhsT=lhsB[:, st, 0, :], rhs=usl, start=True, stop=True), r=['lhsB', utag], w=[('ps', 0)])
                    S.op('pe', lambda e: e.matmul(ps[1][:, :], lhsT=lhsB[:, st, 1, :], rhs=usl, start=True, stop=True), r=['lhsB', utag], w=[('ps', 1)])
                    TT(mm[0][:], ps[0][:, :], C[:], ALU.mult, [('ps', 0), tp + 'C'], [mt(0)])
                    TT(mm[1][:], ps[1][:, :], Sn[:], ALU.mult, [('ps', 1), tp + 'S'], [mt(1)])
                    TT(mm[2][:], ps[1][:, :], C[:], ALU.mult, [('ps', 1), tp + 'C'], [mt(2)])
                    TT(mm[3][:], ps[0][:, :], Sn[:], ALU.mult, [('ps', 0), tp + 'S'], [mt(3)])
                    S.op('pe', lambda e: e.matmul(ps[wb][:, :], lhsT=c_ident[:], rhs=mm[0][:], start=True, stop=False), r=['c_ident', mt(0)], w=[('ps', wb)])
                    S.op('pe', lambda e: e.matmul(ps[wb][:, :], lhsT=c_ident[:], rhs=mm[1][:], start=False, stop=True), r=['c_ident', mt(1)], w=[('ps', wb)])
                    S.op('pe', lambda e: e.matmul(ps[wb + 1][:, :], lhsT=c_ident[:], rhs=mm[2][:], start=True, stop=False), r=['c_ident', mt(2)], w=[('ps', wb + 1)])
                    S.op('pe', lambda e: e.matmul(ps[wb + 1][:, :], lhsT=nident[:], rhs=mm[3][:], start=False, stop=True), r=['nident', mt(3)], w=[('ps', wb + 1)])

                def back(g):
                    own = g >= OWN_G0
                    usl = u_[:, g * 512:(g + 1) * 512]
                    wb = 2 + 2 * (g % 2)
                    zr_, zi_ = zre[g % 2], zim[g % 2]
                    S.op('dve', lambda e: e.tensor_tensor_scan(out=zr_[:], data0=mb[:], data1=ps[wb][:, :], initial=S0[:, 0:1], op0=ALU.mult, op1=ALU.add),
                         r=[tp + 'M', ('ps', wb), 'S0'], w=[('zre', g % 2)])
                    S.op('dve', lambda e: e.tensor_tensor_scan(out=zi_[:], data0=mb[:], data1=ps[wb + 1][:, :], initial=S0[:, 1:2], op0=ALU.mult, op1=ALU.add),
                         r=[tp + 'M', ('ps', wb + 1), 'S0'], w=[('zim', g % 2)])
                    c5, s5 = C[:, 511:512], Sn[:, 511:512]
                    TT(tcol[:, 0:1], zi_[:, 511:512], s5, ALU.mult, [('zim', g % 2), tp + 'S'], ['tcol'])
                    TT(tcol[:, 1:2], zi_[:, 511:512], c5, ALU.mult, [('zim', g % 2), tp + 'C'], ['tcol'])
                    S.op('dve', lambda e: e.scalar_tensor_tensor(out=S0[:, 0:1], in0=zr_[:, 511:512], scalar=c5, in1=tcol[:, 0:1], op0=ALU.mult, op1=ALU.subtract),
                         r=[('zre', g % 2), tp + 'C', 'tcol', 'S0'], w=['S0'])
                    S.op('dve', lambda e: e.scalar_tensor_tensor(out=S0[:, 1:2], in0=zr_[:, 511:512], scalar=s5, in1=tcol[:, 1:2], op0=ALU.mult, op1=ALU.add),
                         r=[('zre', g % 2), tp + 'S', 'tcol', 'S0'], w=['S0'])
                    if own:
                        TT(mo[0][:], zr_[:], C[:], ALU.mult, [('zre', g % 2), tp + 'C', 'mo0'], ['mo0'])
                        TT(mo[1][:], zi_[:], Sn[:], ALU.mult, [('zim', g % 2), tp + 'S', 'mo1'], ['mo1'])
                        TT(mo[2][:], zr_[:], Sn[:], ALU.mult, [('zre', g % 2), tp + 'S', 'mo2'], ['mo2'])
                        TT(mo[3][:], zi_[:], C[:], ALU.mult, [('zim', g % 2), tp + 'C', 'mo3'], ['mo3'])
                        S.op('pool', lambda e: e.tensor_tensor(out=sre[:], in0=mo[0][:], in1=mo[1][:], op=ALU.subtract), r=['mo0', 'mo1'], w=['sre'])
                        S.op('pool', lambda e: e.tensor_tensor(out=sim_[:], in0=mo[2][:], in1=mo[3][:], op=ALU.add), r=['mo2', 'mo3'], w=['sim'])
                        S.op('pe', lambda e: e.matmul(ps[6][0:32, :], lhsT=bdC[:, st, 0, :], rhs=sre[:], start=True, stop=False), r=['bdC', 'sre'], w=[('ps', 6)])
                        S.op('pe', lambda e: e.matmul(ps[6][0:32, :], lhsT=bdC[:, st, 1, :], rhs=sim_[:], start=False, stop=False), r=['bdC', 'sim'], w=[('ps', 6)])
                        S.op('pe', lambda e: e.matmul(ps[6][0:32, :], lhsT=dd[:, st, :], rhs=usl, start=False, stop=True), r=['dd', utag], w=[('ps', 6)])
                        go = g - OWN_G0
                        po = (st % 4) * 32
                        S.op('act', lambda e: e.activation(out=yT_all[po:po + 32, st // 4, go * 512:(go + 1) * 512], in_=ps[6][0:32, :], func=AF.Gelu_apprx_tanh),
                             r=[('ps', 6)], w=['yT_all'])

                da, db, ac = DA[st % 2], DB[st % 2], acol[st % 2]
                S.op('dve', lambda e: e.memset(Sst[0][:], 0.0), r=[('Sst', 0)], w=[('Sst', 0)])
                for g in range(OWN_G0):
                    usl = u_[:, g * 512:(g + 1) * 512]
                    xb = (g % 2) * 2
                    S.op('pe', lambda e: e.matmul(ps[xb][:, :], lhsT=lhsB[:, st, 0, :], rhs=usl, start=True, stop=True), r=['lhsB', utag], w=[('ps', xb)])
                    S.op('pe', lambda e: e.matmul(ps[xb + 1][:, :], lhsT=lhsB[:, st, 1, :], rhs=usl, start=True, stop=True), r=['lhsB', utag], w=[('ps', xb + 1)])
                    xcat = self.psbig[:, xb * 512:(xb + 2) * 512]
                    S.op('dve', lambda e: e.scalar_tensor_tensor(out=junk[:], in0=xcat, scalar=1.0, in1=da[:], op0=ALU.mult, op1=ALU.mult, accum_out=Ecol[:, g, 0:1]),
                         r=[('ps', xb), ('ps', xb + 1), tp + 'DA', 'junk'], w=['junk', ('E', g)])
                    S.op('dve', lambda e: e.scalar_tensor_tensor(out=junk[:], in0=xcat, scalar=1.0, in1=db[:], op0=ALU.mult, op1=ALU.mult, accum_out=Ecol[:, g, 1:2]),
                         r=[('ps', xb), ('ps', xb + 1), tp + 'DB', 'junk'], w=['junk', ('E', g)])
                    cur, nxt = Sst[g % 2], Sst[(g + 1) % 2]
                    ct, nt = ('Sst', g % 2), ('Sst', (g + 1) % 2)
                    S.op('act', lambda e: e.activation(out=tst[:, 0:1], in_=cur[:, 0:1], func=AF.Identity, scale=ac[:, 0:1], bias=Ecol[:, g, 0:1]), r=[ct, tp + 'A', ('E', g)], w=['tst'])
                    S.op('act', lambda e: e.activation(out=nxt[:, 0:1], in_=cur[:, 1:2], func=AF.Identity, scale=ac[:, 2:3], bias=tst[:, 0:1]), r=[ct, tp + 'A', 'tst'], w=[nt])
                    S.op('act', lambda e: e.activation(out=tst[:, 1:2], in_=cur[:, 1:2], func=AF.Identity, scale=ac[:, 0:1], bias=Ecol[:, g, 1:2]), r=[ct, tp + 'A', ('E', g)], w=['tst'])
                    S.op('act', lambda e: e.activation(out=nxt[:, 1:2], in_=cur[:, 0:1], func=AF.Identity, scale=ac[:, 1:2], bias=tst[:, 1:2]), r=[ct, tp + 'A', 'tst'], w=[nt])
                S.op('act', lambda e: e.copy(out=S0[:], in_=Sst[OWN_G0 % 2][:]), r=[('Sst', OWN_G0 % 2), 'S0'], w=['S0'])
                if st + 1 < nst:
                    prepB(st + 1)
                front(OWN_G0)
                for g in range(OWN_G0, NG):
                    if g + 1 < NG:
                        front(g + 1)
                    back(g)
            if 'yT_all' in self.dbg:
                S.dma('sp', L['d_yT'], yT_all[:], r=['yT_all'], w=['d_yT'])
            sg = [P.sb(es, "sg%d" % i, [128, 512]) for i in range(2)]
            yo = [P.sb(es, "yo%d" % i, [128, 512], BF16) for i in range(2)]
            n = 0
            for cc in range(8):
                for tg in range(4):
                    b = 4 + n % 2
                    for kc in range(8):
                        S.op('pe', lambda e: e.matmul(ps[b][:, :], lhsT=wglu[:, kc, cc * 128:(cc + 1) * 128], rhs=yT_all[:, kc, tg * 512:(tg + 1) * 512],
                                                      start=(kc == 0), stop=(kc == 7)), r=['wglu', 'yT_all'], w=[('ps', b)])
                    S.op('act', lambda e: e.activation(out=sg[n % 2][:], in_=ps[b][:, :], func=AF.Sigmoid), r=[('ps', b)], w=[('sg', n % 2)])
                    TT(yo[n % 2][:], yT_all[:, cc, tg * 512:(tg + 1) * 512], sg[n % 2][:], ALU.mult, ['yT_all', ('sg', n % 2)], [('yo', n % 2)])
                    S.dma('sp', ysT_s[cc * 128:(cc + 1) * 128, tg * 512:(tg + 1) * 512], yo[n % 2][:], r=[('yo', n % 2)], w=[('ysT_s', cc, tg)])
                    n += 1
            S.barrier()

    def phase4(self, S, ps, L):
        P = self
        ksT, kwT, qT, Vs, Vw, gates, kcT, Vc1O = (L[k] for k in ['ksT', 'kwT', 'qT', 'Vs', 'Vw', 'gates', 'kcT', 'Vc1O'])
        ynT_s = L['ynT_s']
        c_identb, c_ident, c_pad = self.c_identb, self.c_ident, self.c_pad
        nq = self.nq
        with ExitStack() as es:
            ksTm = P.sb(es, "ksTm", [128, 2, W], BF16)
            kwTm = P.sb(es, "kwTm", [128, 2, NOWN + 512], BF16)
            kcTm = P.sb(es, "kcTm", [128, 2, 512], BF16)
            for src_, dst_, nm_ in ((ksT, ksTm, 'ksT'), (kwT, kwTm, 'kwT'), (kcT, kcTm, 'kcT')):
                S.op('pool', lambda e: e.memset(dst_[:], 0.0), w=[nm_ + 'm'])
                for hp in range(2):
                    S.op('dve', lambda e: e.tensor_copy(out=dst_[64 * hp:64 * hp + 64, hp, :], in_=src_[64 * hp:64 * hp + 64, :]), r=[nm_ + 'm', nm_], w=[nm_ + 'm'])
            ksT, kwT, kcT = ksTm, kwTm, kcTm
            causalT = P.sb(es, "causalT", [128, 128], BF16)
            antiT = P.sb(es, "antiT", [128, 128], BF16)
            cmask = P.sb(es, "cmask", [128, 16, 2, 128], BF16)
            sidx = P.sb(es, "sidx", [128, 128])
            qhalf = P.sb(es, "qhalf", [128, 1])
            real = P.sb(es, "real", [128, 128])
            first = P.sb(es, "first", [128, 128])
            rm2 = P.sb(es, "rm2", [128, 128])
            S.dma('pool', causalT[:], self.ins['causalT'], w=['causalT'])
            S.dma('pool', antiT[:], self.ins['antiT'], w=['antiT'])
            S.dma('pool', cmask[:], self.ins['cmask'], w=['cmask'])
            S.dma('sp', sidx[:], self.ins['sidx'], w=['sidx'])
            S.dma('sp', qhalf[:], self.ins['qhalf'], w=['qhalf'])
            S.op('dve', lambda e: e.tensor_scalar(out=first[:], in0=sidx[:], scalar1=c_pad[:, 0:1], scalar2=None, op0=ALU.subtract), r=['sidx', 'c_pad'], w=['first'])
            S.op('dve', lambda e: e.tensor_scalar(out=real[:], in0=first[:], scalar1=0.0, scalar2=None, op0=ALU.is_ge), r=['first'], w=['real'])
            S.op('dve', lambda e: e.tensor_scalar(out=first[:], in0=first[:], scalar1=0.0, scalar2=None, op0=ALU.is_equal), r=['first'], w=['first'])
            S.op('dve', lambda e: e.tensor_scalar(out=rm2[:], in0=real[:], scalar1=-2.0, scalar2=None, op0=ALU.add), r=['real'], w=['rm2'])
            et = [P.sb(es, "e%d" % i, [128, 512], BF16) for i in range(4)]
            mk = [P.sb(es, "mk%d" % i, [128, 128], BF16) for i in range(3)]
            o_all = P.sb(es, "o_all", [128, 8, 2, 64])
            den8 = P.sb(es, "den8", [128, 8])
            rg8 = P.sb(es, "rg8", [128, 8])
            imp = P.sb(es, "imp", [128, 128])
            A_ = P.sb(es, "A_", [128, 128])
            vld = P.sb(es, "vld", [128, 128])
            fo = P.sb(es, "fo", [128, 128])
            score = P.sb(es, "score", [128, 128])
            swk = P.sb(es, "swk", [128, 128])
            top = P.sb(es, "top", [128, 16])
            selb = P.sb(es, "selb", [128, 128], BF16)
            maskq = P.sb(es, "maskq", [128, W], BF16)
            yst = [P.sb(es, "yst%d" % i, [128, 8, 128], BF16) for i in range(2)]
            cnt = {'u': 0, 'm': 0}

            pend = []
            LA = 3
            cnt['i'] = 0

            def flush(n=0):
                while len(pend) > n:
                    pend.pop(0)()

            def unit(k, qi, keys, ktag, Vrhs, vtag, ncols, mask, mtag, accbank, first_, last_):
                for hf in range(2):
                    i = cnt['i']
                    cnt['i'] += 1
                    sb_ = i % 4
                    pt = ps[sb_]
                    e_ = et[i % 4]
                    etag = ('e', i % 4)
                    S.op('pe', lambda e: e.matmul(pt[:, :], lhsT=keys, rhs=qT[:, qi, 4 * hf:4 * hf + 4, :],
                                                  start=True, stop=True), r=[ktag, 'qT'], w=[('ps', sb_)])
                    S.op('act', lambda e: e.activation(out=e_[:], in_=pt[:, :], func=AF.Exp, scale=0.125), r=[('ps', sb_)], w=[etag])
                    if mask is not None:
                        S.op('dve', lambda e: e.tensor_tensor(out=e_[:].rearrange("p (h q) -> p h q", h=4), in0=e_[:].rearrange("p (h q) -> p h q", h=4),
                                                              in1=mask.unsqueeze(1).to_broadcast([128, 4, 128]), op=ALU.mult), r=[etag, mtag], w=[etag])

                    def B(hf=hf, e_=e_, etag=etag, Vrhs=Vrhs, vtag=vtag, ncols=ncols, accbank=accbank, first_=first_, last_=last_):
                        if accbank is None:
                            S.op('pe', lambda e: e.matmul(ps[4 + hf][0:65, :], lhsT=Vrhs, rhs=e_[:], start=first_, stop=last_), r=[etag, vtag], w=[('ps', 4 + hf)])
                            return
                        for hh in range(4):
                            j = 4 * hf + hh
                            bank, c0 = accbank(j)
                            S.op('pe', lambda e: e.matmul(ps[bank][:, c0:c0 + ncols], lhsT=e_[:, hh * 128:(hh + 1) * 128], rhs=Vrhs,
                                                          start=(first_ and c0 == 0), stop=last_, skip_group_check=True),
                                 r=[etag, vtag], w=[('ps', bank)])
                    pend.append(B)
                    flush(LA)

            aT = P.sb(es, "aT", [65, 1024])

            def finalize():
                flush()
                for hf in range(2):
                    S.op('act', lambda e: e.copy(out=aT[:, hf * 512:(hf + 1) * 512], in_=ps[4 + hf][0:65, :]), r=[('ps', 4 + hf)], w=[('aT', hf)])
                for hf in range(2):
                    for hh in range(4):
                        S.op('pe', lambda e: e.transpose(out=ps[6 + hf][:, hh * 65:(hh + 1) * 65], in_=aT[:, hf * 512 + hh * 128:hf * 512 + (hh + 1) * 128], identity=c_ident[0:65, 0:65]),
                             r=[('aT', hf), 'c_ident'], w=[('ps', 6 + hf)])

            def evac(k, qi, accbank, br, init):
                for j in range(8):
                    bank, c0 = accbank(j)
                    S.op('dve', lambda e: e.tensor_copy(out=den8[:, j:j + 1], in_=ps[bank][:, c0 + 64:c0 + 65]), r=[('ps', bank)], w=['den8'])
                S.op('dve', lambda e: e.tensor_scalar(out=den8[:], in0=den8[:], scalar1=1e-30, scalar2=None, op0=ALU.max), r=['den8'], w=['den8'])
                S.op('dve', lambda e: e.reciprocal(out=den8[:], in_=den8[:]), r=['den8'], w=['den8'])
                gsl = gates[:, qi, 24 * k + br:24 * k + br + 22:3]
                S.op('dve', lambda e: e.tensor_tensor(out=rg8[:], in0=den8[:], in1=gsl, op=ALU.mult), r=['den8', 'gates'], w=['rg8'])
                for j in range(8):
                    bank, c0 = accbank(j)
                    if init:
                        S.op('dve', lambda e: e.tensor_scalar(out=o_all[:, j, k, :], in0=ps[bank][:, c0:c0 + 64], scalar1=rg8[:, j:j + 1], scalar2=None, op0=ALU.mult),
                             r=[('ps', bank), 'rg8'], w=['o_all'])
                    else:
                        S.op('dve', lambda e: e.scalar_tensor_tensor(out=o_all[:, j, k, :], in0=ps[bank][:, c0:c0 + 64], scalar=rg8[:, j:j + 1], in1=o_all[:, j, k, :],
                                                                     op0=ALU.mult, op1=ALU.add), r=[('ps', bank), 'rg8', 'o_all'], w=['o_all'])

            for qi in range(nq):
                wt = 48 + qi
                for k in range(2):
                    accC = lambda j: (4 + j // 2, (j % 2) * 193)
                    for a_ in range(4):
                        m_ = cmask[:, qi, a_ - 2, :] if a_ >= 2 else None
                        unit(k, qi, kcT[:, k, a_ * 128:(a_ + 1) * 128], 'kcTm', Vc1O[:, a_, k, :], 'Vc1O', 193, m_, 'cmask', accC, a_ == 0, a_ == 3)
                    flush()
                    evac(k, qi, accC, 0, True)
                    for j in range(8):
                        bank, c0 = accC(j)
                        if j == 0:
                            S.op('dve', lambda e: e.tensor_scalar(out=imp[:], in0=ps[bank][:, c0 + 65:c0 + 193], scalar1=den8[:, j:j + 1], scalar2=None, op0=ALU.mult),
                                 r=[('ps', bank), 'den8'], w=['imp'])
                        else:
                            S.op('dve', lambda e: e.scalar_tensor_tensor(out=imp[:], in0=ps[bank][:, c0 + 65:c0 + 193], scalar=den8[:, j:j + 1], in1=imp[:],
                                                                         op0=ALU.mult, op1=ALU.add), r=[('ps', bank), 'den8', 'imp'], w=['imp'])
                    S.op('dve', lambda e: e.tensor_scalar(out=A_[:], in0=sidx[:], scalar1=float(-2 * wt), scalar2=qhalf[:, 0:1], op0=ALU.add, op1=ALU.subtract),
                         r=['sidx', 'qhalf'], w=['A_'])
                    S.op('dve', lambda e: e.tensor_scalar(out=vld[:], in0=A_[:], scalar1=0.0, scalar2=None, op0=ALU.is_le), r=['A_'], w=['vld'])
                    S.op('dve', lambda e: e.tensor_scalar(out=fo[:], in0=A_[:], scalar1=-1.0, scalar2=None, op0=ALU.is_ge), r=['A_'], w=['fo'])
                    S.op('dve', lambda e: e.tensor_tensor(out=vld[:], in0=vld[:], in1=real[:], op=ALU.mult), r=['vld', 'real'], w=['vld'])
                    S.op('dve', lambda e: e.tensor_tensor(out=fo[:], in0=fo[:], in1=first[:], op=ALU.max), r=['fo', 'first'], w=['fo'])
                    S.op('dve', lambda e: e.tensor_tensor(out=fo[:], in0=fo[:], in1=vld[:], op=ALU.mult), r=['fo', 'vld'], w=['fo'])
                    S.op('dve', lambda e: e.scalar_tensor_tensor(out=score[:], in0=imp[:], scalar=1.0, in1=vld[:], op0=ALU.add, op1=ALU.mult), r=['imp', 'vld'], w=['score'])
                    S.op('dve', lambda e: e.scalar_tensor_tensor(out=score[:], in0=fo[:], scalar=1e4, in1=score[:], op0=ALU.mult, op1=ALU.add), r=['fo', 'score'], w=['score'])
                    S.op('dve', lambda e: e.tensor_tensor(out=score[:], in0=score[:], in1=rm2[:], op=ALU.add), r=['score', 'rm2'], w=['score'])
                    S.op('dve', lambda e: e.max(out=top[:, 0:8], in_=score[:]), r=['score'], w=['top'])
                    S.op('dve', lambda e: e.match_replace(out=swk[:], in_to_replace=top[:, 0:8], in_values=score[:], imm_value=-1e30), r=['score', 'top'], w=['swk'])
                    S.op('dve', lambda e: e.max(out=top[:, 8:16], in_=swk[:]), r=['swk'], w=['top'])
                    S.op('dve', lambda e: e.tensor_scalar(out=selb[:], in0=score[:], scalar1=top[:, 15:16], scalar2=None, op0=ALU.is_ge), r=['score', 'top'], w=['selb'])
                    if getattr(self, 'dbg_sel', False) and qi == self.dbg_qi and k == self.dbg_k:
                        S.dma('sp', L['d_selb'], selb[:], r=['selb'], w=['d_selb'])
                        S.dma('sp', L['d_imp'], imp[:], r=['imp'], w=['d_imp'])
                    S.op('dve', lambda e: e.tensor_copy(out=maskq[:].rearrange("p (s j) -> p s j", j=64), in_=selb[:].unsqueeze(2).to_broadcast([128, 128, 64])),
                         r=['selb'], w=['maskq'])
                    accF = lambda j: (6 + j // 4, (j % 4) * 65)
                    for kt in range(wt + 1):
                        mi = cnt['m'] % 2
                        cnt['m'] += 1
                        mb = 6 + mi
                        S.op('pe', lambda e: e.matmul(ps[mb][:, 0:128], lhsT=maskq[:, kt * 128:(kt + 1) * 128], rhs=c_identb[:],
                                                      start=True, stop=True), r=['maskq', 'c_identb'], w=[('ps', mb)])
                        if kt == wt:
                            S.op('dve', lambda e: e.tensor_tensor(out=mk[mi][:], in0=ps[mb][:, 0:128], in1=causalT[:], op=ALU.mult), r=[('ps', mb), 'causalT'], w=[('mk', mi)])
                        else:
                            S.op('act', lambda e: e.copy(out=mk[mi][:], in_=ps[mb][:, 0:128]), r=[('ps', mb)], w=[('mk', mi)])
                        unit(k, qi, ksT[:, k, kt * 128:(kt + 1) * 128], 'ksTm', Vs[:, kt, k, :], 'Vs', 65, mk[mi][:], ('mk', mi), None, kt == 0, kt == wt)
                    finalize()
                    evac(k, qi, accF, 1, False)
                    for wi, wkt in enumerate(range(wt - 4, wt + 1)):
                        if wkt == wt:
                            m_, mt_ = causalT[:], 'causalT'
                        elif wkt == wt - 4:
                            m_, mt_ = antiT[:], 'antiT'
                        else:
                            m_, mt_ = None, None
                        lw = wkt - 44
                        unit(k, qi, kwT[:, k, lw * 128:(lw + 1) * 128], 'kwTm', Vw[:, lw, k, :], 'Vw', 65, m_, mt_, None, wi == 0, wi == 4)
                    finalize()
                    evac(k, qi, accF, 2, False)
                ys = yst[qi % 2]
                for half in range(2):
                    tb = 6 + half
                    for i4 in range(4):
                        i = half * 4 + i4
                        S.op('pe', lambda e: e.transpose(out=ps[tb][:, i4 * 128:(i4 + 1) * 128], in_=o_all[:, i, :, :], identity=c_ident[:]),
                             r=['o_all', 'c_ident'], w=[('ps', tb)])
                    S.op('act', lambda e: e.copy(out=ys[:, half * 4:half * 4 + 4, :], in_=ps[tb][:, :].rearrange("p (a b) -> p a b", b=128)),
                         r=[('ps', tb)], w=[('yst', qi % 2)])
                S.dma('sp', ynT_s.rearrange("(i p) t -> p i t", p=128)[:, :, qi * 128:(qi + 1) * 128], ys[:], r=[('yst', qi % 2)], w=[('ynT_s', qi)])
            S.barrier()

    def layer_norm(self, S, h, htag, out, otag, g_bc, b_bc, tmp, k):
        stats, mv = tmp
        for c in range(4):
            S.op('dve', lambda e: e.bn_stats(out=stats[:, c, :], in_=h[:, c * 512:(c + 1) * 512]), r=[htag], w=[('lnst', k)])
        S.op('dve', lambda e: e.bn_aggr(out=mv[:, 0:2], in_=stats[:].rearrange("p a b -> p (a b)")), r=[('lnst', k)], w=[('lnmv', k)])
        S.op('dve', lambda e: e.tensor_scalar(out=mv[:, 2:3], in0=mv[:, 1:2], scalar1=LN_EPS, scalar2=None, op0=ALU.add), r=[('lnmv', k)], w=[('lnmv', k)])
        S.op('act', lambda e: e.activation(out=mv[:, 2:3], in_=mv[:, 2:3], func=AF.Sqrt), r=[('lnmv', k)], w=[('lnmv', k)])
        S.op('dve', lambda e: e.reciprocal(out=mv[:, 2:3], in_=mv[:, 2:3]), r=[('lnmv', k)], w=[('lnmv', k)])
        S.op('dve', lambda e: e.tensor_scalar(out=h, in0=h, scalar1=mv[:, 0:1], scalar2=mv[:, 2:3], op0=ALU.subtract, op1=ALU.mult),
             r=[htag, ('lnmv', k)], w=[htag])
        S.op('dve', lambda e: e.tensor_tensor(out=h, in0=h, in1=g_bc, op=ALU.mult), r=[htag, 'ln_g'], w=[htag])
        S.op('dve', lambda e: e.tensor_tensor(out=out, in0=h, in1=b_bc, op=ALU.add), r=[htag, 'ln_b'], w=[otag])

    def phase5(self, S, ps, L):
        P = self
        ysT_s, ynT_s, x1_s, x1T_s, gT_s = L['ysT_s'], L['ynT_s'], L['x1_s'], L['x1T_s'], L['gT_s']
        x_own = self.ins['x_own']
        c_ident = self.c_ident
        ntt = self.ntt
        with ExitStack() as es:
            wo = P.sb(es, "wo", [128, 16, D], BF16)
            mixT = [P.sb(es, "mixT%d" % i, [128, 16, 128], BF16) for i in range(2)]
            g_bc = P.sb(es, "ln1g", [128, D])
            b_bc = P.sb(es, "ln1b", [128, D])
            wr = P.sb(es, "wr", [128, 16, 64])
            rb = P.sb(es, "rb", [128, 64])
            wov = self.ins['w_out'].rearrange("(c p) n -> p c n", p=128)
            for dcg in range(0, 16, 2):
                S.dma('pool', wo[:, dcg:dcg + 2, :], wov[:, dcg:dcg + 2, :], w=[('wo', dcg)])
            wotags = [('wo', dcg) for dcg in range(0, 16, 2)]
            S.dma('sp', g_bc[:], self.ins['ln1_g'][0:1, :].to_broadcast([128, D]), w=['ln_g'])
            S.dma('sp', b_bc[:], self.ins['ln1_b'][0:1, :].to_broadcast([128, D]), w=['ln_b'])
            S.dma('sp', wr[:], self.ins['w_router'].rearrange("(c p) n -> p c n", p=128), w=['wr'])
            S.dma('sp', rb[:], self.ins['router_bias'][0:1, :].to_broadcast([128, 64]), w=['rb'])
            xo = [P.sb(es, "xo%d" % i, [128, D]) for i in range(2)]
            h_ = [P.sb(es, "h%d" % i, [128, D]) for i in range(2)]
            x1 = [P.sb(es, "x1_%d" % i, [128, D]) for i in range(2)]
            lnt = (P.sb(es, "lnstats", [128, 4, 6]), P.sb(es, "lnmv", [128, 4]))
            xTf = P.sb(es, "xTf", [128, 16, 128])
            xTb = [P.sb(es, "xTb%d" % i, [128, 16, 128], BF16) for i in range(2)]
            r_ = {}
            for nm, shp in [('aff', [128, 64]), ('bia', [128, 64]), ('m1', [128, 8]), ('eq', [128, 64]), ('b2', [128, 64]), ('m2', [128, 8]), ('gs', [128, 8]),
                            ('t8', [128, 8]), ('gm', [128, 8]), ('pen', [128, 8]), ('msk', [128, 64]), ('em', [128, 64]), ('wv', [128, 64]), ('ss', [128, 2]),
                            ('gate', [128, 64])]:
                r_[nm] = P.sb(es, "r_" + nm, shp)
            gst = [P.sb(es, "gst%d" % i, [64, 128]) for i in range(2)]
            TT = lambda o, a_, b_, op, r, w: S.op('dve', lambda e: e.tensor_tensor(out=o, in0=a_, in1=b_, op=op), r=r, w=w)
            TS = lambda o, a_, s1, op, r, w: S.op('dve', lambda e: e.tensor_scalar(out=o, in0=a_, scalar1=s1, scalar2=None, op0=op), r=r, w=w)
            for tt in range(ntt):
                k = tt % 2
                S.dma('sp', xo[k][:], x_own[tt * 128:(tt + 1) * 128, :], w=[('xo', k)])
                S.dma('sp', mixT[k][:, 0:8, :], ysT_s.rearrange("(c p) t -> p c t", p=128)[:, :, tt * 128:(tt + 1) * 128], w=[('mixT0', k)])
                S.dma('sp', mixT[k][:, 8:16, :], ynT_s.rearrange("(c p) t -> p c t", p=128)[:, :, tt * 128:(tt + 1) * 128], w=[('mixT1', k)])
                for dg in range(4):
                    for kc in range(16):
                        S.op('pe', lambda e: e.matmul(ps[dg][:, :], lhsT=mixT[k][:, kc, :], rhs=wo[:, kc, dg * 512:(dg + 1) * 512],
                                                      start=(kc == 0), stop=(kc == 15)), r=wotags + [('mixT0', k), ('mixT1', k)] if kc in (0, 15) else [], w=[('ps', dg)])
                    S.op('dve', lambda e: e.scalar_tensor_tensor(out=h_[k][:, dg * 512:(dg + 1) * 512], in0=xo[k][:, dg * 512:(dg + 1) * 512], scalar=ALPHA,
                                                                 in1=ps[dg][:, :], op0=ALU.mult, op1=ALU.add), r=[('xo', k), ('ps', dg)], w=[('h', k)])
                import os as _os
                cut = int(_os.environ.get('P5CUT', '9'))
                if cut <= 1:
                    S.dma('sp', x1_s[tt * 128:(tt + 1) * 128, :], h_[k][:], r=[('h', k)], w=[('x1_s', tt)])
                    continue
                self.layer_norm(S, h_[k][:], ('h', k), x1[k][:], ('x1', k), g_bc[:], b_bc[:], lnt, 0)
                S.dma('sp', x1_s[tt * 128:(tt + 1) * 128, :], x1[k][:], r=[('x1', k)], w=[('x1_s', tt)])
                if cut <= 2:
                    continue
                for q4 in range(4):
                    tb = 4 + q4 % 2
                    for i4 in range(4):
                        dc = q4 * 4 + i4
                        S.op('pe', lambda e: e.transpose(out=ps[tb][:, i4 * 128:(i4 + 1) * 128], in_=x1[k][:, dc * 128:(dc + 1) * 128], identity=c_ident[:]),
                             r=[('x1', k), 'c_ident'], w=[('ps', tb)])
                    S.op('act', lambda e: e.copy(out=xTf[:, q4 * 4:q4 * 4 + 4, :], in_=ps[tb][:, :].rearrange("p (a b) -> p a b", b=128)), r=[('ps', tb)], w=['xTf'])
                    S.op('dve', lambda e: e.tensor_copy(out=xTb[k][:, q4 * 4:q4 * 4 + 4, :], in_=xTf[:, q4 * 4:q4 * 4 + 4, :]), r=['xTf'], w=[('xTb', k)])
                S.dma('sp', x1T_s.rearrange("(c p) t -> p c t", p=128)[:, :, tt * 128:(tt + 1) * 128], xTb[k][:], r=[('xTb', k)], w=[('x1T_s', tt)])
                if cut <= 3:
                    continue
                for dc in range(16):
                    S.op('pe', lambda e: e.matmul(ps[6][:, 0:64], lhsT=xTf[:, dc, :], rhs=wr[:, dc, :], start=(dc == 0), stop=(dc == 15)), r=['xTf', 'wr'], w=[('ps', 6)])
                S.op('act', lambda e: e.activation(out=r_['aff'][:], in_=ps[6][:, 0:64], func=AF.Sigmoid), r=[('ps', 6)], w=['aff'])
                if cut <= 4:
                    continue
                TT(r_['bia'][:], r_['aff'][:], rb[:], ALU.add, ['aff', 'rb'], ['bia'])
                b3 = r_['bia'][:].rearrange("p (g e) -> p g e", e=8)
                S.op('dve', lambda e: e.tensor_reduce(out=r_['m1'][:], in_=b3, axis=AX.X, op=ALU.max), r=['bia'], w=['m1'])
                TT(r_['eq'][:].rearrange("p (g e) -> p g e", e=8), b3, r_['m1'][:].unsqueeze(2).to_broadcast([128, 8, 8]), ALU.is_equal, ['bia', 'm1'], ['eq'])
                S.op('dve', lambda e: e.scalar_tensor_tensor(out=r_['b2'][:], in0=r_['eq'][:], scalar=-1e9, in1=r_['bia'][:], op0=ALU.mult, op1=ALU.add), r=['eq', 'bia'], w=['b2'])
                S.op('dve', lambda e: e.tensor_reduce(out=r_['m2'][:], in_=r_['b2'][:].rearrange("p (g e) -> p g e", e=8), axis=AX.X, op=ALU.max), r=['b2'], w=['m2'])
                TT(r_['gs'][:], r_['m1'][:], r_['m2'][:], ALU.add, ['m1', 'm2'], ['gs'])
                S.op('dve', lambda e: e.max(out=r_['t8'][:], in_=r_['gs'][:]), r=['gs'], w=['t8'])
                TS(r_['gm'][:], r_['gs'][:], r_['t8'][:, 3:4], ALU.is_ge, ['gs', 't8'], ['gm'])
                S.op('dve', lambda e: e.tensor_scalar(out=r_['pen'][:], in0=r_['gm'][:], scalar1=-1.0, scalar2=1e9, op0=ALU.add, op1=ALU.mult), r=['gm'], w=['pen'])
                m3 = r_['msk'][:].rearrange("p (g e) -> p g e", e=8)
                TT(m3, b3, r_['gm'][:].unsqueeze(2).to_broadcast([128, 8, 8]), ALU.mult, ['bia', 'gm'], ['msk'])
                TT(m3, m3, r_['pen'][:].unsqueeze(2).to_broadcast([128, 8, 8]), ALU.add, ['msk', 'pen'], ['msk'])
                S.op('dve', lambda e: e.max(out=r_['t8'][:], in_=r_['msk'][:]), r=['msk', 'gm'], w=['t8'])
                TS(r_['em'][:], r_['msk'][:], r_['t8'][:, 7:8], ALU.is_ge, ['msk', 't8'], ['em'])
                TT(r_['wv'][:], r_['aff'][:], r_['em'][:], ALU.mult, ['aff', 'em'], ['wv'])
                S.op('dve', lambda e: e.tensor_reduce(out=r_['ss'][:, 0:1], in_=r_['wv'][:], axis=AX.X, op=ALU.add), r=['wv'], w=['ss'])
                S.op('dve', lambda e: e.reciprocal(out=r_['ss'][:, 1:2], in_=r_['ss'][:, 0:1]), r=['ss'], w=['ss'])
                S.op('dve', lambda e: e.tensor_scalar(out=r_['gate'][:], in0=r_['wv'][:], scalar1=r_['ss'][:, 1:2], scalar2=2.5, op0=ALU.mult, op1=ALU.mult), r=['wv', 'ss'], w=['gate'])
                S.op('pe', lambda e: e.transpose(out=ps[7][0:64, 0:128], in_=r_['gate'][:], identity=c_ident[:]), r=['gate', 'c_ident'], w=[('ps', 7)])
                S.op('act', lambda e: e.copy(out=gst[k][:], in_=ps[7][0:64, 0:128]), r=[('ps', 7)], w=[('gst', k)])
                S.dma('sp', gT_s[:, tt * 128:(tt + 1) * 128], gst[k][:], r=[('gst', k)], w=[('gT_s', tt)])
            S.barrier()

    def phase6(self, S, ps, L):
        P = self
        x1_s, x1T_s, gT_s = L['x1_s'], L['x1T_s'], L['gT_s']
        out = L['out']
        c_ident = self.c_ident
        nexp = self.nexp
        with ExitStack() as es:
            acc = P.sb(es, "acc", [128, 8, D])
            xT = P.sb(es, "x1T", [128, 16, 1024], BF16)
            gT = P.sb(es, "gTsb", [64, 1024])
            g_bc = P.sb(es, "ln2g", [128, D])
            b_bc = P.sb(es, "ln2b", [128, D])
            S.dma('sp', g_bc[:], self.ins['ln2_g'][0:1, :].to_broadcast([128, D]), w=['ln_g'])
            S.dma('sp', b_bc[:], self.ins['ln2_b'][0:1, :].to_broadcast([128, D]), w=['ln_b'])
            wgb = [P.sb(es, "wgb%d" % i, [128, 16, 256], BF16) for i in range(2)]
            wub = [P.sb(es, "wub%d" % i, [128, 16, 256], BF16) for i in range(2)]
            wdb = [P.sb(es, "wdb%d" % i, [128, 4, D], BF16) for i in range(2)]
            hT = [P.sb(es, "hT%d" % i, [128, 4, 1024], BF16) for i in range(2)]
            gbc = [P.sb(es, "gbc%d" % tg, [128, 512]) for tg in range(2)]
            sg = [P.sb(es, "sgm%d" % i, [128, 512]) for i in range(2)]
            tm = [P.sb(es, "tm%d" % i, [128, 512]) for i in range(1)] * 2
            lnt = (P.sb(es, "ln2stats", [128, 4, 6]), P.sb(es, "ln2mv", [128, 4]))
            cnt = {'gu': 0, 'y': 0}
            NE = nexp

            def wsrc(e_):
                if e_ == 64:
                    return self.ins['ws_gate'], self.ins['ws_up'], self.ins['ws_down']
                return self.ins['w_gate'][e_], self.ins['w_up'][e_], self.ins['w_down'][e_]

            def loads_gu(ge, fh):
                wgs, wus, wds = wsrc(ge % NE)
                S.dma('pool', wgb[fh][:], wgs.rearrange("(c p) f -> p c f", p=128)[:, :, fh * 256:(fh + 1) * 256], w=[('wg', fh)])
                S.dma('pool', wub[fh][:], wus.rearrange("(c p) f -> p c f", p=128)[:, :, fh * 256:(fh + 1) * 256], w=[('wu', fh)])

            def loads_d(ge):
                wgs, wus, wds = wsrc(ge % NE)
                S.dma('pool', wdb[ge % 2][:], wds.rearrange("(c p) d -> p c d", p=128), w=[('wd', ge % 2)])

            def gate_bc(ge):
                e_ = ge % NE
                if e_ == 64:
                    return
                for tg in range(2):
                    S.op('pe', lambda e: e.matmul(ps[6][:, :], lhsT=c_ident[0:64, e_:e_ + 1].to_broadcast([64, 128]), rhs=gT[:, tg * 512:(tg + 1) * 512],
                                                  start=True, stop=True), r=['c_ident', 'gTsb'], w=[('ps', 6)])
                    S.op('act', lambda e: e.copy(out=gbc[tg][:], in_=ps[6][:, :]), r=[('ps', 6)], w=[('gbc', tg)])

            def GU(ge, fh):
                e_ = ge % NE
                shared = (e_ == 64)
                wg_, wu_ = wgb[fh], wub[fh]
                hb = hT[ge % 2]
                htag = ('hT', ge % 2)
                for tg in range(2):
                    for fc in range(2):
                        bg = (cnt['gu'] % 2) * 2
                        cnt['gu'] += 1
                        k = cnt['gu'] % 2
                        for dc in range(16):
                            S.op('pe', lambda e: e.matmul(ps[bg][:, :], lhsT=wg_[:, dc, fc * 128:(fc + 1) * 128], rhs=xT[:, dc, tg * 512:(tg + 1) * 512],
                                                          start=(dc == 0), stop=(dc == 15)), r=[('wg', fh), 'x1T'] if dc in (0, 15) else [], w=[('ps', bg)])
                        for dc in range(16):
                            S.op('pe', lambda e: e.matmul(ps[bg + 1][:, :], lhsT=wu_[:, dc, fc * 128:(fc + 1) * 128], rhs=xT[:, dc, tg * 512:(tg + 1) * 512],
                                                          start=(dc == 0), stop=(dc == 15)), r=[('wu', fh), 'x1T'] if dc in (0, 15) else [], w=[('ps', bg + 1)])
                        S.op('act', lambda e: e.activation(out=sg[k][:], in_=ps[bg][:, :], func=AF.Silu), r=[('ps', bg)], w=[('sg', k)])
                        hdst = hb[:, fh * 2 + fc, tg * 512:(tg + 1) * 512]
                        if shared:
                            S.op('dve', lambda e: e.tensor_tensor(out=hdst, in0=ps[bg + 1][:, :], in1=sg[k][:], op=ALU.mult), r=[('ps', bg + 1), ('sg', k)], w=[htag])
                        else:
                            S.op('dve', lambda e: e.tensor_tensor(out=tm[k][:], in0=ps[bg + 1][:, :], in1=sg[k][:], op=ALU.mult), r=[('ps', bg + 1), ('sg', k)], w=[('tm', 0)])
                            S.op('dve', lambda e: e.tensor_tensor(out=hdst, in0=tm[k][:], in1=gbc[tg][:], op=ALU.mult), r=[('tm', 0), ('gbc', tg)], w=[htag])

            def DN(ge):
                wd_ = wdb[ge % 2]
                hb = hT[ge % 2]
                htag = ('hT', ge % 2)
                for t8 in range(8):
                    for dg in range(4):
                        by = (4, 5, 7)[cnt['y'] % 3]
                        cnt['y'] += 1
                        for fc in range(4):
                            S.op('pe', lambda e: e.matmul(ps[by][:, :], lhsT=hb[:, fc, t8 * 128:(t8 + 1) * 128], rhs=wd_[:, fc, dg * 512:(dg + 1) * 512],
                                                          start=(fc == 0), stop=(fc == 3)), r=[htag, ('wd', ge % 2)], w=[('ps', by)])
                        asl = acc[:, t8, dg * 512:(dg + 1) * 512]
                        atag = ('acc', t8, dg)
                        S.op('dve', lambda e: e.tensor_tensor(out=asl, in0=asl, in1=ps[by][:, :], op=ALU.add), r=[('ps', by), atag], w=[atag])

            acctags = [('acc', t8, dg) for t8 in range(8) for dg in range(4)] + [('accrow', t8) for t8 in range(8)]
            for half in range(self.nhalf):
                tk0 = half * 1024
                S.dma('sp', xT[:], x1T_s.rearrange("(c p) t -> p c t", p=128)[:, :, tk0:tk0 + 1024], w=['x1T'])
                S.dma('sp', gT[:], gT_s[:, tk0:tk0 + 1024], w=['gTsb'])
                S.dma('sp', acc[:], x1_s[tk0:tk0 + 1024, :].rearrange("(t p) d -> p t d", p=128), w=acctags)
                for t8 in range(8):
                    S.op('act', lambda e: e.activation(out=acc[:, t8, :], in_=acc[:, t8, :], func=AF.Copy, scale=ALPHA),
                         r=[('acc', t8, dg) for dg in range(4)], w=[('acc', t8, dg) for dg in range(4)] + [('accrow', t8)])
                g0 = half * NE
                loads_gu(g0, 0)
                loads_gu(g0, 1)
                loads_d(g0)
                gate_bc(g0)
                GU(g0, 0)
                if NE > 1:
                    loads_gu(g0 + 1, 0)
                GU(g0, 1)
                if NE > 1:
                    loads_gu(g0 + 1, 1)
                    loads_d(g0 + 1)
                for ei in range(NE):
                    ge = g0 + ei
                    if ei + 1 < NE:
                        gate_bc(ge + 1)
                        GU(ge + 1, 0)
                        if ei + 2 < NE:
                            loads_gu(ge + 2, 0)
                        GU(ge + 1, 1)
                        if ei + 2 < NE:
                            loads_gu(ge + 2, 1)
                    DN(ge)
                    if ei + 2 < NE:
                        loads_d(ge + 2)
                for t8 in range(8):
                    k = t8 % 2
                    k = 0
                    S.op('dve', lambda e: e.tensor_copy(out=lnt[1][:, 3:4], in_=acc[:, t8, 0:1]), r=[('acc', t8, dg) for dg in range(4)], w=[('accrow', t8)] + [('acc', t8, dg) for dg in range(4)])
                    self.layer_norm(S, acc[:, t8, :], ('accrow', t8), acc[:, t8, :], ('accrow', t8), g_bc[:], b_bc[:], lnt, 1)
                    r0 = tk0 + t8 * 128
                    S.dma('sp', out[r0:r0 + 128, :], acc[:, t8, :], r=[('accrow', t8)], w=[('out', r0)])
            S.barrier()

    def finish_debug(self, S, L):
        for name in sorted(self.dbg):
            t = L[name]
            if name == 'ynT_s':
                t = t[:, 0:self.nq * 128]
            shape = list(t.shape)
            o = self.dout("dbg_" + name, shape, t.dtype)
            S.dma('sp', o, t if isinstance(t, bass.AP) else t[:], w=[('dbg', name)])
        S.barrier()


def host_constants():
    c = {}
    c["identf"] = np.eye(128, dtype=np.float32)
    pm = np.zeros((128, 128), np.float32)
    for m in range(128):
        k = (m % 64 + 32) % 64 + 64 * (m // 64)
        pm[k, m] = 1.0
    c["pswap"] = pm
    rc = np.zeros((128, 4), np.float32)
    inv = 10000.0 ** (-(np.arange(32, dtype=np.float64)) / 32.0)
    for p in range(128):
        rc[p, 0] = inv[p % 32] / TWO_PI
        rc[p, 1] = (-TWO_PI if (p % 64) < 32 else TWO_PI)
    c["ropecol"] = rc
    n_ = (np.arange(4)[None, :, None] * 128 + np.arange(128)[:, None, None])
    s_ = np.arange(128)[None, None, :]
    c["omat"] = ((16 * n_ <= 64 * s_ + 63) & (16 * n_ + 31 >= 64 * s_)).astype(np.float32)
    kk = np.arange(128)[:, None]
    qq = np.arange(128)[None, :]
    c["causalT"] = (kk <= qq).astype(np.float32)
    c["antiT"] = (kk > qq).astype(np.float32)
    nl = np.arange(128)[:, None, None, None]
    qi = np.arange(16)[None, :, None, None]
    aa = np.arange(2)[None, None, :, None] + 2
    ql = np.arange(128)[None, None, None, :]
    c["cmask"] = (16 * (128 * aa + nl) + 31 <= (48 + qi) * 128 + ql).astype(np.float32)
    c["sidx"] = np.broadcast_to(np.arange(128, dtype=np.float32)[None, :], (128, 128)).copy()
    c["qhalf"] = (np.arange(128) >= 64).astype(np.float32)[:, None]
    c["iota1"] = np.broadcast_to(np.arange(1, 513, dtype=np.float32)[None, :], (128, 512)).copy()
    c["iotar"] = np.broadcast_to((511.0 - np.arange(512, dtype=np.float32))[None, :], (128, 512)).copy()
    return c


def core_inputs(c, x, positions, shared):
    b, j = c // 4, c % 4
    t0 = NOWN * j
    pad = W - (t0 + NOWN)
    m = {}
    xT = np.zeros((D, W), np.float32)
    xT[:, pad:] = x[b, :t0 + NOWN, :].T
    m["xT"] = xT
    m["x_own"] = np.ascontiguousarray(x[b, t0:t0 + NOWN, :])
    pw = np.zeros((1, W), np.int32)
    pw[0, pad:] = positions[b, :t0 + NOWN]
    m["posw"] = pw
    pc = np.zeros((1, 512), np.int32)
    pc[0, :] = pw[0, np.minimum(np.arange(512) * 16, W - 1)]
    m["posc"] = pc
    widx = np.arange(64)[None, :] * 128 + np.arange(128)[:, None]
    m["kvalid"] = (widx >= pad).astype(np.float32)
    nidx = np.arange(4)[None, :] * 128 + np.arange(128)[:, None]
    m["nvalid"] = (nidx >= pad // 16).astype(np.float32)
    pc2 = np.zeros((128, 2), np.float32)
    pc2[:, 0] = pad // 64
    pc2[:, 1] = pad
    m["padcol"] = pc2
    m.update(shared)
    return m


def make_shared(inp):
    sh = host_constants()
    sh["w_in"] = np.ascontiguousarray(inp["w_in"][0][:, w_in_columns()])
    for nm in ["w_cmp_k1", "w_cmp_v1", "w_cmp_k2", "w_cmp_v2"]:
        sh[nm] = np.ascontiguousarray(inp[nm][0])
    G2 = lambda a: np.asarray(a[0]).reshape(32, 2, *a.shape[2:])
    lam = np.stack([G2(inp["lam_re"]), G2(inp["lam_im"]),
                    np.broadcast_to(G2(inp["log_dt"])[:, :, None], (32, 2, 64))], -1)
    sh["s5_lam"] = np.ascontiguousarray(lam.transpose(1, 2, 0, 3).reshape(128, 32, 3)).astype(np.float32)
    bb = np.stack([G2(inp["ssm_b_re"]), G2(inp["ssm_b_im"])], 3)
    sh["s5_b"] = np.ascontiguousarray(bb.transpose(1, 2, 0, 3, 4).reshape(128, 32, 2, 16)).astype(np.float32)
    cc = np.stack([G2(inp["ssm_c_re"]), G2(inp["ssm_c_im"])], 2)
    sh["s5_c"] = np.ascontiguousarray(cc.transpose(1, 4, 0, 2, 3).reshape(128, 32, 2, 16)).astype(np.float32)
    sh["s5_d"] = np.ascontiguousarray(G2(inp["ssm_d"]).transpose(1, 2, 0).reshape(32, 32)).astype(np.float32)
    sh["w_glu"] = np.ascontiguousarray(inp["w_glu"][0])
    wo = np.asarray(inp["w_out"][0])
    sh["w_out"] = np.ascontiguousarray(np.concatenate([wo[:1024], wo[1024:].reshape(16, 64, D)[q_head_order()].reshape(1024, D)], 0))
    for nm in ["ln1_g", "ln1_b", "ln2_g", "ln2_b", "w_router", "router_bias", "w_gate", "w_up", "w_down", "ws_gate", "ws_up", "ws_down"]:
        sh[nm] = np.ascontiguousarray(inp[nm][0])
    for nm in ["cmp_pos_k", "cmp_pos_v"]:
        t = np.asarray(inp[nm][0]).T
        sh[nm + "T"] = np.ascontiguousarray(np.concatenate([t, t], 0))
    return sh


_CACHE = {}


def kernel(**inputs):
    x = np.asarray(inputs["x"], np.float32)
    positions = np.asarray(inputs["positions"], np.int32)
    shared = make_shared(inputs)
    if 'nc' not in _CACHE:
        _CACHE['nc'] = Prog('all').build()
    nc = _CACHE['nc']
    in_maps = [core_inputs(c, x, positions, shared) for c in range(8)]
    res = run_bass_kernel_spmd(nc, in_maps, core_ids=list(range(8)))
    out = np.zeros((2, SEQ, D), np.float32)
    for c in range(8):
        b, j = c // 4, c % 4
        out[b, j * NOWN:(j + 1) * NOWN] = res.results[c]["out"]
    return out
```

```python
import math
from contextlib import ExitStack

import numpy as np
import concourse.bass as bass
import concourse.mybir as mybir
from concourse.bass_utils import run_bass_kernel_spmd

F32 = mybir.dt.float32
BF16 = mybir.dt.bfloat16
I32 = mybir.dt.int32
AF = mybir.ActivationFunctionType
ALU = mybir.AluOpType
AX = mybir.AxisListType

D = 2048
SEQ = 8192
W = 8192
NOWN = 2048
NG = W // 512
OWN_G0 = (W - NOWN) // 512
ALPHA = 2.0 ** 0.25
LN_EPS = 1e-5
TWO_PI = 2.0 * math.pi


class Sched:
    SEM_LIMIT = 20000

    def __init__(self, nc, es, n_dma_sems=40):
        self.nc = nc
        self.es = es
        self.E = {'pe': nc.tensor, 'act': nc.scalar, 'dve': nc.vector, 'pool': nc.gpsimd, 'sp': nc.sync}
        self.cur = {}
        self.known = {e: {} for e in self.E}
        self.lastw = {}
        self.reads = {}
        self.nsem = 0
        self.dma_slots = {'hw': [[self._newsem('dmah'), 0] for _ in range(n_dma_sems // 2)],
                          'sw': [[self._newsem('dmas'), 0] for _ in range(n_dma_sems // 2)]}
        self.dma_rr = {'hw': 0, 'sw': 0}
        self.ninst = {e: 0 for e in self.E}

    def _newsem(self, name):
        self.nsem += 1
        return self.es.enter_context(self.nc.semaphore('%s_%d' % (name, self.nsem)))

    def _tick(self, eng):
        c = self.cur.get(eng)
        if c is None or c[1] >= self.SEM_LIMIT:
            c = [self._newsem(eng), 0]
            self.cur[eng] = c
        c[1] += 1
        return c[0], c[1]

    def _wait(self, eng, dep):
        sem, val, deng = dep
        k = id(sem)
        if self.known[eng].get(k, 0) >= val:
            return
        self.E[eng].wait_ge(sem, val)
        self.known[eng][k] = val

    def _deps(self, eng, r, w):
        deps = []
        for res in r:
            lw = self.lastw.get(res)
            if lw is not None:
                deps.append(lw)
        for res in w:
            lw = self.lastw.get(res)
            if lw is not None:
                deps.append(lw)
            rd = self.reads.get(res)
            if rd:
                deps.extend(rd[0].values())
                deps.extend(rd[1])
        for d in deps:
            if d[2] == eng and eng == 'pe':
                continue
            self._wait(eng, d)

    def _commit(self, me, r, w):
        for res in w:
            self.lastw[res] = me
            self.reads[res] = ({}, [])
        for res in r:
            if res in w:
                continue
            rd = self.reads.get(res)
            if rd is None:
                rd = ({}, [])
                self.reads[res] = rd
            if me[2] == 'dma':
                rd[1].append(me)
            else:
                rd[0][me[2]] = me

    def op(self, eng, fn, r=(), w=()):
        self._deps(eng, r, w)
        sem, val = self._tick(eng)
        inst = fn(self.E[eng])
        inst.then_inc(sem, 1)
        self.ninst[eng] += 1
        me = (sem, val, eng)
        self._commit(me, r, w)
        return me

    def dma(self, q, out, in_, r=(), w=(), **kw):
        self._deps(q, r, w)
        kind = 'sw' if q == 'pool' else 'hw'
        slots = self.dma_slots[kind]
        slot = slots[self.dma_rr[kind]]
        self.dma_rr[kind] = (self.dma_rr[kind] + 1) % len(slots)
        if slot[1] > 0:
            self._wait(q, (slot[0], slot[1], 'dma'))
        if slot[1] >= self.SEM_LIMIT:
            slot[0] = self._newsem('dmax')
            slot[1] = 0
        slot[1] += 16
        self.E[q].dma_start(out=out, in_=in_, **kw).then_inc(slot[0], 16)
        self.ninst[q] += 1
        me = (slot[0], slot[1], 'dma')
        self._commit(me, r, w)
        return me

    def barrier(self):
        marks = []
        for e, c in self.cur.items():
            if c[1] > 0:
                marks.append((c[0], c[1], e))
        for slots in self.dma_slots.values():
            for slot in slots:
                if slot[1] > 0:
                    marks.append((slot[0], slot[1], 'dma'))
        for eng in self.E:
            for m in marks:
                if m[2] == eng and eng == 'pe':
                    continue
                self._wait(eng, m)
        self.lastw = {}
        self.reads = {}


C_U = 0
C_KC = 8 * 128
C_VC = 9 * 128
NC1 = 10 * 128
C_KS = 0
C_Q = 128
C_KW = 9 * 128
C_VS = 10 * 128
C_VW = 11 * 128
C_G = 12 * 128
NC2 = 12 * 128 + 48
NCOL = NC1 + NC2


def q_head_order():
    return [h for i in range(8) for h in (i, 8 + i)]


def w_in_columns():
    u = np.arange(0, 1024)
    q = np.arange(1024, 2048).reshape(16, 64)[q_head_order()].reshape(-1)
    kc = np.arange(2048, 2176)
    vc = np.arange(2176, 2304)
    ks = np.arange(2304, 2432)
    vs = np.arange(2432, 2560)
    kw = np.arange(2560, 2688)
    vw = np.arange(2688, 2816)
    g = np.arange(2816, 2864)
    cols = np.concatenate([u, kc, vc, ks, q, kw, vs, vw, g])
    assert cols.shape[0] == NCOL
    return cols


class Prog:
    def __init__(self, upto='all', dbg=()):
        self.upto = upto
        self.dbg = set(dbg)
        self.nc = bass.Bass("TRN2", target_bir_lowering=False)
        self.nq = 16
        self.nst = 32
        self.ntt = 16
        self.nexp = 65
        self.nhalf = 2
        self.skip = set()
        self.ext_scratch = set()
        self.dbg_qi, self.dbg_k = 0, 0
        self.ins = {}
        self.outs = {}

    def din(self, name, shape, dt=F32):
        t = self.nc.dram_tensor(name, list(shape), dt, kind="ExternalInput").ap()
        self.ins[name] = t
        return t

    def dout(self, name, shape, dt=F32):
        t = self.nc.dram_tensor(name, list(shape), dt, kind="ExternalOutput").ap()
        self.outs[name] = t
        return t

    def dscr(self, name, shape, dt=F32):
        if name in getattr(self, 'ext_scratch', ()):
            return self.din(name, shape, dt)
        return self.nc.dram_tensor(name, list(shape), dt, kind="Internal").ap()

    def sb(self, es, name, shape, dt=F32):
        self._nsb = getattr(self, '_nsb', 0) + 1
        return es.enter_context(self.nc.sbuf_tensor("%s_%d" % (name, self._nsb), list(shape), dt))

    def range_reduce(self, S, eng, fr, r, ti, tf, rtag, wtags):
        MAGIC = 12582912.0
        S.op(eng, lambda e: e.tensor_scalar(out=tf, in0=r, scalar1=MAGIC, scalar2=-MAGIC, op0=ALU.add, op1=ALU.add), r=[rtag], w=[wtags[1]])
        S.op(eng, lambda e: e.tensor_tensor(out=fr, in0=r, in1=tf, op=ALU.subtract), r=[rtag, wtags[1]], w=[wtags[2]])

    def build(self):
        nc = self.nc
        P = self
        xT = P.din("xT", [D, W])
        x_own = P.din("x_own", [NOWN, D])
        posw = P.din("posw", [1, W], I32)
        posc = P.din("posc", [1, 512], I32)
        kvalid = P.din("kvalid", [128, 64])
        nvalid = P.din("nvalid", [128, 4])
        padcol = P.din("padcol", [128, 2])
        w_in = P.din("w_in", [D, NCOL])
        ropecol = P.din("ropecol", [128, 4])
        identf = P.din("identf", [128, 128])
        pswap = P.din("pswap", [128, 128])
        P.din("w_cmp_k1", [2048, 128]); P.din("w_cmp_v1", [2048, 128])
        P.din("w_cmp_k2", [128, 64]); P.din("w_cmp_v2", [128, 64])
        P.din("cmp_pos_kT", [128, 32]); P.din("cmp_pos_vT", [128, 32])
        P.din("omat", [128, 4, 128]); P.din("causalT", [128, 128]); P.din("antiT", [128, 128])
        P.din("cmask", [128, 16, 2, 128]); P.din("sidx", [128, 128]); P.din("qhalf", [128, 1])
        P.din("s5_lam", [128, 32, 3]); P.din("s5_b", [128, 32, 2, 16]); P.din("s5_c", [128, 32, 2, 16]); P.din("s5_d", [32, 32])
        P.din("iota1", [128, 512]); P.din("iotar", [128, 512]); P.din("w_glu", [1024, 1024])
        P.din("w_out", [D, D]); P.din("ln1_g", [1, D]); P.din("ln1_b", [1, D]); P.din("ln2_g", [1, D]); P.din("ln2_b", [1, D])
        P.din("w_router", [D, 64]); P.din("router_bias", [1, 64])
        P.din("w_gate", [64, D, 512]); P.din("w_up", [64, D, 512]); P.din("w_down", [64, 512, D])
        P.din("ws_gate", [D, 512]); P.din("ws_up", [D, 512]); P.din("ws_down", [512, D])
        uT_s = P.dscr("uT_s", [1024, W], BF16)
        kcT_s = P.dscr("kcT_s", [128, W], BF16)
        vcT_s = P.dscr("vcT_s", [128, W], BF16)

        with ExitStack() as es0:
            S = Sched(nc, es0)
            self.S = S
            psbig = es0.enter_context(nc.psum_tensor("psbig", [128, 4096], F32))
            self.psbig = psbig
            ps = [psbig[:, i * 512:(i + 1) * 512] for i in range(8)]
            c_ident = P.sb(es0, "c_ident", [128, 128])
            c_identb = P.sb(es0, "c_identb", [128, 128], BF16)
            c_pswap = P.sb(es0, "c_pswap", [128, 128], BF16)
            c_rope = P.sb(es0, "c_rope", [128, 4])
            c_kvalid = P.sb(es0, "c_kvalid", [128, 64])
            c_nvalid = P.sb(es0, "c_nvalid", [128, 4])
            c_pad = P.sb(es0, "c_pad", [128, 2])
            S.dma('sp', c_ident[:], identf[:, :], w=['c_ident'])
            S.dma('pool', c_identb[:], identf[:, :], w=['c_identb'])
            S.dma('pool', c_pswap[:], pswap[:, :], w=['c_pswap'])
            S.dma('sp', c_rope[:], ropecol[:, :], w=['c_rope'])
            S.dma('sp', c_kvalid[:], kvalid[:, :], w=['c_kvalid'])
            S.dma('sp', c_nvalid[:], nvalid[:, :], w=['c_nvalid'])
            S.dma('sp', c_pad[:], padcol[:, :], w=['c_pad'])
            self.c_rope, self.c_pswap, self.c_ident, self.c_identb = c_rope, c_pswap, c_ident, c_identb
            self.c_kvalid, self.c_nvalid, self.c_pad = c_kvalid, c_nvalid, c_pad
            S.barrier()

            if 'p1' not in self.skip:
                self.phase1(S, ps, 1, locals())
            if self.upto == 'p1a':
                self.finish_debug(S, locals())
                return nc
            ysT_s = P.dscr("ysT_s", [1024, NOWN], BF16)
            if 'yT_all' in self.dbg:
                d_yT = P.dout("dbg_yT_all", [128, 8, NOWN], BF16)
            if 'p3' not in self.skip:
                self.phase3(S, ps, locals())
            self.dbg.discard('yT_all')
            if self.upto == 'p3':
                self.finish_debug(S, locals())
                return nc

            es1 = es0.enter_context(ExitStack())
            ksT = P.sb(es1, "ksT", [128, W], BF16)
            kwT = P.sb(es1, "kwT", [128, NOWN + 512], BF16)
            qT = P.sb(es1, "qT", [128, 16, 8, 128], BF16)
            Vs = P.sb(es1, "Vs", [128, 64, 2, 65], BF16)
            Vw = P.sb(es1, "Vw", [128, 20, 2, 65], BF16)
            gates = P.sb(es1, "gates", [128, 16, 48])
            if 'p1' not in self.skip:
                self.phase1(S, ps, 2, locals())
            if self.upto == 'p1':
                self.finish_debug(S, locals())
                return nc
            kcT = P.sb(es1, "kcT", [128, 512], BF16)
            Vc1O = P.sb(es1, "Vc1O", [128, 4, 2, 193], BF16)
            if 'p2' not in self.skip:
                self.phase2(S, ps, locals())
            if self.upto == 'p2':
                self.finish_debug(S, locals())
                return nc
            ynT_s = P.dscr("ynT_s", [1024, NOWN], BF16)
            if 'selb' in self.dbg:
                d_selb = P.dout("dbg_selb", [128, 128], BF16)
                d_imp = P.dout("dbg_imp", [128, 128])
                self.dbg.discard('selb')
                self.dbg_sel = True
            if 'p4' not in self.skip:
                self.phase4(S, ps, locals())
            if self.upto == 'p4':
                self.finish_debug(S, locals())
                return nc
            es1.close()
            x1_s = P.dscr("x1_s", [NOWN, D])
            x1T_s = P.dscr("x1T_s", [D, NOWN], BF16)
            gT_s = P.dscr("gT_s", [64, NOWN])
            if 'p5' not in self.skip:
                self.phase5(S, ps, locals())
            if self.upto == 'p5':
                self.finish_debug(S, locals())
                return nc
            out = P.dout("out", [NOWN, D])
            self.phase6(S, ps, locals())
            self.finish_debug(S, locals())
        return nc

    def rope_tables(self, S, eng, pos_i, cs, scol, tagp, tmp):
        c_rope = self.c_rope
        posf, r, fr, ti, tf = tmp
        C, Ss = cs
        S.op(eng, lambda e: e.tensor_copy(out=posf, in_=pos_i), r=[tagp + 'pi'], w=[tagp + 'pf'])
        S.op(eng, lambda e: e.tensor_scalar(out=r, in0=posf, scalar1=scol, scalar2=c_rope[:, 0:1], op0=ALU.add, op1=ALU.mult),
             r=[tagp + 'pf', 'c_rope'], w=[tagp + 'r'])
        self.range_reduce(S, eng, fr, r, ti, tf, tagp + 'r', [tagp + 'ti', tagp + 'tf', tagp + 'fr'])
        S.op('act', lambda e: e.activation(out=Ss, in_=fr, func=AF.Sin, scale=c_rope[:, 1:2]), r=[tagp + 'fr', 'c_rope'], w=[tagp + 'S'])
        S.op(eng, lambda e: e.tensor_scalar(out=r, in0=r, scalar1=0.25, scalar2=None, op0=ALU.add), r=[tagp + 'r'], w=[tagp + 'r'])
        self.range_reduce(S, eng, fr, r, ti, tf, tagp + 'r', [tagp + 'ti', tagp + 'tf', tagp + 'fr'])
        S.op('act', lambda e: e.activation(out=C, in_=fr, func=AF.Sin, scale=TWO_PI), r=[tagp + 'fr'], w=[tagp + 'C'])

    def rope_apply(self, S, src_ps, src_tag, dst, dst_tag, C, Ss, ctag, stag, raw, t1, t2, ps_sw, k):
        c_pswap = self.c_pswap
        S.op('act', lambda e: e.copy(out=raw, in_=src_ps), r=[src_tag], w=[('raw', k)])
        S.op('pe', lambda e: e.matmul(ps_sw, lhsT=c_pswap[:], rhs=raw, start=True, stop=True), r=[('raw', k), 'c_pswap'], w=[('pssw', k)])
        S.op('dve', lambda e: e.tensor_tensor(out=t1, in0=raw, in1=C, op=ALU.mult), r=[('raw', k), ctag], w=[('t1', k)])
        S.op('dve', lambda e: e.tensor_tensor(out=t2, in0=ps_sw, in1=Ss, op=ALU.mult), r=[('pssw', k), stag], w=[('t2', k)])
        if isinstance(dst, tuple):
            for hp, d_ in enumerate(dst):
                sl = slice(64 * hp, 64 * hp + 64)
                S.op('dve', lambda e: e.tensor_tensor(out=d_, in0=t1[sl], in1=t2[sl], op=ALU.add), r=[('t1', k), ('t2', k)], w=[dst_tag])
        elif len(dst.shape) == 3:
            S.op('dve', lambda e: e.tensor_tensor(out=dst, in0=t1.rearrange("p (a b) -> p a b", b=128), in1=t2.rearrange("p (a b) -> p a b", b=128), op=ALU.add),
                 r=[('t1', k), ('t2', k)], w=[dst_tag])
        else:
            S.op('dve', lambda e: e.tensor_tensor(out=dst, in0=t1, in1=t2, op=ALU.add), r=[('t1', k), ('t2', k)], w=[dst_tag])

    def phase1(self, S, ps, npass, L):
        nc = self.nc
        P = self
        xT = self.ins["xT"]
        w_in = self.ins["w_in"]
        posw = self.ins["posw"]
        c_kvalid = self.c_kvalid
        if npass == 1:
            uT_s, kcT_s, vcT_s = L['uT_s'], L['kcT_s'], L['vcT_s']
            wc0, wn = 0, NC1
        else:
            ksT, kwT, qT, Vs, Vw, gates = (L[k] for k in ['ksT', 'kwT', 'qT', 'Vs', 'Vw', 'gates'])
            wc0, wn = NC1, NC2
        with ExitStack() as es:
            w_sb = P.sb(es, "w_sb", [128, 16, wn], BF16)
            xg = [P.sb(es, "xg%d" % i, [128, 16, 512], BF16) for i in range(2)]
            if npass == 1:
                ust = [P.sb(es, "ust%d" % i, [128, 512], BF16) for i in range(4)]
            else:
                raw = [P.sb(es, "raw%d" % i, [128, 512], BF16) for i in range(2)]
                t1 = [P.sb(es, "t1_%d" % i, [128, 512]) for i in range(2)]
                t2 = [P.sb(es, "t2_%d" % i, [128, 512]) for i in range(2)]
                Ct = [P.sb(es, "Ct%d" % i, [128, 512]) for i in range(2)]
                St = [P.sb(es, "St%d" % i, [128, 512]) for i in range(2)]
                posi = [P.sb(es, "posi%d" % i, [128, 512], I32) for i in range(2)]
                rtmp = (P.sb(es, "r_pf", [128, 512]), P.sb(es, "r_r", [128, 512]), P.sb(es, "r_fr", [128, 512]),
                        P.sb(es, "r_ti", [128, 512], I32), P.sb(es, "r_tf", [128, 512]))
            wv = w_in.rearrange("(c p) n -> p c n", p=128)
            wblocks = [(c0, min(wn, c0 + 800)) for c0 in range(0, wn, 800)]
            for (c0, c1) in wblocks:
                for dcg in range(0, 16, 4):
                    S.dma('pool', w_sb[:, dcg:dcg + 4, c0:c1], wv[:, dcg:dcg + 4, wc0 + c0:wc0 + c1], w=[('w_sb', c0, dcg)])
            wtags = [('w_sb', c0, dcg) for (c0, c1) in wblocks for dcg in range(0, 16, 4)]
            xv = xT.rearrange("(c p) t -> p c t", p=128)
            nfm = 0
            ropek = 0
            nst = 0
            for g in range(NG):
                own = g >= OWN_G0
                xb = xg[g % 2]
                xtag = ('xg', g % 2)
                for dcg in range(0, 16, 4):
                    S.dma('pool', xb[:, dcg:dcg + 4, :], xv[:, dcg:dcg + 4, g * 512:(g + 1) * 512], w=[(xtag, dcg)])
                xtags = [(xtag, dcg) for dcg in range(0, 16, 4)]
                if npass == 1:
                    fm = [('u', i, C_U + 128 * i) for i in range(8)] + [('kc', 0, C_KC), ('vc', 0, C_VC)]
                else:
                    pi = posi[g % 2]
                    tp = 'rt%d' % (g % 2)
                    S.dma('sp', pi[:], posw[0:1, g * 512:(g + 1) * 512].to_broadcast([128, 512]), w=[tp + 'pi'])
                    C, Ss = Ct[g % 2], St[g % 2]
                    self.rope_tables(S, 'dve', pi[:], (C[:], Ss[:]), 0.0, tp, tuple(t[:] for t in rtmp))
                    fm = [('ks', 0, C_KS)]
                    if own:
                        fm += [('q', i, C_Q + 128 * i) for i in range(8)]
                    if g >= OWN_G0 - 1:
                        fm += [('kw', 0, C_KW)]
                for (kind, i, c0) in fm:
                    bank = nfm % 4
                    nfm += 1
                    pt = ps[bank]
                    ptag = ('ps', bank)
                    for dc in range(16):
                        S.op('pe', lambda e: e.matmul(pt[:, :], lhsT=w_sb[:, dc, c0:c0 + 128], rhs=xb[:, dc, :], start=(dc == 0), stop=(dc == 15)),
                             r=(wtags + xtags) if dc in (0, 15) else [], w=[ptag])
                    if kind in ('u', 'kc', 'vc'):
                        ub = ust[nst % 4]
                        utag = ('ust', nst % 4)
                        nst += 1
                        S.op('act', lambda e: e.copy(out=ub[:], in_=pt[:, :]), r=[ptag], w=[utag])
                        if kind == 'u':
                            dst = uT_s[i * 128:(i + 1) * 128, g * 512:(g + 1) * 512]
                        elif kind == 'kc':
                            dst = kcT_s[:, g * 512:(g + 1) * 512]
                        else:
                            dst = vcT_s[:, g * 512:(g + 1) * 512]
                        S.dma('sp', dst, ub[:], r=[utag], w=[('scr', kind, i, g)])
                    else:
                        if kind == 'ks':
                            dst, dtag = ksT[:, g * 512:(g + 1) * 512], ('ksT', g)
                        elif kind == 'q':
                            go = g - OWN_G0
                            dst, dtag = qT[:, 4 * go:4 * go + 4, i, :], ('qT', i, go)
                        else:
                            go = g - (OWN_G0 - 1)
                            dst, dtag = kwT[:, go * 512:(go + 1) * 512], ('kwT', go)
                        k = ropek % 2
                        ropek += 1
                        self.rope_apply(S, pt[:, :], ptag, dst, dtag, C[:], Ss[:], tp + 'C', tp + 'S',
                                        raw[k][:], t1[k][:], t2[k][:], ps[4 + k][:, :], k)
                if npass == 2:
                    for tt in range(4):
                        wt = g * 4 + tt
                        bank = 6 + (wt % 2)
                        pt = ps[bank]
                        ptag = ('ps', bank)
                        ncols = 128 + (176 if g >= OWN_G0 - 1 else 0)
                        for dc in range(16):
                            S.op('pe', lambda e: e.matmul(pt[:, 0:ncols], lhsT=xb[:, dc, tt * 128:(tt + 1) * 128], rhs=w_sb[:, dc, C_VS:C_VS + ncols],
                                                          start=(dc == 0), stop=(dc == 15)),
                                 r=(wtags + xtags) if dc in (0, 15) else [], w=[ptag])
                        S.op('act', lambda e: e.copy(out=Vs[:, wt, :, 0:64], in_=pt[:, 0:128].rearrange("p (h d) -> p h d", h=2)), r=[ptag], w=[('Vs', wt)])
                        if g >= OWN_G0 - 1:
                            lw = wt - (OWN_G0 - 1) * 4
                            S.op('act', lambda e: e.copy(out=Vw[:, lw, :, 0:64], in_=pt[:, 128:256].rearrange("p (h d) -> p h d", h=2)), r=[ptag], w=[('Vw', lw)])
                        if own:
                            lo = wt - OWN_G0 * 4
                            S.op('act', lambda e: e.activation(out=gates[:, lo, :], in_=pt[:, 256:304], func=AF.Sigmoid), r=[ptag], w=[('gates', lo)])
            if npass == 2:
                for h in range(2):
                    S.op('dve', lambda e: e.tensor_copy(out=Vs[:, :, h, 64], in_=c_kvalid[:, 0:64]), r=['c_kvalid'], w=[('Vs1', h)])
                    S.op('dve', lambda e: e.tensor_copy(out=Vw[:, :, h, 64], in_=c_kvalid[:, 44:64]), r=['c_kvalid'], w=[('Vw1', h)])
            S.barrier()

    def phase2(self, S, ps, L):
        P = self
        kcT_s, vcT_s = L['kcT_s'], L['vcT_s']
        kcT, Vc1O = L['kcT'], L['Vc1O']
        posc = self.ins['posc']
        c_nvalid = self.c_nvalid
        with ExitStack() as es:
            raws = [P.sb(es, "cr_k", [128, W], BF16), P.sb(es, "cr_v", [128, W], BF16)]
            w1 = [P.sb(es, "w1k", [128, 32, 128], BF16), P.sb(es, "w1v", [128, 32, 128], BF16)]
            w2k = [P.sb(es, "w2k%d" % h, [128, 128], BF16) for h in range(2)]
            w2v = P.sb(es, "w2v", [128, 64], BF16)
            peT = [P.sb(es, "peTk", [128, 32], BF16), P.sb(es, "peTv", [128, 32], BF16)]
            cb = P.sb(es, "cb", [128, 2])
            gT = [P.sb(es, "gT%d" % h, [128, 512], BF16) for h in range(2)]
            Omat = P.sb(es, "Omat", [128, 4, 128], BF16)
            posi = P.sb(es, "cposi", [128, 512], I32)
            Ct, St = P.sb(es, "cCt", [128, 512]), P.sb(es, "cSt", [128, 512])
            rtmp = (P.sb(es, "c_pf", [128, 512]), P.sb(es, "c_r", [128, 512]), P.sb(es, "c_fr", [128, 512]),
                    P.sb(es, "c_ti", [128, 512], I32), P.sb(es, "c_tf", [128, 512]))
            raw, t1, t2 = P.sb(es, "craw", [128, 512], BF16), P.sb(es, "ct1", [128, 512]), P.sb(es, "ct2", [128, 512])
            S.dma('sp', raws[0][:], kcT_s, w=['cr0'])
            S.dma('sp', raws[1][:], vcT_s, w=['cr1'])
            for kv, nm in enumerate(['w_cmp_k1', 'w_cmp_v1']):
                src = self.ins[nm].rearrange("(i d) c -> d i c", d=64)
                for h in range(2):
                    S.dma('pool', w1[kv][64 * h:64 * h + 64, :, :], src, w=[('w1', kv, h)])
            for h in range(2):
                S.op('dve', lambda e: e.memset(w2k[h][:], 0.0), w=[('w2k', h)])
                S.dma('pool', w2k[h][:, 64 * h:64 * h + 64], self.ins['w_cmp_k2'], w=[('w2k', h)])
                S.op('dve', lambda e: e.memset(gT[h][:], 0.0), w=[('gT', h)])
            S.dma('pool', w2v[:], self.ins['w_cmp_v2'], w=['w2v'])
            S.dma('pool', peT[0][:], self.ins['cmp_pos_kT'], w=['peT0'])
            S.dma('pool', peT[1][:], self.ins['cmp_pos_vT'], w=['peT1'])
            S.dma('pool', Omat[:], self.ins['omat'], w=['Omat'])
            S.dma('sp', posi[:], posc[0:1, :].to_broadcast([128, 512]), w=['cpi'])
            self.rope_tables(S, 'pool', posi[:], (Ct[:], St[:]), 15.5, 'c', tuple(t[:] for t in rtmp))
            for kv in range(2):
                for i in range(32):
                    S.op('pe', lambda e: e.matmul(ps[7][:, 0:1], lhsT=w1[kv][0:64, i, :], rhs=peT[kv][0:64, i:i + 1], start=(i == 0), stop=(i == 31)),
                         r=[('w1', kv, 0), 'peT%d' % kv], w=[('ps', 7)])
                S.op('act', lambda e: e.copy(out=cb[:, kv:kv + 1], in_=ps[7][:, 0:1]), r=[('ps', 7)], w=[('cb', kv)])
                for h in range(2):
                    pt = ps[h]
                    for i in range(32):
                        S.op('pe', lambda e: e.matmul(pt[:, 0:511], lhsT=w1[kv][64 * h:64 * h + 64, i, :],
                                                      rhs=raws[kv][64 * h:64 * h + 64, i:i + 16 * 510 + 1:16], start=(i == 0), stop=(i == 31)),
                             r=[('w1', kv, h), 'cr%d' % kv], w=[('ps', h)])
                    S.op('act', lambda e: e.activation(out=gT[h][:, 0:511], in_=pt[:, 0:511], func=AF.Gelu_apprx_tanh, bias=cb[:, kv:kv + 1]),
                         r=[('ps', h), ('cb', kv)], w=[('gT', h)])
                if kv == 0:
                    for h in range(2):
                        S.op('pe', lambda e: e.matmul(ps[2][:, :], lhsT=w2k[h][:], rhs=gT[h][:], start=(h == 0), stop=(h == 1)),
                             r=[('gT', h), ('w2k', h)], w=[('ps', 2)])
                    self.rope_apply(S, ps[2][:, :], ('ps', 2), kcT[:], 'kcT', Ct[:], St[:], 'cC', 'cS', raw[:], t1[:], t2[:], ps[3][:, :], 0)
                else:
                    for a_ in range(4):
                        for h in range(2):
                            S.op('pe', lambda e: e.matmul(ps[2][:, (a_ * 2 + h) * 64:(a_ * 2 + h + 1) * 64], lhsT=gT[h][:, a_ * 128:(a_ + 1) * 128], rhs=w2v[:],
                                                          start=True, stop=True), r=[('gT', h), 'w2v'], w=[('ps', 2)])
                    for a_ in range(4):
                        S.op('dve', lambda e: e.tensor_scalar(out=Vc1O[:, a_, :, 0:64], in0=ps[2][:, a_ * 128:(a_ + 1) * 128].rearrange("p (h d) -> p h d", h=2),
                                                              scalar1=c_nvalid[:, a_:a_ + 1], scalar2=None, op0=ALU.mult),
                             r=[('ps', 2), 'c_nvalid'], w=[('Vc1O', a_)])
                        for h in range(2):
                            S.op('dve', lambda e: e.tensor_copy(out=Vc1O[:, a_, h, 64:65], in_=c_nvalid[:, a_:a_ + 1]), r=['c_nvalid'], w=[('Vc1O', a_)])
                            S.op('dve', lambda e: e.tensor_scalar(out=Vc1O[:, a_, h, 65:193], in0=Omat[:, a_, :], scalar1=c_nvalid[:, a_:a_ + 1], scalar2=None, op0=ALU.mult),
                                 r=['Omat', 'c_nvalid'], w=[('Vc1O', a_)])
            S.barrier()

    def phase3(self, S, ps, L):
        P = self
        uT_s, ysT_s = L['uT_s'], L['ysT_s']
        c_ident = self.c_ident
        nst = self.nst
        with ExitStack() as es:
            lam = P.sb(es, "lam", [128, 32, 3])
            sbD = P.sb(es, "sbD", [32, 32])
            iota1 = P.sb(es, "iota1", [128, 512])
            iotar = P.sb(es, "iotar", [128, 512])
            wglu = P.sb(es, "wglu", [128, 8, 1024], BF16)
            S.dma('sp', lam[:], self.ins['s5_lam'], w=['lam'])
            S.dma('sp', sbD[:], self.ins['s5_d'], w=['sbD'])
            S.dma('sp', iota1[:], self.ins['iota1'], w=['iota1'])
            S.dma('sp', iotar[:], self.ins['iotar'], w=['iotar'])
            S.dma('pool', wglu[:], self.ins['w_glu'].rearrange("(c p) n -> p c n", p=128), w=['wglu'])
            v = {}
            for nm in ['dt', 'mag', 'thc', 'sn', 'cs', 'ar', 'ai', 'zr', 'den', 'fr', 'fi', 'ta', 'tb', 'tfr', 'ttf', 'lrdt', 'm512']:
                v[nm] = P.sb(es, "v_" + nm, [128, 32])
            v_ti = P.sb(es, "v_ti", [128, 32], I32)
            lhsB = P.sb(es, "lhsB", [32, 32, 2, 128], BF16)
            bdC = P.sb(es, "bdC", [128, 32, 2, 32], BF16)
            dd = P.sb(es, "Ddiag", [32, 32, 32], BF16)
            esp = ExitStack()
            sbB = P.sb(esp, "sbB", [128, 32, 2, 16])
            sbC = P.sb(esp, "sbC", [128, 32, 2, 16])
            bd = P.sb(esp, "bdB", [128, 32, 2, 32])
            bt1 = P.sb(esp, "bt1", [128, 32, 16])
            bt2 = P.sb(esp, "bt2", [128, 32, 16])
            S.dma('sp', sbB[:], self.ins['s5_b'], w=['sbB'])
            S.dma('sp', sbC[:], self.ins['s5_c'], w=['sbC'])
            lr, li, ldt = lam[:, :, 0], lam[:, :, 1], lam[:, :, 2]
            TT = lambda o, a_, b_, op, r, w: S.op('dve', lambda e: e.tensor_tensor(out=o, in0=a_, in1=b_, op=op), r=r, w=w)
            TS = lambda o, a_, s1, op, r, w: S.op('dve', lambda e: e.tensor_scalar(out=o, in0=a_, scalar1=s1, scalar2=None, op0=op), r=r, w=w)
            S.op('act', lambda e: e.activation(out=v['dt'][:], in_=ldt, func=AF.Exp), r=['lam'], w=['v_dt'])
            TT(v['ta'][:], lr, v['dt'][:], ALU.mult, ['lam', 'v_dt'], ['v_ta'])
            S.op('act', lambda e: e.activation(out=v['mag'][:], in_=v['ta'][:], func=AF.Exp), r=['v_ta'], w=['v_mag'])
            S.op('act', lambda e: e.activation(out=v['m512'][:], in_=v['ta'][:], func=AF.Exp, scale=512.0), r=['v_ta'], w=['v_m512'])
            S.op('dve', lambda e: e.tensor_copy(out=v['lrdt'][:], in_=v['ta'][:]), r=['v_ta'], w=['v_lrdt'])
            TT(v['thc'][:], li, v['dt'][:], ALU.mult, ['lam', 'v_dt'], ['v_thc'])
            TS(v['thc'][:], v['thc'][:], 1.0 / TWO_PI, ALU.mult, ['v_thc'], ['v_thc'])
            self.range_reduce(S, 'dve', v['tfr'][:], v['thc'][:], v_ti[:], v['ttf'][:], 'v_thc', ['v_ti', 'v_ttf', 'v_tfr'])
            S.op('act', lambda e: e.activation(out=v['sn'][:], in_=v['tfr'][:], func=AF.Sin, scale=TWO_PI), r=['v_tfr'], w=['v_sn'])
            TS(v['tb'][:], v['thc'][:], 0.25, ALU.add, ['v_thc'], ['v_tb'])
            self.range_reduce(S, 'dve', v['tfr'][:], v['tb'][:], v_ti[:], v['ttf'][:], 'v_tb', ['v_ti', 'v_ttf', 'v_tfr'])
            S.op('act', lambda e: e.activation(out=v['cs'][:], in_=v['tfr'][:], func=AF.Sin, scale=TWO_PI), r=['v_tfr'], w=['v_cs'])
            TT(v['ar'][:], v['mag'][:], v['cs'][:], ALU.mult, ['v_mag', 'v_cs'], ['v_ar'])
            TT(v['ai'][:], v['mag'][:], v['sn'][:], ALU.mult, ['v_mag', 'v_sn'], ['v_ai'])
            TS(v['zr'][:], v['ar'][:], -1.0, ALU.add, ['v_ar'], ['v_zr'])
            TT(v['den'][:], lr, lr, ALU.mult, ['lam'], ['v_den'])
            TT(v['ta'][:], li, li, ALU.mult, ['lam'], ['v_ta'])
            TT(v['den'][:], v['den'][:], v['ta'][:], ALU.add, ['v_den', 'v_ta'], ['v_den'])
            S.op('dve', lambda e: e.reciprocal(out=v['den'][:], in_=v['den'][:]), r=['v_den'], w=['v_den'])
            TT(v['ta'][:], v['zr'][:], lr, ALU.mult, ['v_zr', 'lam'], ['v_ta'])
            TT(v['tb'][:], v['ai'][:], li, ALU.mult, ['v_ai', 'lam'], ['v_tb'])
            TT(v['fr'][:], v['ta'][:], v['tb'][:], ALU.add, ['v_ta', 'v_tb'], ['v_fr'])
            TT(v['fr'][:], v['fr'][:], v['den'][:], ALU.mult, ['v_fr', 'v_den'], ['v_fr'])
            TT(v['ta'][:], v['ai'][:], lr, ALU.mult, ['v_ai', 'lam'], ['v_ta'])
            TT(v['tb'][:], v['zr'][:], li, ALU.mult, ['v_zr', 'lam'], ['v_tb'])
            TT(v['fi'][:], v['ta'][:], v['tb'][:], ALU.subtract, ['v_ta', 'v_tb'], ['v_fi'])
            TT(v['fi'][:], v['fi'][:], v['den'][:], ALU.mult, ['v_fi', 'v_den'], ['v_fi'])
            S.op('pool', lambda e: e.memset(bd[:], 0.0), w=['bd'])
            frb = v['fr'][:].unsqueeze(2).to_broadcast([128, 32, 16])
            fib = v['fi'][:].unsqueeze(2).to_broadcast([128, 32, 16])
            br_, bi_ = sbB[:, :, 0, :], sbB[:, :, 1, :]
            TT(bt1[:], br_, frb, ALU.mult, ['sbB', 'v_fr'], ['bt1'])
            TT(bt2[:], bi_, fib, ALU.mult, ['sbB', 'v_fi'], ['bt2'])
            for gl in range(2):
                sl = slice(64 * gl, 64 * gl + 64)
                TT(bd[sl, :, 0, 16 * gl:16 * gl + 16], bt1[sl], bt2[sl], ALU.subtract, ['bt1', 'bt2', 'bd'], ['bd'])
            TT(bt1[:], bi_, frb, ALU.mult, ['sbB', 'v_fr', 'bd'], ['bt1'])
            TT(bt2[:], br_, fib, ALU.mult, ['sbB', 'v_fi', 'bd'], ['bt2'])
            for gl in range(2):
                sl = slice(64 * gl, 64 * gl + 64)
                TT(bd[sl, :, 1, 16 * gl:16 * gl + 16], bt1[sl], bt2[sl], ALU.add, ['bt1', 'bt2', 'bd'], ['bd'])
            for st in range(nst):
                for ri in range(2):
                    S.op('pe', lambda e: e.transpose(out=ps[0][0:32, ri * 128:(ri + 1) * 128], in_=bd[:, st, ri, :], identity=c_ident[:]), r=['bd', 'c_ident'], w=[('ps', 0)])
                S.op('act', lambda e: e.copy(out=lhsB[:, st, :, :], in_=ps[0][0:32, 0:256].rearrange("p (a b) -> p a b", b=128)), r=[('ps', 0)], w=['lhsB'])
            S.op('pool', lambda e: e.memset(bdC[:], 0.0), w=['bdC'])
            for gl in range(2):
                sl = slice(64 * gl, 64 * gl + 64)
                S.op('dve', lambda e: e.tensor_copy(out=bdC[sl, :, 0, 16 * gl:16 * gl + 16], in_=sbC[sl, :, 0, :]), r=['sbC', 'bdC'], w=['bdC'])
                TS(bdC[sl, :, 1, 16 * gl:16 * gl + 16], sbC[sl, :, 1, :], -1.0, ALU.mult, ['sbC', 'bdC'], ['bdC'])
            TT(dd[:], c_ident[0:32, 0:32].unsqueeze(1).to_broadcast([32, 32, 32]), sbD[:].unsqueeze(2).to_broadcast([32, 32, 32]), ALU.mult, ['c_ident', 'sbD'], ['dd'])

            S.barrier()
            esp.close()
            ub = [P.sb(es, "ub%d" % i, [32, W], BF16) for i in range(2)]
            Ct = [P.sb(es, "sCt%d" % i, [128, 512]) for i in range(2)]
            St = [P.sb(es, "sSt%d" % i, [128, 512]) for i in range(2)]
            magb = [P.sb(es, "magb%d" % i, [128, 512]) for i in range(2)]
            rtmp = (P.sb(es, "s_r", [128, 512]), P.sb(es, "s_fr", [128, 512]), None, P.sb(es, "s_tf", [128, 512]))
            Gt = P.sb(es, "s_G", [128, 512])
            rtmp2 = [P.sb(es, "s_fr%d" % i, [128, 512]) for i in range(3)]
            DA = [P.sb(es, "DA%d" % i, [128, 1024]) for i in range(2)]
            DB = [P.sb(es, "DB%d" % i, [128, 1024]) for i in range(2)]
            acol = [P.sb(es, "acol%d" % i, [128, 4]) for i in range(2)]
            junk = P.sb(es, "junk", [128, 1024], BF16)
            Ecol = P.sb(es, "Ecol", [128, 12, 2])
            Sst = [P.sb(es, "Sst%d" % i, [128, 2]) for i in range(2)]
            tst = P.sb(es, "tst", [128, 2])
            m_ = [[P.sb(es, "m%d_%d" % (k_, i), [128, 512]) for i in range(4)] for k_ in range(2)]
            mo = [P.sb(es, "mo%d" % i, [128, 512]) for i in range(4)]
            zre = [P.sb(es, "zre%d" % i, [128, 512]) for i in range(2)]
            zim = [P.sb(es, "zim%d" % i, [128, 512]) for i in range(2)]
            sre, sim_ = P.sb(es, "sre", [128, 512], BF16), P.sb(es, "sim", [128, 512], BF16)
            S0 = P.sb(es, "S0", [128, 2])
            tcol = P.sb(es, "tcol", [128, 2])
            nident = P.sb(es, "nident", [128, 128])
            S.op('dve', lambda e: e.tensor_scalar(out=nident[:], in0=c_ident[:], scalar1=-1.0, scalar2=None, op0=ALU.mult), r=['c_ident'], w=['nident'])
            yT_all = P.sb(es, "yT_all", [128, 8, NOWN], BF16)
            if nst < 32:
                S.op('pool', lambda e: e.memset(yT_all[:], 0.0), w=['yT_all'])
            def prepA(st):
                S.dma('sp', ub[st % 2][:], uT_s[st * 32:(st + 1) * 32, :], w=[('ub', st % 2)])
                C, Sn, mb = Ct[st % 2], St[st % 2], magb[st % 2]
                tp = 's5t%d' % (st % 2)
                da, db = DA[st % 2], DB[st % 2]
                r_, fr_, tf_ = rtmp[0][:], rtmp[1][:], rtmp[3][:]
                E_ = 'dve'
                S.op(E_, lambda e: e.tensor_scalar(out=r_, in0=iota1[:], scalar1=v['thc'][:, st:st + 1], scalar2=None, op0=ALU.mult), r=['iota1', 'v_thc'], w=['s_r'])
                self.range_reduce(S, E_, fr_, r_, None, tf_, 's_r', ['s_ti', 's_tf', 's_fr'])
                S.op('act', lambda e: e.activation(out=Sn[:], in_=fr_, func=AF.Sin, scale=TWO_PI), r=['s_fr'], w=[tp + 'S'])
                S.op(E_, lambda e: e.tensor_scalar(out=r_, in0=r_, scalar1=0.25, scalar2=None, op0=ALU.add), r=['s_r'], w=['s_r'])
                self.range_reduce(S, E_, rtmp2[0][:], r_, None, tf_, 's_r', ['s_ti', 's_tf', 's_fr2'])
                S.op('act', lambda e: e.activation(out=C[:], in_=rtmp2[0][:], func=AF.Sin, scale=TWO_PI), r=['s_fr2'], w=[tp + 'C'])
                S.op(E_, lambda e: e.tensor_scalar(out=r_, in0=iotar[:], scalar1=v['thc'][:, st:st + 1], scalar2=None, op0=ALU.mult), r=['iotar', 'v_thc'], w=['s_r'])
                self.range_reduce(S, E_, rtmp2[1][:], r_, None, tf_, 's_r', ['s_ti', 's_tf', 's_fr3'])
                S.op('act', lambda e: e.activation(out=db[:, 0:512], in_=rtmp2[1][:], func=AF.Sin, scale=TWO_PI), r=['s_fr3'], w=[tp + 'DB'])
                S.op(E_, lambda e: e.tensor_scalar(out=r_, in0=r_, scalar1=0.25, scalar2=None, op0=ALU.add), r=['s_r'], w=['s_r'])
                self.range_reduce(S, E_, rtmp2[2][:], r_, None, tf_, 's_r', ['s_ti', 's_tf', 's_fr4'])
                S.op('act', lambda e: e.activation(out=da[:, 0:512], in_=rtmp2[2][:], func=AF.Sin, scale=TWO_PI), r=['s_fr4'], w=[tp + 'DA'])
                S.op('act', lambda e: e.activation(out=Gt[:], in_=iotar[:], func=AF.Exp, scale=v['lrdt'][:, st:st + 1]), r=['iotar', 'v_lrdt'], w=['s_G'])

            def prepB(st):
                C, Sn, mb = Ct[st % 2], St[st % 2], magb[st % 2]
                tp = 's5t%d' % (st % 2)
                da, db, ac = DA[st % 2], DB[st % 2], acol[st % 2]
                E_ = 'dve'
                S.op(E_, lambda e: e.tensor_copy(out=mb[:], in_=v['mag'][:, st:st + 1].to_broadcast([128, 512])), r=['v_mag'], w=[tp + 'M'])
                S.op(E_, lambda e: e.tensor_tensor(out=da[:, 0:512], in0=da[:, 0:512], in1=Gt[:], op=ALU.mult), r=[tp + 'DA', 's_G'], w=[tp + 'DA'])
                S.op(E_, lambda e: e.tensor_tensor(out=db[:, 0:512], in0=db[:, 0:512], in1=Gt[:], op=ALU.mult), r=[tp + 'DB', 's_G'], w=[tp + 'DB'])
                S.op(E_, lambda e: e.tensor_copy(out=db[:, 512:1024], in_=da[:, 0:512]), r=[tp + 'DA', tp + 'DB'], w=[tp + 'DB'])
                S.op(E_, lambda e: e.tensor_scalar(out=da[:, 512:1024], in0=db[:, 0:512], scalar1=-1.0, scalar2=None, op0=ALU.mult), r=[tp + 'DB', tp + 'DA'], w=[tp + 'DA'])
                S.op(E_, lambda e: e.tensor_tensor(out=ac[:, 0:1], in0=C[:, 511:512], in1=v['m512'][:, st:st + 1], op=ALU.mult), r=[tp + 'C', 'v_m512'], w=[tp + 'A'])
                S.op(E_, lambda e: e.tensor_tensor(out=ac[:, 1:2], in0=Sn[:, 511:512], in1=v['m512'][:, st:st + 1], op=ALU.mult), r=[tp + 'S', 'v_m512'], w=[tp + 'A'])
                S.op(E_, lambda e: e.tensor_scalar(out=ac[:, 2:3], in0=ac[:, 1:2], scalar1=-1.0, scalar2=None, op0=ALU.mult), r=[tp + 'A'], w=[tp + 'A'])

            prepA(0)
            prepB(0)
            for st in range(nst):
                u_ = ub[st % 2]
                utag = ('ub', st % 2)
                C, Sn, mb = Ct[st % 2], St[st % 2], magb[st % 2]
                tp = 's5t%d' % (st % 2)
                if st + 1 < nst:
                    prepA(st + 1)
                def front(g):
                    usl = u_[:, g * 512:(g + 1) * 512]
                    mm = m_[g % 2]
                    mt = lambda i: ('m', g % 2, i)
                    wb = 2 + 2 * (g % 2)
                    S.op('pe', lambda e: e.matmul(ps[0][:, :], lhsT=lhsB[:, st, 0, :], rhs=usl, start=True, stop=True), r=['lhsB', utag], w=[('ps', 0)])
                    S.op('pe', lambda e: e.matmul(ps[1][:, :], lhsT=lhsB[:, st, 1, :], rhs=usl, start=True, stop=True), r=['lhsB', utag], w=[('ps', 1)])
                    TT(mm[0][:], ps[0][:, :], C[:], ALU.mult, [('ps', 0), tp + 'C'], [mt(0)])
                    TT(mm[1][:], ps[1][:, :], Sn[:], ALU.mult, [('ps', 1), tp + 'S'], [mt(1)])
                    TT(mm[2][:], ps[1][:, :], C[:], ALU.mult, [('ps', 1), tp + 'C'], [mt(2)])
                    TT(mm[3][:], ps[0][:, :], Sn[:], ALU.mult, [('ps', 0), tp + 'S'], [mt(3)])
                    S.op('pe', lambda e: e.matmul(ps[wb][:, :], lhsT=c_ident[:], rhs=mm[0][:], start=True, stop=False), r=['c_ident', mt(0)], w=[('ps', wb)])
                    S.op('pe', lambda e: e.matmul(ps[wb][:, :], lhsT=c_ident[:], rhs=mm[1][:], start=False, stop=True), r=['c_ident', mt(1)], w=[('ps', wb)])
                    S.op('pe', lambda e: e.matmul(ps[wb + 1][:, :], lhsT=c_ident[:], rhs=mm[2][:], start=True, stop=False), r=['c_ident', mt(2)], w=[('ps', wb + 1)])
                    S.op('pe', lambda e: e.matmul(ps[wb + 1][:, :], lhsT=nident[:], rhs=mm[3][:], start=False, stop=True), r=['nident', mt(3)], w=[('ps', wb + 1)])

                def back(g):
                    own = g >= OWN_G0
                    usl = u_[:, g * 512:(g + 1) * 512]
                    wb = 2 + 2 * (g % 2)
                    zr_, zi_ = zre[g % 2], zim[g % 2]
                    S.op('dve', lambda e: e.tensor_tensor_scan(out=zr_[:], data0=mb[:], data1=ps[wb][:, :], initial=S0[:, 0:1], op0=ALU.mult, op1=ALU.add),
                         r=[tp + 'M', ('ps', wb), 'S0'], w=[('zre', g % 2)])
                    S.op('dve', lambda e: e.tensor_tensor_scan(out=zi_[:], data0=mb[:], data1=ps[wb + 1][:, :], initial=S0[:, 1:2], op0=ALU.mult, op1=ALU.add),
                         r=[tp + 'M', ('ps', wb + 1), 'S0'], w=[('zim', g % 2)])
                    c5, s5 = C[:, 511:512], Sn[:, 511:512]
                    TT(tcol[:, 0:1], zi_[:, 511:512], s5, ALU.mult, [('zim', g % 2), tp + 'S'], ['tcol'])
                    TT(tcol[:, 1:2], zi_[:, 511:512], c5, ALU.mult, [('zim', g % 2), tp + 'C'], ['tcol'])
                    S.op('dve', lambda e: e.scalar_tensor_tensor(out=S0[:, 0:1], in0=zr_[:, 511:512], scalar=c5, in1=tcol[:, 0:1], op0=ALU.mult, op1=ALU.subtract),
                         r=[('zre', g % 2), tp + 'C', 'tcol', 'S0'], w=['S0'])
                    S.op('dve', lambda e: e.scalar_tensor_tensor(out=S0[:, 1:2], in0=zr_[:, 511:512], scalar=s5, in1=tcol[:, 1:2], op0=ALU.mult, op1=ALU.add),
                         r=[('zre', g % 2), tp + 'S', 'tcol', 'S0'], w=['S0'])
                    if own:
                        TT(mo[0][:], zr_[:], C[:], ALU.mult, [('zre', g % 2), tp + 'C', 'mo0'], ['mo0'])
                        TT(mo[1][:], zi_[:], Sn[:], ALU.mult, [('zim', g % 2), tp + 'S', 'mo1'], ['mo1'])
                        TT(mo[2][:], zr_[:], Sn[:], ALU.mult, [('zre', g % 2), tp + 'S', 'mo2'], ['mo2'])
                        TT(mo[3][:], zi_[:], C[:], ALU.mult, [('zim', g % 2), tp + 'C', 'mo3'], ['mo3'])
                        S.op('pool', lambda e: e.tensor_tensor(out=sre[:], in0=mo[0][:], in1=mo[1][:], op=ALU.subtract), r=['mo0', 'mo1'], w=['sre'])
                        S.op('pool', lambda e: e.tensor_tensor(out=sim_[:], in0=mo[2][:], in1=mo[3][:], op=ALU.add), r=['mo2', 'mo3'], w=['sim'])
                        S.op('pe', lambda e: e.matmul(ps[6][0:32, :], lhsT=bdC[:, st, 0, :], rhs=sre[:], start=True, stop=False), r=['bdC', 'sre'], w=[('ps', 6)])
                        S.op('pe', lambda e: e.matmul(ps[6][0:32, :], lhsT=bdC[:, st, 1, :], rhs=sim_[:], start=False, stop=False), r=['bdC', 'sim'], w=[('ps', 6)])
                        S.op('pe', lambda e: e.matmul(ps[6][0:32, :], lhsT=dd[:, st, :], rhs=usl, start=False, stop=True), r=['dd', utag], w=[('ps', 6)])
                        go = g - OWN_G0
                        po = (st % 4) * 32
                        S.op('act', lambda e: e.activation(out=yT_all[po:po + 32, st // 4, go * 512:(go + 1) * 512], in_=ps[6][0:32, :], func=AF.Gelu_apprx_tanh),
                             r=[('ps', 6)], w=['yT_all'])

                da, db, ac = DA[st % 2], DB[st % 2], acol[st % 2]
                S.op('dve', lambda e: e.memset(Sst[0][:], 0.0), r=[('Sst', 0)], w=[('Sst', 0)])
                for g in range(OWN_G0):
                    usl = u_[:, g * 512:(g + 1) * 512]
                    xb = (g % 2) * 2
                    S.op('pe', lambda e: e.matmul(ps[xb][:, :], lhsT=lhsB[:, st, 0, :], rhs=usl, start=True, stop=True), r=['lhsB', utag], w=[('ps', xb)])
                    S.op('pe', lambda e: e.matmul(ps[xb + 1][:, :], lhsT=lhsB[:, st, 1, :], rhs=usl, start=True, stop=True), r=['lhsB', utag], w=[('ps', xb + 1)])
                    xcat = self.psbig[:, xb * 512:(xb + 2) * 512]
                    S.op('dve', lambda e: e.scalar_tensor_tensor(out=junk[:], in0=xcat, scalar=1.0, in1=da[:], op0=ALU.mult, op1=ALU.mult, accum_out=Ecol[:, g, 0:1]),
                         r=[('ps', xb), ('ps', xb + 1), tp + 'DA', 'junk'], w=['junk', ('E', g)])
                    S.op('dve', lambda e: e.scalar_tensor_tensor(out=junk[:], in0=xcat, scalar=1.0, in1=db[:], op0=ALU.mult, op1=ALU.mult, accum_out=Ecol[:, g, 1:2]),
                         r=[('ps', xb), ('ps', xb + 1), tp + 'DB', 'junk'], w=['junk', ('E', g)])
                    cur, nxt = Sst[g % 2], Sst[(g + 1) % 2]
                    ct, nt = ('Sst', g % 2), ('Sst', (g + 1) % 2)
                    S.op('act', lambda e: e.activation(out=tst[:, 0:1], in_=cur[:, 0:1], func=AF.Identity, scale=ac[:, 0:1], bias=Ecol[:, g, 0:1]), r=[ct, tp + 'A', ('E', g)], w=['tst'])
                    S.op('act', lambda e: e.activation(out=nxt[:, 0:1], in_=cur[:, 1:2], func=AF.Identity, scale=ac[:, 2:3], bias=tst[:, 0:1]), r=[ct, tp + 'A', 'tst'], w=[nt])
                    S.op('act', lambda e: e.activation(out=tst[:, 1:2], in_=cur[:, 1:2], func=AF.Identity, scale=ac[:, 0:1], bias=Ecol[:, g, 1:2]), r=[ct, tp + 'A', ('E', g)], w=['tst'])
                    S.op('act', lambda e: e.activation(out=nxt[:, 1:2], in_=cur[:, 0:1], func=AF.Identity, scale=ac[:, 1:2], bias=tst[:, 1:2]), r=[ct, tp + 'A', 'tst'], w=[nt])
                S.op('act', lambda e: e.copy(out=S0[:], in_=Sst[OWN_G0 % 2][:]), r=[('Sst', OWN_G0 % 2), 'S0'], w=['S0'])
                if st + 1 < nst:
                    prepB(st + 1)
                front(OWN_G0)
                for g in range(OWN_G0, NG):
                    if g + 1 < NG:
                        front(g + 1)
                    back(g)
            if 'yT_all' in self.dbg:
                S.dma('sp', L['d_yT'], yT_all[:], r=['yT_all'], w=['d_yT'])
            sg = [P.sb(es, "sg%d" % i, [128, 512]) for i in range(2)]
            yo = [P.sb(es, "yo%d" % i, [128, 512], BF16) for i in range(2)]
            n = 0
            for cc in range(8):
                for tg in range(4):
                    b = 4 + n % 2
                    for kc in range(8):
                        S.op('pe', lambda e: e.matmul(ps[b][:, :], lhsT=wglu[:, kc, cc * 128:(cc + 1) * 128], rhs=yT_all[:, kc, tg * 512:(tg + 1) * 512],
                                                      start=(kc == 0), stop=(kc == 7)), r=['wglu', 'yT_all'], w=[('ps', b)])
                    S.op('act', lambda e: e.activation(out=sg[n % 2][:], in_=ps[b][:, :], func=AF.Sigmoid), r=[('ps', b)], w=[('sg', n % 2)])
                    TT(yo[n % 2][:], yT_all[:, cc, tg * 512:(tg + 1) * 512], sg[n % 2][:], ALU.mult, ['yT_all', ('sg', n % 2)], [('yo', n % 2)])
                    S.dma('sp', ysT_s[cc * 128:(cc + 1) * 128, tg * 512:(tg + 1) * 512], yo[n % 2][:], r=[('yo', n % 2)], w=[('ysT_s', cc, tg)])
                    n += 1
            S.barrier()

    def phase4(self, S, ps, L):
        P = self
        ksT, kwT, qT, Vs, Vw, gates, kcT, Vc1O = (L[k] for k in ['ksT', 'kwT', 'qT', 'Vs', 'Vw', 'gates', 'kcT', 'Vc1O'])
        ynT_s = L['ynT_s']
        c_identb, c_ident, c_pad = self.c_identb, self.c_ident, self.c_pad
        nq = self.nq
        with ExitStack() as es:
            ksTm = P.sb(es, "ksTm", [128, 2, W], BF16)
            kwTm = P.sb(es, "kwTm", [128, 2, NOWN + 512], BF16)
            kcTm = P.sb(es, "kcTm", [128, 2, 512], BF16)
            for src_, dst_, nm_ in ((ksT, ksTm, 'ksT'), (kwT, kwTm, 'kwT'), (kcT, kcTm, 'kcT')):
                S.op('pool', lambda e: e.memset(dst_[:], 0.0), w=[nm_ + 'm'])
                for hp in range(2):
                    S.op('dve', lambda e: e.tensor_copy(out=dst_[64 * hp:64 * hp + 64, hp, :], in_=src_[64 * hp:64 * hp + 64, :]), r=[nm_ + 'm', nm_], w=[nm_ + 'm'])
            ksT, kwT, kcT = ksTm, kwTm, kcTm
            causalT = P.sb(es, "causalT", [128, 128], BF16)
            antiT = P.sb(es, "antiT", [128, 128], BF16)
            cmask = P.sb(es, "cmask", [128, 16, 2, 128], BF16)
            sidx = P.sb(es, "sidx", [128, 128])
            qhalf = P.sb(es, "qhalf", [128, 1])
            real = P.sb(es, "real", [128, 128])
            first = P.sb(es, "first", [128, 128])
            rm2 = P.sb(es, "rm2", [128, 128])
            S.dma('pool', causalT[:], self.ins['causalT'], w=['causalT'])
            S.dma('pool', antiT[:], self.ins['antiT'], w=['antiT'])
            S.dma('pool', cmask[:], self.ins['cmask'], w=['cmask'])
            S.dma('sp', sidx[:], self.ins['sidx'], w=['sidx'])
            S.dma('sp', qhalf[:], self.ins['qhalf'], w=['qhalf'])
            S.op('dve', lambda e: e.tensor_scalar(out=first[:], in0=sidx[:], scalar1=c_pad[:, 0:1], scalar2=None, op0=ALU.subtract), r=['sidx', 'c_pad'], w=['first'])
            S.op('dve', lambda e: e.tensor_scalar(out=real[:], in0=first[:], scalar1=0.0, scalar2=None, op0=ALU.is_ge), r=['first'], w=['real'])
            S.op('dve', lambda e: e.tensor_scalar(out=first[:], in0=first[:], scalar1=0.0, scalar2=None, op0=ALU.is_equal), r=['first'], w=['first'])
            S.op('dve', lambda e: e.tensor_scalar(out=rm2[:], in0=real[:], scalar1=-2.0, scalar2=None, op0=ALU.add), r=['real'], w=['rm2'])
            et = [P.sb(es, "e%d" % i, [128, 512], BF16) for i in range(4)]
            mk = [P.sb(es, "mk%d" % i, [128, 128], BF16) for i in range(3)]
            o_all = P.sb(es, "o_all", [128, 8, 2, 64])
            den8 = P.sb(es, "den8", [128, 8])
            rg8 = P.sb(es, "rg8", [128, 8])
            imp = P.sb(es, "imp", [128, 128])
            A_ = P.sb(es, "A_", [128, 128])
            vld = P.sb(es, "vld", [128, 128])
            fo = P.sb(es, "fo", [128, 128])
            score = P.sb(es, "score", [128, 128])
            swk = P.sb(es, "swk", [128, 128])
            top = P.sb(es, "top", [128, 16])
            selb = P.sb(es, "selb", [128, 128], BF16)
            maskq = P.sb(es, "maskq", [128, W], BF16)
            yst = [P.sb(es, "yst%d" % i, [128, 8, 128], BF16) for i in range(2)]
            cnt = {'u': 0, 'm': 0}

            pend = []
            LA = 3
            cnt['i'] = 0

            def flush(n=0):
                while len(pend) > n:
                    pend.pop(0)()

            def unit(k, qi, keys, ktag, Vrhs, vtag, ncols, mask, mtag, accbank, first_, last_):
                for hf in range(2):
                    i = cnt['i']
                    cnt['i'] += 1
                    sb_ = i % 4
                    pt = ps[sb_]
                    e_ = et[i % 4]
                    etag = ('e', i % 4)
                    S.op('pe', lambda e: e.matmul(pt[:, :], lhsT=keys, rhs=qT[:, qi, 4 * hf:4 * hf + 4, :],
                                                  start=True, stop=True), r=[ktag, 'qT'], w=[('ps', sb_)])
                    S.op('act', lambda e: e.activation(out=e_[:], in_=pt[:, :], func=AF.Exp, scale=0.125), r=[('ps', sb_)], w=[etag])
                    if mask is not None:
                        S.op('dve', lambda e: e.tensor_tensor(out=e_[:].rearrange("p (h q) -> p h q", h=4), in0=e_[:].rearrange("p (h q) -> p h q", h=4),
                                                              in1=mask.unsqueeze(1).to_broadcast([128, 4, 128]), op=ALU.mult), r=[etag, mtag], w=[etag])

                    def B(hf=hf, e_=e_, etag=etag, Vrhs=Vrhs, vtag=vtag, ncols=ncols, accbank=accbank, first_=first_, last_=last_):
                        if accbank is None:
                            S.op('pe', lambda e: e.matmul(ps[4 + hf][0:65, :], lhsT=Vrhs, rhs=e_[:], start=first_, stop=last_), r=[etag, vtag], w=[('ps', 4 + hf)])
                            return
                        for hh in range(4):
                            j = 4 * hf + hh
                            bank, c0 = accbank(j)
                            S.op('pe', lambda e: e.matmul(ps[bank][:, c0:c0 + ncols], lhsT=e_[:, hh * 128:(hh + 1) * 128], rhs=Vrhs,
                                                          start=(first_ and c0 == 0), stop=last_, skip_group_check=True),
                                 r=[etag, vtag], w=[('ps', bank)])
                    pend.append(B)
                    flush(LA)

            aT = P.sb(es, "aT", [65, 1024])

            def finalize():
                flush()
                for hf in range(2):
                    S.op('act', lambda e: e.copy(out=aT[:, hf * 512:(hf + 1) * 512], in_=ps[4 + hf][0:65, :]), r=[('ps', 4 + hf)], w=[('aT', hf)])
                for hf in range(2):
                    for hh in range(4):
                        S.op('pe', lambda e: e.transpose(out=ps[6 + hf][:, hh * 65:(hh + 1) * 65], in_=aT[:, hf * 512 + hh * 128:hf * 512 + (hh + 1) * 128], identity=c_ident[0:65, 0:65]),
                             r=[('aT', hf), 'c_ident'], w=[('ps', 6 + hf)])

            def evac(k, qi, accbank, br, init):
                for j in range(8):
                    bank, c0 = accbank(j)
                    S.op('dve', lambda e: e.tensor_copy(out=den8[:, j:j + 1], in_=ps[bank][:, c0 + 64:c0 + 65]), r=[('ps', bank)], w=['den8'])
                S.op('dve', lambda e: e.tensor_scalar(out=den8[:], in0=den8[:], scalar1=1e-30, scalar2=None, op0=ALU.max), r=['den8'], w=['den8'])
                S.op('dve', lambda e: e.reciprocal(out=den8[:], in_=den8[:]), r=['den8'], w=['den8'])
                gsl = gates[:, qi, 24 * k + br:24 * k + br + 22:3]
                S.op('dve', lambda e: e.tensor_tensor(out=rg8[:], in0=den8[:], in1=gsl, op=ALU.mult), r=['den8', 'gates'], w=['rg8'])
                for j in range(8):
                    bank, c0 = accbank(j)
                    if init:
                        S.op('dve', lambda e: e.tensor_scalar(out=o_all[:, j, k, :], in0=ps[bank][:, c0:c0 + 64], scalar1=rg8[:, j:j + 1], scalar2=None, op0=ALU.mult),
                             r=[('ps', bank), 'rg8'], w=['o_all'])
                    else:
                        S.op('dve', lambda e: e.scalar_tensor_tensor(out=o_all[:, j, k, :], in0=ps[bank][:, c0:c0 + 64], scalar=rg8[:, j:j + 1], in1=o_all[:, j, k, :],
                                                                     op0=ALU.mult, op1=ALU.add), r=[('ps', bank), 'rg8', 'o_all'], w=['o_all'])

            for qi in range(nq):
                wt = 48 + qi
                for k in range(2):
                    accC = lambda j: (4 + j // 2, (j % 2) * 193)
                    for a_ in range(4):
                        m_ = cmask[:, qi, a_ - 2, :] if a_ >= 2 else None
                        unit(k, qi, kcT[:, k, a_ * 128:(a_ + 1) * 128], 'kcTm', Vc1O[:, a_, k, :], 'Vc1O', 193, m_, 'cmask', accC, a_ == 0, a_ == 3)
                    flush()
                    evac(k, qi, accC, 0, True)
                    for j in range(8):
                        bank, c0 = accC(j)
                        if j == 0:
                            S.op('dve', lambda e: e.tensor_scalar(out=imp[:], in0=ps[bank][:, c0 + 65:c0 + 193], scalar1=den8[:, j:j + 1], scalar2=None, op0=ALU.mult),
                                 r=[('ps', bank), 'den8'], w=['imp'])
                        else:
                            S.op('dve', lambda e: e.scalar_tensor_tensor(out=imp[:], in0=ps[bank][:, c0 + 65:c0 + 193], scalar=den8[:, j:j + 1], in1=imp[:],
                                                                         op0=ALU.mult, op1=ALU.add), r=[('ps', bank), 'den8', 'imp'], w=['imp'])
                    S.op('dve', lambda e: e.tensor_scalar(out=A_[:], in0=sidx[:], scalar1=float(-2 * wt), scalar2=qhalf[:, 0:1], op0=ALU.add, op1=ALU.subtract),
                         r=['sidx', 'qhalf'], w=['A_'])
                    S.op('dve', lambda e: e.tensor_scalar(out=vld[:], in0=A_[:], scalar1=0.0, scalar2=None, op0=ALU.is_le), r=['A_'], w=['vld'])
                    S.op('dve', lambda e: e.tensor_scalar(out=fo[:], in0=A_[:], scalar1=-1.0, scalar2=None, op0=ALU.is_ge), r=['A_'], w=['fo'])
                    S.op('dve', lambda e: e.tensor_tensor(out=vld[:], in0=vld[:], in1=real[:], op=ALU.mult), r=['vld', 'real'], w=['vld'])
                    S.op('dve', lambda e: e.tensor_tensor(out=fo[:], in0=fo[:], in1=first[:], op=ALU.max), r=['fo', 'first'], w=['fo'])
                    S.op('dve', lambda e: e.tensor_tensor(out=fo[:], in0=fo[:], in1=vld[:], op=ALU.mult), r=['fo', 'vld'], w=['fo'])
                    S.op('dve', lambda e: e.scalar_tensor_tensor(out=score[:], in0=imp[:], scalar=1.0, in1=vld[:], op0=ALU.add, op1=ALU.mult), r=['imp', 'vld'], w=['score'])
                    S.op('dve', lambda e: e.scalar_tensor_tensor(out=score[:], in0=fo[:], scalar=1e4, in1=score[:], op0=ALU.mult, op1=ALU.add), r=['fo', 'score'], w=['score'])
                    S.op('dve', lambda e: e.tensor_tensor(out=score[:], in0=score[:], in1=rm2[:], op=ALU.add), r=['score', 'rm2'], w=['score'])
                    S.op('dve', lambda e: e.max(out=top[:, 0:8], in_=score[:]), r=['score'], w=['top'])
                    S.op('dve', lambda e: e.match_replace(out=swk[:], in_to_replace=top[:, 0:8], in_values=score[:], imm_value=-1e30), r=['score', 'top'], w=['swk'])
                    S.op('dve', lambda e: e.max(out=top[:, 8:16], in_=swk[:]), r=['swk'], w=['top'])
                    S.op('dve', lambda e: e.tensor_scalar(out=selb[:], in0=score[:], scalar1=top[:, 15:16], scalar2=None, op0=ALU.is_ge), r=['score', 'top'], w=['selb'])
                    if getattr(self, 'dbg_sel', False) and qi == self.dbg_qi and k == self.dbg_k:
                        S.dma('sp', L['d_selb'], selb[:], r=['selb'], w=['d_selb'])
                        S.dma('sp', L['d_imp'], imp[:], r=['imp'], w=['d_imp'])
                    S.op('dve', lambda e: e.tensor_copy(out=maskq[:].rearrange("p (s j) -> p s j", j=64), in_=selb[:].unsqueeze(2).to_broadcast([128, 128, 64])),
                         r=['selb'], w=['maskq'])
                    accF = lambda j: (6 + j // 4, (j % 4) * 65)
                    for kt in range(wt + 1):
                        mi = cnt['m'] % 2
                        cnt['m'] += 1
                        mb = 6 + mi
                        S.op('pe', lambda e: e.matmul(ps[mb][:, 0:128], lhsT=maskq[:, kt * 128:(kt + 1) * 128], rhs=c_identb[:],
                                                      start=True, stop=True), r=['maskq', 'c_identb'], w=[('ps', mb)])
                        if kt == wt:
                            S.op('dve', lambda e: e.tensor_tensor(out=mk[mi][:], in0=ps[mb][:, 0:128], in1=causalT[:], op=ALU.mult), r=[('ps', mb), 'causalT'], w=[('mk', mi)])
                        else:
                            S.op('act', lambda e: e.copy(out=mk[mi][:], in_=ps[mb][:, 0:128]), r=[('ps', mb)], w=[('mk', mi)])
                        unit(k, qi, ksT[:, k, kt * 128:(kt + 1) * 128], 'ksTm', Vs[:, kt, k, :], 'Vs', 65, mk[mi][:], ('mk', mi), None, kt == 0, kt == wt)
                    finalize()
                    evac(k, qi, accF, 1, False)
                    for wi, wkt in enumerate(range(wt - 4, wt + 1)):
                        if wkt == wt:
                            m_, mt_ = causalT[:], 'causalT'
                        elif wkt == wt - 4:
                            m_, mt_ = antiT[:], 'antiT'
                        else:
                            m_, mt_ = None, None
                        lw = wkt - 44
                        unit(k, qi, kwT[:, k, lw * 128:(lw + 1) * 128], 'kwTm', Vw[:, lw, k, :], 'Vw', 65, m_, mt_, None, wi == 0, wi == 4)
                    finalize()
                    evac(k, qi, accF, 2, False)
                ys = yst[qi % 2]
                for half in range(2):
                    tb = 6 + half
                    for i4 in range(4):
                        i = half * 4 + i4
                        S.op('pe', lambda e: e.transpose(out=ps[tb][:, i4 * 128:(i4 + 1) * 128], in_=o_all[:, i, :, :], identity=c_ident[:]),
                             r=['o_all', 'c_ident'], w=[('ps', tb)])
                    S.op('act', lambda e: e.copy(out=ys[:, half * 4:half * 4 + 4, :], in_=ps[tb][:, :].rearrange("p (a b) -> p a b", b=128)),
                         r=[('ps', tb)], w=[('yst', qi % 2)])
                S.dma('sp', ynT_s.rearrange("(i p) t -> p i t", p=128)[:, :, qi * 128:(qi + 1) * 128], ys[:], r=[('yst', qi % 2)], w=[('ynT_s', qi)])
            S.barrier()

    def layer_norm(self, S, h, htag, out, otag, g_bc, b_bc, tmp, k):
        stats, mv = tmp
        for c in range(4):
            S.op('dve', lambda e: e.bn_stats(out=stats[:, c, :], in_=h[:, c * 512:(c + 1) * 512]), r=[htag], w=[('lnst', k)])
        S.op('dve', lambda e: e.bn_aggr(out=mv[:, 0:2], in_=stats[:].rearrange("p a b -> p (a b)")), r=[('lnst', k)], w=[('lnmv', k)])
        S.op('dve', lambda e: e.tensor_scalar(out=mv[:, 2:3], in0=mv[:, 1:2], scalar1=LN_EPS, scalar2=None, op0=ALU.add), r=[('lnmv', k)], w=[('lnmv', k)])
        S.op('act', lambda e: e.activation(out=mv[:, 2:3], in_=mv[:, 2:3], func=AF.Sqrt), r=[('lnmv', k)], w=[('lnmv', k)])
        S.op('dve', lambda e: e.reciprocal(out=mv[:, 2:3], in_=mv[:, 2:3]), r=[('lnmv', k)], w=[('lnmv', k)])
        S.op('dve', lambda e: e.tensor_scalar(out=h, in0=h, scalar1=mv[:, 0:1], scalar2=mv[:, 2:3], op0=ALU.subtract, op1=ALU.mult),
             r=[htag, ('lnmv', k)], w=[htag])
        S.op('dve', lambda e: e.tensor_tensor(out=h, in0=h, in1=g_bc, op=ALU.mult), r=[htag, 'ln_g'], w=[htag])
        S.op('dve', lambda e: e.tensor_tensor(out=out, in0=h, in1=b_bc, op=ALU.add), r=[htag, 'ln_b'], w=[otag])

    def phase5(self, S, ps, L):
        P = self
        ysT_s, ynT_s, x1_s, x1T_s, gT_s = L['ysT_s'], L['ynT_s'], L['x1_s'], L['x1T_s'], L['gT_s']
        x_own = self.ins['x_own']
        c_ident = self.c_ident
        ntt = self.ntt
        with ExitStack() as es:
            wo = P.sb(es, "wo", [128, 16, D], BF16)
            mixT = [P.sb(es, "mixT%d" % i, [128, 16, 128], BF16) for i in range(2)]
            g_bc = P.sb(es, "ln1g", [128, D])
            b_bc = P.sb(es, "ln1b", [128, D])
            wr = P.sb(es, "wr", [128, 16, 64])
            rb = P.sb(es, "rb", [128, 64])
            wov = self.ins['w_out'].rearrange("(c p) n -> p c n", p=128)
            for dcg in range(0, 16, 2):
                S.dma('pool', wo[:, dcg:dcg + 2, :], wov[:, dcg:dcg + 2, :], w=[('wo', dcg)])
            wotags = [('wo', dcg) for dcg in range(0, 16, 2)]
            S.dma('sp', g_bc[:], self.ins['ln1_g'][0:1, :].to_broadcast([128, D]), w=['ln_g'])
            S.dma('sp', b_bc[:], self.ins['ln1_b'][0:1, :].to_broadcast([128, D]), w=['ln_b'])
            S.dma('sp', wr[:], self.ins['w_router'].rearrange("(c p) n -> p c n", p=128), w=['wr'])
            S.dma('sp', rb[:], self.ins['router_bias'][0:1, :].to_broadcast([128, 64]), w=['rb'])
            xo = [P.sb(es, "xo%d" % i, [128, D]) for i in range(2)]
            h_ = [P.sb(es, "h%d" % i, [128, D]) for i in range(2)]
            x1 = [P.sb(es, "x1_%d" % i, [128, D]) for i in range(2)]
            lnt = (P.sb(es, "lnstats", [128, 4, 6]), P.sb(es, "lnmv", [128, 4]))
            xTf = P.sb(es, "xTf", [128, 16, 128])
            xTb = [P.sb(es, "xTb%d" % i, [128, 16, 128], BF16) for i in range(2)]
            r_ = {}
            for nm, shp in [('aff', [128, 64]), ('bia', [128, 64]), ('m1', [128, 8]), ('eq', [128, 64]), ('b2', [128, 64]), ('m2', [128, 8]), ('gs', [128, 8]),
                            ('t8', [128, 8]), ('gm', [128, 8]), ('pen', [128, 8]), ('msk', [128, 64]), ('em', [128, 64]), ('wv', [128, 64]), ('ss', [128, 2]),
                            ('gate', [128, 64])]:
                r_[nm] = P.sb(es, "r_" + nm, shp)
            gst = [P.sb(es, "gst%d" % i, [64, 128]) for i in range(2)]
            TT = lambda o, a_, b_, op, r, w: S.op('dve', lambda e: e.tensor_tensor(out=o, in0=a_, in1=b_, op=op), r=r, w=w)
            TS = lambda o, a_, s1, op, r, w: S.op('dve', lambda e: e.tensor_scalar(out=o, in0=a_, scalar1=s1, scalar2=None, op0=op), r=r, w=w)
            for tt in range(ntt):
                k = tt % 2
                S.dma('sp', xo[k][:], x_own[tt * 128:(tt + 1) * 128, :], w=[('xo', k)])
                S.dma('sp', mixT[k][:, 0:8, :], ysT_s.rearrange("(c p) t -> p c t", p=128)[:, :, tt * 128:(tt + 1) * 128], w=[('mixT0', k)])
                S.dma('sp', mixT[k][:, 8:16, :], ynT_s.rearrange("(c p) t -> p c t", p=128)[:, :, tt * 128:(tt + 1) * 128], w=[('mixT1', k)])
                for dg in range(4):
                    for kc in range(16):
                        S.op('pe', lambda e: e.matmul(ps[dg][:, :], lhsT=mixT[k][:, kc, :], rhs=wo[:, kc, dg * 512:(dg + 1) * 512],
                                                      start=(kc == 0), stop=(kc == 15)), r=wotags + [('mixT0', k), ('mixT1', k)] if kc in (0, 15) else [], w=[('ps', dg)])
                    S.op('dve', lambda e: e.scalar_tensor_tensor(out=h_[k][:, dg * 512:(dg + 1) * 512], in0=xo[k][:, dg * 512:(dg + 1) * 512], scalar=ALPHA,
                                                                 in1=ps[dg][:, :], op0=ALU.mult, op1=ALU.add), r=[('xo', k), ('ps', dg)], w=[('h', k)])
                import os as _os
                cut = int(_os.environ.get('P5CUT', '9'))
                if cut <= 1:
                    S.dma('sp', x1_s[tt * 128:(tt + 1) * 128, :], h_[k][:], r=[('h', k)], w=[('x1_s', tt)])
                    continue
                self.layer_norm(S, h_[k][:], ('h', k), x1[k][:], ('x1', k), g_bc[:], b_bc[:], lnt, 0)
                S.dma('sp', x1_s[tt * 128:(tt + 1) * 128, :], x1[k][:], r=[('x1', k)], w=[('x1_s', tt)])
                if cut <= 2:
                    continue
                for q4 in range(4):
                    tb = 4 + q4 % 2
                    for i4 in range(4):
                        dc = q4 * 4 + i4
                        S.op('pe', lambda e: e.transpose(out=ps[tb][:, i4 * 128:(i4 + 1) * 128], in_=x1[k][:, dc * 128:(dc + 1) * 128], identity=c_ident[:]),
                             r=[('x1', k), 'c_ident'], w=[('ps', tb)])
                    S.op('act', lambda e: e.copy(out=xTf[:, q4 * 4:q4 * 4 + 4, :], in_=ps[tb][:, :].rearrange("p (a b) -> p a b", b=128)), r=[('ps', tb)], w=['xTf'])
                    S.op('dve', lambda e: e.tensor_copy(out=xTb[k][:, q4 * 4:q4 * 4 + 4, :], in_=xTf[:, q4 * 4:q4 * 4 + 4, :]), r=['xTf'], w=[('xTb', k)])
                S.dma('sp', x1T_s.rearrange("(c p) t -> p c t", p=128)[:, :, tt * 128:(tt + 1) * 128], xTb[k][:], r=[('xTb', k)], w=[('x1T_s', tt)])
                if cut <= 3:
                    continue
                for dc in range(16):
                    S.op('pe', lambda e: e.matmul(ps[6][:, 0:64], lhsT=xTf[:, dc, :], rhs=wr[:, dc, :], start=(dc == 0), stop=(dc == 15)), r=['xTf', 'wr'], w=[('ps', 6)])
                S.op('act', lambda e: e.activation(out=r_['aff'][:], in_=ps[6][:, 0:64], func=AF.Sigmoid), r=[('ps', 6)], w=['aff'])
                if cut <= 4:
                    continue
                TT(r_['bia'][:], r_['aff'][:], rb[:], ALU.add, ['aff', 'rb'], ['bia'])
                b3 = r_['bia'][:].rearrange("p (g e) -> p g e", e=8)
                S.op('dve', lambda e: e.tensor_reduce(out=r_['m1'][:], in_=b3, axis=AX.X, op=ALU.max), r=['bia'], w=['m1'])
                TT(r_['eq'][:].rearrange("p (g e) -> p g e", e=8), b3, r_['m1'][:].unsqueeze(2).to_broadcast([128, 8, 8]), ALU.is_equal, ['bia', 'm1'], ['eq'])
                S.op('dve', lambda e: e.scalar_tensor_tensor(out=r_['b2'][:], in0=r_['eq'][:], scalar=-1e9, in1=r_['bia'][:], op0=ALU.mult, op1=ALU.add), r=['eq', 'bia'], w=['b2'])
                S.op('dve', lambda e: e.tensor_reduce(out=r_['m2'][:], in_=r_['b2'][:].rearrange("p (g e) -> p g e", e=8), axis=AX.X, op=ALU.max), r=['b2'], w=['m2'])
                TT(r_['gs'][:], r_['m1'][:], r_['m2'][:], ALU.add, ['m1', 'm2'], ['gs'])
                S.op('dve', lambda e: e.max(out=r_['t8'][:], in_=r_['gs'][:]), r=['gs'], w=['t8'])
                TS(r_['gm'][:], r_['gs'][:], r_['t8'][:, 3:4], ALU.is_ge, ['gs', 't8'], ['gm'])
                S.op('dve', lambda e: e.tensor_scalar(out=r_['pen'][:], in0=r_['gm'][:], scalar1=-1.0, scalar2=1e9, op0=ALU.add, op1=ALU.mult), r=['gm'], w=['pen'])
                m3 = r_['msk'][:].rearrange("p (g e) -> p g e", e=8)
                TT(m3, b3, r_['gm'][:].unsqueeze(2).to_broadcast([128, 8, 8]), ALU.mult, ['bia', 'gm'], ['msk'])
                TT(m3, m3, r_['pen'][:].unsqueeze(2).to_broadcast([128, 8, 8]), ALU.add, ['msk', 'pen'], ['msk'])
                S.op('dve', lambda e: e.max(out=r_['t8'][:], in_=r_['msk'][:]), r=['msk', 'gm'], w=['t8'])
                TS(r_['em'][:], r_['msk'][:], r_['t8'][:, 7:8], ALU.is_ge, ['msk', 't8'], ['em'])
                TT(r_['wv'][:], r_['aff'][:], r_['em'][:], ALU.mult, ['aff', 'em'], ['wv'])
                S.op('dve', lambda e: e.tensor_reduce(out=r_['ss'][:, 0:1], in_=r_['wv'][:], axis=AX.X, op=ALU.add), r=['wv'], w=['ss'])
                S.op('dve', lambda e: e.reciprocal(out=r_['ss'][:, 1:2], in_=r_['ss'][:, 0:1]), r=['ss'], w=['ss'])
                S.op('dve', lambda e: e.tensor_scalar(out=r_['gate'][:], in0=r_['wv'][:], scalar1=r_['ss'][:, 1:2], scalar2=2.5, op0=ALU.mult, op1=ALU.mult), r=['wv', 'ss'], w=['gate'])
                S.op('pe', lambda e: e.transpose(out=ps[7][0:64, 0:128], in_=r_['gate'][:], identity=c_ident[:]), r=['gate', 'c_ident'], w=[('ps', 7)])
                S.op('act', lambda e: e.copy(out=gst[k][:], in_=ps[7][0:64, 0:128]), r=[('ps', 7)], w=[('gst', k)])
                S.dma('sp', gT_s[:, tt * 128:(tt + 1) * 128], gst[k][:], r=[('gst', k)], w=[('gT_s', tt)])
            S.barrier()

    def phase6(self, S, ps, L):
        P = self
        x1_s, x1T_s, gT_s = L['x1_s'], L['x1T_s'], L['gT_s']
        out = L['out']
        c_ident = self.c_ident
        nexp = self.nexp
        with ExitStack() as es:
            acc = P.sb(es, "acc", [128, 8, D])
            xT = P.sb(es, "x1T", [128, 16, 1024], BF16)
            gT = P.sb(es, "gTsb", [128, 1024])
            S.op('dve', lambda e: e.memset(gT[:], 0.0), w=['gTsb'])
            g_bc = P.sb(es, "ln2g", [128, D])
            b_bc = P.sb(es, "ln2b", [128, D])
            S.dma('sp', g_bc[:], self.ins['ln2_g'][0:1, :].to_broadcast([128, D]), w=['ln_g'])
            S.dma('sp', b_bc[:], self.ins['ln2_b'][0:1, :].to_broadcast([128, D]), w=['ln_b'])
            wgb = [P.sb(es, "wgb%d" % i, [128, 16, 256], BF16) for i in range(2)]
            wub = [P.sb(es, "wub%d" % i, [128, 16, 256], BF16) for i in range(2)]
            wdb = [P.sb(es, "wdb%d" % i, [128, 4, D], BF16) for i in range(2)]
            hT = [P.sb(es, "hT%d" % i, [128, 4, 1024], BF16) for i in range(2)]
            gbc = [P.sb(es, "gbc%d" % tg, [128, 512]) for tg in range(2)]
            sg = [P.sb(es, "sgm%d" % i, [128, 512]) for i in range(2)]
            tm = [P.sb(es, "tm%d" % i, [128, 512]) for i in range(1)] * 2
            lnt = (P.sb(es, "ln2stats", [128, 4, 6]), P.sb(es, "ln2mv", [128, 4]))
            cnt = {'gu': 0, 'y': 0}
            NE = nexp

            def wsrc(e_):
                if e_ == 64:
                    return self.ins['ws_gate'], self.ins['ws_up'], self.ins['ws_down']
                return self.ins['w_gate'][e_], self.ins['w_up'][e_], self.ins['w_down'][e_]

            def loads_gu(ge, fh):
                wgs, wus, wds = wsrc(ge % NE)
                S.dma('pool', wgb[fh][:], wgs.rearrange("(c p) f -> p c f", p=128)[:, :, fh * 256:(fh + 1) * 256], w=[('wg', fh)])
                S.dma('pool', wub[fh][:], wus.rearrange("(c p) f -> p c f", p=128)[:, :, fh * 256:(fh + 1) * 256], w=[('wu', fh)])

            def loads_d(ge):
                wgs, wus, wds = wsrc(ge % NE)
                S.dma('pool', wdb[ge % 2][:], wds.rearrange("(c p) d -> p c d", p=128), w=[('wd', ge % 2)])

            def gate_bc(ge):
                e_ = ge % NE
                if e_ == 64:
                    return
                for tg in range(2):
                    S.op('pe', lambda e: e.matmul(ps[6][:, :], lhsT=c_ident[:, e_:e_ + 1].to_broadcast([128, 128]), rhs=gT[:, tg * 512:(tg + 1) * 512],
                                                  start=True, stop=True), r=['c_ident', 'gTsb'], w=[('ps', 6)])
                    S.op('act', lambda e: e.copy(out=gbc[tg][:], in_=ps[6][:, :]), r=[('ps', 6)], w=[('gbc', tg)])

            def GU(ge, fh):
                e_ = ge % NE
                shared = (e_ == 64)
                wg_, wu_ = wgb[fh], wub[fh]
                hb = hT[ge % 2]
                htag = ('hT', ge % 2)
                for tg in range(2):
                    for fc in range(2):
                        bg = (cnt['gu'] % 2) * 2
                        cnt['gu'] += 1
                        k = cnt['gu'] % 2
                        for dc in range(16):
                            S.op('pe', lambda e: e.matmul(ps[bg][:, :], lhsT=wg_[:, dc, fc * 128:(fc + 1) * 128], rhs=xT[:, dc, tg * 512:(tg + 1) * 512],
                                                          start=(dc == 0), stop=(dc == 15)), r=[('wg', fh), 'x1T'] if dc in (0, 15) else [], w=[('ps', bg)])
                        for dc in range(16):
                            S.op('pe', lambda e: e.matmul(ps[bg + 1][:, :], lhsT=wu_[:, dc, fc * 128:(fc + 1) * 128], rhs=xT[:, dc, tg * 512:(tg + 1) * 512],
                                                          start=(dc == 0), stop=(dc == 15)), r=[('wu', fh), 'x1T'] if dc in (0, 15) else [], w=[('ps', bg + 1)])
                        S.op('act', lambda e: e.activation(out=sg[k][:], in_=ps[bg][:, :], func=AF.Silu), r=[('ps', bg)], w=[('sg', k)])
                        hdst = hb[:, fh * 2 + fc, tg * 512:(tg + 1) * 512]
                        if shared:
                            S.op('dve', lambda e: e.tensor_tensor(out=hdst, in0=ps[bg + 1][:, :], in1=sg[k][:], op=ALU.mult), r=[('ps', bg + 1), ('sg', k)], w=[htag])
                        else:
                            S.op('dve', lambda e: e.tensor_tensor(out=tm[k][:], in0=ps[bg + 1][:, :], in1=sg[k][:], op=ALU.mult), r=[('ps', bg + 1), ('sg', k)], w=[('tm', 0)])
                            S.op('dve', lambda e: e.tensor_tensor(out=hdst, in0=tm[k][:], in1=gbc[tg][:], op=ALU.mult), r=[('tm', 0), ('gbc', tg)], w=[htag])

            def DN(ge):
                wd_ = wdb[ge % 2]
                hb = hT[ge % 2]
                htag = ('hT', ge % 2)
                for t8 in range(8):
                    for dg in range(4):
                        by = (4, 5, 7)[cnt['y'] % 3]
                        cnt['y'] += 1
                        for fc in range(4):
                            S.op('pe', lambda e: e.matmul(ps[by][:, :], lhsT=hb[:, fc, t8 * 128:(t8 + 1) * 128], rhs=wd_[:, fc, dg * 512:(dg + 1) * 512],
                                                          start=(fc == 0), stop=(fc == 3)), r=[htag, ('wd', ge % 2)], w=[('ps', by)])
                        asl = acc[:, t8, dg * 512:(dg + 1) * 512]
                        atag = ('acc', t8, dg)
                        S.op('dve', lambda e: e.tensor_tensor(out=asl, in0=asl, in1=ps[by][:, :], op=ALU.add), r=[('ps', by), atag], w=[atag])

            acctags = [('acc', t8, dg) for t8 in range(8) for dg in range(4)] + [('accrow', t8) for t8 in range(8)]
            for half in range(self.nhalf):
                tk0 = half * 1024
                S.dma('sp', xT[:], x1T_s.rearrange("(c p) t -> p c t", p=128)[:, :, tk0:tk0 + 1024], w=['x1T'])
                S.dma('sp', gT[0:64, :], gT_s[:, tk0:tk0 + 1024], w=['gTsb'])
                S.dma('sp', acc[:], x1_s[tk0:tk0 + 1024, :].rearrange("(t p) d -> p t d", p=128), w=acctags)
                for t8 in range(8):
                    S.op('act', lambda e: e.activation(out=acc[:, t8, :], in_=acc[:, t8, :], func=AF.Copy, scale=ALPHA),
                         r=[('acc', t8, dg) for dg in range(4)], w=[('acc', t8, dg) for dg in range(4)] + [('accrow', t8)])
                g0 = half * NE
                loads_gu(g0, 0)
                loads_gu(g0, 1)
                loads_d(g0)
                gate_bc(g0)
                GU(g0, 0)
                if NE > 1:
                    loads_gu(g0 + 1, 0)
                GU(g0, 1)
                if NE > 1:
                    loads_gu(g0 + 1, 1)
                    loads_d(g0 + 1)
                for ei in range(NE):
                    ge = g0 + ei
                    if ei + 1 < NE:
                        gate_bc(ge + 1)
                        GU(ge + 1, 0)
                        if ei + 2 < NE:
                            loads_gu(ge + 2, 0)
                        GU(ge + 1, 1)
                        if ei + 2 < NE:
                            loads_gu(ge + 2, 1)
                    DN(ge)
                    if ei + 2 < NE:
                        loads_d(ge + 2)
                for t8 in range(8):
                    k = t8 % 2
                    k = 0
                    S.op('dve', lambda e: e.tensor_copy(out=lnt[1][:, 3:4], in_=acc[:, t8, 0:1]), r=[('acc', t8, dg) for dg in range(4)], w=[('accrow', t8)] + [('acc', t8, dg) for dg in range(4)])
                    self.layer_norm(S, acc[:, t8, :], ('accrow', t8), acc[:, t8, :], ('accrow', t8), g_bc[:], b_bc[:], lnt, 1)
                    r0 = tk0 + t8 * 128
                    S.dma('sp', out[r0:r0 + 128, :], acc[:, t8, :], r=[('accrow', t8)], w=[('out', r0)])
            S.barrier()

    def finish_debug(self, S, L):
        for name in sorted(self.dbg):
            t = L[name]
            if name == 'ynT_s':
                t = t[:, 0:self.nq * 128]
            shape = list(t.shape)
            o = self.dout("dbg_" + name, shape, t.dtype)
            S.dma('sp', o, t if isinstance(t, bass.AP) else t[:], w=[('dbg', name)])
        S.barrier()


def host_constants():
    c = {}
    c["identf"] = np.eye(128, dtype=np.float32)
    pm = np.zeros((128, 128), np.float32)
    for m in range(128):
        k = (m % 64 + 32) % 64 + 64 * (m // 64)
        pm[k, m] = 1.0
    c["pswap"] = pm
    rc = np.zeros((128, 4), np.float32)
    inv = 10000.0 ** (-(np.arange(32, dtype=np.float64)) / 32.0)
    for p in range(128):
        rc[p, 0] = inv[p % 32] / TWO_PI
        rc[p, 1] = (-TWO_PI if (p % 64) < 32 else TWO_PI)
    c["ropecol"] = rc
    n_ = (np.arange(4)[None, :, None] * 128 + np.arange(128)[:, None, None])
    s_ = np.arange(128)[None, None, :]
    c["omat"] = ((16 * n_ <= 64 * s_ + 63) & (16 * n_ + 31 >= 64 * s_)).astype(np.float32)
    kk = np.arange(128)[:, None]
    qq = np.arange(128)[None, :]
    c["causalT"] = (kk <= qq).astype(np.float32)
    c["antiT"] = (kk > qq).astype(np.float32)
    nl = np.arange(128)[:, None, None, None]
    qi = np.arange(16)[None, :, None, None]
    aa = np.arange(2)[None, None, :, None] + 2
    ql = np.arange(128)[None, None, None, :]
    c["cmask"] = (16 * (128 * aa + nl) + 31 <= (48 + qi) * 128 + ql).astype(np.float32)
    c["sidx"] = np.broadcast_to(np.arange(128, dtype=np.float32)[None, :], (128, 128)).copy()
    c["qhalf"] = (np.arange(128) >= 64).astype(np.float32)[:, None]
    c["iota1"] = np.broadcast_to(np.arange(1, 513, dtype=np.float32)[None, :], (128, 512)).copy()
    c["iotar"] = np.broadcast_to((511.0 - np.arange(512, dtype=np.float32))[None, :], (128, 512)).copy()
    return c


def core_inputs(c, x, positions, shared):
    b, j = c // 4, c % 4
    t0 = NOWN * j
    pad = W - (t0 + NOWN)
    m = {}
    xT = np.zeros((D, W), np.float32)
    xT[:, pad:] = x[b, :t0 + NOWN, :].T
    m["xT"] = xT
    m["x_own"] = np.ascontiguousarray(x[b, t0:t0 + NOWN, :])
    pw = np.zeros((1, W), np.int32)
    pw[0, pad:] = positions[b, :t0 + NOWN]
    m["posw"] = pw
    pc = np.zeros((1, 512), np.int32)
    pc[0, :] = pw[0, np.minimum(np.arange(512) * 16, W - 1)]
    m["posc"] = pc
    widx = np.arange(64)[None, :] * 128 + np.arange(128)[:, None]
    m["kvalid"] = (widx >= pad).astype(np.float32)
    nidx = np.arange(4)[None, :] * 128 + np.arange(128)[:, None]
    m["nvalid"] = (nidx >= pad // 16).astype(np.float32)
    pc2 = np.zeros((128, 2), np.float32)
    pc2[:, 0] = pad // 64
    pc2[:, 1] = pad
    m["padcol"] = pc2
    m.update(shared)
    return m


def make_shared(inp):
    sh = host_constants()
    sh["w_in"] = np.ascontiguousarray(inp["w_in"][0][:, w_in_columns()])
    for nm in ["w_cmp_k1", "w_cmp_v1", "w_cmp_k2", "w_cmp_v2"]:
        sh[nm] = np.ascontiguousarray(inp[nm][0])
    G2 = lambda a: np.asarray(a[0]).reshape(32, 2, *a.shape[2:])
    lam = np.stack([G2(inp["lam_re"]), G2(inp["lam_im"]),
                    np.broadcast_to(G2(inp["log_dt"])[:, :, None], (32, 2, 64))], -1)
    sh["s5_lam"] = np.ascontiguousarray(lam.transpose(1, 2, 0, 3).reshape(128, 32, 3)).astype(np.float32)
    bb = np.stack([G2(inp["ssm_b_re"]), G2(inp["ssm_b_im"])], 3)
    sh["s5_b"] = np.ascontiguousarray(bb.transpose(1, 2, 0, 3, 4).reshape(128, 32, 2, 16)).astype(np.float32)
    cc = np.stack([G2(inp["ssm_c_re"]), G2(inp["ssm_c_im"])], 2)
    sh["s5_c"] = np.ascontiguousarray(cc.transpose(1, 4, 0, 2, 3).reshape(128, 32, 2, 16)).astype(np.float32)
    sh["s5_d"] = np.ascontiguousarray(G2(inp["ssm_d"]).transpose(1, 2, 0).reshape(32, 32)).astype(np.float32)
    sh["w_glu"] = np.ascontiguousarray(inp["w_glu"][0])
    wo = np.asarray(inp["w_out"][0])
    sh["w_out"] = np.ascontiguousarray(np.concatenate([wo[:1024], wo[1024:].reshape(16, 64, D)[q_head_order()].reshape(1024, D)], 0))
    for nm in ["ln1_g", "ln1_b", "ln2_g", "ln2_b", "w_router", "router_bias", "w_gate", "w_up", "w_down", "ws_gate", "ws_up", "ws_down"]:
        sh[nm] = np.ascontiguousarray(inp[nm][0])
    for nm in ["cmp_pos_k", "cmp_pos_v"]:
        t = np.asarray(inp[nm][0]).T
        sh[nm + "T"] = np.ascontiguousarray(np.concatenate([t, t], 0))
    return sh


_CACHE = {}


def kernel(**inputs):
    x = np.asarray(inputs["x"], np.float32)
    positions = np.asarray(inputs["positions"], np.int32)
    shared = make_shared(inputs)
    if 'nc' not in _CACHE:
        _CACHE['nc'] = Prog('all').build()
    nc = _CACHE['nc']
    in_maps = [core_inputs(c, x, positions, shared) for c in range(8)]
    res = run_bass_kernel_spmd(nc, in_maps, core_ids=list(range(8)))
    out = np.zeros((2, SEQ, D), np.float32)
    for c in range(8):
        b, j = c // 4, c % 4
        out[b, j * NOWN:(j + 1) * NOWN] = res.results[c]["out"]
    return out
```
